# Optimizing a Trainium2 kernel written in Bass

```python
import math
import jax, jax.numpy as jnp
from jax import lax
import numpy as np

D_MODEL = 1024
BATCH = 4
SEQ = 8192
DEPTH = 1

ATT_HEADS = 4
ATT_HEAD_DIM = 64
ATT_Q_WIDTH = ATT_HEADS * 2 * ATT_HEAD_DIM
ATT_V_DIM = 2 * ATT_HEAD_DIM
ATT_V_WIDTH = ATT_HEADS * ATT_V_DIM
ROPE_THETA = 10000.0
Q_BLOCK = 128
SUBLN_EPS = 1e-5
CONV_WIDTH = D_MODEL // 2
CONV_K = 3
N_BRANCHES = 2
IN_SIZES = (ATT_Q_WIDTH, ATT_Q_WIDTH, ATT_V_WIDTH, CONV_WIDTH, CONV_WIDTH, CONV_WIDTH, N_BRANCHES * D_MODEL)
IN_COLS = sum(IN_SIZES)
N_GROUPS = 4
EXPERTS_PER_GROUP = 8
N_EXPERTS = N_GROUPS * EXPERTS_PER_GROUP
TOP_K = 2
EXPERT_FF = D_MODEL // 2
MOE_BLOCK = 128
LN_EPS = 1e-5
DEEPNORM_ALPHA = (2.0 * DEPTH) ** 0.25
DEEPNORM_BETA = (8.0 * DEPTH) ** -0.25

kernel_name = "hybrid_diffattn_shortconv_hmoe_deepnorm"


def layer_norm(x, g, b):
    xf = x.astype(jnp.float32)
    mu = jnp.mean(xf, -1, keepdims=True)
    var = jnp.mean(jnp.square(xf - mu), -1, keepdims=True)
    return ((xf - mu) * lax.rsqrt(var + LN_EPS) * g.astype(jnp.float32) + b.astype(jnp.float32)).astype(x.dtype)


def rope_tables(positions):
    inv_freq = ROPE_THETA ** (-jnp.arange(0, ATT_HEAD_DIM, 2, dtype=jnp.float32) / ATT_HEAD_DIM)
    ang = positions.astype(jnp.float32)[..., None] * inv_freq
    ang = jnp.concatenate([ang, ang], -1)
    return jnp.cos(ang), jnp.sin(ang)


def apply_rope(t, cos, sin):
    c = cos[:, :, None, None, :]
    s = sin[:, :, None, None, :]
    t1, t2 = jnp.split(t, 2, axis=-1)
    rot = jnp.concatenate([-t2, t1], -1)
    return (t.astype(jnp.float32) * c + rot.astype(jnp.float32) * s).astype(t.dtype)


def diff_attention(q, k, v, lam):
    B, S = q.shape[0], q.shape[1]
    nqb = S // Q_BLOCK
    scale = ATT_HEAD_DIM ** -0.5
    kt = k.transpose(0, 2, 3, 1, 4)
    vt = v.transpose(0, 2, 1, 3)
    qb = q.reshape(B, nqb, Q_BLOCK, ATT_HEADS, 2, ATT_HEAD_DIM).transpose(1, 0, 3, 4, 2, 5)
    kpos = jnp.arange(S)
    neg = jnp.finfo(jnp.float32).min

    def block(args):
        qi, blk = args
        s = jnp.einsum('bhmqd,bhmkd->bhmqk', qi, kt, preferred_element_type=jnp.float32) * scale
        qpos = blk * Q_BLOCK + jnp.arange(Q_BLOCK)
        mask = kpos[None, :] <= qpos[:, None]
        p = jax.nn.softmax(jnp.where(mask, s, neg), axis=-1)
        a = p[:, :, 0] - lam * p[:, :, 1]
        return jnp.einsum('bhqk,bhkv->bhqv', a.astype(vt.dtype), vt)

    o = lax.map(block, (qb, jnp.arange(nqb)))
    return o.transpose(1, 0, 3, 2, 4).reshape(B, S, ATT_HEADS, ATT_V_DIM)


def short_conv(u, w):
    S = u.shape[1]
    up = jnp.pad(u, ((0, 0), (CONV_K - 1, 0), (0, 0)))
    y = w[0] * up[:, 0:S]
    for j in range(1, CONV_K):
        y = y + w[j] * up[:, j:j + S]
    return y


def hier_moe(x, w_rg, b_rg, w_re, b_re, w_gate, w_up, w_down):
    B, S, D = x.shape
    T = B * S
    xf = x.reshape(T, D)
    g_logits = (xf @ w_rg).astype(jnp.float32) + b_rg.astype(jnp.float32)
    g_prob = jax.nn.softmax(g_logits, -1)
    g_sel = jnp.argmax(g_logits, -1)
    g_w = jnp.take_along_axis(g_prob, g_sel[:, None], -1)[:, 0]
    e_logits = ((xf @ w_re).astype(jnp.float32) + b_re.astype(jnp.float32)).reshape(T, N_GROUPS, EXPERTS_PER_GROUP)
    e_logits = jnp.take_along_axis(e_logits, g_sel[:, None, None], 1)[:, 0]
    top_v, top_i = lax.top_k(e_logits, TOP_K)
    top_p = jax.nn.softmax(top_v, -1) * g_w[:, None]
    expert_id = g_sel[:, None].astype(jnp.int32) * EXPERTS_PER_GROUP + top_i.astype(jnp.int32)

    A = T * TOP_K
    e_flat = expert_id.reshape(A)
    tok_flat = jnp.repeat(jnp.arange(T, dtype=jnp.int32), TOP_K)
    w_flat = top_p.reshape(A)
    order = jnp.argsort(e_flat)
    e_sorted = e_flat[order]
    tok_sorted = tok_flat[order]
    w_sorted = w_flat[order]
    counts = jnp.bincount(e_flat, length=N_EXPERTS).astype(jnp.int32)
    padded = (counts + MOE_BLOCK - 1) // MOE_BLOCK * MOE_BLOCK
    start = jnp.cumsum(counts) - counts
    pend = jnp.cumsum(padded)
    pstart = pend - padded
    dest = pstart[e_sorted] + jnp.arange(A, dtype=jnp.int32) - start[e_sorted]
    P = A + N_EXPERTS * MOE_BLOCK
    NB = P // MOE_BLOCK
    slot_tok = jnp.full((P,), T, jnp.int32).at[dest].set(tok_sorted)
    slot_w = jnp.zeros((P,), jnp.float32).at[dest].set(w_sorted)
    block_expert = jnp.minimum(
        jnp.searchsorted(pend, jnp.arange(NB, dtype=jnp.int32) * MOE_BLOCK, side='right'), N_EXPERTS - 1)
    x_pad = jnp.concatenate([xf, jnp.zeros((1, D), xf.dtype)], 0)
    xs = x_pad[slot_tok].reshape(NB, MOE_BLOCK, D)

    def run(args):
        xb, e = args
        h = jax.nn.silu(xb @ w_gate[e]) * (xb @ w_up[e])
        return h @ w_down[e]

    ys = lax.map(run, (xs, block_expert)).reshape(P, D)
    out = jnp.zeros((T + 1, D), jnp.float32).at[slot_tok].add(ys.astype(jnp.float32) * slot_w[:, None])
    return out[:T].reshape(B, S, D).astype(x.dtype)


def setup_inputs(seed: int = 0) -> dict:
    key = jax.random.key(seed)
    ks = jax.random.split(key, 24)
    f32 = jnp.float32
    nrm = lambda k, shape, s: jax.random.normal(k, shape, f32) * s
    x = jax.random.normal(ks[0], (BATCH, SEQ, D_MODEL), f32)
    positions = jnp.broadcast_to(jnp.arange(SEQ, dtype=jnp.int32), (BATCH, SEQ))
    col_scale = jnp.concatenate([
        jnp.ones((2 * ATT_Q_WIDTH,), f32),
        jnp.full((ATT_V_WIDTH,), DEEPNORM_BETA, f32),
        jnp.ones((3 * CONV_WIDTH + N_BRANCHES * D_MODEL,), f32)])
    w_in = nrm(ks[1], (DEPTH, D_MODEL, IN_COLS), D_MODEL ** -0.5) * col_scale
    b_gate = nrm(ks[2], (DEPTH, N_BRANCHES * D_MODEL), 0.02)
    lambda_q1 = nrm(ks[3], (DEPTH, ATT_HEAD_DIM), 0.1)
    lambda_k1 = nrm(ks[4], (DEPTH, ATT_HEAD_DIM), 0.1)
    lambda_q2 = nrm(ks[5], (DEPTH, ATT_HEAD_DIM), 0.1)
    lambda_k2 = nrm(ks[6], (DEPTH, ATT_HEAD_DIM), 0.1)
    subln_g = 1.0 + nrm(ks[7], (DEPTH, ATT_V_DIM), 0.02)
    w_o_att = nrm(ks[8], (DEPTH, ATT_V_WIDTH, D_MODEL), ATT_V_WIDTH ** -0.5 * DEEPNORM_BETA)
    conv_w = nrm(ks[9], (DEPTH, CONV_K, CONV_WIDTH), CONV_K ** -0.5)
    w_o_conv = nrm(ks[10], (DEPTH, CONV_WIDTH, D_MODEL), CONV_WIDTH ** -0.5 * DEEPNORM_BETA)
    w_mix_out = nrm(ks[11], (DEPTH, D_MODEL, D_MODEL), D_MODEL ** -0.5 * DEEPNORM_BETA)
    ln1_g = 1.0 + nrm(ks[12], (DEPTH, D_MODEL), 0.02)
    ln1_b = nrm(ks[13], (DEPTH, D_MODEL), 0.02)
    w_router_group = nrm(ks[14], (DEPTH, D_MODEL, N_GROUPS), D_MODEL ** -0.5)
    b_router_group = nrm(ks[15], (DEPTH, N_GROUPS), 0.01)
    w_router_expert = nrm(ks[16], (DEPTH, D_MODEL, N_EXPERTS), D_MODEL ** -0.5)
    b_router_expert = nrm(ks[17], (DEPTH, N_EXPERTS), 0.01)
    w_exp_gate = nrm(ks[18], (DEPTH, N_EXPERTS, D_MODEL, EXPERT_FF), D_MODEL ** -0.5)
    w_exp_up = nrm(ks[19], (DEPTH, N_EXPERTS, D_MODEL, EXPERT_FF), D_MODEL ** -0.5 * DEEPNORM_BETA)
    w_exp_down = nrm(ks[20], (DEPTH, N_EXPERTS, EXPERT_FF, D_MODEL), EXPERT_FF ** -0.5 * DEEPNORM_BETA)
    ln2_g = 1.0 + nrm(ks[21], (DEPTH, D_MODEL), 0.02)
    ln2_b = nrm(ks[22], (DEPTH, D_MODEL), 0.02)
    return {"x": x, "positions": positions, "w_in": w_in, "b_gate": b_gate,
            "lambda_q1": lambda_q1, "lambda_k1": lambda_k1, "lambda_q2": lambda_q2, "lambda_k2": lambda_k2,
            "subln_g": subln_g, "w_o_att": w_o_att, "conv_w": conv_w, "w_o_conv": w_o_conv,
            "w_mix_out": w_mix_out, "ln1_g": ln1_g, "ln1_b": ln1_b,
            "w_router_group": w_router_group, "b_router_group": b_router_group,
            "w_router_expert": w_router_expert, "b_router_expert": b_router_expert,
            "w_exp_gate": w_exp_gate, "w_exp_up": w_exp_up, "w_exp_down": w_exp_down,
            "ln2_g": ln2_g, "ln2_b": ln2_b}


def reference(x, positions, w_in, b_gate, lambda_q1, lambda_k1, lambda_q2, lambda_k2, subln_g, w_o_att,
              conv_w, w_o_conv, w_mix_out, ln1_g, ln1_b, w_router_group, b_router_group, w_router_expert,
              b_router_expert, w_exp_gate, w_exp_up, w_exp_down, ln2_g, ln2_b):
    B, S, D = x.shape
    cos, sin = rope_tables(positions)
    split_pts = [int(v) for v in np.cumsum(IN_SIZES)[:-1]]
    h = x
    for l in range(DEPTH):
        lambda_init = 0.8 - 0.6 * math.exp(-0.3 * l)
        proj = h @ w_in[l]
        q, k, v, cb, cc, cx, gates = jnp.split(proj, split_pts, axis=-1)
        q = apply_rope(q.reshape(B, S, ATT_HEADS, 2, ATT_HEAD_DIM), cos, sin)
        k = apply_rope(k.reshape(B, S, ATT_HEADS, 2, ATT_HEAD_DIM), cos, sin)
        v = v.reshape(B, S, ATT_HEADS, ATT_V_DIM)
        lam = (jnp.exp(jnp.sum(lambda_q1[l].astype(jnp.float32) * lambda_k1[l].astype(jnp.float32)))
               - jnp.exp(jnp.sum(lambda_q2[l].astype(jnp.float32) * lambda_k2[l].astype(jnp.float32)))
               + lambda_init)
        o = diff_attention(q, k, v, lam).astype(jnp.float32)
        o = o * lax.rsqrt(jnp.mean(jnp.square(o), -1, keepdims=True) + SUBLN_EPS)
        o = (o * subln_g[l].astype(jnp.float32) * (1.0 - lambda_init)).astype(h.dtype)
        y_att = o.reshape(B, S, ATT_V_WIDTH) @ w_o_att[l]
        y_conv = (cb * short_conv(cc * cx, conv_w[l])) @ w_o_conv[l]
        g = jax.nn.sigmoid(gates.astype(jnp.float32) + b_gate[l].astype(jnp.float32)).reshape(B, S, N_BRANCHES, D)
        merged = (g[:, :, 0] * y_att.astype(jnp.float32) + g[:, :, 1] * y_conv.astype(jnp.float32)).astype(h.dtype)
        h = layer_norm(DEEPNORM_ALPHA * h + merged @ w_mix_out[l], ln1_g[l], ln1_b[l])
        ffn = hier_moe(h, w_router_group[l], b_router_group[l], w_router_expert[l], b_router_expert[l],
                       w_exp_gate[l], w_exp_up[l], w_exp_down[l])
        h = layer_norm(DEEPNORM_ALPHA * h + ffn, ln2_g[l], ln2_b[l])
    return h
```

```python
import math
import numpy as np
import ml_dtypes
from contextlib import ExitStack
import concourse.bass as bass
import concourse.mybir as mybir
from concourse.bass_utils import run_bass_kernel_spmd

F32 = mybir.dt.float32
BF16 = mybir.dt.bfloat16
I32 = mybir.dt.int32
U32 = mybir.dt.uint32
AF = mybir.ActivationFunctionType
ALU = mybir.AluOpType
AX = mybir.AxisListType
ENGS = ["tensor", "vector", "scalar", "gpsimd", "sync"]

S = 8192
D = 1024
NOWN = 4096
CAP = 384
NSLOT = 32 * CAP
ALPHA = 2.0 ** 0.25
LAMBDA_INIT = 0.8 - 0.6 * math.exp(0.0)
TWO_PI = 2.0 * math.pi
NEG = -1.0e30


class Prog:
    def __init__(self, nc, es):
        self.nc = nc
        self.es = es
        self.ops = {e: [] for e in ENGS}
        self.cnt = {e: 0 for e in ENGS}
        self.esem = {e: es.enter_context(nc.semaphore("es_" + e)) for e in ENGS}
        self.dsem = {}
        self.dcnt = {}
        self.waited = {e: {} for e in ENGS}
        self.base_waits = []

    def _waits(self, eng, waits):
        best = {}
        for w in list(waits) + list(self.base_waits):
            if w is None:
                continue
            sem, val, key = w
            if key not in best or best[key][1] < val:
                best[key] = (sem, val)
        out = []
        for key, (sem, val) in best.items():
            if self.waited[eng].get(key, 0) >= val:
                continue
            self.waited[eng][key] = val
            out.append((sem, val))
        return out

    def op(self, eng, fn, waits=(), signal=True):
        ws = self._waits(eng, waits)
        inc = None
        tok = None
        if signal:
            self.cnt[eng] += 1
            inc = (self.esem[eng], 1)
            tok = (self.esem[eng], self.cnt[eng], "e_" + eng)
        self.ops[eng].append((fn, ws, inc))
        return tok

    def _dsem(self, sem):
        if sem not in self.dsem:
            self.dsem[sem] = self.es.enter_context(self.nc.semaphore("ds_" + sem))
            self.dcnt[sem] = 0
        return self.dsem[sem]

    def dma(self, q, out, in_, sem, waits=(), **kw):
        return self.cdma(q, lambda e: e.dma_start(out=out, in_=in_, **kw), sem, waits)

    def cdma(self, q, fn, sem, waits=()):
        s = self._dsem(sem)
        ws = self._waits(q, waits)
        self.dcnt[sem] += 16
        self.ops[q].append((fn, ws, (s, 16)))
        return (s, self.dcnt[sem], "d_" + sem)

    def _auto_waits(self, reads, writes, psum):
        if not hasattr(self, "lw"):
            self.lw, self.rd, self.pa = {}, {}, {}
        ws = []
        for r in reads:
            ws.append(self.lw.get(r))
        for w in writes:
            ws.append(self.lw.get(w))
            ws.extend(self.rd.get(w, []))
        for p in psum:
            ws.append(self.pa.get(p))
        return ws

    def _auto_done(self, tok, reads, writes, psum):
        for r in reads:
            self.rd.setdefault(r, []).append(tok)
        for w in writes:
            self.lw[w] = tok
            self.rd[w] = []
        for p in psum:
            self.pa[p] = tok

    def auto(self, eng, fns, reads=(), writes=(), psum=(), extra=()):
        if not isinstance(fns, (list, tuple)):
            fns = [fns]
        ws = self._auto_waits(reads, writes, psum) + list(extra)
        tok = None
        for k, fn in enumerate(fns):
            tok = self.op(eng, fn, waits=ws if k == 0 else (), signal=(k == len(fns) - 1))
        self._auto_done(tok, reads, writes, psum)
        return tok

    def adma(self, q, out, in_, sem, reads=(), writes=(), extra=(), **kw):
        ws = self._auto_waits(reads, writes, ()) + list(extra)
        tok = self.dma(q, out, in_, sem, waits=ws, **kw)
        self._auto_done(tok, reads, writes, ())
        return tok

    def acdma(self, q, fn, sem, reads=(), writes=(), extra=()):
        ws = self._auto_waits(reads, writes, ()) + list(extra)
        tok = self.cdma(q, fn, sem, waits=ws)
        self._auto_done(tok, reads, writes, ())
        return tok

    def emit(self, final_waits):
        nc = self.nc
        with nc.Block() as block:
            def mk(eng):
                def body(e):
                    for fn, ws, inc in self.ops[eng]:
                        for sem, val in ws:
                            e.wait_ge(sem, val)
                        ins = fn(e)
                        if inc is not None:
                            ins.then_inc(inc[0], inc[1])
                    if eng == "sync":
                        for w in final_waits:
                            if w is not None:
                                e.wait_ge(w[0], w[1])
                return body
            block.tensor(mk("tensor"))
            block.vector(mk("vector"))
            block.scalar(mk("scalar"))
            block.gpsimd(mk("gpsimd"))
            block.sync(mk("sync"))


class Arena:
    def __init__(self, t, nwords):
        self.t = t
        self.n = nwords
        self.top = 0

    def mark(self):
        return self.top

    def reset(self, m):
        self.top = m

    def alloc(self, shape, dt):
        per = 1
        for s_ in shape[1:]:
            per *= s_
        if dt == BF16:
            words = (per + 1) // 2
        else:
            words = per
        words = (words + 7) // 8 * 8
        a = self.top
        self.top += words
        assert self.top <= self.n, ("arena overflow", self.top, self.n)
        v = self.t[:, a:a + words]
        if dt == BF16:
            v = v.bitcast(BF16)[:, 0:per]
        elif dt == I32:
            v = v.bitcast(I32)[:, 0:per]
        else:
            v = v[:, 0:per]
        if len(shape) == 3:
            v = v.rearrange("p (a b) -> p a b", a=shape[1])
        elif len(shape) == 4:
            v = v.rearrange("p (a b c) -> p a b c", a=shape[1], b=shape[2])
        if shape[0] != 128:
            v = v[0:shape[0]]
        return v


def build(upto="all", taps=(), NQT=8):
    nc = bass.Bass("TRN2", target_bir_lowering=False)
    din = lambda name, shape, dt: nc.dram_tensor(name, shape, dt, kind="ExternalInput").ap()
    xf = din("xf", [S, D], F32)
    xo = din("xo", [NOWN, D], F32)
    xh = din("xh", [64, D], F32)
    posf = din("posf", [1, S], I32)
    poso = din("poso", [1, NOWN], I32)
    w_in = din("w_in", [D, 5120], F32)
    b_gate = din("b_gate", [1, 2048], F32)
    lam_in = din("lam_in", [1, 256], F32)
    subln_g = din("subln_g", [1, 128], F32)
    w_o_att = din("w_o_att", [512, D], F32)
    conv_w = din("conv_w", [3, 512], F32)
    w_o_conv = din("w_o_conv", [512, D], F32)
    w_mix = din("w_mix", [D, D], F32)
    ln1 = din("ln1", [2, D], F32)
    ln2 = din("ln2", [2, D], F32)
    w_rt = din("w_rt", [D, 36], F32)
    b_rt = din("b_rt", [1, 36], F32)
    w_eg = din("w_eg", [32, D, 512], F32)
    w_eu = din("w_eu", [32, D, 512], F32)
    w_ed = din("w_ed", [32, 512, D], F32)
    cbf = din("cbf", [128, 384 + 4096], F32)
    cf32 = din("cf32", [128, 512], F32)
    out = nc.dram_tensor("out", [NOWN, D], F32, kind="ExternalOutput").ap()
    h1f = nc.dram_tensor("h1f", [NOWN, D], F32, kind="Internal").ap()
    xs_d = nc.dram_tensor("xs_d", [NSLOT, D], BF16, kind="Internal").ap()
    ys_d = nc.dram_tensor("ys_d", [NSLOT, D], F32, kind="Internal").ap()
    tap_out = {}

    with ExitStack() as es:
        P = Prog(nc, es)
        NW = 51 * 1024
        arena_t = es.enter_context(nc.sbuf_tensor("arena", [128, NW], F32))
        AR = Arena(arena_t, NW)
        psum = es.enter_context(nc.psum_tensor("psum", [128, 8, 512], F32))
        PB = [psum[:, i, :] for i in range(8)]
        PBb = [psum[:, i, :].bitcast(BF16) for i in range(8)]
        finals = []

        def tap(name, ap, waits, dt=F32):
            if name not in taps:
                return
            shp = list(ap.shape)
            o = nc.dram_tensor("tap_" + name, shp, dt, kind="ExternalOutput").ap()
            tap_out[name] = shp
            finals.append(P.dma("sync", o, ap, "tap_" + name, waits=waits))

        cb_t = AR.alloc([128, 384], BF16)
        ident = cb_t[:, 0:128]
        rotm = cb_t[:, 128:256]
        onesb = cb_t[:, 256:384]
        cf_t = AR.alloc([128, 512], F32)
        identf = cf_t[:, 0:128]
        onesf = cf_t[:, 128:256]
        trif = cf_t[:, 256:384]
        invf = cf_t[:, 384:385]
        ecap = cf_t[:, 392:424]
        mhalf = cf_t[:, 424:425]
        epsc = cf_t[:, 425:426]
        d_cb = P.dma("gpsimd", cb_t, cbf[:, 0:384], "cb")
        d_cf = P.dma("sync", cf_t, cf32, "cf")
        QO = AR.alloc([128, 4, NOWN], BF16)
        small = AR.alloc([128, 64], F32)
        neglam = small[:, 0:1]
        gsc = small[:, 1:2]
        lamv = AR.alloc([128, 256], F32)
        slg = AR.alloc([128, 128], F32)
        d_lam = P.dma("sync", lamv, lam_in.to_broadcast([128, 256]), "lam")
        d_slg = P.dma("sync", slg[:, 0:1], subln_g.rearrange("o p -> p o"), "slg", allow_slow_non_contiguous=True)
        lt = small[:, 8:10]
        t_l1 = P.op("vector", lambda e: e.tensor_tensor(out=lamv[:, 0:64], in0=lamv[:, 0:64], in1=lamv[:, 64:128], op=ALU.mult), waits=[d_lam])
        t_l2 = P.op("vector", lambda e: e.tensor_tensor(out=lamv[:, 128:192], in0=lamv[:, 128:192], in1=lamv[:, 192:256], op=ALU.mult), waits=[d_lam])
        t_l3 = P.op("vector", lambda e: e.tensor_reduce(out=lt[:, 0:1], in_=lamv[:, 0:64], axis=AX.X, op=ALU.add), waits=[t_l1])
        t_l4 = P.op("vector", lambda e: e.tensor_reduce(out=lt[:, 1:2], in_=lamv[:, 128:192], axis=AX.X, op=ALU.add), waits=[t_l2])
        t_l5 = P.op("scalar", lambda e: e.activation(out=small[:, 10:12], in_=lt, func=AF.Exp), waits=[t_l3, t_l4])
        t_l6 = P.op("vector", lambda e: e.tensor_tensor(out=small[:, 12:13], in0=small[:, 11:12], in1=small[:, 10:11], op=ALU.subtract), waits=[t_l5])
        t_l7 = P.op("vector", lambda e: e.tensor_scalar(out=neglam, in0=small[:, 12:13], scalar1=-LAMBDA_INIT, scalar2=None, op0=ALU.add), waits=[t_l6])
        t_g = P.op("vector", lambda e: e.tensor_scalar(out=gsc, in0=slg[:, 0:1], scalar1=1.0 - LAMBDA_INIT, scalar2=None, op0=ALU.mult), waits=[d_slg])
        t_consts = [d_cb, d_cf, t_l7, t_g]
        LG = AR.alloc([128, 32, 36], F32)
        PW = AR.alloc([128, 2, 32], F32)
        SLI = AR.alloc([128, 2, 32], I32)
        pers_mark = AR.mark()

        KT = AR.alloc([128, 2, S], BF16)
        maskt_t = AR.alloc([128, 4096], BF16)
        maskt = maskt_t.rearrange("p (r q) -> p r q", r=8)
        d_mask = P.dma("gpsimd", maskt_t, cbf[:, 384:384 + 4096], "mask", max_dma_last_dim=4096)
        VV = AR.alloc([128, 64, 256], BF16)
        wq = AR.alloc([128, 8, 512], BF16)
        wkv = AR.alloc([128, 8, 512], BF16)
        XBt = [AR.alloc([128, 4, D], BF16) for _ in range(2)]
        XTt = [AR.alloc([128, 8, 512], BF16) for _ in range(2)]
        post = AR.alloc([128, 512], F32)
        tq = AR.alloc([128, 512], F32)
        ki = AR.alloc([128, 512], I32)
        cst = AR.alloc([128, 512], F32)
        snt = AR.alloc([128, 512], F32)
        qsb = [AR.alloc([128, 512], BF16) for _ in range(2)]
        ra = [AR.alloc([128, 512], F32) for _ in range(2)]
        rb = [AR.alloc([128, 512], F32) for _ in range(2)]
        PT = [AR.alloc([128, 2, 512], BF16) for _ in range(3)]
        EE = [AR.alloc([128, 512], F32) for _ in range(4)]

        st = {"xb_free": [None, None], "xt_free": [None, None], "tp_free": [None, None],
              "kp_free": [[], []], "rp_free": [None, None], "vp_free": [None, None],
              "tab_free": [], "qs_free": [None, None], "ra_free": [None, None],
              "n_kp": 0, "n_tp": 0, "n_vp": 0, "n_x": 0}

        def load_w(dst, src_cols, sem, waits):
            return P.dma("gpsimd", dst, src_cols.rearrange("(c p) n -> p c n", p=128), sem, waits=waits)

        def tables(pos_src, t0):
            w0 = list(st["tab_free"])
            d = P.dma("gpsimd", post, pos_src[0:1, t0:t0 + 512].to_broadcast([128, 512]), "pos", waits=w0)
            prev = []
            for dst, add in ((snt, 0.0), (cst, 0.25)):
                a = P.op("vector", lambda e, add=add: e.tensor_scalar(out=tq, in0=post, scalar1=invf, scalar2=add, op0=ALU.mult, op1=ALU.add), waits=[d, d_cf] + prev)
                b = P.op("vector", lambda e: e.tensor_copy(out=ki, in_=tq), waits=[a])
                c = P.op("vector", lambda e: e.tensor_tensor(out=tq, in0=tq, in1=ki, op=ALU.subtract), waits=[b])
                s_ = P.op("scalar", lambda e, dst=dst: e.activation(out=dst, in_=tq, func=AF.Sin, scale=TWO_PI), waits=[c] + w0)
                prev = [s_]
            return prev[0]

        def load_x_tile(src, row0):
            i = st["n_x"] % 2
            st["n_x"] += 1
            xb = XBt[i]
            d = P.dma("gpsimd", xb, src[row0:row0 + 512, :].rearrange("(b p) d -> p b d", p=128),
                      "xb%d" % i, waits=[st["xb_free"][i]])
            return i, d

        def transpose_tile(i, dx):
            xb = XBt[i]
            xt = XTt[i]
            evs = []
            last_t = None
            for g in range(4):
                bk = st["n_tp"] % 2
                st["n_tp"] += 1
                for cc in range(2):
                    c = 2 * g + cc
                    for blk in range(4):
                        last = (cc == 1 and blk == 3)
                        o_ = PBb[bk][:, cc * 512 + blk * 128: cc * 512 + blk * 128 + 128]
                        i_ = xb[:, blk, c * 128:(c + 1) * 128]
                        tk = P.op("tensor", lambda e, o_=o_, i_=i_: e.transpose(out=o_, in_=i_, identity=ident),
                                  waits=[dx, d_cb, st["tp_free"][bk]], signal=last)
                        if last:
                            last_t = tk
                dst = xt[:, 2 * g:2 * g + 2, :].rearrange("p a b -> p (a b)")
                src = PBb[bk]
                if g % 2 == 0:
                    ev = P.op("vector", lambda e, dst=dst, src=src: e.tensor_copy(out=dst, in_=src), waits=[last_t, st["xt_free"][i]])
                else:
                    ev = P.op("scalar", lambda e, dst=dst, src=src: e.copy(out=dst, in_=src), waits=[last_t, st["xt_free"][i]])
                st["tp_free"][bk] = ev
                evs.append(ev)
            st["xb_free"][i] = last_t
            return evs

        def proj_rope(xt, evs, wt, wcol, tab_tok, dst, extra_w):
            kb = st["n_kp"] % 2
            st["n_kp"] += 1
            kp = PB[2 + kb]
            rp = PB[4 + kb]
            q_ = qsb[kb]
            ra_ = ra[kb]
            rb_ = rb[kb]
            mm = None
            for c in range(8):
                l_ = wt[:, c, wcol:wcol + 128]
                r_ = xt[:, c, :]
                mm = P.op("tensor", lambda e, l_=l_, r_=r_, c=c: e.matmul(kp, lhsT=l_, rhs=r_, start=(c == 0), stop=(c == 7)),
                          waits=evs + st["kp_free"][kb] + extra_w, signal=(c == 7))
            cp = P.op("scalar", lambda e: e.copy(out=q_, in_=kp), waits=[mm, st["qs_free"][kb]])
            rm = P.op("tensor", lambda e: e.matmul(rp, lhsT=rotm, rhs=q_, start=True, stop=True), waits=[cp, d_cb, st["rp_free"][kb]])
            st["qs_free"][kb] = rm
            a = P.op("vector", lambda e: e.tensor_tensor(out=ra_, in0=kp, in1=cst, op=ALU.mult), waits=[mm, cp, tab_tok, st["ra_free"][kb]])
            b = P.op("vector", lambda e: e.tensor_tensor(out=rb_, in0=rp, in1=snt, op=ALU.mult), waits=[rm, tab_tok, st["ra_free"][kb]])
            st["kp_free"][kb] = [a, cp]
            st["rp_free"][kb] = b
            f = P.op("gpsimd", lambda e: e.tensor_tensor(out=dst, in0=ra_, in1=rb_, op=ALU.add), waits=[a, b])
            st["ra_free"][kb] = f
            return f, b, rm

        import os
        KD = int(os.environ.get("KDBG", "99"))

        def a_tile(T, dwk, dwv, last):
            i, dx = load_x_tile(xf, T * 512)
            last.append(dx)
            if KD < 1:
                return
            tab = tables(posf, T * 512)
            last.append(tab)
            if KD < 2:
                return
            evs = transpose_tile(i, dx)
            last.extend(evs)
            if KD < 3:
                return
            tabfree = []
            for hl in range(2):
                f, b, rm = proj_rope(XTt[i], evs, wkv, hl * 128, tab, KT[:, hl, T * 512:(T + 1) * 512], [dwk])
                tabfree.append(b)
                last.append(f)
            st["tab_free"] = tabfree
            if KD < 4:
                return
            mm = None
            for blk in range(4):
                vb = st["n_vp"] % 2
                st["n_vp"] += 1
                vp = PB[6 + vb][:, 0:256]
                for c in range(8):
                    l_ = XTt[i][:, c, blk * 128:(blk + 1) * 128]
                    r_ = wkv[:, c, 256:512]
                    mm = P.op("tensor", lambda e, l_=l_, r_=r_, c=c, vp=vp: e.matmul(vp, lhsT=l_, rhs=r_, start=(c == 0), stop=(c == 7)),
                              waits=evs + [dwv, st["vp_free"][vb]], signal=(c == 7))
                o_ = VV[:, T * 4 + blk, :]
                vts = P.op("scalar", lambda e, o_=o_, vp=vp: e.copy(out=o_, in_=vp), waits=[mm])
                st["vp_free"][vb] = vts
                last.append(vts)
            st["xt_free"][i] = mm

        def phase_A(hp):
            dwk = load_w(wkv[:, :, 0:256], w_in[:, 512 + hp * 256: 512 + hp * 256 + 256], "wkv", [])
            dwv = load_w(wkv[:, :, 256:512], w_in[:, 1024 + hp * 256: 1024 + hp * 256 + 256], "wkv", [])
            last = [dwk, dwv]
            for T in range(16 if KD >= 99 else 2):
                a_tile(T, dwk, dwv, last)
            return last

        def q_tile(T, dwq, last):
            i, dx = load_x_tile(xo, T * 512)
            tab = tables(poso, T * 512)
            evs = transpose_tile(i, dx)
            tabfree = []
            rm = None
            for h in range(4):
                f, b, rm = proj_rope(XTt[i], evs, wq, h * 128, tab, QO[:, h, T * 512:(T + 1) * 512], [dwq])
                tabfree.append(b)
                last.append(f)
            st["tab_free"] = tabfree
            st["xt_free"][i] = rm

        def phase_Q():
            dwq = load_w(wq, w_in[:, 0:512], "wq", [])
            last = []
            for T in range(NQT):
                q_tile(T, dwq, last)
            return last

        att = {"s_free": [None, None], "pt_free": [None, None, None], "acc_free": None, "n_s": 0, "n_pt": 0, "ee_free": None}

        def att_unit(i, hl, h):
            nkb = 8 * i + 8
            qk_tok = {}
            qrange = slice(i * 512, (i + 1) * 512)

            def issue_qk(kb):
                s = att["n_s"] % 2
                att["n_s"] += 1
                krange = slice(kb * 128, (kb + 1) * 128)
                P.op("tensor", lambda e: e.matmul(psum[:, 2 * s, :], lhsT=KT[0:64, hl, krange], rhs=QO[0:64, h, qrange], start=True, stop=True),
                     waits=[att["s_free"][s]], signal=False)
                t = P.op("tensor", lambda e: e.matmul(psum[:, 2 * s + 1, :], lhsT=KT[64:128, hl, krange], rhs=QO[64:128, h, qrange], start=True, stop=True))
                qk_tok[kb] = (t, s)

            def pv_step(kb):
                t, s = qk_tok[kb]
                pi = att["n_pt"] % 3
                att["n_pt"] += 1
                pt = PT[pi]
                ex = P.op("scalar", lambda e: e.activation(out=pt, in_=psum[:, 2 * s:2 * s + 2, :], func=AF.Exp, scale=0.125), waits=[t, att["pt_free"][pi]])
                att["s_free"][s] = ex
                pv_w = ex
                if kb >= 8 * i:
                    r = kb - 8 * i
                    pv_w = P.op("vector", lambda e: e.tensor_tensor(out=pt, in0=pt, in1=maskt[:, r, :].unsqueeze(1).to_broadcast([128, 2, 512]), op=ALU.mult),
                                waits=[ex, d_mask])
                s0 = (kb == 0)
                s1 = (kb == nkb - 1)
                w0 = [pv_w, att["acc_free"]] if kb == 0 else [pv_w]
                vv = VV[:, kb, hl * 128:(hl + 1) * 128]
                P.op("tensor", lambda e: e.matmul(PB[4], lhsT=vv, rhs=pt[:, 0, :], start=s0, stop=s1), waits=w0, signal=False)
                P.op("tensor", lambda e: e.matmul(PB[5], lhsT=vv, rhs=pt[:, 1, :], start=s0, stop=s1), signal=False)
                P.op("tensor", lambda e: e.matmul(PB[6], lhsT=onesb, rhs=pt[:, 0, :], start=s0, stop=s1), signal=False)
                pv = P.op("tensor", lambda e: e.matmul(PB[7], lhsT=onesb, rhs=pt[:, 1, :], start=s0, stop=s1))
                att["pt_free"][pi] = pv
                return pv

            issue_qk(0)
            pv = None
            for kb in range(nkb):
                if kb + 1 < nkb:
                    issue_qk(kb + 1)
                pv = pv_step(kb)
            wfree = [att["ee_free"]]
            e0 = P.op("vector", lambda e: e.reciprocal(out=EE[0], in_=PB[6]), waits=[pv] + wfree)
            e1 = P.op("vector", lambda e: e.reciprocal(out=EE[1], in_=PB[7]), waits=[pv] + wfree)
            e2 = P.op("vector", lambda e: e.tensor_tensor(out=EE[0], in0=PB[4], in1=EE[0], op=ALU.mult), waits=[e0])
            e3 = P.op("vector", lambda e: e.tensor_tensor(out=EE[1], in0=PB[5], in1=EE[1], op=ALU.mult), waits=[e1])
            att["acc_free"] = e3
            e4 = P.op("vector", lambda e: e.scalar_tensor_tensor(out=EE[2], in0=EE[1], scalar=neglam, in1=EE[0], op0=ALU.mult, op1=ALU.add), waits=[e2, e3, t_l7] + wfree)
            e5 = P.op("gpsimd", lambda e: e.tensor_tensor(out=EE[3], in0=EE[2], in1=EE[2], op=ALU.mult), waits=[e4] + wfree)
            s = att["n_s"] % 2
            att["n_s"] += 1
            ss = P.op("tensor", lambda e: e.matmul(psum[:, 2 * s, :], lhsT=onesf, rhs=EE[3], start=True, stop=True), waits=[e5, d_cf, att["s_free"][s]])
            e6 = P.op("scalar", lambda e: e.activation(out=EE[3], in_=psum[:, 2 * s, :], func=AF.Ln, scale=1.0 / 128.0, bias=epsc), waits=[ss])
            att["s_free"][s] = e6
            e7 = P.op("scalar", lambda e: e.activation(out=EE[3], in_=EE[3], func=AF.Exp, scale=-0.5), waits=[e6])
            e8 = P.op("vector", lambda e: e.tensor_tensor(out=EE[2], in0=EE[2], in1=EE[3], op=ALU.mult), waits=[e7])
            e9 = P.op("vector", lambda e: e.tensor_scalar(out=QO[:, h, qrange], in0=EE[2], scalar1=gsc, scalar2=None, op0=ALU.mult), waits=[e8, t_g])
            att["ee_free"] = e9
            return e9

        def attention(hp):
            last_tok = None
            for i in range(NQT):
                for hl in range(2):
                    last_tok = att_unit(i, hl, 2 * hp + hl)
            return last_tok

        att_last = None
        if upto == "consts":
            tap("small", small, t_consts)
            P.emit(finals)
            return nc, tap_out
        for hp in range(2):
            kvt = phase_A(hp)
            if upto == "A":
                tap("kt", KT[:, :, 0:2048], kvt, BF16)
                tap("vv", VV[:, 0:8, :], kvt, BF16)
                P.emit(finals)
                return nc, tap_out
            qtoks = phase_Q() if hp == 0 else []
            P.base_waits = [t for t in kvt + qtoks if t is not None] + t_consts
            att_last = attention(hp)
            P.base_waits = [att_last]
            if upto == "att0":
                break
        tap("qo", QO, [att_last], BF16)
        tap("kt", KT[:, :, 0:2048], [att_last], BF16)
        tap("vv", VV[:, 0:8, :], [att_last], BF16)

        if upto in ("att0", "att"):
            P.emit(finals)
            return nc, tap_out

        AR.reset(pers_mark)
        wp = AR.alloc([128, 8, 3584], BF16)
        woa = AR.alloc([128, 4, 1024], BF16)
        woc = AR.alloc([128, 4, 1024], BF16)
        wmx = AR.alloc([128, 8, 1024], BF16)
        xb2 = AR.alloc([128, 2, 1024], BF16)
        xhb = AR.alloc([128, 1024], BF16)
        xres = AR.alloc([128, 2, 1024], F32)
        xT2 = AR.alloc([128, 8, 256], BF16)
        xhT = AR.alloc([128, 32], BF16)
        ccs = AR.alloc([128, 264], F32)
        Ub = AR.alloc([128, 2, 130], F32)
        T1 = AR.alloc([128, 2, 128], F32)
        Zb = AR.alloc([128, 4, 256], BF16)
        g0 = AR.alloc([128, 256], F32)
        g1 = AR.alloc([128, 256], F32)
        m0 = AR.alloc([128, 256], F32)
        m1 = AR.alloc([128, 256], F32)
        MT = AR.alloc([128, 8, 256], BF16)
        Rb = AR.alloc([128, 1024], F32)
        lng = AR.alloc([128, 1024], F32)
        lnb = AR.alloc([128, 1024], F32)
        H1T = AR.alloc([128, 8, 128], F32)
        stt_b = AR.alloc([128, 16], F32)
        bgs = AR.alloc([128, 16], F32)
        cws = AR.alloc([128, 12], F32)
        rbias = AR.alloc([128, 36], F32)
        wr = AR.alloc([128, 8, 36], F32)
        for i7 in range(7):
            P.adma("gpsimd", wp[:, :, i7 * 512:(i7 + 1) * 512], w_in[:, 1536 + i7 * 512:1536 + (i7 + 1) * 512].rearrange("(c p) n -> p c n", p=128), "wp", writes=["wp"])
        P.adma("gpsimd", woa, w_o_att.rearrange("(c p) n -> p c n", p=128), "woa", writes=["woa"])
        P.adma("gpsimd", woc, w_o_conv.rearrange("(c p) n -> p c n", p=128), "woc", writes=["woc"])
        P.adma("gpsimd", wmx, w_mix.rearrange("(c p) n -> p c n", p=128), "wmx", writes=["wmx"])
        P.adma("sync", wr, w_rt.rearrange("(c p) n -> p c n", p=128), "wr", writes=["wr"])
        P.adma("sync", bgs, b_gate.rearrange("o (q p) -> p (o q)", p=128), "bgs", writes=["bgs"], allow_slow_non_contiguous=True)
        P.adma("sync", cws.rearrange("p (k q) -> p k q", k=3), conv_w.rearrange("k (q p) -> p k q", p=128), "cws", writes=["cws"], allow_slow_non_contiguous=True)
        P.adma("sync", rbias, b_rt.to_broadcast([128, 36]), "rbias", writes=["rbias"])
        P.adma("sync", lng, ln1[0:1, :].to_broadcast([128, 1024]), "lng", writes=["lng"])
        P.adma("sync", lnb, ln1[1:2, :].to_broadcast([128, 1024]), "lnb", writes=["lnb"])

        def layer_norm(buf, gname, bname, gt, bt, stt=None):
            stt = stt_b if stt is None else stt
            mv = stt[:, 12:14]
            ve = stt[:, 14:15]
            rs = stt[:, 15:16]
            P.auto("vector", lambda e: e.bn_stats(out=stt[:, 0:6], in_=buf[:, 0:512]), reads=["R"], writes=["stt"])
            P.auto("vector", lambda e: e.bn_stats(out=stt[:, 6:12], in_=buf[:, 512:1024]), reads=["R"], writes=["stt2"])
            P.auto("vector", lambda e: e.bn_aggr(out=mv, in_=stt[:, 0:12]), reads=["stt", "stt2"], writes=["mv"])
            P.auto("vector", lambda e: e.tensor_scalar(out=ve, in0=mv[:, 1:2], scalar1=1e-5, scalar2=None, op0=ALU.add), reads=["mv"], writes=["ve"])
            P.auto("gpsimd", lambda e: e.tensor_tensor(out=rs, in0=ve, in1=mhalf, op=ALU.pow), reads=["ve"], writes=["rs"], extra=[d_cf])
            P.auto("vector", lambda e: e.tensor_scalar(out=buf, in0=buf, scalar1=mv[:, 0:1], scalar2=rs, op0=ALU.subtract, op1=ALU.mult), reads=["mv", "rs"], writes=["R"])
            P.auto("gpsimd", lambda e: e.tensor_tensor(out=buf, in0=buf, in1=gt, op=ALU.mult), reads=[gname], writes=["R"])
            return P.auto("gpsimd", lambda e: e.tensor_tensor(out=buf, in0=buf, in1=bt, op=ALU.add), reads=[bname], writes=["R"])

        def b_tile(tb):
            r0 = tb * 256
            P.adma("gpsimd", xb2, xo[r0:r0 + 256, :].rearrange("(b p) d -> p b d", p=128), "xb2", writes=["xb2"])
            P.adma("gpsimd", xhb[0:4, :], xh[tb * 4:tb * 4 + 4, :], "xhb", writes=["xhb"])
            P.adma("sync", xres, xo[r0:r0 + 256, :].rearrange("(b p) d -> p b d", p=128), "xres", writes=["xres"])
            for half in range(2):
                fns = []
                for cc in range(4):
                    c = half * 4 + cc
                    for blk in range(2):
                        o_ = PBb[half][:, cc * 256 + blk * 128: cc * 256 + blk * 128 + 128]
                        i_ = xb2[:, blk, c * 128:(c + 1) * 128]
                        fns.append(lambda e, o_=o_, i_=i_: e.transpose(out=o_, in_=i_, identity=ident))
                P.auto("tensor", fns, reads=["xb2"], psum=["b%d" % half], extra=[d_cb])
                dst = xT2[:, half * 4:half * 4 + 4, :].rearrange("p a b -> p (a b)")
                if half == 0:
                    P.auto("vector", lambda e, dst=dst: e.tensor_copy(out=dst, in_=PBb[0]), writes=["xT2a"], psum=["b0"])
                else:
                    P.auto("scalar", lambda e, dst=dst: e.copy(out=dst, in_=PBb[1]), writes=["xT2b"], psum=["b1"])
            fns = []
            for c in range(8):
                o_ = PBb[6][:, c * 4:(c + 1) * 4]
                i_ = xhb[0:4, c * 128:(c + 1) * 128]
                fns.append(lambda e, o_=o_, i_=i_: e.transpose(out=o_, in_=i_, identity=ident[0:4, 0:4]))
            P.auto("tensor", fns, reads=["xhb"], psum=["b6"], extra=[d_cb])
            P.auto("vector", lambda e: e.tensor_copy(out=xhT, in_=PBb[6][:, 0:32]), writes=["xhT"], psum=["b6"])
            xhT3 = xhT.rearrange("p (c k) -> p c k", c=8)
            for q in range(4):
                fns = []
                for c in range(8):
                    l_ = wp[:, c, 512 + q * 128: 512 + q * 128 + 128]
                    fns.append(lambda e, l_=l_, c=c: e.matmul(PB[2][:, 0:256], lhsT=l_, rhs=xT2[:, c, :], start=(c == 0), stop=(c == 7)))
                for c in range(8):
                    l_ = wp[:, c, 512 + q * 128: 512 + q * 128 + 128]
                    fns.append(lambda e, l_=l_, c=c: e.matmul(PB[2][:, 256:260], lhsT=l_, rhs=xhT3[:, c, :], start=(c == 0), stop=(c == 7)))
                for c in range(8):
                    l_ = wp[:, c, 1024 + q * 128: 1024 + q * 128 + 128]
                    fns.append(lambda e, l_=l_, c=c: e.matmul(PB[2][:, 260:264], lhsT=l_, rhs=xhT3[:, c, :], start=(c == 0), stop=(c == 7)))
                P.auto("tensor", fns, reads=["wp", "xT2a", "xT2b", "xhT"], psum=["b2"])
                fns = []
                for c in range(8):
                    l_ = wp[:, c, 1024 + q * 128: 1024 + q * 128 + 128]
                    fns.append(lambda e, l_=l_, c=c: e.matmul(PB[3][:, 0:256], lhsT=l_, rhs=xT2[:, c, :], start=(c == 0), stop=(c == 7)))
                for c in range(8):
                    l_ = wp[:, c, q * 128: q * 128 + 128]
                    fns.append(lambda e, l_=l_, c=c: e.matmul(PB[3][:, 256:512], lhsT=l_, rhs=xT2[:, c, :], start=(c == 0), stop=(c == 7)))
                P.auto("tensor", fns, reads=["wp", "xT2a", "xT2b"], psum=["b3"])
                P.auto("scalar", lambda e: e.copy(out=ccs, in_=PB[2][:, 0:264]), writes=["ccs"], psum=["b2"])
                P.auto("vector", lambda e: e.tensor_tensor(out=Ub[:, :, 2:130], in0=ccs[:, 0:256].rearrange("p (b t) -> p b t", b=2),
                                                          in1=PB[3][:, 0:256].rearrange("p (b t) -> p b t", b=2), op=ALU.mult),
                       reads=["ccs"], writes=["U"], psum=["b3"])
                P.auto("vector", lambda e: e.tensor_tensor(out=Ub[:, :, 0:2], in0=ccs[:, 256:260].rearrange("p (b t) -> p b t", b=2),
                                                          in1=ccs[:, 260:264].rearrange("p (b t) -> p b t", b=2), op=ALU.mult),
                       reads=["ccs"], writes=["U2"])
                P.auto("vector", lambda e, q=q: e.tensor_scalar(out=T1, in0=Ub[:, :, 0:128], scalar1=cws[:, q:q + 1], scalar2=None, op0=ALU.mult),
                       reads=["U", "U2", "cws"], writes=["T1"])
                P.auto("vector", lambda e, q=q: e.scalar_tensor_tensor(out=T1, in0=Ub[:, :, 1:129], scalar=cws[:, 4 + q:5 + q], in1=T1, op0=ALU.mult, op1=ALU.add),
                       reads=["U", "U2"], writes=["T1"])
                P.auto("vector", lambda e, q=q: e.scalar_tensor_tensor(out=T1, in0=Ub[:, :, 2:130], scalar=cws[:, 8 + q:9 + q], in1=T1, op0=ALU.mult, op1=ALU.add),
                       reads=["U", "U2"], writes=["T1"])
                P.auto("vector", lambda e, q=q: e.tensor_tensor(out=Zb[:, q, :], in0=T1.rearrange("p b t -> p (b t)"), in1=PB[3][:, 256:512], op=ALU.mult),
                       reads=["T1"], writes=["Z"], psum=["b3"])
            for c8 in range(8):
                fns = []
                for c in range(8):
                    l_ = wp[:, c, 1536 + c8 * 128: 1536 + c8 * 128 + 128]
                    fns.append(lambda e, l_=l_, c=c: e.matmul(PB[4][:, 0:256], lhsT=l_, rhs=xT2[:, c, :], start=(c == 0), stop=(c == 7)))
                for c in range(8):
                    l_ = wp[:, c, 2560 + c8 * 128: 2560 + c8 * 128 + 128]
                    fns.append(lambda e, l_=l_, c=c: e.matmul(PB[4][:, 256:512], lhsT=l_, rhs=xT2[:, c, :], start=(c == 0), stop=(c == 7)))
                P.auto("tensor", fns, reads=["wp", "xT2a", "xT2b"], psum=["b4"])
                P.auto("scalar", lambda e, c8=c8: e.activation(out=g0, in_=PB[4][:, 0:256], func=AF.Sigmoid, bias=bgs[:, c8:c8 + 1]), reads=["bgs"], writes=["g0"], psum=["b4"])
                P.auto("scalar", lambda e, c8=c8: e.activation(out=g1, in_=PB[4][:, 256:512], func=AF.Sigmoid, bias=bgs[:, 8 + c8:9 + c8]), reads=["bgs"], writes=["g1"], psum=["b4"])
                fns = []
                for h in range(4):
                    l_ = woa[:, h, c8 * 128:(c8 + 1) * 128]
                    r_ = QO[:, h, r0:r0 + 256]
                    fns.append(lambda e, l_=l_, r_=r_, h=h: e.matmul(PB[5][:, 0:256], lhsT=l_, rhs=r_, start=(h == 0), stop=(h == 3)))
                for q in range(4):
                    l_ = woc[:, q, c8 * 128:(c8 + 1) * 128]
                    r_ = Zb[:, q, :]
                    fns.append(lambda e, l_=l_, r_=r_, q=q: e.matmul(PB[5][:, 256:512], lhsT=l_, rhs=r_, start=(q == 0), stop=(q == 3)))
                P.auto("tensor", fns, reads=["woa", "woc", "Z"], psum=["b5"])
                P.auto("vector", lambda e: e.tensor_tensor(out=m0, in0=g0, in1=PB[5][:, 0:256], op=ALU.mult), reads=["g0"], writes=["m0"], psum=["b5"])
                P.auto("vector", lambda e: e.tensor_tensor(out=m1, in0=g1, in1=PB[5][:, 256:512], op=ALU.mult), reads=["g1"], writes=["m1"], psum=["b5"])
                P.auto("gpsimd", lambda e, c8=c8: e.tensor_tensor(out=MT[:, c8, :], in0=m0, in1=m1, op=ALU.add), reads=["m0", "m1"], writes=["MT"])
            for blk in range(2):
                gb = tb * 2 + blk
                fns = []
                for half in range(2):
                    for c in range(8):
                        l_ = MT[:, c, blk * 128:(blk + 1) * 128]
                        r_ = wmx[:, c, half * 512:(half + 1) * 512]
                        fns.append(lambda e, l_=l_, r_=r_, c=c, half=half: e.matmul(PB[6 + half], lhsT=l_, rhs=r_, start=(c == 0), stop=(c == 7)))
                P.auto("tensor", fns, reads=["MT", "wmx"], psum=["b6", "b7"])
                P.auto("vector", lambda e, blk=blk: e.scalar_tensor_tensor(out=Rb[:, 0:512], in0=xres[:, blk, 0:512], scalar=ALPHA, in1=PB[6], op0=ALU.mult, op1=ALU.add),
                       reads=["xres"], writes=["R"], psum=["b6"])
                P.auto("vector", lambda e, blk=blk: e.scalar_tensor_tensor(out=Rb[:, 512:1024], in0=xres[:, blk, 512:1024], scalar=ALPHA, in1=PB[7], op0=ALU.mult, op1=ALU.add),
                       reads=["xres"], writes=["R"], psum=["b7"])
                layer_norm(Rb, "lng", "lnb", lng, lnb)
                finals_h1.append(P.adma("sync", h1f[gb * 128:(gb + 1) * 128, :], Rb, "h1w", reads=["R"]))
                fns = []
                for c in range(8):
                    o_ = PB[c // 4][:, (c % 4) * 128:(c % 4) * 128 + 128]
                    i_ = Rb[:, c * 128:(c + 1) * 128]
                    fns.append(lambda e, o_=o_, i_=i_: e.transpose(out=o_, in_=i_, identity=identf))
                P.auto("tensor", fns, reads=["R"], psum=["b0", "b1"], extra=[d_cf])
                P.auto("scalar", lambda e: e.copy(out=H1T[:, 0:4, :].rearrange("p a b -> p (a b)"), in_=PB[0]), writes=["H1Ta"], psum=["b0"])
                P.auto("vector", lambda e: e.tensor_copy(out=H1T[:, 4:8, :].rearrange("p a b -> p (a b)"), in_=PB[1]), writes=["H1Tb"], psum=["b1"])
                fns = []
                for c in range(8):
                    fns.append(lambda e, c=c: e.matmul(PB[0][:, 0:36], lhsT=H1T[:, c, :], rhs=wr[:, c, :], start=(c == 0), stop=(c == 7)))
                P.auto("tensor", fns, reads=["H1Ta", "H1Tb", "wr"], psum=["b0"])
                P.auto("vector", lambda e, gb=gb: e.tensor_tensor(out=LG[:, gb, :], in0=PB[0][:, 0:36], in1=rbias, op=ALU.add), reads=["rbias"], writes=["LG"], psum=["b0"])

        finals_h1 = []
        NTB = NQT * 2
        for tb in range(NTB):
            b_tile(tb)
        tap("lg", LG, [P.lw["LG"]])
        tap("h1", h1f[0:256, :], finals_h1)
        if upto == "B":
            P.emit(finals + finals_h1)
            return nc, tap_out

        P.base_waits = [P.lw["LG"]] + finals_h1
        AR.reset(pers_mark)
        NB = NTB * 2
        gm = AR.alloc([128, 32], F32)
        gd = AR.alloc([128, 32, 4], F32)
        pen = AR.alloc([128, 32, 4], F32)
        ge = AR.alloc([128, 32, 4], F32)
        gs = AR.alloc([128, 32], F32)
        gw = AR.alloc([128, 32], F32)
        em = AR.alloc([128, 32, 32], F32)
        em2 = AR.alloc([128, 32, 32], F32)
        oh1 = AR.alloc([128, 32, 32], F32)
        oh2 = AR.alloc([128, 32, 32], F32)
        Aa = AR.alloc([128, 32, 32], F32)
        Tt = AR.alloc([128, 32, 32], F32)
        sc0 = AR.alloc([128, 32, 32], F32)
        sc1 = AR.alloc([128, 32, 32], F32)
        rank = AR.alloc([128, 32, 32], F32)
        tmpc = AR.alloc([128, 32, 32], F32)
        v1 = AR.alloc([128, 32], F32)
        v2 = AR.alloc([128, 32], F32)
        dv = AR.alloc([128, 32], F32)
        s12 = AR.alloc([128, 2, 32], F32)
        stt_c = AR.alloc([128, 16], F32)
        fl = lambda t: t.rearrange("p a b -> p (a b)")
        gl = LG[:, :, 0:4]
        el = LG[:, :, 4:36]
        bc3 = lambda t: t.unsqueeze(2).to_broadcast([128, 32, 32])
        V = "vector"
        P.auto(V, lambda e: e.tensor_reduce(out=gm, in_=gl, axis=AX.X, op=ALU.max), reads=["LG"], writes=["gm"])
        P.auto(V, lambda e: e.tensor_tensor(out=gd, in0=gl, in1=gm.unsqueeze(2).to_broadcast([128, 32, 4]), op=ALU.subtract), reads=["LG", "gm"], writes=["gd"])
        P.auto(V, lambda e: e.tensor_scalar(out=pen, in0=gd, scalar1=0.0, scalar2=NEG, op0=ALU.is_lt, op1=ALU.mult), reads=["gd"], writes=["pen"])
        P.auto("scalar", lambda e: e.activation(out=ge, in_=gd, func=AF.Exp), reads=["gd"], writes=["ge"])
        P.auto(V, lambda e: e.tensor_reduce(out=gs, in_=ge, axis=AX.X, op=ALU.add), reads=["ge"], writes=["gs"])
        P.auto(V, lambda e: e.reciprocal(out=gw, in_=gs), reads=["gs"], writes=["gw"])
        P.auto(V, lambda e: e.tensor_tensor(out=em.rearrange("p b (g k) -> p b g k", g=4), in0=el.rearrange("p b (g k) -> p b g k", g=4),
                                            in1=pen.unsqueeze(3).to_broadcast([128, 32, 4, 8]), op=ALU.add), reads=["LG", "pen"], writes=["em"])
        P.auto(V, lambda e: e.tensor_reduce(out=v1, in_=em, axis=AX.X, op=ALU.max), reads=["em"], writes=["v1"])
        P.auto(V, lambda e: e.tensor_tensor(out=oh1, in0=em, in1=bc3(v1), op=ALU.is_equal), reads=["em", "v1"], writes=["oh1"])
        P.auto(V, lambda e: e.scalar_tensor_tensor(out=fl(em2), in0=fl(oh1), scalar=NEG, in1=fl(em), op0=ALU.mult, op1=ALU.add), reads=["oh1", "em"], writes=["em2"])
        P.auto(V, lambda e: e.tensor_reduce(out=v2, in_=em2, axis=AX.X, op=ALU.max), reads=["em2"], writes=["v2"])
        P.auto(V, lambda e: e.tensor_tensor(out=oh2, in0=em2, in1=bc3(v2), op=ALU.is_equal), reads=["em2", "v2"], writes=["oh2"])
        P.auto(V, lambda e: e.tensor_tensor(out=dv, in0=v2, in1=v1, op=ALU.subtract), reads=["v1", "v2"], writes=["dv"])
        P.auto("scalar", lambda e: e.activation(out=dv, in_=dv, func=AF.Exp), reads=[], writes=["dv"])
        P.auto(V, lambda e: e.tensor_scalar(out=dv, in0=dv, scalar1=1.0, scalar2=None, op0=ALU.add), writes=["dv"])
        P.auto(V, lambda e: e.reciprocal(out=dv, in_=dv), writes=["dv"])
        P.auto(V, lambda e: e.tensor_tensor(out=PW[:, 0, :], in0=dv, in1=gw, op=ALU.mult), reads=["dv", "gw"], writes=["PW0"])
        P.auto(V, lambda e: e.tensor_tensor(out=PW[:, 1, :], in0=gw, in1=PW[:, 0, :], op=ALU.subtract), reads=["PW0", "gw"], writes=["PW1"])
        P.auto(V, lambda e: e.tensor_tensor(out=fl(Aa), in0=fl(oh1), in1=fl(oh2), op=ALU.add), reads=["oh1", "oh2"], writes=["Aa"])
        Af = fl(Aa)
        P.auto("tensor", [lambda e: e.matmul(PB[0], lhsT=trif, rhs=Af[:, 0:512], start=True, stop=True),
                          lambda e: e.matmul(PB[1], lhsT=trif, rhs=Af[:, 512:1024], start=True, stop=True),
                          lambda e: e.matmul(PB[2], lhsT=onesf, rhs=Af[:, 0:512], start=True, stop=True),
                          lambda e: e.matmul(PB[3], lhsT=onesf, rhs=Af[:, 512:1024], start=True, stop=True)],
               reads=["Aa"], psum=["b0", "b1", "b2", "b3"], extra=[d_cf])
        P.auto(V, lambda e: e.tensor_copy(out=fl(Tt)[:, 0:512], in_=PB[2]), writes=["Tt"], psum=["b2"])
        P.auto(V, lambda e: e.tensor_copy(out=fl(Tt)[:, 512:1024], in_=PB[3]), writes=["Tt"], psum=["b3"])
        cur, cname = Tt, "Tt"
        for si, sh in enumerate((1, 2, 4, 8, 16)):
            nxt, nname = (sc0, "sc0") if si % 2 == 0 else (sc1, "sc1")
            P.auto(V, lambda e, cur=cur, nxt=nxt, sh=sh: e.tensor_tensor(out=nxt[:, sh:32, :], in0=cur[:, sh:32, :], in1=cur[:, 0:32 - sh, :], op=ALU.add), reads=[cname], writes=[nname])
            P.auto(V, lambda e, cur=cur, nxt=nxt, sh=sh: e.tensor_copy(out=nxt[:, 0:sh, :], in_=cur[:, 0:sh, :]), reads=[cname], writes=[nname])
            cur, cname = nxt, nname
        P.auto(V, lambda e, cur=cur: e.tensor_tensor(out=fl(tmpc), in0=fl(cur), in1=fl(Tt), op=ALU.subtract), reads=[cname, "Tt"], writes=["tmpc"])
        P.auto(V, lambda e: e.tensor_tensor(out=fl(rank)[:, 0:512], in0=fl(tmpc)[:, 0:512], in1=PB[0], op=ALU.add), reads=["tmpc"], writes=["rank"], psum=["b0"])
        P.auto(V, lambda e: e.tensor_tensor(out=fl(rank)[:, 512:1024], in0=fl(tmpc)[:, 512:1024], in1=PB[1], op=ALU.add), reads=["tmpc"], writes=["rank"], psum=["b1"])
        P.auto(V, lambda e: e.tensor_scalar(out=fl(rank), in0=fl(rank), scalar1=float(CAP - 1), scalar2=None, op0=ALU.min), writes=["rank"])
        P.auto(V, lambda e: e.tensor_tensor(out=rank, in0=rank, in1=ecap.unsqueeze(1).to_broadcast([128, 32, 32]), op=ALU.add), writes=["rank"], extra=[d_cf])
        P.auto(V, lambda e: e.tensor_tensor(out=fl(tmpc), in0=fl(oh1), in1=fl(rank), op=ALU.mult), reads=["oh1", "rank"], writes=["tmpc"])
        P.auto(V, lambda e: e.tensor_reduce(out=s12[:, 0, :], in_=tmpc, axis=AX.X, op=ALU.add), reads=["tmpc"], writes=["s12a"])
        P.auto(V, lambda e: e.tensor_tensor(out=fl(tmpc), in0=fl(oh2), in1=fl(rank), op=ALU.mult), reads=["oh2", "rank"], writes=["tmpc"])
        P.auto(V, lambda e: e.tensor_reduce(out=s12[:, 1, :], in_=tmpc, axis=AX.X, op=ALU.add), reads=["tmpc"], writes=["s12b"])
        P.auto(V, lambda e: e.tensor_scalar(out=fl(s12), in0=fl(s12), scalar1=float(NSLOT - 1), scalar2=0.0, op0=ALU.min, op1=ALU.max), reads=["s12a", "s12b"], writes=["s12a", "s12b"])
        P.auto(V, lambda e: e.tensor_copy(out=SLI, in_=s12), reads=["s12a", "s12b"], writes=["SLI"])
        tap("sli", SLI, [P.lw["SLI"]], I32)
        tap("pw", PW, [P.lw["PW1"]])

        hb = [AR.alloc([128, 1024], F32) for _ in range(2)]
        sc_toks = []
        for blk in range(NB):
            i = blk % 2
            P.adma("sync", hb[i], h1f[blk * 128:(blk + 1) * 128, :], "hb%d" % i, writes=["hb%d" % i])
            for k in range(2):
                off = SLI[:, k, blk:blk + 1].bitcast(U32)
                src = hb[i]
                sc_toks.append(P.acdma("gpsimd", lambda e, off=off, src=src: e.indirect_dma_start(
                    out=xs_d, out_offset=bass.IndirectOffsetOnAxis(ap=off, axis=0), in_=src, in_offset=None),
                    "sc%d_%d" % (i, k), reads=["hb%d" % i, "SLI"]))

        wg = [AR.alloc([128, 8, 512], BF16) for _ in range(2)]
        wu = [AR.alloc([128, 8, 512], BF16) for _ in range(2)]
        wd = [AR.alloc([128, 4, 1024], BF16) for _ in range(2)]
        XE = [AR.alloc([128, 3, 1024], BF16) for _ in range(2)]
        XT = AR.alloc([128, 8, 384], BF16)
        sg = AR.alloc([128, 384], F32)
        HT = AR.alloc([128, 4, 384], BF16)
        YSb = [AR.alloc([128, 1024], F32) for _ in range(2)]
        NE = 32
        ys_toks = []

        def e_loads(e_):
            i = e_ % 2
            P.adma("gpsimd", wg[i], w_eg[e_].rearrange("(c p) n -> p c n", p=128), "wg%d" % i, writes=["wg%d" % i])
            P.adma("gpsimd", wu[i], w_eu[e_].rearrange("(c p) n -> p c n", p=128), "wu%d" % i, writes=["wu%d" % i])
            P.adma("gpsimd", wd[i], w_ed[e_].rearrange("(c p) n -> p c n", p=128), "wd%d" % i, writes=["wd%d" % i])
            P.adma("sync", XE[i], xs_d[e_ * CAP:(e_ + 1) * CAP, :].rearrange("(b p) d -> p b d", p=128), "xe%d" % i, writes=["xe%d" % i], extra=sc_toks)

        def e_compute(e_):
            i = e_ % 2
            for pr in range(4):
                bank = pr % 2
                fns = []
                for cc in range(2):
                    c = 2 * pr + cc
                    for sb in range(3):
                        o_ = PBb[bank][:, cc * 384 + sb * 128: cc * 384 + sb * 128 + 128]
                        i_ = XE[i][:, sb, c * 128:(c + 1) * 128]
                        fns.append(lambda e, o_=o_, i_=i_: e.transpose(out=o_, in_=i_, identity=ident))
                P.auto("tensor", fns, reads=["xe%d" % i], psum=["b%d" % bank])
                dst = XT[:, 2 * pr:2 * pr + 2, :].rearrange("p a b -> p (a b)")
                if bank == 0:
                    P.auto("vector", lambda e, dst=dst: e.tensor_copy(out=dst, in_=PBb[0][:, 0:768]), writes=["XT%d" % pr], psum=["b0"])
                else:
                    P.auto("scalar", lambda e, dst=dst: e.copy(out=dst, in_=PBb[1][:, 0:768]), writes=["XT%d" % pr], psum=["b1"])
            xt_names = ["XT%d" % pr for pr in range(4)]
            for f in range(4):
                gb_ = 2 + 2 * (f % 2)
                ub_ = 3 + 2 * (f % 2)
                fns = []
                for c in range(8):
                    l_ = wg[i][:, c, f * 128:(f + 1) * 128]
                    fns.append(lambda e, l_=l_, c=c, gb_=gb_: e.matmul(PB[gb_][:, 0:384], lhsT=l_, rhs=XT[:, c, :], start=(c == 0), stop=(c == 7)))
                P.auto("tensor", fns, reads=["wg%d" % i] + xt_names, psum=["b%d" % gb_])
                fns = []
                for c in range(8):
                    l_ = wu[i][:, c, f * 128:(f + 1) * 128]
                    fns.append(lambda e, l_=l_, c=c, ub_=ub_: e.matmul(PB[ub_][:, 0:384], lhsT=l_, rhs=XT[:, c, :], start=(c == 0), stop=(c == 7)))
                P.auto("tensor", fns, reads=["wu%d" % i] + xt_names, psum=["b%d" % ub_])
                P.auto("scalar", lambda e, gb_=gb_: e.activation(out=sg, in_=PB[gb_][:, 0:384], func=AF.Silu), writes=["sg"], psum=["b%d" % gb_])
                P.auto("vector", lambda e, ub_=ub_, f=f: e.tensor_tensor(out=HT[:, f, :], in0=sg, in1=PB[ub_][:, 0:384], op=ALU.mult), reads=["sg"], writes=["HT%d" % f], psum=["b%d" % ub_])
            ht_names = ["HT%d" % f for f in range(4)]
            for sb in range(3):
                k = (e_ * 3 + sb) % 2
                fns = []
                for half in range(2):
                    for f in range(4):
                        l_ = HT[:, f, sb * 128:(sb + 1) * 128]
                        r_ = wd[i][:, f, half * 512:(half + 1) * 512]
                        fns.append(lambda e, l_=l_, r_=r_, f=f, half=half: e.matmul(PB[6 + half], lhsT=l_, rhs=r_, start=(f == 0), stop=(f == 3)))
                P.auto("tensor", fns, reads=["wd%d" % i] + ht_names, psum=["b6", "b7"])
                ysb = YSb[k]
                P.auto("scalar", lambda e, ysb=ysb: e.copy(out=ysb[:, 0:512], in_=PB[6]), writes=["ysa%d" % k], psum=["b6"])
                P.auto("vector", lambda e, ysb=ysb: e.tensor_copy(out=ysb[:, 512:1024], in_=PB[7]), writes=["ysb%d" % k], psum=["b7"])
                r0 = e_ * CAP + sb * 128
                ys_toks.append(P.adma("sync", ys_d[r0:r0 + 128, :], ysb, "ysw%d" % k, reads=["ysa%d" % k, "ysb%d" % k]))

        e_loads(0)
        for e_ in range(NE):
            if e_ + 1 < NE:
                e_loads(e_ + 1)
            e_compute(e_)

        Y1 = AR.alloc([128, 1024], F32)
        Y2 = AR.alloc([128, 1024], F32)
        hc = AR.alloc([128, 1024], F32)
        R2 = AR.alloc([128, 1024], F32)
        lng2 = AR.alloc([128, 1024], F32)
        lnb2 = AR.alloc([128, 1024], F32)
        P.adma("sync", lng2, ln2[0:1, :].to_broadcast([128, 1024]), "lng2", writes=["lng2"])
        P.adma("sync", lnb2, ln2[1:2, :].to_broadcast([128, 1024]), "lnb2", writes=["lnb2"])
        for blk in range(NB):
            P.adma("sync", hc, h1f[blk * 128:(blk + 1) * 128, :], "hc", writes=["hc"])
            for k, yb in ((0, Y1), (1, Y2)):
                off = SLI[:, k, blk:blk + 1].bitcast(U32)
                P.acdma("gpsimd", lambda e, off=off, yb=yb: e.indirect_dma_start(
                    out=yb, out_offset=None, in_=ys_d, in_offset=bass.IndirectOffsetOnAxis(ap=off, axis=0)),
                    "yg%d" % k, reads=["SLI"], writes=["Y%d" % k], extra=ys_toks)
            P.auto("scalar", lambda e: e.mul(out=R2, in_=hc, mul=ALPHA), reads=["hc"], writes=["R"])
            P.auto(V, lambda e, blk=blk: e.scalar_tensor_tensor(out=R2, in0=Y1, scalar=PW[:, 0, blk:blk + 1], in1=R2, op0=ALU.mult, op1=ALU.add), reads=["Y0", "PW0"], writes=["R"])
            P.auto(V, lambda e, blk=blk: e.scalar_tensor_tensor(out=R2, in0=Y2, scalar=PW[:, 1, blk:blk + 1], in1=R2, op0=ALU.mult, op1=ALU.add), reads=["Y1", "PW1"], writes=["R"])
            layer_norm(R2, "lng2", "lnb2", lng2, lnb2, stt_c)
            finals.append(P.adma("sync", out[blk * 128:(blk + 1) * 128, :], R2, "outw", reads=["R"]))

        P.emit(finals)
    return nc, tap_out


def _consts(j):
    ident = np.eye(128, dtype=np.float32)
    rot = np.zeros((128, 128), np.float32)
    for m in range(128):
        if (m % 64) < 32:
            rot[m + 32, m] = -1.0
        else:
            rot[m - 32, m] = 1.0
    ones = np.ones((128, 128), np.float32)
    kk = np.arange(128)[:, None, None]
    r = np.arange(8)[None, :, None]
    qq = np.arange(512)[None, None, :]
    mask = ((r * 128 + kk) <= ((2 * (qq // 128) + j) * 128 + (qq % 128))).astype(np.float32)
    cbf = np.concatenate([ident, rot, ones, mask.reshape(128, 4096)], axis=1)
    cf = np.zeros((128, 512), np.float32)
    cf[:, 0:128] = ident
    cf[:, 128:256] = 1.0
    cf[:, 256:384] = (np.arange(128)[:, None] < np.arange(128)[None, :]).astype(np.float32)
    inv_freq = (np.float32(10000.0) ** (-np.arange(0, 64, 2, dtype=np.float32) / np.float32(64))).astype(np.float32)
    p = np.arange(128)
    cf[:, 384] = (inv_freq[(p % 64) % 32].astype(np.float64) / (2.0 * np.pi)).astype(np.float32)
    cf[:, 392:424] = (np.arange(32) * CAP)[None, :]
    cf[:, 424] = -0.5
    cf[:, 425] = 1e-5
    return np.ascontiguousarray(cbf), cf


def make_core_inputs(inp, c):
    b, j = c // 2, c % 2
    x = inp["x"]
    xb_ = x[b]
    blocks = xb_.reshape(64, 128, D)
    xo = np.ascontiguousarray(blocks[j::2].reshape(NOWN, D))
    xh = np.zeros((32, 2, D), np.float32)
    for m in range(32):
        blk = 2 * m + j
        if blk > 0:
            xh[m] = xb_[blk * 128 - 2: blk * 128]
    pos = np.asarray(inp["positions"][b], dtype=np.int32)
    poso = np.ascontiguousarray(pos.reshape(64, 128)[j::2].reshape(1, NOWN))
    cbf, cf = _consts(j)
    f = lambda a: np.ascontiguousarray(np.asarray(a, dtype=np.float32))
    return {
        "xf": f(xb_), "xo": xo, "xh": f(xh.reshape(64, D)), "posf": np.ascontiguousarray(pos.reshape(1, S)), "poso": poso,
        "w_in": f(inp["w_in"][0]), "b_gate": f(inp["b_gate"][0].reshape(1, 2048)),
        "lam_in": f(np.concatenate([inp["lambda_q1"][0], inp["lambda_k1"][0], inp["lambda_q2"][0], inp["lambda_k2"][0]]).reshape(1, 256)),
        "subln_g": f(inp["subln_g"][0].reshape(1, 128)), "w_o_att": f(inp["w_o_att"][0]), "conv_w": f(inp["conv_w"][0]),
        "w_o_conv": f(inp["w_o_conv"][0]), "w_mix": f(inp["w_mix_out"][0]),
        "ln1": f(np.stack([inp["ln1_g"][0], inp["ln1_b"][0]])), "ln2": f(np.stack([inp["ln2_g"][0], inp["ln2_b"][0]])),
        "w_rt": f(np.concatenate([inp["w_router_group"][0], inp["w_router_expert"][0]], axis=1)),
        "b_rt": f(np.concatenate([inp["b_router_group"][0], inp["b_router_expert"][0]]).reshape(1, 36)),
        "w_eg": f(inp["w_exp_gate"][0]), "w_eu": f(inp["w_exp_up"][0]), "w_ed": f(inp["w_exp_down"][0]),
        "cbf": cbf, "cf32": cf,
    }


def kernel(**inputs):
    inp = {k: np.asarray(v) for k, v in inputs.items()}
    nc, _ = build()
    in_maps = [make_core_inputs(inp, c) for c in range(8)]
    res = run_bass_kernel_spmd(nc, in_maps, core_ids=list(range(8)))
    outp = np.zeros((4, S, D), np.float32)
    for c in range(8):
        b, j = c // 2, c % 2
        o = np.asarray(res.results[c]["out"]).reshape(32, 128, D)
        outp[b].reshape(64, 128, D)[j::2] = o
    return outp
```

```python
import math
import numpy as np
import ml_dtypes
from contextlib import ExitStack
import concourse.bass as bass
import concourse.mybir as mybir
from concourse.bass_utils import run_bass_kernel_spmd

F32 = mybir.dt.float32
BF16 = mybir.dt.bfloat16
I32 = mybir.dt.int32
U32 = mybir.dt.uint32
AF = mybir.ActivationFunctionType
ALU = mybir.AluOpType
AX = mybir.AxisListType
ENGS = ["tensor", "vector", "scalar", "gpsimd", "sync"]

S = 8192
D = 1024
NOWN = 4096
CAP = 384
NSLOT = 32 * CAP
ALPHA = 2.0 ** 0.25
LAMBDA_INIT = 0.8 - 0.6 * math.exp(0.0)
TWO_PI = 2.0 * math.pi
NEG = -1.0e30


class Prog:
    def __init__(self, nc, es):
        self.nc = nc
        self.es = es
        self.ops = {e: [] for e in ENGS}
        self.cnt = {e: 0 for e in ENGS}
        self.esem = {e: es.enter_context(nc.semaphore("es_" + e)) for e in ENGS}
        self.dsem = {}
        self.dcnt = {}
        self.waited = {e: {} for e in ENGS}
        self.base_waits = []

    def _waits(self, eng, waits):
        best = {}
        for w in list(waits) + list(self.base_waits):
            if w is None:
                continue
            sem, val, key = w
            if key not in best or best[key][1] < val:
                best[key] = (sem, val)
        out = []
        for key, (sem, val) in best.items():
            if self.waited[eng].get(key, 0) >= val:
                continue
            self.waited[eng][key] = val
            out.append((sem, val))
        return out

    def op(self, eng, fn, waits=(), signal=True):
        ws = self._waits(eng, waits)
        inc = None
        tok = None
        if signal:
            self.cnt[eng] += 1
            inc = (self.esem[eng], 1)
            tok = (self.esem[eng], self.cnt[eng], "e_" + eng)
        self.ops[eng].append((fn, ws, inc))
        return tok

    def _dsem(self, sem):
        if sem not in self.dsem:
            self.dsem[sem] = self.es.enter_context(self.nc.semaphore("ds_" + sem))
            self.dcnt[sem] = 0
        return self.dsem[sem]

    def dma(self, q, out, in_, sem, waits=(), **kw):
        return self.cdma(q, lambda e: e.dma_start(out=out, in_=in_, **kw), sem, waits)

    def cdma(self, q, fn, sem, waits=()):
        s = self._dsem(sem)
        ws = self._waits(q, waits)
        self.dcnt[sem] += 16
        self.ops[q].append((fn, ws, (s, 16)))
        return (s, self.dcnt[sem], "d_" + sem)

    def _auto_waits(self, reads, writes, psum):
        if not hasattr(self, "lw"):
            self.lw, self.rd, self.pa = {}, {}, {}
        ws = []
        for r in reads:
            ws.append(self.lw.get(r))
        for w in writes:
            ws.append(self.lw.get(w))
            ws.extend(self.rd.get(w, []))
        for p in psum:
            ws.append(self.pa.get(p))
        return ws

    def _auto_done(self, tok, reads, writes, psum):
        for r in reads:
            self.rd.setdefault(r, []).append(tok)
        for w in writes:
            self.lw[w] = tok
            self.rd[w] = []
        for p in psum:
            self.pa[p] = tok

    def auto(self, eng, fns, reads=(), writes=(), psum=(), extra=()):
        if not isinstance(fns, (list, tuple)):
            fns = [fns]
        ws = self._auto_waits(reads, writes, psum) + list(extra)
        tok = None
        for k, fn in enumerate(fns):
            tok = self.op(eng, fn, waits=ws if k == 0 else (), signal=(k == len(fns) - 1))
        self._auto_done(tok, reads, writes, psum)
        return tok

    def adma(self, q, out, in_, sem, reads=(), writes=(), extra=(), **kw):
        ws = self._auto_waits(reads, writes, ()) + list(extra)
        tok = self.dma(q, out, in_, sem, waits=ws, **kw)
        self._auto_done(tok, reads, writes, ())
        return tok

    def acdma(self, q, fn, sem, reads=(), writes=(), extra=()):
        ws = self._auto_waits(reads, writes, ()) + list(extra)
        tok = self.cdma(q, fn, sem, waits=ws)
        self._auto_done(tok, reads, writes, ())
        return tok

    def emit(self, final_waits):
        nc = self.nc
        with nc.Block() as block:
            def mk(eng):
                def body(e):
                    for fn, ws, inc in self.ops[eng]:
                        for sem, val in ws:
                            e.wait_ge(sem, val)
                        ins = fn(e)
                        if inc is not None:
                            ins.then_inc(inc[0], inc[1])
                    if eng == "sync":
                        for w in final_waits:
                            if w is not None:
                                e.wait_ge(w[0], w[1])
                return body
            block.tensor(mk("tensor"))
            block.vector(mk("vector"))
            block.scalar(mk("scalar"))
            block.gpsimd(mk("gpsimd"))
            block.sync(mk("sync"))


class Arena:
    def __init__(self, t, nwords):
        self.t = t
        self.n = nwords
        self.top = 0

    def mark(self):
        return self.top

    def reset(self, m):
        self.top = m

    def alloc(self, shape, dt):
        per = 1
        for s_ in shape[1:]:
            per *= s_
        if dt == BF16:
            words = (per + 1) // 2
        else:
            words = per
        words = (words + 7) // 8 * 8
        a = self.top
        self.top += words
        assert self.top <= self.n, ("arena overflow", self.top, self.n)
        v = self.t[:, a:a + words]
        if dt == BF16:
            v = v.bitcast(BF16)[:, 0:per]
        elif dt == I32:
            v = v.bitcast(I32)[:, 0:per]
        else:
            v = v[:, 0:per]
        if len(shape) == 3:
            v = v.rearrange("p (a b) -> p a b", a=shape[1])
        elif len(shape) == 4:
            v = v.rearrange("p (a b c) -> p a b c", a=shape[1], b=shape[2])
        if shape[0] != 128:
            v = v[0:shape[0]]
        return v


def build(upto="all", taps=(), NQT=8):
    nc = bass.Bass("TRN2", target_bir_lowering=False)
    din = lambda name, shape, dt: nc.dram_tensor(name, shape, dt, kind="ExternalInput").ap()
    xf = din("xf", [S, D], F32)
    xo = din("xo", [NOWN, D], F32)
    xh = din("xh", [64, D], F32)
    posf = din("posf", [1, S], I32)
    poso = din("poso", [1, NOWN], I32)
    w_in = din("w_in", [D, 5120], F32)
    b_gate = din("b_gate", [1, 2048], F32)
    lam_in = din("lam_in", [1, 256], F32)
    subln_g = din("subln_g", [1, 128], F32)
    w_o_att = din("w_o_att", [512, D], F32)
    conv_w = din("conv_w", [3, 512], F32)
    w_o_conv = din("w_o_conv", [512, D], F32)
    w_mix = din("w_mix", [D, D], F32)
    ln1 = din("ln1", [2, D], F32)
    ln2 = din("ln2", [2, D], F32)
    w_rt = din("w_rt", [D, 36], F32)
    b_rt = din("b_rt", [1, 36], F32)
    w_eg = din("w_eg", [32, D, 512], F32)
    w_eu = din("w_eu", [32, D, 512], F32)
    w_ed = din("w_ed", [32, 512, D], F32)
    cbf = din("cbf", [128, 384 + 4096], F32)
    cf32 = din("cf32", [128, 512], F32)
    out = nc.dram_tensor("out", [NOWN, D], F32, kind="ExternalOutput").ap()
    h1f = nc.dram_tensor("h1f", [NOWN, D], F32, kind="Internal").ap()
    xs_d = nc.dram_tensor("xs_d", [NSLOT, D], BF16, kind="Internal").ap()
    ys_d = nc.dram_tensor("ys_d", [NSLOT, D], F32, kind="Internal").ap()
    tap_out = {}

    with ExitStack() as es:
        P = Prog(nc, es)
        NW = 51 * 1024
        arena_t = es.enter_context(nc.sbuf_tensor("arena", [128, NW], F32))
        AR = Arena(arena_t, NW)
        psum = es.enter_context(nc.psum_tensor("psum", [128, 8, 512], F32))
        PB = [psum[:, i, :] for i in range(8)]
        PBb = [psum[:, i, :].bitcast(BF16) for i in range(8)]
        finals = []

        def tap(name, ap, waits, dt=F32):
            if name not in taps:
                return
            shp = list(ap.shape)
            o = nc.dram_tensor("tap_" + name, shp, dt, kind="ExternalOutput").ap()
            tap_out[name] = shp
            finals.append(P.dma("sync", o, ap, "tap_" + name, waits=waits))

        cb_t = AR.alloc([128, 384], BF16)
        ident = cb_t[:, 0:128]
        rotm = cb_t[:, 128:256]
        onesb = cb_t[:, 256:384]
        cf_t = AR.alloc([128, 512], F32)
        identf = cf_t[:, 0:128]
        onesf = cf_t[:, 128:256]
        trif = cf_t[:, 256:384]
        invf = cf_t[:, 384:385]
        ecap = cf_t[:, 392:424]
        mhalf = cf_t[:, 424:425]
        epsc = cf_t[:, 425:426]
        d_cb = P.dma("gpsimd", cb_t, cbf[:, 0:384], "cb")
        d_cf = P.dma("sync", cf_t, cf32, "cf")
        QO = AR.alloc([128, 4, NOWN], BF16)
        small = AR.alloc([128, 64], F32)
        neglam = small[:, 0:1]
        gsc = small[:, 1:2]
        lamv = AR.alloc([128, 256], F32)
        slg = AR.alloc([128, 128], F32)
        d_lam = P.dma("sync", lamv, lam_in.to_broadcast([128, 256]), "lam")
        d_slg = P.dma("sync", slg[:, 0:1], subln_g.rearrange("o p -> p o"), "slg", allow_slow_non_contiguous=True)
        lt = small[:, 8:10]
        t_l1 = P.op("vector", lambda e: e.tensor_tensor(out=lamv[:, 0:64], in0=lamv[:, 0:64], in1=lamv[:, 64:128], op=ALU.mult), waits=[d_lam])
        t_l2 = P.op("vector", lambda e: e.tensor_tensor(out=lamv[:, 128:192], in0=lamv[:, 128:192], in1=lamv[:, 192:256], op=ALU.mult), waits=[d_lam])
        t_l3 = P.op("vector", lambda e: e.tensor_reduce(out=lt[:, 0:1], in_=lamv[:, 0:64], axis=AX.X, op=ALU.add), waits=[t_l1])
        t_l4 = P.op("vector", lambda e: e.tensor_reduce(out=lt[:, 1:2], in_=lamv[:, 128:192], axis=AX.X, op=ALU.add), waits=[t_l2])
        t_l5 = P.op("scalar", lambda e: e.activation(out=small[:, 10:12], in_=lt, func=AF.Exp), waits=[t_l3, t_l4])
        t_l6 = P.op("vector", lambda e: e.tensor_tensor(out=small[:, 12:13], in0=small[:, 11:12], in1=small[:, 10:11], op=ALU.subtract), waits=[t_l5])
        t_l7 = P.op("vector", lambda e: e.tensor_scalar(out=neglam, in0=small[:, 12:13], scalar1=-LAMBDA_INIT, scalar2=None, op0=ALU.add), waits=[t_l6])
        t_g = P.op("vector", lambda e: e.tensor_scalar(out=gsc, in0=slg[:, 0:1], scalar1=1.0 - LAMBDA_INIT, scalar2=None, op0=ALU.mult), waits=[d_slg])
        t_consts = [d_cb, d_cf, t_l7, t_g]
        LG = AR.alloc([128, 32, 36], F32)
        PW = AR.alloc([128, 2, 32], F32)
        SLI = AR.alloc([128, 2, 32], I32)
        pers_mark = AR.mark()

        KT = AR.alloc([128, 2, S], BF16)
        maskt_t = AR.alloc([128, 4096], BF16)
        maskt = maskt_t.rearrange("p (r q) -> p r q", r=8)
        d_mask = P.dma("gpsimd", maskt_t, cbf[:, 384:384 + 4096], "mask", max_dma_last_dim=4096)
        VV = AR.alloc([128, 64, 256], BF16)
        wq = AR.alloc([128, 8, 512], BF16)
        wkv = AR.alloc([128, 8, 512], BF16)
        XBt = [AR.alloc([128, 4, D], BF16) for _ in range(2)]
        XTt = [AR.alloc([128, 8, 512], BF16) for _ in range(2)]
        post2 = [AR.alloc([128, 512], F32) for _ in range(2)]
        tq = AR.alloc([128, 512], F32)
        ki = AR.alloc([128, 512], I32)
        cst2 = [AR.alloc([128, 512], F32) for _ in range(2)]
        snt2 = [AR.alloc([128, 512], F32) for _ in range(2)]
        qsb = [AR.alloc([128, 512], BF16) for _ in range(2)]
        ra = [AR.alloc([128, 512], F32) for _ in range(2)]
        rb = [AR.alloc([128, 512], F32) for _ in range(2)]
        PT = [AR.alloc([128, 2, 512], BF16) for _ in range(3)]
        EE = [AR.alloc([128, 512], F32) for _ in range(4)]

        st = {"xb_free": [None, None], "xt_free": [None, None], "tp_free": [None, None],
              "kp_free": [[], []], "rp_free": [None, None], "vp_free": [None, None],
              "tab_free": [[], []], "qs_free": [None, None], "ra_free": [None, None],
              "n_kp": 0, "n_tp": 0, "n_vp": 0, "n_x": 0, "n_tab": 0, "last_sin": None}

        def load_w(dst, src_cols, sem, waits):
            return P.dma("gpsimd", dst, src_cols.rearrange("(c p) n -> p c n", p=128), sem, waits=waits)

        def tables(pos_src, t0):
            ti = st["n_tab"] % 2
            st["n_tab"] += 1
            w0 = list(st["tab_free"][ti])
            post = post2[ti]
            d = P.dma("gpsimd", post, pos_src[0:1, t0:t0 + 512].to_broadcast([128, 512]), "pos%d" % ti, waits=w0)
            prev = [st["last_sin"]]
            for dst, add in ((snt2[ti], 0.0), (cst2[ti], 0.25)):
                a = P.op("vector", lambda e, add=add: e.tensor_scalar(out=tq, in0=post, scalar1=invf, scalar2=add, op0=ALU.mult, op1=ALU.add), waits=[d, d_cf] + prev)
                b = P.op("vector", lambda e: e.tensor_copy(out=ki, in_=tq), waits=[a])
                c = P.op("vector", lambda e: e.tensor_tensor(out=tq, in0=tq, in1=ki, op=ALU.subtract), waits=[b])
                s_ = P.op("scalar", lambda e, dst=dst: e.activation(out=dst, in_=tq, func=AF.Sin, scale=TWO_PI), waits=[c] + w0)
                prev = [s_]
            st["last_sin"] = prev[0]
            return prev[0], ti

        def load_x_tile(src, row0):
            i = st["n_x"] % 2
            st["n_x"] += 1
            xb = XBt[i]
            d = P.dma("gpsimd", xb, src[row0:row0 + 512, :].rearrange("(b p) d -> p b d", p=128),
                      "xb%d" % i, waits=[st["xb_free"][i]])
            return i, d

        def transpose_tile(i, dx):
            xb = XBt[i]
            xt = XTt[i]
            evs = []
            last_t = None
            for g in range(4):
                bk = st["n_tp"] % 2
                st["n_tp"] += 1
                for cc in range(2):
                    c = 2 * g + cc
                    for blk in range(4):
                        last = (cc == 1 and blk == 3)
                        o_ = PBb[bk][:, cc * 512 + blk * 128: cc * 512 + blk * 128 + 128]
                        i_ = xb[:, blk, c * 128:(c + 1) * 128]
                        tk = P.op("tensor", lambda e, o_=o_, i_=i_: e.transpose(out=o_, in_=i_, identity=ident),
                                  waits=[dx, d_cb, st["tp_free"][bk]], signal=last)
                        if last:
                            last_t = tk
                dst = xt[:, 2 * g:2 * g + 2, :].rearrange("p a b -> p (a b)")
                src = PBb[bk]
                if g % 2 == 0:
                    ev = P.op("vector", lambda e, dst=dst, src=src: e.tensor_copy(out=dst, in_=src), waits=[last_t, st["xt_free"][i]])
                else:
                    ev = P.op("scalar", lambda e, dst=dst, src=src: e.copy(out=dst, in_=src), waits=[last_t, st["xt_free"][i]])
                st["tp_free"][bk] = ev
                evs.append(ev)
            st["xb_free"][i] = last_t
            return evs

        def proj_part1(xt, evs, wt, wcol, tab_tok, ti, extra_w):
            kb = st["n_kp"] % 2
            st["n_kp"] += 1
            kp = PB[2 + kb]
            q_ = qsb[kb]
            ra_ = ra[kb]
            cs_ = cst2[ti]
            mm = None
            for c in range(8):
                l_ = wt[:, c, wcol:wcol + 128]
                r_ = xt[:, c, :]
                mm = P.op("tensor", lambda e, l_=l_, r_=r_, c=c: e.matmul(kp, lhsT=l_, rhs=r_, start=(c == 0), stop=(c == 7)),
                          waits=evs + st["kp_free"][kb] + extra_w, signal=(c == 7))
            cp = P.op("scalar", lambda e: e.copy(out=q_, in_=kp), waits=[mm, st["qs_free"][kb]])
            a = P.op("vector", lambda e: e.tensor_tensor(out=ra_, in0=kp, in1=cs_, op=ALU.mult), waits=[mm, cp, tab_tok, st["ra_free"][kb]])
            st["kp_free"][kb] = [a, cp]
            return {"kb": kb, "cp": cp, "a": a, "tab": tab_tok, "ti": ti, "mm": mm}

        def proj_part2(cx, dst):
            kb = cx["kb"]
            rp = PB[4 + kb]
            q_ = qsb[kb]
            ra_ = ra[kb]
            rb_ = rb[kb]
            sn_ = snt2[cx["ti"]]
            rm = P.op("tensor", lambda e: e.matmul(rp, lhsT=rotm, rhs=q_, start=True, stop=True), waits=[cx["cp"], d_cb, st["rp_free"][kb]])
            st["qs_free"][kb] = rm
            b = P.op("vector", lambda e: e.tensor_tensor(out=rb_, in0=rp, in1=sn_, op=ALU.mult), waits=[rm, cx["tab"], st["ra_free"][kb]])
            st["rp_free"][kb] = b
            f = P.op("vector", lambda e: e.tensor_tensor(out=dst, in0=ra_, in1=rb_, op=ALU.add), waits=[cx["a"], b])
            st["ra_free"][kb] = f
            return f, b, rm

        pre = {}

        def prefetch(key, src, pos_src, T):
            i, dx = load_x_tile(src, T * 512)
            tab, ti = tables(pos_src, T * 512)
            pre[key] = (i, dx, tab, ti)

        def a_tile(T, dwk, dwv, last, nxt):
            i, dx, tab, ti = pre.pop(("a", T))
            if nxt is not None:
                prefetch(*nxt)
            evs = transpose_tile(i, dx)
            cxs = [proj_part1(XTt[i], evs, wkv, hl * 128, tab, ti, [dwk]) for hl in range(2)]
            mm = None
            for blk in range(4):
                vb = st["n_vp"] % 2
                st["n_vp"] += 1
                vp = PB[6 + vb][:, 0:256]
                for c in range(8):
                    l_ = XTt[i][:, c, blk * 128:(blk + 1) * 128]
                    r_ = wkv[:, c, 256:512]
                    mm = P.op("tensor", lambda e, l_=l_, r_=r_, c=c, vp=vp: e.matmul(vp, lhsT=l_, rhs=r_, start=(c == 0), stop=(c == 7)),
                              waits=evs + [dwv, st["vp_free"][vb]], signal=(c == 7))
                o_ = VV[:, T * 4 + blk, :]
                vts = P.op("scalar", lambda e, o_=o_, vp=vp: e.copy(out=o_, in_=vp), waits=[mm])
                st["vp_free"][vb] = vts
                last.append(vts)
            st["xt_free"][i] = mm
            tabfree = []
            for hl in range(2):
                f, b, rm = proj_part2(cxs[hl], KT[:, hl, T * 512:(T + 1) * 512])
                tabfree.append(b)
                last.append(f)
            st["tab_free"][ti] = tabfree

        def phase_A(hp, then_q):
            dwk = load_w(wkv[:, :, 0:256], w_in[:, 512 + hp * 256: 512 + hp * 256 + 256], "wkv", [])
            dwv = load_w(wkv[:, :, 256:512], w_in[:, 1024 + hp * 256: 1024 + hp * 256 + 256], "wkv", [])
            last = []
            prefetch(("a", 0), xf, posf, 0)
            for T in range(16):
                if T + 1 < 16:
                    nxt = (("a", T + 1), xf, posf, T + 1)
                elif then_q:
                    nxt = (("q", 0), xo, poso, 0)
                else:
                    nxt = None
                a_tile(T, dwk, dwv, last, nxt)
            return last

        def q_tile(T, dwq, last):
            i, dx, tab, ti = pre.pop(("q", T))
            if T + 1 < NQT:
                prefetch(("q", T + 1), xo, poso, T + 1)
            evs = transpose_tile(i, dx)
            tabfree = []
            rm = None
            cxs = {}
            cxs[0] = proj_part1(XTt[i], evs, wq, 0, tab, ti, [dwq])
            for h in range(4):
                if h + 1 < 4:
                    cxs[h + 1] = proj_part1(XTt[i], evs, wq, (h + 1) * 128, tab, ti, [dwq])
                f, b, rm = proj_part2(cxs[h], QO[:, h, T * 512:(T + 1) * 512])
                tabfree.append(b)
                last.append(f)
            st["tab_free"][ti] = tabfree
            st["xt_free"][i] = rm

        def phase_Q():
            dwq = load_w(wq, w_in[:, 0:512], "wq", [])
            last = []
            for T in range(NQT):
                q_tile(T, dwq, last)
            return last

        ACC0, ACC1 = ra[0], ra[1]
        att = {"a0": None, "a1": None, "accL_free": None, "s_free": [None, None], "pt_free": [[], [], []], "acc_free": None, "n_s": 0, "n_pt": 0, "ee_free": None, "pending": None}

        def att_unit(i, hl, h):
            nkb = 8 * i + 8
            qk_tok = {}
            qrange = slice(i * 512, (i + 1) * 512)

            def issue_qk(kb):
                s = att["n_s"] % 2
                att["n_s"] += 1
                krange = slice(kb * 128, (kb + 1) * 128)
                P.op("tensor", lambda e: e.matmul(psum[:, 2 * s, :], lhsT=KT[0:64, hl, krange], rhs=QO[0:64, h, qrange], start=True, stop=True),
                     waits=[att["s_free"][s]], signal=False)
                t = P.op("tensor", lambda e: e.matmul(psum[:, 2 * s + 1, :], lhsT=KT[64:128, hl, krange], rhs=QO[64:128, h, qrange], start=True, stop=True))
                qk_tok[kb] = (t, s)

            def pv_step(kb):
                t, s = qk_tok[kb]
                pi = att["n_pt"] % 3
                att["n_pt"] += 1
                pt = PT[pi]
                ex = P.op("scalar", lambda e: e.activation(out=pt, in_=psum[:, 2 * s:2 * s + 2, :], func=AF.Exp, scale=0.125), waits=[t] + att["pt_free"][pi])
                att["s_free"][s] = ex
                pv_w = ex
                if kb >= 8 * i:
                    r = kb - 8 * i
                    pv_w = P.op("vector", lambda e: e.tensor_tensor(out=pt, in0=pt, in1=maskt[:, r, :].unsqueeze(1).to_broadcast([128, 2, 512]), op=ALU.mult),
                                waits=[ex, d_mask])
                s0 = (kb == 0)
                s1 = (kb == nkb - 1)
                w0 = [pv_w, att["acc_free"]] if kb == 0 else [pv_w]
                vv = VV[:, kb, hl * 128:(hl + 1) * 128]
                P.op("tensor", lambda e: e.matmul(PB[4], lhsT=vv, rhs=pt[:, 0, :], start=s0, stop=s1), waits=w0, signal=False)
                pv = P.op("tensor", lambda e: e.matmul(PB[5], lhsT=vv, rhs=pt[:, 1, :], start=s0, stop=s1))
                if kb == 0:
                    a0 = P.op("vector", lambda e: e.tensor_copy(out=ACC0, in_=pt[:, 0, :]), waits=[pv_w, att["accL_free"]])
                    a1 = P.op("gpsimd", lambda e: e.tensor_copy(out=ACC1, in_=pt[:, 1, :]), waits=[pv_w, att["accL_free"]])
                else:
                    a0 = P.op("vector", lambda e: e.tensor_tensor(out=ACC0, in0=ACC0, in1=pt[:, 0, :], op=ALU.add), waits=[pv_w, att["a0"]])
                    a1 = P.op("gpsimd", lambda e: e.tensor_tensor(out=ACC1, in0=ACC1, in1=pt[:, 1, :], op=ALU.add), waits=[pv_w, att["a1"]])
                att["a0"], att["a1"] = a0, a1
                att["pt_free"][pi] = [pv, a0, a1]
                return pv

            issue_qk(0)
            pv = None
            for kb in range(nkb):
                if kb + 1 < nkb:
                    issue_qk(kb + 1)
                pv = pv_step(kb)
                if kb == 2 and att["pending"] is not None:
                    att["pending"]()
                    att["pending"] = None
            if att["pending"] is not None:
                att["pending"]()
                att["pending"] = None
            wfree = [att["ee_free"]]
            P.op("tensor", lambda e: e.matmul(PB[6], lhsT=onesf, rhs=ACC0, start=True, stop=True), waits=[att["a0"], att["acc_free"], d_cf], signal=False)
            lsum = P.op("tensor", lambda e: e.matmul(PB[7], lhsT=onesf, rhs=ACC1, start=True, stop=True), waits=[att["a1"]])
            att["accL_free"] = lsum
            e0 = P.op("vector", lambda e: e.reciprocal(out=EE[0], in_=PB[6]), waits=[pv, lsum] + wfree)
            e1 = P.op("vector", lambda e: e.reciprocal(out=EE[1], in_=PB[7]), waits=[pv, lsum] + wfree)
            e2 = P.op("vector", lambda e: e.tensor_tensor(out=EE[0], in0=PB[4], in1=EE[0], op=ALU.mult), waits=[e0])
            e3 = P.op("vector", lambda e: e.tensor_tensor(out=EE[1], in0=PB[5], in1=EE[1], op=ALU.mult), waits=[e1])
            att["acc_free"] = e3
            e4 = P.op("vector", lambda e: e.scalar_tensor_tensor(out=EE[2], in0=EE[1], scalar=neglam, in1=EE[0], op0=ALU.mult, op1=ALU.add), waits=[e2, e3, t_l7] + wfree)
            e5 = P.op("gpsimd", lambda e: e.tensor_tensor(out=EE[3], in0=EE[2], in1=EE[2], op=ALU.mult), waits=[e4] + wfree)

            def finish():
                s = att["n_s"] % 2
                att["n_s"] += 2
                ss = P.op("tensor", lambda e: e.matmul(psum[:, 2 * s, :], lhsT=onesf, rhs=EE[3], start=True, stop=True), waits=[e5, d_cf, att["s_free"][s]])
                e6 = P.op("scalar", lambda e: e.activation(out=EE[3], in_=psum[:, 2 * s, :], func=AF.Ln, scale=1.0 / 128.0, bias=epsc), waits=[ss])
                att["s_free"][s] = e6
                e7 = P.op("scalar", lambda e: e.activation(out=EE[3], in_=EE[3], func=AF.Exp, scale=-0.5), waits=[e6])
                e8 = P.op("vector", lambda e: e.tensor_tensor(out=EE[2], in0=EE[2], in1=EE[3], op=ALU.mult), waits=[e7])
                e9 = P.op("vector", lambda e: e.tensor_scalar(out=QO[:, h, qrange], in0=EE[2], scalar1=gsc, scalar2=None, op0=ALU.mult), waits=[e8, t_g])
                att["ee_free"] = e9
                att["last"] = e9
            att["pending"] = finish

        def attention(hp):
            for i in range(NQT):
                for hl in range(2):
                    att_unit(i, hl, 2 * hp + hl)
            att["pending"]()
            att["pending"] = None
            return att["last"]

        att_last = None
        if upto == "consts":
            tap("small", small, t_consts)
            P.emit(finals)
            return nc, tap_out
        for hp in range(2):
            kvt = phase_A(hp, hp == 0)
            if upto == "A":
                tap("kt", KT[:, :, 0:2048], kvt, BF16)
                tap("vv", VV[:, 0:8, :], kvt, BF16)
                P.emit(finals)
                return nc, tap_out
            qtoks = phase_Q() if hp == 0 else []
            P.base_waits = [t for t in kvt + qtoks if t is not None] + t_consts
            att_last = attention(hp)
            P.base_waits = [att_last]
            if upto == "att0":
                break
        tap("qo", QO, [att_last], BF16)
        tap("kt", KT[:, :, 0:2048], [att_last], BF16)
        tap("vv", VV[:, 0:8, :], [att_last], BF16)

        if upto in ("att0", "att"):
            P.emit(finals)
            return nc, tap_out

        AR.reset(pers_mark)
        wp = AR.alloc([128, 8, 3584], BF16)
        woa = AR.alloc([128, 4, 1024], BF16)
        woc = AR.alloc([128, 4, 1024], BF16)
        wmx = AR.alloc([128, 8, 1024], BF16)
        xb2_ = [AR.alloc([128, 2, 1024], BF16) for _ in range(2)]
        xhb_ = [AR.alloc([128, 1024], BF16) for _ in range(2)]
        xres_ = [AR.alloc([128, 2, 1024], F32) for _ in range(2)]
        xT2 = AR.alloc([128, 8, 256], BF16)
        xhT = AR.alloc([128, 32], BF16)
        ccs_ = [AR.alloc([128, 264], F32) for _ in range(2)]
        Ub_ = [AR.alloc([128, 2, 130], F32) for _ in range(2)]
        T1_ = [AR.alloc([128, 2, 128], F32) for _ in range(2)]
        Zb = AR.alloc([128, 4, 256], BF16)
        g0_ = [AR.alloc([128, 256], F32) for _ in range(2)]
        g1_ = [AR.alloc([128, 256], F32) for _ in range(2)]
        m0_ = [AR.alloc([128, 256], F32) for _ in range(2)]
        m1_ = [AR.alloc([128, 256], F32) for _ in range(2)]
        MT = AR.alloc([128, 8, 256], BF16)
        Rb = AR.alloc([128, 1024], F32)
        lng = AR.alloc([128, 1024], F32)
        lnb = AR.alloc([128, 1024], F32)
        H1T = AR.alloc([128, 8, 128], F32)
        stt_b = AR.alloc([128, 16], F32)
        bgs = AR.alloc([128, 16], F32)
        cws = AR.alloc([128, 12], F32)
        rbias = AR.alloc([128, 36], F32)
        wr = AR.alloc([128, 8, 36], F32)
        for i7 in range(7):
            P.adma("gpsimd", wp[:, :, i7 * 512:(i7 + 1) * 512], w_in[:, 1536 + i7 * 512:1536 + (i7 + 1) * 512].rearrange("(c p) n -> p c n", p=128), "wp", writes=["wp"])
        P.adma("gpsimd", woa, w_o_att.rearrange("(c p) n -> p c n", p=128), "woa", writes=["woa"])
        P.adma("gpsimd", woc, w_o_conv.rearrange("(c p) n -> p c n", p=128), "woc", writes=["woc"])
        P.adma("gpsimd", wmx, w_mix.rearrange("(c p) n -> p c n", p=128), "wmx", writes=["wmx"])
        P.adma("sync", wr, w_rt.rearrange("(c p) n -> p c n", p=128), "wr", writes=["wr"])
        P.adma("sync", bgs, b_gate.rearrange("o (q p) -> p (o q)", p=128), "bgs", writes=["bgs"], allow_slow_non_contiguous=True)
        P.adma("sync", cws.rearrange("p (k q) -> p k q", k=3), conv_w.rearrange("k (q p) -> p k q", p=128), "cws", writes=["cws"], allow_slow_non_contiguous=True)
        P.adma("sync", rbias, b_rt.to_broadcast([128, 36]), "rbias", writes=["rbias"])
        P.adma("sync", lng, ln1[0:1, :].to_broadcast([128, 1024]), "lng", writes=["lng"])
        P.adma("sync", lnb, ln1[1:2, :].to_broadcast([128, 1024]), "lnb", writes=["lnb"])

        def layer_norm(buf, gname, bname, gt, bt, stt=None, rn="R"):
            stt = stt_b if stt is None else stt
            mv = stt[:, 12:14]
            ve = stt[:, 14:15]
            rs = stt[:, 15:16]
            P.auto("vector", lambda e: e.bn_stats(out=stt[:, 0:6], in_=buf[:, 0:512]), reads=[rn], writes=["stt"])
            P.auto("vector", lambda e: e.bn_stats(out=stt[:, 6:12], in_=buf[:, 512:1024]), reads=[rn], writes=["stt2"])
            P.auto("vector", lambda e: e.bn_aggr(out=mv, in_=stt[:, 0:12]), reads=["stt", "stt2"], writes=["mv"])
            P.auto("vector", lambda e: e.tensor_scalar(out=ve, in0=mv[:, 1:2], scalar1=1e-5, scalar2=None, op0=ALU.add), reads=["mv"], writes=["ve"])
            P.auto("gpsimd", lambda e: e.tensor_tensor(out=rs, in0=ve, in1=mhalf, op=ALU.pow), reads=["ve"], writes=["rs"], extra=[d_cf])
            P.auto("vector", lambda e: e.tensor_scalar(out=buf, in0=buf, scalar1=mv[:, 0:1], scalar2=rs, op0=ALU.subtract, op1=ALU.mult), reads=["mv", "rs"], writes=[rn])
            P.auto("vector", lambda e: e.tensor_tensor(out=buf, in0=buf, in1=gt, op=ALU.mult), reads=[gname], writes=[rn])
            return P.auto("gpsimd", lambda e: e.tensor_tensor(out=buf, in0=buf, in1=bt, op=ALU.add), reads=[bname], writes=[rn])

        def b_loads(tb):
            r0 = tb * 256
            pb = tb % 2
            P.adma("gpsimd", xb2_[pb], xo[r0:r0 + 256, :].rearrange("(b p) d -> p b d", p=128), "xb2%d" % pb, writes=["xb2%d" % pb])
            P.adma("gpsimd", xhb_[pb][0:4, :], xh[tb * 4:tb * 4 + 4, :], "xhb%d" % pb, writes=["xhb%d" % pb])
            P.adma("sync", xres_[pb], xo[r0:r0 + 256, :].rearrange("(b p) d -> p b d", p=128), "xres%d" % pb, writes=["xres%d" % pb])

        def b_tile(tb):
            r0 = tb * 256
            pb = tb % 2
            xb2, xhb, xres = xb2_[pb], xhb_[pb], xres_[pb]
            n_xb2, n_xhb, n_xres = "xb2%d" % pb, "xhb%d" % pb, "xres%d" % pb
            if tb + 1 < NTB:
                b_loads(tb + 1)
            for half in range(2):
                fns = []
                for cc in range(4):
                    c = half * 4 + cc
                    for blk in range(2):
                        o_ = PBb[half][:, cc * 256 + blk * 128: cc * 256 + blk * 128 + 128]
                        i_ = xb2[:, blk, c * 128:(c + 1) * 128]
                        fns.append(lambda e, o_=o_, i_=i_: e.transpose(out=o_, in_=i_, identity=ident))
                P.auto("tensor", fns, reads=[n_xb2], psum=["b%d" % half], extra=[d_cb])
                dst = xT2[:, half * 4:half * 4 + 4, :].rearrange("p a b -> p (a b)")
                if half == 0:
                    P.auto("vector", lambda e, dst=dst: e.tensor_copy(out=dst, in_=PBb[0]), writes=["xT2a"], psum=["b0"])
                else:
                    P.auto("scalar", lambda e, dst=dst: e.copy(out=dst, in_=PBb[1]), writes=["xT2b"], psum=["b1"])
            fns = []
            for c in range(8):
                o_ = PBb[6][:, c * 4:(c + 1) * 4]
                i_ = xhb[0:4, c * 128:(c + 1) * 128]
                fns.append(lambda e, o_=o_, i_=i_: e.transpose(out=o_, in_=i_, identity=ident[0:4, 0:4]))
            P.auto("tensor", fns, reads=[n_xhb], psum=["b6"], extra=[d_cb])
            P.auto("vector", lambda e: e.tensor_copy(out=xhT, in_=PBb[6][:, 0:32]), writes=["xhT"], psum=["b6"])
            xhT3 = xhT.rearrange("p (c k) -> p c k", c=8)
            for q in range(4):
                qp = q % 2
                BA, BB = PB[2 + 2 * qp], PB[3 + 2 * qp]
                nBA, nBB = "b%d" % (2 + 2 * qp), "b%d" % (3 + 2 * qp)
                ccs, Ub, T1 = ccs_[qp], Ub_[qp], T1_[qp]
                nccs, nU, nU2, nT1 = "ccs%d" % qp, "U%d" % qp, "U2%d" % qp, "T1%d" % qp
                fns = []
                for c in range(8):
                    l_ = wp[:, c, 512 + q * 128: 512 + q * 128 + 128]
                    fns.append(lambda e, l_=l_, c=c, BA=BA: e.matmul(BA[:, 0:256], lhsT=l_, rhs=xT2[:, c, :], start=(c == 0), stop=(c == 7)))
                for c in range(8):
                    l_ = wp[:, c, 512 + q * 128: 512 + q * 128 + 128]
                    fns.append(lambda e, l_=l_, c=c, BA=BA: e.matmul(BA[:, 256:260], lhsT=l_, rhs=xhT3[:, c, :], start=(c == 0), stop=(c == 7)))
                for c in range(8):
                    l_ = wp[:, c, 1024 + q * 128: 1024 + q * 128 + 128]
                    fns.append(lambda e, l_=l_, c=c, BA=BA: e.matmul(BA[:, 260:264], lhsT=l_, rhs=xhT3[:, c, :], start=(c == 0), stop=(c == 7)))
                P.auto("tensor", fns, reads=["wp", "xT2a", "xT2b", "xhT"], psum=[nBA])
                fns = []
                for c in range(8):
                    l_ = wp[:, c, 1024 + q * 128: 1024 + q * 128 + 128]
                    fns.append(lambda e, l_=l_, c=c, BB=BB: e.matmul(BB[:, 0:256], lhsT=l_, rhs=xT2[:, c, :], start=(c == 0), stop=(c == 7)))
                for c in range(8):
                    l_ = wp[:, c, q * 128: q * 128 + 128]
                    fns.append(lambda e, l_=l_, c=c, BB=BB: e.matmul(BB[:, 256:512], lhsT=l_, rhs=xT2[:, c, :], start=(c == 0), stop=(c == 7)))
                P.auto("tensor", fns, reads=["wp", "xT2a", "xT2b"], psum=[nBB])
                P.auto("scalar", lambda e, ccs=ccs, BA=BA: e.copy(out=ccs, in_=BA[:, 0:264]), writes=[nccs], psum=[nBA])
                P.auto("vector", lambda e, ccs=ccs, Ub=Ub, BB=BB: e.tensor_tensor(out=Ub[:, :, 2:130], in0=ccs[:, 0:256].rearrange("p (b t) -> p b t", b=2),
                                                          in1=BB[:, 0:256].rearrange("p (b t) -> p b t", b=2), op=ALU.mult),
                       reads=[nccs], writes=[nU], psum=[nBB])
                P.auto("vector", lambda e, ccs=ccs, Ub=Ub: e.tensor_tensor(out=Ub[:, :, 0:2], in0=ccs[:, 256:260].rearrange("p (b t) -> p b t", b=2),
                                                          in1=ccs[:, 260:264].rearrange("p (b t) -> p b t", b=2), op=ALU.mult),
                       reads=[nccs], writes=[nU2])
                P.auto("vector", lambda e, q=q, T1=T1, Ub=Ub: e.tensor_scalar(out=T1, in0=Ub[:, :, 0:128], scalar1=cws[:, q:q + 1], scalar2=None, op0=ALU.mult),
                       reads=[nU, nU2, "cws"], writes=[nT1])
                P.auto("vector", lambda e, q=q, T1=T1, Ub=Ub: e.scalar_tensor_tensor(out=T1, in0=Ub[:, :, 1:129], scalar=cws[:, 4 + q:5 + q], in1=T1, op0=ALU.mult, op1=ALU.add),
                       reads=[nU, nU2], writes=[nT1])
                P.auto("vector", lambda e, q=q, T1=T1, Ub=Ub: e.scalar_tensor_tensor(out=T1, in0=Ub[:, :, 2:130], scalar=cws[:, 8 + q:9 + q], in1=T1, op0=ALU.mult, op1=ALU.add),
                       reads=[nU, nU2], writes=[nT1])
                P.auto("vector", lambda e, q=q, T1=T1, BB=BB: e.tensor_tensor(out=Zb[:, q, :], in0=T1.rearrange("p b t -> p (b t)"), in1=BB[:, 256:512], op=ALU.mult),
                       reads=[nT1], writes=["Z%d" % q], psum=[nBB])
            for c8 in range(8):
                cp_ = c8 % 2
                BG, BY = PB[2 + 2 * cp_], PB[3 + 2 * cp_]
                nBG, nBY = "b%d" % (2 + 2 * cp_), "b%d" % (3 + 2 * cp_)
                g0, g1, m0, m1 = g0_[cp_], g1_[cp_], m0_[cp_], m1_[cp_]
                ng0, ng1, nm0, nm1 = "g0%d" % cp_, "g1%d" % cp_, "m0%d" % cp_, "m1%d" % cp_
                fns = []
                for c in range(8):
                    l_ = wp[:, c, 1536 + c8 * 128: 1536 + c8 * 128 + 128]
                    fns.append(lambda e, l_=l_, c=c, BG=BG: e.matmul(BG[:, 0:256], lhsT=l_, rhs=xT2[:, c, :], start=(c == 0), stop=(c == 7)))
                for c in range(8):
                    l_ = wp[:, c, 2560 + c8 * 128: 2560 + c8 * 128 + 128]
                    fns.append(lambda e, l_=l_, c=c, BG=BG: e.matmul(BG[:, 256:512], lhsT=l_, rhs=xT2[:, c, :], start=(c == 0), stop=(c == 7)))
                P.auto("tensor", fns, reads=["wp", "xT2a", "xT2b"], psum=[nBG])
                P.auto("scalar", lambda e, c8=c8, g0=g0, BG=BG: e.activation(out=g0, in_=BG[:, 0:256], func=AF.Sigmoid, bias=bgs[:, c8:c8 + 1]), reads=["bgs"], writes=[ng0], psum=[nBG])
                P.auto("scalar", lambda e, c8=c8, g1=g1, BG=BG: e.activation(out=g1, in_=BG[:, 256:512], func=AF.Sigmoid, bias=bgs[:, 8 + c8:9 + c8]), reads=["bgs"], writes=[ng1], psum=[nBG])
                fns = []
                for h in range(4):
                    l_ = woa[:, h, c8 * 128:(c8 + 1) * 128]
                    r_ = QO[:, h, r0:r0 + 256]
                    fns.append(lambda e, l_=l_, r_=r_, h=h, BY=BY: e.matmul(BY[:, 0:256], lhsT=l_, rhs=r_, start=(h == 0), stop=(h == 3)))
                for q in range(4):
                    l_ = woc[:, q, c8 * 128:(c8 + 1) * 128]
                    r_ = Zb[:, q, :]
                    fns.append(lambda e, l_=l_, r_=r_, q=q, BY=BY: e.matmul(BY[:, 256:512], lhsT=l_, rhs=r_, start=(q == 0), stop=(q == 3)))
                P.auto("tensor", fns, reads=["woa", "woc", "Z0", "Z1", "Z2", "Z3"], psum=[nBY])
                P.auto("vector", lambda e, m0=m0, g0=g0, BY=BY: e.tensor_tensor(out=m0, in0=g0, in1=BY[:, 0:256], op=ALU.mult), reads=[ng0], writes=[nm0], psum=[nBY])
                P.auto("vector", lambda e, m1=m1, g1=g1, BY=BY: e.tensor_tensor(out=m1, in0=g1, in1=BY[:, 256:512], op=ALU.mult), reads=[ng1], writes=[nm1], psum=[nBY])
                P.auto("vector", lambda e, c8=c8, m0=m0, m1=m1: e.tensor_tensor(out=MT[:, c8, :], in0=m0, in1=m1, op=ALU.add), reads=[nm0, nm1], writes=["MT%d" % c8])
            for blk in range(2):
                gb = tb * 2 + blk
                fns = []
                for half in range(2):
                    for c in range(8):
                        l_ = MT[:, c, blk * 128:(blk + 1) * 128]
                        r_ = wmx[:, c, half * 512:(half + 1) * 512]
                        fns.append(lambda e, l_=l_, r_=r_, c=c, half=half: e.matmul(PB[6 + half], lhsT=l_, rhs=r_, start=(c == 0), stop=(c == 7)))
                P.auto("tensor", fns, reads=["MT%d" % c_ for c_ in range(8)] + ["wmx"], psum=["b6", "b7"])
                P.auto("vector", lambda e, blk=blk: e.scalar_tensor_tensor(out=Rb[:, 0:512], in0=xres[:, blk, 0:512], scalar=ALPHA, in1=PB[6], op0=ALU.mult, op1=ALU.add),
                       reads=[n_xres], writes=["R"], psum=["b6"])
                P.auto("vector", lambda e, blk=blk: e.scalar_tensor_tensor(out=Rb[:, 512:1024], in0=xres[:, blk, 512:1024], scalar=ALPHA, in1=PB[7], op0=ALU.mult, op1=ALU.add),
                       reads=[n_xres], writes=["R"], psum=["b7"])
                layer_norm(Rb, "lng", "lnb", lng, lnb)
                finals_h1.append(P.adma("sync", h1f[gb * 128:(gb + 1) * 128, :], Rb, "h1w", reads=["R"]))
                fns = []
                for c in range(8):
                    o_ = PB[c // 4][:, (c % 4) * 128:(c % 4) * 128 + 128]
                    i_ = Rb[:, c * 128:(c + 1) * 128]
                    fns.append(lambda e, o_=o_, i_=i_: e.transpose(out=o_, in_=i_, identity=identf))
                P.auto("tensor", fns, reads=["R"], psum=["b0", "b1"], extra=[d_cf])
                P.auto("scalar", lambda e: e.copy(out=H1T[:, 0:4, :].rearrange("p a b -> p (a b)"), in_=PB[0]), writes=["H1Ta"], psum=["b0"])
                P.auto("vector", lambda e: e.tensor_copy(out=H1T[:, 4:8, :].rearrange("p a b -> p (a b)"), in_=PB[1]), writes=["H1Tb"], psum=["b1"])
                fns = []
                for c in range(8):
                    fns.append(lambda e, c=c: e.matmul(PB[0][:, 0:36], lhsT=H1T[:, c, :], rhs=wr[:, c, :], start=(c == 0), stop=(c == 7)))
                P.auto("tensor", fns, reads=["H1Ta", "H1Tb", "wr"], psum=["b0"])
                P.auto("vector", lambda e, gb=gb: e.tensor_tensor(out=LG[:, gb, :], in0=PB[0][:, 0:36], in1=rbias, op=ALU.add), reads=["rbias"], writes=["LG"], psum=["b0"])

        finals_h1 = []
        NTB = NQT * 2
        b_loads(0)
        for tb in range(NTB):
            b_tile(tb)
        tap("lg", LG, [P.lw["LG"]])
        tap("h1", h1f[0:256, :], finals_h1)
        if upto == "B":
            P.emit(finals + finals_h1)
            return nc, tap_out

        P.base_waits = [P.lw["LG"]] + finals_h1
        AR.reset(pers_mark)
        NB = NTB * 2
        gm = AR.alloc([128, 32], F32)
        gd = AR.alloc([128, 32, 4], F32)
        pen = AR.alloc([128, 32, 4], F32)
        ge = AR.alloc([128, 32, 4], F32)
        gs = AR.alloc([128, 32], F32)
        gw = AR.alloc([128, 32], F32)
        em = AR.alloc([128, 32, 32], F32)
        em2 = AR.alloc([128, 32, 32], F32)
        oh1 = AR.alloc([128, 32, 32], F32)
        oh2 = AR.alloc([128, 32, 32], F32)
        Aa = AR.alloc([128, 32, 32], F32)
        Tt = AR.alloc([128, 32, 32], F32)
        sc0 = AR.alloc([128, 32, 32], F32)
        sc1 = AR.alloc([128, 32, 32], F32)
        rank = AR.alloc([128, 32, 32], F32)
        tmpc = AR.alloc([128, 32, 32], F32)
        v1 = AR.alloc([128, 32], F32)
        v2 = AR.alloc([128, 32], F32)
        dv = AR.alloc([128, 32], F32)
        s12 = AR.alloc([128, 2, 32], F32)
        stt_c = AR.alloc([128, 16], F32)
        fl = lambda t: t.rearrange("p a b -> p (a b)")
        gl = LG[:, :, 0:4]
        el = LG[:, :, 4:36]
        bc3 = lambda t: t.unsqueeze(2).to_broadcast([128, 32, 32])
        V = "vector"
        P.auto(V, lambda e: e.tensor_reduce(out=gm, in_=gl, axis=AX.X, op=ALU.max), reads=["LG"], writes=["gm"])
        P.auto(V, lambda e: e.tensor_tensor(out=gd, in0=gl, in1=gm.unsqueeze(2).to_broadcast([128, 32, 4]), op=ALU.subtract), reads=["LG", "gm"], writes=["gd"])
        P.auto(V, lambda e: e.tensor_scalar(out=pen, in0=gd, scalar1=0.0, scalar2=NEG, op0=ALU.is_lt, op1=ALU.mult), reads=["gd"], writes=["pen"])
        P.auto("scalar", lambda e: e.activation(out=ge, in_=gd, func=AF.Exp), reads=["gd"], writes=["ge"])
        P.auto(V, lambda e: e.tensor_reduce(out=gs, in_=ge, axis=AX.X, op=ALU.add), reads=["ge"], writes=["gs"])
        P.auto(V, lambda e: e.reciprocal(out=gw, in_=gs), reads=["gs"], writes=["gw"])
        P.auto(V, lambda e: e.tensor_tensor(out=em.rearrange("p b (g k) -> p b g k", g=4), in0=el.rearrange("p b (g k) -> p b g k", g=4),
                                            in1=pen.unsqueeze(3).to_broadcast([128, 32, 4, 8]), op=ALU.add), reads=["LG", "pen"], writes=["em"])
        P.auto(V, lambda e: e.tensor_reduce(out=v1, in_=em, axis=AX.X, op=ALU.max), reads=["em"], writes=["v1"])
        P.auto(V, lambda e: e.tensor_tensor(out=oh1, in0=em, in1=bc3(v1), op=ALU.is_equal), reads=["em", "v1"], writes=["oh1"])
        P.auto(V, lambda e: e.scalar_tensor_tensor(out=fl(em2), in0=fl(oh1), scalar=NEG, in1=fl(em), op0=ALU.mult, op1=ALU.add), reads=["oh1", "em"], writes=["em2"])
        P.auto(V, lambda e: e.tensor_reduce(out=v2, in_=em2, axis=AX.X, op=ALU.max), reads=["em2"], writes=["v2"])
        P.auto(V, lambda e: e.tensor_tensor(out=oh2, in0=em2, in1=bc3(v2), op=ALU.is_equal), reads=["em2", "v2"], writes=["oh2"])
        P.auto(V, lambda e: e.tensor_tensor(out=dv, in0=v2, in1=v1, op=ALU.subtract), reads=["v1", "v2"], writes=["dv"])
        P.auto("scalar", lambda e: e.activation(out=dv, in_=dv, func=AF.Exp), reads=[], writes=["dv"])
        P.auto(V, lambda e: e.tensor_scalar(out=dv, in0=dv, scalar1=1.0, scalar2=None, op0=ALU.add), writes=["dv"])
        P.auto(V, lambda e: e.reciprocal(out=dv, in_=dv), writes=["dv"])
        P.auto(V, lambda e: e.tensor_tensor(out=PW[:, 0, :], in0=dv, in1=gw, op=ALU.mult), reads=["dv", "gw"], writes=["PW0"])
        P.auto(V, lambda e: e.tensor_tensor(out=PW[:, 1, :], in0=gw, in1=PW[:, 0, :], op=ALU.subtract), reads=["PW0", "gw"], writes=["PW1"])
        P.auto(V, lambda e: e.tensor_tensor(out=fl(Aa), in0=fl(oh1), in1=fl(oh2), op=ALU.add), reads=["oh1", "oh2"], writes=["Aa"])
        Af = fl(Aa)
        P.auto("tensor", [lambda e: e.matmul(PB[0], lhsT=trif, rhs=Af[:, 0:512], start=True, stop=True),
                          lambda e: e.matmul(PB[1], lhsT=trif, rhs=Af[:, 512:1024], start=True, stop=True),
                          lambda e: e.matmul(PB[2], lhsT=onesf, rhs=Af[:, 0:512], start=True, stop=True),
                          lambda e: e.matmul(PB[3], lhsT=onesf, rhs=Af[:, 512:1024], start=True, stop=True)],
               reads=["Aa"], psum=["b0", "b1", "b2", "b3"], extra=[d_cf])
        P.auto(V, lambda e: e.tensor_copy(out=fl(Tt)[:, 0:512], in_=PB[2]), writes=["Tt"], psum=["b2"])
        P.auto(V, lambda e: e.tensor_copy(out=fl(Tt)[:, 512:1024], in_=PB[3]), writes=["Tt"], psum=["b3"])
        cur, cname = Tt, "Tt"
        for si, sh in enumerate((1, 2, 4, 8, 16)):
            nxt, nname = (sc0, "sc0") if si % 2 == 0 else (sc1, "sc1")
            P.auto(V, lambda e, cur=cur, nxt=nxt, sh=sh: e.tensor_tensor(out=nxt[:, sh:32, :], in0=cur[:, sh:32, :], in1=cur[:, 0:32 - sh, :], op=ALU.add), reads=[cname], writes=[nname])
            P.auto(V, lambda e, cur=cur, nxt=nxt, sh=sh: e.tensor_copy(out=nxt[:, 0:sh, :], in_=cur[:, 0:sh, :]), reads=[cname], writes=[nname])
            cur, cname = nxt, nname
        P.auto(V, lambda e, cur=cur: e.tensor_tensor(out=fl(tmpc), in0=fl(cur), in1=fl(Tt), op=ALU.subtract), reads=[cname, "Tt"], writes=["tmpc"])
        P.auto(V, lambda e: e.tensor_tensor(out=fl(rank)[:, 0:512], in0=fl(tmpc)[:, 0:512], in1=PB[0], op=ALU.add), reads=["tmpc"], writes=["rank"], psum=["b0"])
        P.auto(V, lambda e: e.tensor_tensor(out=fl(rank)[:, 512:1024], in0=fl(tmpc)[:, 512:1024], in1=PB[1], op=ALU.add), reads=["tmpc"], writes=["rank"], psum=["b1"])
        P.auto(V, lambda e: e.tensor_scalar(out=fl(rank), in0=fl(rank), scalar1=float(CAP - 1), scalar2=None, op0=ALU.min), writes=["rank"])
        P.auto(V, lambda e: e.tensor_tensor(out=rank, in0=rank, in1=ecap.unsqueeze(1).to_broadcast([128, 32, 32]), op=ALU.add), writes=["rank"], extra=[d_cf])
        P.auto(V, lambda e: e.tensor_tensor(out=fl(tmpc), in0=fl(oh1), in1=fl(rank), op=ALU.mult), reads=["oh1", "rank"], writes=["tmpc"])
        P.auto(V, lambda e: e.tensor_reduce(out=s12[:, 0, :], in_=tmpc, axis=AX.X, op=ALU.add), reads=["tmpc"], writes=["s12a"])
        P.auto(V, lambda e: e.tensor_tensor(out=fl(tmpc), in0=fl(oh2), in1=fl(rank), op=ALU.mult), reads=["oh2", "rank"], writes=["tmpc"])
        P.auto(V, lambda e: e.tensor_reduce(out=s12[:, 1, :], in_=tmpc, axis=AX.X, op=ALU.add), reads=["tmpc"], writes=["s12b"])
        P.auto(V, lambda e: e.tensor_scalar(out=fl(s12), in0=fl(s12), scalar1=float(NSLOT - 1), scalar2=0.0, op0=ALU.min, op1=ALU.max), reads=["s12a", "s12b"], writes=["s12a", "s12b"])
        P.auto(V, lambda e: e.tensor_copy(out=SLI, in_=s12), reads=["s12a", "s12b"], writes=["SLI"])
        tap("sli", SLI, [P.lw["SLI"]], I32)
        tap("pw", PW, [P.lw["PW1"]])

        P.base_waits = [P.lw["SLI"], P.lw["PW1"], P.lw["PW0"]] + finals_h1
        AR.reset(pers_mark)
        stt_c = AR.alloc([128, 16], F32)
        wg = [AR.alloc([128, 8, 512], BF16) for _ in range(2)]
        wu = [AR.alloc([128, 8, 512], BF16) for _ in range(2)]
        wd = [AR.alloc([128, 4, 1024], BF16) for _ in range(2)]

        def e_loads_w(e_):
            i = e_ % 2
            P.adma("gpsimd", wg[i], w_eg[e_].rearrange("(c p) n -> p c n", p=128), "wg%d" % i, writes=["wg%d" % i])
            P.adma("gpsimd", wu[i], w_eu[e_].rearrange("(c p) n -> p c n", p=128), "wu%d" % i, writes=["wu%d" % i])
            P.adma("gpsimd", wd[i], w_ed[e_].rearrange("(c p) n -> p c n", p=128), "wd%d" % i, writes=["wd%d" % i])

        e_loads_w(0)
        hb = [AR.alloc([128, 1024], F32) for _ in range(2)]
        sc_toks = []
        for blk in range(NB):
            i = blk % 2
            P.adma("sync", hb[i], h1f[blk * 128:(blk + 1) * 128, :], "hb%d" % i, writes=["hb%d" % i])
            for k in range(2):
                off = SLI[:, k, blk:blk + 1].bitcast(U32)
                src = hb[i]
                sc_toks.append(P.acdma("gpsimd", lambda e, off=off, src=src: e.indirect_dma_start(
                    out=xs_d, out_offset=bass.IndirectOffsetOnAxis(ap=off, axis=0), in_=src, in_offset=None),
                    "sc%d_%d" % (i, k), reads=["hb%d" % i, "SLI"]))

        XE = [AR.alloc([128, 3, 1024], BF16) for _ in range(2)]
        XT = AR.alloc([128, 8, 384], BF16)
        sg = AR.alloc([128, 384], F32)
        HT = AR.alloc([128, 4, 384], BF16)
        YSb = [AR.alloc([128, 1024], F32) for _ in range(2)]
        NE = 32
        ys_toks = []

        def e_loads(e_):
            i = e_ % 2
            if e_ > 0:
                e_loads_w(e_)
            P.adma("sync", XE[i], xs_d[e_ * CAP:(e_ + 1) * CAP, :].rearrange("(b p) d -> p b d", p=128), "xe%d" % i, writes=["xe%d" % i], extra=sc_toks)

        def e_compute(e_):
            i = e_ % 2
            for pr in range(4):
                bank = pr % 2
                fns = []
                for cc in range(2):
                    c = 2 * pr + cc
                    for sb in range(3):
                        o_ = PBb[bank][:, cc * 384 + sb * 128: cc * 384 + sb * 128 + 128]
                        i_ = XE[i][:, sb, c * 128:(c + 1) * 128]
                        fns.append(lambda e, o_=o_, i_=i_: e.transpose(out=o_, in_=i_, identity=ident))
                P.auto("tensor", fns, reads=["xe%d" % i], psum=["b%d" % bank])
                dst = XT[:, 2 * pr:2 * pr + 2, :].rearrange("p a b -> p (a b)")
                if bank == 0:
                    P.auto("vector", lambda e, dst=dst: e.tensor_copy(out=dst, in_=PBb[0][:, 0:768]), writes=["XT%d" % pr], psum=["b0"])
                else:
                    P.auto("scalar", lambda e, dst=dst: e.copy(out=dst, in_=PBb[1][:, 0:768]), writes=["XT%d" % pr], psum=["b1"])
            xt_names = ["XT%d" % pr for pr in range(4)]
            for f in range(4):
                gb_ = 2 + 2 * (f % 2)
                ub_ = 3 + 2 * (f % 2)
                fns = []
                for c in range(8):
                    l_ = wg[i][:, c, f * 128:(f + 1) * 128]
                    fns.append(lambda e, l_=l_, c=c, gb_=gb_: e.matmul(PB[gb_][:, 0:384], lhsT=l_, rhs=XT[:, c, :], start=(c == 0), stop=(c == 7)))
                P.auto("tensor", fns, reads=["wg%d" % i] + xt_names, psum=["b%d" % gb_])
                fns = []
                for c in range(8):
                    l_ = wu[i][:, c, f * 128:(f + 1) * 128]
                    fns.append(lambda e, l_=l_, c=c, ub_=ub_: e.matmul(PB[ub_][:, 0:384], lhsT=l_, rhs=XT[:, c, :], start=(c == 0), stop=(c == 7)))
                P.auto("tensor", fns, reads=["wu%d" % i] + xt_names, psum=["b%d" % ub_])
                P.auto("scalar", lambda e, gb_=gb_: e.activation(out=sg, in_=PB[gb_][:, 0:384], func=AF.Silu), writes=["sg"], psum=["b%d" % gb_])
                P.auto("vector", lambda e, ub_=ub_, f=f: e.tensor_tensor(out=HT[:, f, :], in0=sg, in1=PB[ub_][:, 0:384], op=ALU.mult), reads=["sg"], writes=["HT%d" % f], psum=["b%d" % ub_])
            ht_names = ["HT%d" % f for f in range(4)]
            for sb in range(3):
                k = (e_ * 3 + sb) % 2
                fns = []
                for half in range(2):
                    for f in range(4):
                        l_ = HT[:, f, sb * 128:(sb + 1) * 128]
                        r_ = wd[i][:, f, half * 512:(half + 1) * 512]
                        fns.append(lambda e, l_=l_, r_=r_, f=f, half=half: e.matmul(PB[6 + half], lhsT=l_, rhs=r_, start=(f == 0), stop=(f == 3)))
                P.auto("tensor", fns, reads=["wd%d" % i] + ht_names, psum=["b6", "b7"])
                ysb = YSb[k]
                P.auto("scalar", lambda e, ysb=ysb: e.copy(out=ysb[:, 0:512], in_=PB[6]), writes=["ysa%d" % k], psum=["b6"])
                P.auto("vector", lambda e, ysb=ysb: e.tensor_copy(out=ysb[:, 512:1024], in_=PB[7]), writes=["ysb%d" % k], psum=["b7"])
                r0 = e_ * CAP + sb * 128
                ys_toks.append(P.adma("sync", ys_d[r0:r0 + 128, :], ysb, "ysw%d" % k, reads=["ysa%d" % k, "ysb%d" % k]))

        e_loads(0)
        for e_ in range(NE):
            if e_ + 1 < NE:
                e_loads(e_ + 1)
            e_compute(e_)

        Y1_ = [AR.alloc([128, 1024], F32) for _ in range(2)]
        Y2_ = [AR.alloc([128, 1024], F32) for _ in range(2)]
        hc_ = [AR.alloc([128, 1024], F32) for _ in range(2)]
        R2_ = [AR.alloc([128, 1024], F32) for _ in range(2)]
        lng2 = AR.alloc([128, 1024], F32)
        lnb2 = AR.alloc([128, 1024], F32)
        P.adma("sync", lng2, ln2[0:1, :].to_broadcast([128, 1024]), "lng2", writes=["lng2"])
        P.adma("sync", lnb2, ln2[1:2, :].to_broadcast([128, 1024]), "lnb2", writes=["lnb2"])

        def c_loads(blk):
            pb = blk % 2
            P.adma("sync", hc_[pb], h1f[blk * 128:(blk + 1) * 128, :], "hc%d" % pb, writes=["hc%d" % pb])
            for k, yb in ((0, Y1_[pb]), (1, Y2_[pb])):
                off = SLI[:, k, blk:blk + 1].bitcast(U32)
                P.acdma("gpsimd", lambda e, off=off, yb=yb: e.indirect_dma_start(
                    out=yb, out_offset=None, in_=ys_d, in_offset=bass.IndirectOffsetOnAxis(ap=off, axis=0)),
                    "yg%d%d" % (k, pb), reads=["SLI"], writes=["Y%d%d" % (k, pb)], extra=ys_toks)

        def c_block(blk):
            pb = blk % 2
            if blk + 1 < NB:
                c_loads(blk + 1)
            Y1, Y2, hc, R2 = Y1_[pb], Y2_[pb], hc_[pb], R2_[pb]
            rn = "R2%d" % pb
            P.auto("scalar", lambda e: e.mul(out=R2, in_=hc, mul=ALPHA), reads=["hc%d" % pb], writes=[rn])
            P.auto(V, lambda e: e.scalar_tensor_tensor(out=R2, in0=Y1, scalar=PW[:, 0, blk:blk + 1], in1=R2, op0=ALU.mult, op1=ALU.add), reads=["Y0%d" % pb, "PW0"], writes=[rn])
            P.auto(V, lambda e: e.scalar_tensor_tensor(out=R2, in0=Y2, scalar=PW[:, 1, blk:blk + 1], in1=R2, op0=ALU.mult, op1=ALU.add), reads=["Y1%d" % pb, "PW1"], writes=[rn])
            layer_norm(R2, "lng2", "lnb2", lng2, lnb2, stt_c, rn)
            finals.append(P.adma("sync", out[blk * 128:(blk + 1) * 128, :], R2, "outw%d" % pb, reads=[rn]))

        c_loads(0)
        for blk in range(NB):
            c_block(blk)

        P.emit(finals)
    return nc, tap_out


def _consts(j):
    ident = np.eye(128, dtype=np.float32)
    rot = np.zeros((128, 128), np.float32)
    for m in range(128):
        if (m % 64) < 32:
            rot[m + 32, m] = -1.0
        else:
            rot[m - 32, m] = 1.0
    ones = np.ones((128, 128), np.float32)
    kk = np.arange(128)[:, None, None]
    r = np.arange(8)[None, :, None]
    qq = np.arange(512)[None, None, :]
    mask = ((r * 128 + kk) <= ((2 * (qq // 128) + j) * 128 + (qq % 128))).astype(np.float32)
    cbf = np.concatenate([ident, rot, ones, mask.reshape(128, 4096)], axis=1)
    cf = np.zeros((128, 512), np.float32)
    cf[:, 0:128] = ident
    cf[:, 128:256] = 1.0
    cf[:, 256:384] = (np.arange(128)[:, None] < np.arange(128)[None, :]).astype(np.float32)
    inv_freq = (np.float32(10000.0) ** (-np.arange(0, 64, 2, dtype=np.float32) / np.float32(64))).astype(np.float32)
    p = np.arange(128)
    cf[:, 384] = (inv_freq[(p % 64) % 32].astype(np.float64) / (2.0 * np.pi)).astype(np.float32)
    cf[:, 392:424] = (np.arange(32) * CAP)[None, :]
    cf[:, 424] = -0.5
    cf[:, 425] = 1e-5
    return np.ascontiguousarray(cbf), cf


def make_core_inputs(inp, c):
    b, j = c // 2, c % 2
    x = inp["x"]
    xb_ = x[b]
    blocks = xb_.reshape(64, 128, D)
    xo = np.ascontiguousarray(blocks[j::2].reshape(NOWN, D))
    xh = np.zeros((32, 2, D), np.float32)
    for m in range(32):
        blk = 2 * m + j
        if blk > 0:
            xh[m] = xb_[blk * 128 - 2: blk * 128]
    pos = np.asarray(inp["positions"][b], dtype=np.int32)
    poso = np.ascontiguousarray(pos.reshape(64, 128)[j::2].reshape(1, NOWN))
    cbf, cf = _consts(j)
    f = lambda a: np.ascontiguousarray(np.asarray(a, dtype=np.float32))
    return {
        "xf": f(xb_), "xo": xo, "xh": f(xh.reshape(64, D)), "posf": np.ascontiguousarray(pos.reshape(1, S)), "poso": poso,
        "w_in": f(inp["w_in"][0]), "b_gate": f(inp["b_gate"][0].reshape(1, 2048)),
        "lam_in": f(np.concatenate([inp["lambda_q1"][0], inp["lambda_k1"][0], inp["lambda_q2"][0], inp["lambda_k2"][0]]).reshape(1, 256)),
        "subln_g": f(inp["subln_g"][0].reshape(1, 128)), "w_o_att": f(inp["w_o_att"][0]), "conv_w": f(inp["conv_w"][0]),
        "w_o_conv": f(inp["w_o_conv"][0]), "w_mix": f(inp["w_mix_out"][0]),
        "ln1": f(np.stack([inp["ln1_g"][0], inp["ln1_b"][0]])), "ln2": f(np.stack([inp["ln2_g"][0], inp["ln2_b"][0]])),
        "w_rt": f(np.concatenate([inp["w_router_group"][0], inp["w_router_expert"][0]], axis=1)),
        "b_rt": f(np.concatenate([inp["b_router_group"][0], inp["b_router_expert"][0]]).reshape(1, 36)),
        "w_eg": f(inp["w_exp_gate"][0]), "w_eu": f(inp["w_exp_up"][0]), "w_ed": f(inp["w_exp_down"][0]),
        "cbf": cbf, "cf32": cf,
    }


def kernel(**inputs):
    inp = {k: np.asarray(v) for k, v in inputs.items()}
    nc, _ = build()
    in_maps = [make_core_inputs(inp, c) for c in range(8)]
    res = run_bass_kernel_spmd(nc, in_maps, core_ids=list(range(8)))
    outp = np.zeros((4, S, D), np.float32)
    for c in range(8):
        b, j = c // 2, c % 2
        o = np.asarray(res.results[c]["out"]).reshape(32, 128, D)
        outp[b].reshape(64, 128, D)[j::2] = o
    return outp
```

```python
import math
import numpy as np
import ml_dtypes
from contextlib import ExitStack
import concourse.bass as bass
import concourse.mybir as mybir
from concourse.bass_utils import run_bass_kernel_spmd

F32 = mybir.dt.float32
BF16 = mybir.dt.bfloat16
I32 = mybir.dt.int32
U32 = mybir.dt.uint32
AF = mybir.ActivationFunctionType
ALU = mybir.AluOpType
AX = mybir.AxisListType
ENGS = ["tensor", "vector", "scalar", "gpsimd", "sync"]

S = 8192
D = 1024
NOWN = 4096
CAP = 384
NSLOT = 32 * CAP
ALPHA = 2.0 ** 0.25
LAMBDA_INIT = 0.8 - 0.6 * math.exp(0.0)
TWO_PI = 2.0 * math.pi
NEG = -1.0e30


class Prog:
    def __init__(self, nc, es):
        self.nc = nc
        self.es = es
        self.ops = {e: [] for e in ENGS}
        self.cnt = {e: 0 for e in ENGS}
        self.esem = {e: es.enter_context(nc.semaphore("es_" + e)) for e in ENGS}
        self.dsem = {}
        self.dcnt = {}
        self.waited = {e: {} for e in ENGS}
        self.base_waits = []

    def _waits(self, eng, waits):
        best = {}
        for w in list(waits) + list(self.base_waits):
            if w is None:
                continue
            sem, val, key = w
            if key not in best or best[key][1] < val:
                best[key] = (sem, val)
        out = []
        for key, (sem, val) in best.items():
            if self.waited[eng].get(key, 0) >= val:
                continue
            self.waited[eng][key] = val
            out.append((sem, val))
        return out

    def op(self, eng, fn, waits=(), signal=True):
        ws = self._waits(eng, waits)
        inc = None
        tok = None
        if signal:
            self.cnt[eng] += 1
            inc = (self.esem[eng], 1)
            tok = (self.esem[eng], self.cnt[eng], "e_" + eng)
        self.ops[eng].append((fn, ws, inc))
        return tok

    def _dsem(self, sem):
        if sem not in self.dsem:
            self.dsem[sem] = self.es.enter_context(self.nc.semaphore("ds_" + sem))
            self.dcnt[sem] = 0
        return self.dsem[sem]

    def dma(self, q, out, in_, sem, waits=(), **kw):
        return self.cdma(q, lambda e: e.dma_start(out=out, in_=in_, **kw), sem, waits)

    def cdma(self, q, fn, sem, waits=()):
        s = self._dsem(sem)
        ws = self._waits(q, waits)
        self.dcnt[sem] += 16
        self.ops[q].append((fn, ws, (s, 16)))
        return (s, self.dcnt[sem], "d_" + sem)

    def _auto_waits(self, reads, writes, psum):
        if not hasattr(self, "lw"):
            self.lw, self.rd, self.pa = {}, {}, {}
        ws = []
        for r in reads:
            ws.append(self.lw.get(r))
        for w in writes:
            ws.append(self.lw.get(w))
            ws.extend(self.rd.get(w, []))
        for p in psum:
            ws.append(self.pa.get(p))
        return ws

    def _auto_done(self, tok, reads, writes, psum):
        for r in reads:
            self.rd.setdefault(r, []).append(tok)
        for w in writes:
            self.lw[w] = tok
            self.rd[w] = []
        for p in psum:
            self.pa[p] = tok

    def auto(self, eng, fns, reads=(), writes=(), psum=(), extra=()):
        if not isinstance(fns, (list, tuple)):
            fns = [fns]
        ws = self._auto_waits(reads, writes, psum) + list(extra)
        tok = None
        for k, fn in enumerate(fns):
            tok = self.op(eng, fn, waits=ws if k == 0 else (), signal=(k == len(fns) - 1))
        self._auto_done(tok, reads, writes, psum)
        return tok

    def adma(self, q, out, in_, sem, reads=(), writes=(), extra=(), **kw):
        ws = self._auto_waits(reads, writes, ()) + list(extra)
        tok = self.dma(q, out, in_, sem, waits=ws, **kw)
        self._auto_done(tok, reads, writes, ())
        return tok

    def acdma(self, q, fn, sem, reads=(), writes=(), extra=()):
        ws = self._auto_waits(reads, writes, ()) + list(extra)
        tok = self.cdma(q, fn, sem, waits=ws)
        self._auto_done(tok, reads, writes, ())
        return tok

    def emit(self, final_waits):
        nc = self.nc
        with nc.Block() as block:
            def mk(eng):
                def body(e):
                    for fn, ws, inc in self.ops[eng]:
                        for sem, val in ws:
                            e.wait_ge(sem, val)
                        ins = fn(e)
                        if inc is not None:
                            ins.then_inc(inc[0], inc[1])
                    if eng == "sync":
                        for w in final_waits:
                            if w is not None:
                                e.wait_ge(w[0], w[1])
                return body
            block.tensor(mk("tensor"))
            block.vector(mk("vector"))
            block.scalar(mk("scalar"))
            block.gpsimd(mk("gpsimd"))
            block.sync(mk("sync"))


class Arena:
    def __init__(self, t, nwords):
        self.t = t
        self.n = nwords
        self.top = 0

    def mark(self):
        return self.top

    def reset(self, m):
        self.top = m

    def alloc(self, shape, dt):
        per = 1
        for s_ in shape[1:]:
            per *= s_
        if dt == BF16:
            words = (per + 1) // 2
        else:
            words = per
        words = (words + 7) // 8 * 8
        a = self.top
        self.top += words
        assert self.top <= self.n, ("arena overflow", self.top, self.n)
        v = self.t[:, a:a + words]
        if dt == BF16:
            v = v.bitcast(BF16)[:, 0:per]
        elif dt == I32:
            v = v.bitcast(I32)[:, 0:per]
        else:
            v = v[:, 0:per]
        if len(shape) == 3:
            v = v.rearrange("p (a b) -> p a b", a=shape[1])
        elif len(shape) == 4:
            v = v.rearrange("p (a b c) -> p a b c", a=shape[1], b=shape[2])
        if shape[0] != 128:
            v = v[0:shape[0]]
        return v


def build(upto="all", taps=(), NQT=8):
    nc = bass.Bass("TRN2", target_bir_lowering=False)
    din = lambda name, shape, dt: nc.dram_tensor(name, shape, dt, kind="ExternalInput").ap()
    xf = din("xf", [S, D], F32)
    xo = din("xo", [NOWN, D], F32)
    xh = din("xh", [64, D], F32)
    posf = din("posf", [1, S], I32)
    poso = din("poso", [1, NOWN], I32)
    w_in = din("w_in", [D, 5120], F32)
    b_gate = din("b_gate", [1, 2048], F32)
    lam_in = din("lam_in", [1, 256], F32)
    subln_g = din("subln_g", [1, 128], F32)
    w_o_att = din("w_o_att", [512, D], F32)
    conv_w = din("conv_w", [3, 512], F32)
    w_o_conv = din("w_o_conv", [512, D], F32)
    w_mix = din("w_mix", [D, D], F32)
    ln1 = din("ln1", [2, D], F32)
    ln2 = din("ln2", [2, D], F32)
    w_rt = din("w_rt", [D, 36], F32)
    b_rt = din("b_rt", [1, 36], F32)
    w_eg = din("w_eg", [32, D, 512], F32)
    w_eu = din("w_eu", [32, D, 512], F32)
    w_ed = din("w_ed", [32, 512, D], F32)
    cbf = din("cbf", [128, 384 + 4096], F32)
    cf32 = din("cf32", [128, 512], F32)
    out = nc.dram_tensor("out", [NOWN, D], F32, kind="ExternalOutput").ap()
    h1f = nc.dram_tensor("h1f", [NOWN, D], F32, kind="Internal").ap()
    xs_d = nc.dram_tensor("xs_d", [NSLOT, D], BF16, kind="Internal").ap()
    ys_d = nc.dram_tensor("ys_d", [NSLOT, D], F32, kind="Internal").ap()
    tap_out = {}

    with ExitStack() as es:
        P = Prog(nc, es)
        NW = 51 * 1024
        arena_t = es.enter_context(nc.sbuf_tensor("arena", [128, NW], F32))
        AR = Arena(arena_t, NW)
        psum = es.enter_context(nc.psum_tensor("psum", [128, 8, 512], F32))
        PB = [psum[:, i, :] for i in range(8)]
        PBb = [psum[:, i, :].bitcast(BF16) for i in range(8)]
        finals = []

        def tap(name, ap, waits, dt=F32):
            if name not in taps:
                return
            shp = list(ap.shape)
            o = nc.dram_tensor("tap_" + name, shp, dt, kind="ExternalOutput").ap()
            tap_out[name] = shp
            finals.append(P.dma("sync", o, ap, "tap_" + name, waits=waits))

        cb_t = AR.alloc([128, 384], BF16)
        ident = cb_t[:, 0:128]
        rotm = cb_t[:, 128:256]
        onesb = cb_t[:, 256:384]
        cf_t = AR.alloc([128, 512], F32)
        identf = cf_t[:, 0:128]
        onesf = cf_t[:, 128:256]
        trif = cf_t[:, 256:384]
        invf = cf_t[:, 384:385]
        ecap = cf_t[:, 392:424]
        mhalf = cf_t[:, 424:425]
        epsc = cf_t[:, 425:426]
        d_cb = P.dma("gpsimd", cb_t, cbf[:, 0:384], "cb")
        d_cf = P.dma("sync", cf_t, cf32, "cf")
        QO = AR.alloc([128, 4, NOWN], BF16)
        small = AR.alloc([128, 64], F32)
        neglam = small[:, 0:1]
        gsc = small[:, 1:2]
        lamv = AR.alloc([128, 256], F32)
        slg = AR.alloc([128, 128], F32)
        d_lam = P.dma("sync", lamv, lam_in.to_broadcast([128, 256]), "lam")
        d_slg = P.dma("sync", slg[:, 0:1], subln_g.rearrange("o p -> p o"), "slg", allow_slow_non_contiguous=True)
        lt = small[:, 8:10]
        t_l1 = P.op("vector", lambda e: e.tensor_tensor(out=lamv[:, 0:64], in0=lamv[:, 0:64], in1=lamv[:, 64:128], op=ALU.mult), waits=[d_lam])
        t_l2 = P.op("vector", lambda e: e.tensor_tensor(out=lamv[:, 128:192], in0=lamv[:, 128:192], in1=lamv[:, 192:256], op=ALU.mult), waits=[d_lam])
        t_l3 = P.op("vector", lambda e: e.tensor_reduce(out=lt[:, 0:1], in_=lamv[:, 0:64], axis=AX.X, op=ALU.add), waits=[t_l1])
        t_l4 = P.op("vector", lambda e: e.tensor_reduce(out=lt[:, 1:2], in_=lamv[:, 128:192], axis=AX.X, op=ALU.add), waits=[t_l2])
        t_l5 = P.op("scalar", lambda e: e.activation(out=small[:, 10:12], in_=lt, func=AF.Exp), waits=[t_l3, t_l4])
        t_l6 = P.op("vector", lambda e: e.tensor_tensor(out=small[:, 12:13], in0=small[:, 11:12], in1=small[:, 10:11], op=ALU.subtract), waits=[t_l5])
        t_l7 = P.op("vector", lambda e: e.tensor_scalar(out=neglam, in0=small[:, 12:13], scalar1=-LAMBDA_INIT, scalar2=None, op0=ALU.add), waits=[t_l6])
        t_g = P.op("vector", lambda e: e.tensor_scalar(out=gsc, in0=slg[:, 0:1], scalar1=1.0 - LAMBDA_INIT, scalar2=None, op0=ALU.mult), waits=[d_slg])
        t_consts = [d_cb, d_cf, t_l7, t_g]
        LG = AR.alloc([128, 32, 36], F32)
        PW = AR.alloc([128, 2, 32], F32)
        SLI = AR.alloc([128, 2, 32], I32)
        pers_mark = AR.mark()

        KT = AR.alloc([128, 2, S], BF16)
        maskt_t = AR.alloc([128, 4096], BF16)
        maskt = maskt_t.rearrange("p (r q) -> p r q", r=8)
        d_mask = P.dma("gpsimd", maskt_t, cbf[:, 384:384 + 4096], "mask", max_dma_last_dim=4096)
        VV = AR.alloc([128, 64, 256], BF16)
        wq = AR.alloc([128, 8, 512], BF16)
        wkv = AR.alloc([128, 8, 512], BF16)
        XBt = [AR.alloc([128, 4, D], BF16) for _ in range(2)]
        XTt = [AR.alloc([128, 8, 512], BF16) for _ in range(2)]
        post2 = [AR.alloc([128, 512], F32) for _ in range(2)]
        tq = AR.alloc([128, 512], F32)
        ki = AR.alloc([128, 512], I32)
        cst2 = [AR.alloc([128, 512], F32) for _ in range(2)]
        snt2 = [AR.alloc([128, 512], F32) for _ in range(2)]
        qsb = [AR.alloc([128, 512], BF16) for _ in range(2)]
        ra = [AR.alloc([128, 512], F32) for _ in range(2)]
        rb = [AR.alloc([128, 512], F32) for _ in range(2)]
        PT = [AR.alloc([128, 2, 512], BF16) for _ in range(3)]
        EE = [AR.alloc([128, 512], F32) for _ in range(4)]

        st = {"xb_free": [None, None], "xt_free": [None, None], "tp_free": [None, None],
              "kp_free": [[], []], "rp_free": [None, None], "vp_free": [None, None],
              "tab_free": [[], []], "qs_free": [None, None], "ra_free": [None, None],
              "n_kp": 0, "n_tp": 0, "n_vp": 0, "n_x": 0, "n_tab": 0, "last_sin": None}

        def load_w(dst, src_cols, sem, waits):
            return P.dma("gpsimd", dst, src_cols.rearrange("(c p) n -> p c n", p=128), sem, waits=waits)

        def tables_dma(pos_src, t0):
            ti = st["n_tab"] % 2
            st["n_tab"] += 1
            w0 = list(st["tab_free"][ti])
            d = P.dma("gpsimd", post2[ti], pos_src[0:1, t0:t0 + 512].to_broadcast([128, 512]), "pos%d" % ti, waits=w0)
            return ti, d, w0

        def tables(pos_src, t0):
            return tables_compute(*tables_dma(pos_src, t0))

        def tables_compute(ti, d, w0):
            post = post2[ti]
            prev = [st["last_sin"]]
            for dst, add in ((snt2[ti], 0.0), (cst2[ti], 0.25)):
                a = P.op("vector", lambda e, add=add: e.tensor_scalar(out=tq, in0=post, scalar1=invf, scalar2=add, op0=ALU.mult, op1=ALU.add), waits=[d, d_cf] + prev)
                b = P.op("vector", lambda e: e.tensor_copy(out=ki, in_=tq), waits=[a])
                c = P.op("vector", lambda e: e.tensor_tensor(out=tq, in0=tq, in1=ki, op=ALU.subtract), waits=[b])
                s_ = P.op("scalar", lambda e, dst=dst: e.activation(out=dst, in_=tq, func=AF.Sin, scale=TWO_PI), waits=[c] + w0)
                prev = [s_]
            st["last_sin"] = prev[0]
            return prev[0], ti

        def load_x_tile(src, row0):
            i = st["n_x"] % 2
            st["n_x"] += 1
            xb = XBt[i]
            d = P.dma("gpsimd", xb, src[row0:row0 + 512, :].rearrange("(b p) d -> p b d", p=128),
                      "xb%d" % i, waits=[st["xb_free"][i]])
            return i, d

        def transpose_tile(i, dx):
            xb = XBt[i]
            xt = XTt[i]
            evs = []
            last_t = None
            for g in range(4):
                bk = st["n_tp"] % 2
                st["n_tp"] += 1
                for cc in range(2):
                    c = 2 * g + cc
                    for blk in range(4):
                        last = (cc == 1 and blk == 3)
                        o_ = PBb[bk][:, cc * 512 + blk * 128: cc * 512 + blk * 128 + 128]
                        i_ = xb[:, blk, c * 128:(c + 1) * 128]
                        tk = P.op("tensor", lambda e, o_=o_, i_=i_: e.transpose(out=o_, in_=i_, identity=ident),
                                  waits=[dx, d_cb, st["tp_free"][bk]], signal=last)
                        if last:
                            last_t = tk
                dst = xt[:, 2 * g:2 * g + 2, :].rearrange("p a b -> p (a b)")
                src = PBb[bk]
                if g % 2 == 0:
                    ev = P.op("vector", lambda e, dst=dst, src=src: e.tensor_copy(out=dst, in_=src), waits=[last_t, st["xt_free"][i]])
                else:
                    ev = P.op("scalar", lambda e, dst=dst, src=src: e.copy(out=dst, in_=src), waits=[last_t, st["xt_free"][i]])
                st["tp_free"][bk] = ev
                evs.append(ev)
            st["xb_free"][i] = last_t
            return evs

        def proj_part1(xt, evs, wt, wcol, tab_tok, ti, extra_w):
            kb = st["n_kp"] % 2
            st["n_kp"] += 1
            kp = PB[2 + kb]
            q_ = qsb[kb]
            ra_ = ra[kb]
            cs_ = cst2[ti]
            mm = None
            for c in range(8):
                l_ = wt[:, c, wcol:wcol + 128]
                r_ = xt[:, c, :]
                mm = P.op("tensor", lambda e, l_=l_, r_=r_, c=c: e.matmul(kp, lhsT=l_, rhs=r_, start=(c == 0), stop=(c == 7)),
                          waits=evs + st["kp_free"][kb] + extra_w, signal=(c == 7))
            cp = P.op("scalar", lambda e: e.copy(out=q_, in_=kp), waits=[mm, st["qs_free"][kb]])
            a = P.op("vector", lambda e: e.tensor_tensor(out=ra_, in0=kp, in1=cs_, op=ALU.mult), waits=[mm, cp, tab_tok, st["ra_free"][kb]])
            st["kp_free"][kb] = [a, cp]
            return {"kb": kb, "cp": cp, "a": a, "tab": tab_tok, "ti": ti, "mm": mm}

        def proj_part2(cx, dst):
            kb = cx["kb"]
            rp = PB[4 + kb]
            q_ = qsb[kb]
            ra_ = ra[kb]
            rb_ = rb[kb]
            sn_ = snt2[cx["ti"]]
            rm = P.op("tensor", lambda e: e.matmul(rp, lhsT=rotm, rhs=q_, start=True, stop=True), waits=[cx["cp"], d_cb, st["rp_free"][kb]])
            st["qs_free"][kb] = rm
            b = P.op("vector", lambda e: e.tensor_tensor(out=rb_, in0=rp, in1=sn_, op=ALU.mult), waits=[rm, cx["tab"], st["ra_free"][kb]])
            st["rp_free"][kb] = b
            f = P.op("vector", lambda e: e.tensor_tensor(out=dst, in0=ra_, in1=rb_, op=ALU.add), waits=[cx["a"], b])
            st["ra_free"][kb] = f
            return f, b, rm

        pre = {}

        def prefetch(key, src, pos_src, T):
            i, dx = load_x_tile(src, T * 512)
            tab, ti = tables(pos_src, T * 512)
            pre[key] = (i, dx, tab, ti)

        def prefetch_dma(key, src, pos_src, T):
            i, dx = load_x_tile(src, T * 512)
            return (key, i, dx, tables_dma(pos_src, T * 512))

        def prefetch_compute(pf):
            key, i, dx, td = pf
            tab, ti = tables_compute(*td)
            pre[key] = (i, dx, tab, ti)

        def a_tile(T, dwk, dwv, last, nxt):
            i, dx, tab, ti = pre.pop(("a", T))
            pf = prefetch_dma(*nxt) if nxt is not None else None
            evs = transpose_tile(i, dx)
            cxs = [proj_part1(XTt[i], evs, wkv, hl * 128, tab, ti, [dwk]) for hl in range(2)]
            if pf is not None:
                prefetch_compute(pf)
            mm = None
            for blk in range(4):
                vb = st["n_vp"] % 2
                st["n_vp"] += 1
                vp = PB[6 + vb][:, 0:256]
                for c in range(8):
                    l_ = XTt[i][:, c, blk * 128:(blk + 1) * 128]
                    r_ = wkv[:, c, 256:512]
                    mm = P.op("tensor", lambda e, l_=l_, r_=r_, c=c, vp=vp: e.matmul(vp, lhsT=l_, rhs=r_, start=(c == 0), stop=(c == 7)),
                              waits=evs + [dwv, st["vp_free"][vb]], signal=(c == 7))
                o_ = VV[:, T * 4 + blk, :]
                vts = P.op("scalar", lambda e, o_=o_, vp=vp: e.copy(out=o_, in_=vp), waits=[mm])
                st["vp_free"][vb] = vts
                last.append(vts)
            st["xt_free"][i] = mm
            tabfree = []
            for hl in range(2):
                f, b, rm = proj_part2(cxs[hl], KT[:, hl, T * 512:(T + 1) * 512])
                tabfree.append(b)
                last.append(f)
            st["tab_free"][ti] = tabfree

        def phase_A(hp, then_q):
            dwk = load_w(wkv[:, :, 0:256], w_in[:, 512 + hp * 256: 512 + hp * 256 + 256], "wk", [])
            dwv = load_w(wkv[:, :, 256:512], w_in[:, 1024 + hp * 256: 1024 + hp * 256 + 256], "wv", [])
            last = []
            prefetch(("a", 0), xf, posf, 0)
            for T in range(16):
                if T + 1 < 16:
                    nxt = (("a", T + 1), xf, posf, T + 1)
                elif then_q:
                    nxt = (("q", 0), xo, poso, 0)
                else:
                    nxt = None
                a_tile(T, dwk, dwv, last, nxt)
            return last

        def q_tile(T, dwq, last):
            i, dx, tab, ti = pre.pop(("q", T))
            pf = prefetch_dma(("q", T + 1), xo, poso, T + 1) if T + 1 < NQT else None
            evs = transpose_tile(i, dx)
            tabfree = []
            rm = None
            cxs = {}
            cxs[0] = proj_part1(XTt[i], evs, wq, 0, tab, ti, [dwq])
            if pf is not None:
                prefetch_compute(pf)
            for h in range(4):
                if h + 1 < 4:
                    cxs[h + 1] = proj_part1(XTt[i], evs, wq, (h + 1) * 128, tab, ti, [dwq])
                f, b, rm = proj_part2(cxs[h], QO[:, h, T * 512:(T + 1) * 512])
                tabfree.append(b)
                last.append(f)
            st["tab_free"][ti] = tabfree
            st["xt_free"][i] = rm

        def phase_Q():
            dwq = load_w(wq, w_in[:, 0:512], "wq", [])
            last = []
            for T in range(NQT):
                q_tile(T, dwq, last)
            return last

        ACC0 = ra[0]
        att = {"a0": None, "accL_free": None, "s_free": [None, None], "pt_free": [[], [], []], "acc_free": None, "n_s": 0, "n_pt": 0, "ee_free": None, "pending": None}

        def att_unit(i, hl, h):
            nkb = 8 * i + 8
            qk_tok = {}
            qrange = slice(i * 512, (i + 1) * 512)

            def issue_qk(kb):
                s = att["n_s"] % 2
                att["n_s"] += 1
                krange = slice(kb * 128, (kb + 1) * 128)
                P.op("tensor", lambda e: e.matmul(psum[:, 2 * s, :], lhsT=KT[0:64, hl, krange], rhs=QO[0:64, h, qrange], start=True, stop=True),
                     waits=[att["s_free"][s]], signal=False)
                t = P.op("tensor", lambda e: e.matmul(psum[:, 2 * s + 1, :], lhsT=KT[64:128, hl, krange], rhs=QO[64:128, h, qrange], start=True, stop=True))
                qk_tok[kb] = (t, s)

            def pv_step(kb):
                t, s = qk_tok[kb]
                pi = att["n_pt"] % 3
                att["n_pt"] += 1
                pt = PT[pi]
                ex = P.op("scalar", lambda e: e.activation(out=pt, in_=psum[:, 2 * s:2 * s + 2, :], func=AF.Exp, scale=0.125), waits=[t] + att["pt_free"][pi])
                att["s_free"][s] = ex
                pv_w = ex
                if kb >= 8 * i:
                    r = kb - 8 * i
                    pv_w = P.op("vector", lambda e: e.tensor_tensor(out=pt, in0=pt, in1=maskt[:, r, :].unsqueeze(1).to_broadcast([128, 2, 512]), op=ALU.mult),
                                waits=[ex, d_mask])
                s0 = (kb == 0)
                s1 = (kb == nkb - 1)
                w0 = [pv_w, att["acc_free"]] if kb == 0 else [pv_w]
                vv = VV[:, kb, hl * 128:(hl + 1) * 128]
                P.op("tensor", lambda e: e.matmul(PB[4], lhsT=vv, rhs=pt[:, 0, :], start=s0, stop=s1), waits=w0, signal=False)
                P.op("tensor", lambda e: e.matmul(PB[5], lhsT=vv, rhs=pt[:, 1, :], start=s0, stop=s1), signal=False)
                pv = P.op("tensor", lambda e: e.matmul(PB[7], lhsT=onesb, rhs=pt[:, 1, :], start=s0, stop=s1))
                if kb == 0:
                    a0 = P.op("vector", lambda e: e.tensor_copy(out=ACC0, in_=pt[:, 0, :]), waits=[pv_w, att["accL_free"]])
                else:
                    a0 = P.op("vector", lambda e: e.tensor_tensor(out=ACC0, in0=ACC0, in1=pt[:, 0, :], op=ALU.add), waits=[pv_w, att["a0"]])
                att["a0"] = a0
                att["pt_free"][pi] = [pv, a0]
                return pv

            issue_qk(0)
            pv = None
            for kb in range(nkb):
                if kb + 1 < nkb:
                    issue_qk(kb + 1)
                pv = pv_step(kb)
                if kb == 2 and att["pending"] is not None:
                    att["pending"]()
                    att["pending"] = None
            if att["pending"] is not None:
                att["pending"]()
                att["pending"] = None
            wfree = [att["ee_free"]]
            lsum = P.op("tensor", lambda e: e.matmul(PB[6], lhsT=onesf, rhs=ACC0, start=True, stop=True), waits=[att["a0"], att["acc_free"], d_cf])
            att["accL_free"] = lsum
            e0 = P.op("vector", lambda e: e.reciprocal(out=EE[0], in_=PB[6]), waits=[pv, lsum] + wfree)
            e1 = P.op("vector", lambda e: e.reciprocal(out=EE[1], in_=PB[7]), waits=[pv, lsum] + wfree)
            e2 = P.op("vector", lambda e: e.tensor_tensor(out=EE[0], in0=PB[4], in1=EE[0], op=ALU.mult), waits=[e0])
            e3 = P.op("vector", lambda e: e.tensor_tensor(out=EE[1], in0=PB[5], in1=EE[1], op=ALU.mult), waits=[e1])
            att["acc_free"] = e3
            e4 = P.op("vector", lambda e: e.scalar_tensor_tensor(out=EE[2], in0=EE[1], scalar=neglam, in1=EE[0], op0=ALU.mult, op1=ALU.add), waits=[e2, e3, t_l7] + wfree)
            e5 = P.op("gpsimd", lambda e: e.tensor_tensor(out=EE[3], in0=EE[2], in1=EE[2], op=ALU.mult), waits=[e4] + wfree)

            def finish():
                s = att["n_s"] % 2
                att["n_s"] += 2
                ss = P.op("tensor", lambda e: e.matmul(psum[:, 2 * s, :], lhsT=onesf, rhs=EE[3], start=True, stop=True), waits=[e5, d_cf, att["s_free"][s]])
                e6 = P.op("scalar", lambda e: e.activation(out=EE[3], in_=psum[:, 2 * s, :], func=AF.Ln, scale=1.0 / 128.0, bias=epsc), waits=[ss])
                att["s_free"][s] = e6
                e7 = P.op("scalar", lambda e: e.activation(out=EE[3], in_=EE[3], func=AF.Exp, scale=-0.5), waits=[e6])
                e8 = P.op("vector", lambda e: e.tensor_tensor(out=EE[2], in0=EE[2], in1=EE[3], op=ALU.mult), waits=[e7])
                e9 = P.op("vector", lambda e: e.tensor_scalar(out=QO[:, h, qrange], in0=EE[2], scalar1=gsc, scalar2=None, op0=ALU.mult), waits=[e8, t_g])
                att["ee_free"] = e9
                att["last"] = e9
            att["pending"] = finish

        def attention(hp):
            for i in range(NQT):
                for hl in range(2):
                    att_unit(i, hl, 2 * hp + hl)
            att["pending"]()
            att["pending"] = None
            return att["last"]

        att_last = None
        if upto == "consts":
            tap("small", small, t_consts)
            P.emit(finals)
            return nc, tap_out
        for hp in range(2):
            kvt = phase_A(hp, hp == 0)
            if upto == "A":
                tap("kt", KT[:, :, 0:2048], kvt, BF16)
                tap("vv", VV[:, 0:8, :], kvt, BF16)
                P.emit(finals)
                return nc, tap_out
            qtoks = phase_Q() if hp == 0 else []
            P.base_waits = [t for t in kvt + qtoks if t is not None] + t_consts
            att_last = attention(hp)
            P.base_waits = [att_last]
            if upto == "att0":
                break
        tap("qo", QO, [att_last], BF16)
        tap("kt", KT[:, :, 0:2048], [att_last], BF16)
        tap("vv", VV[:, 0:8, :], [att_last], BF16)

        if upto in ("att0", "att"):
            P.emit(finals)
            return nc, tap_out

        AR.reset(pers_mark)
        wp = AR.alloc([128, 8, 3584], BF16)
        woa = AR.alloc([128, 4, 1024], BF16)
        woc = AR.alloc([128, 4, 1024], BF16)
        wmx = AR.alloc([128, 8, 1024], BF16)
        xb2_ = [AR.alloc([128, 2, 1024], BF16) for _ in range(2)]
        xhb_ = [AR.alloc([128, 1024], BF16) for _ in range(2)]
        xres_ = [AR.alloc([128, 2, 1024], F32) for _ in range(2)]
        xT2 = AR.alloc([128, 8, 256], BF16)
        xhT = AR.alloc([128, 32], BF16)
        ccs_ = [AR.alloc([128, 264], F32) for _ in range(2)]
        Ub_ = [AR.alloc([128, 2, 130], F32) for _ in range(2)]
        T1_ = [AR.alloc([128, 2, 128], F32) for _ in range(2)]
        Zb = AR.alloc([128, 4, 256], BF16)
        g0_ = [AR.alloc([128, 256], F32) for _ in range(2)]
        g1_ = [AR.alloc([128, 256], F32) for _ in range(2)]
        m0_ = [AR.alloc([128, 256], F32) for _ in range(2)]
        m1_ = [AR.alloc([128, 256], F32) for _ in range(2)]
        MT = AR.alloc([128, 8, 256], BF16)
        Rb = AR.alloc([128, 1024], F32)
        lng = AR.alloc([128, 1024], F32)
        lnb = AR.alloc([128, 1024], F32)
        H1T = AR.alloc([128, 8, 128], F32)
        stt_b = AR.alloc([128, 16], F32)
        bgs = AR.alloc([128, 16], F32)
        cws = AR.alloc([128, 12], F32)
        rbias = AR.alloc([128, 36], F32)
        wr = AR.alloc([128, 8, 36], F32)
        for i7 in range(7):
            P.adma("gpsimd", wp[:, :, i7 * 512:(i7 + 1) * 512], w_in[:, 1536 + i7 * 512:1536 + (i7 + 1) * 512].rearrange("(c p) n -> p c n", p=128), "wp", writes=["wp"])
        P.adma("gpsimd", woa, w_o_att.rearrange("(c p) n -> p c n", p=128), "woa", writes=["woa"])
        P.adma("gpsimd", woc, w_o_conv.rearrange("(c p) n -> p c n", p=128), "woc", writes=["woc"])
        P.adma("gpsimd", wmx, w_mix.rearrange("(c p) n -> p c n", p=128), "wmx", writes=["wmx"])
        P.adma("sync", wr, w_rt.rearrange("(c p) n -> p c n", p=128), "wr", writes=["wr"])
        P.adma("sync", bgs, b_gate.rearrange("o (q p) -> p (o q)", p=128), "bgs", writes=["bgs"], allow_slow_non_contiguous=True)
        P.adma("sync", cws.rearrange("p (k q) -> p k q", k=3), conv_w.rearrange("k (q p) -> p k q", p=128), "cws", writes=["cws"], allow_slow_non_contiguous=True)
        P.adma("sync", rbias, b_rt.to_broadcast([128, 36]), "rbias", writes=["rbias"])
        P.adma("sync", lng, ln1[0:1, :].to_broadcast([128, 1024]), "lng", writes=["lng"])
        P.adma("sync", lnb, ln1[1:2, :].to_broadcast([128, 1024]), "lnb", writes=["lnb"])

        def layer_norm(buf, gname, bname, gt, bt, stt=None, rn="R"):
            stt = stt_b if stt is None else stt
            mv = stt[:, 12:14]
            ve = stt[:, 14:15]
            rs = stt[:, 15:16]
            P.auto("vector", lambda e: e.bn_stats(out=stt[:, 0:6], in_=buf[:, 0:512]), reads=[rn], writes=["stt"])
            P.auto("vector", lambda e: e.bn_stats(out=stt[:, 6:12], in_=buf[:, 512:1024]), reads=[rn], writes=["stt2"])
            P.auto("vector", lambda e: e.bn_aggr(out=mv, in_=stt[:, 0:12]), reads=["stt", "stt2"], writes=["mv"])
            P.auto("vector", lambda e: e.tensor_scalar(out=ve, in0=mv[:, 1:2], scalar1=1e-5, scalar2=None, op0=ALU.add), reads=["mv"], writes=["ve"])
            P.auto("gpsimd", lambda e: e.tensor_tensor(out=rs, in0=ve, in1=mhalf, op=ALU.pow), reads=["ve"], writes=["rs"], extra=[d_cf])
            P.auto("vector", lambda e: e.tensor_scalar(out=buf, in0=buf, scalar1=mv[:, 0:1], scalar2=rs, op0=ALU.subtract, op1=ALU.mult), reads=["mv", "rs"], writes=[rn])
            P.auto("vector", lambda e: e.tensor_tensor(out=buf, in0=buf, in1=gt, op=ALU.mult), reads=[gname], writes=[rn])
            return P.auto("gpsimd", lambda e: e.tensor_tensor(out=buf, in0=buf, in1=bt, op=ALU.add), reads=[bname], writes=[rn])

        def b_loads(tb):
            r0 = tb * 256
            pb = tb % 2
            P.adma("gpsimd", xb2_[pb], xo[r0:r0 + 256, :].rearrange("(b p) d -> p b d", p=128), "xb2%d" % pb, writes=["xb2%d" % pb])
            P.adma("gpsimd", xhb_[pb][0:4, :], xh[tb * 4:tb * 4 + 4, :], "xhb%d" % pb, writes=["xhb%d" % pb])
            P.adma("sync", xres_[pb], xo[r0:r0 + 256, :].rearrange("(b p) d -> p b d", p=128), "xres%d" % pb, writes=["xres%d" % pb])

        def b_tile(tb):
            r0 = tb * 256
            pb = tb % 2
            xb2, xhb, xres = xb2_[pb], xhb_[pb], xres_[pb]
            n_xb2, n_xhb, n_xres = "xb2%d" % pb, "xhb%d" % pb, "xres%d" % pb
            if tb + 1 < NTB:
                b_loads(tb + 1)
            for half in range(2):
                fns = []
                for cc in range(4):
                    c = half * 4 + cc
                    for blk in range(2):
                        o_ = PBb[half][:, cc * 256 + blk * 128: cc * 256 + blk * 128 + 128]
                        i_ = xb2[:, blk, c * 128:(c + 1) * 128]
                        fns.append(lambda e, o_=o_, i_=i_: e.transpose(out=o_, in_=i_, identity=ident))
                P.auto("tensor", fns, reads=[n_xb2], psum=["b%d" % half], extra=[d_cb])
                dst = xT2[:, half * 4:half * 4 + 4, :].rearrange("p a b -> p (a b)")
                if half == 0:
                    P.auto("vector", lambda e, dst=dst: e.tensor_copy(out=dst, in_=PBb[0]), writes=["xT2a"], psum=["b0"])
                else:
                    P.auto("scalar", lambda e, dst=dst: e.copy(out=dst, in_=PBb[1]), writes=["xT2b"], psum=["b1"])
            fns = []
            for c in range(8):
                o_ = PBb[6][:, c * 4:(c + 1) * 4]
                i_ = xhb[0:4, c * 128:(c + 1) * 128]
                fns.append(lambda e, o_=o_, i_=i_: e.transpose(out=o_, in_=i_, identity=ident[0:4, 0:4]))
            P.auto("tensor", fns, reads=[n_xhb], psum=["b6"], extra=[d_cb])
            P.auto("vector", lambda e: e.tensor_copy(out=xhT, in_=PBb[6][:, 0:32]), writes=["xhT"], psum=["b6"])
            xhT3 = xhT.rearrange("p (c k) -> p c k", c=8)
            for q in range(4):
                qp = q % 2
                BA, BB = PB[2 + 2 * qp], PB[3 + 2 * qp]
                nBA, nBB = "b%d" % (2 + 2 * qp), "b%d" % (3 + 2 * qp)
                ccs, Ub, T1 = ccs_[qp], Ub_[qp], T1_[qp]
                nccs, nU, nU2, nT1 = "ccs%d" % qp, "U%d" % qp, "U2%d" % qp, "T1%d" % qp
                fns = []
                for c in range(8):
                    l_ = wp[:, c, 512 + q * 128: 512 + q * 128 + 128]
                    fns.append(lambda e, l_=l_, c=c, BA=BA: e.matmul(BA[:, 0:256], lhsT=l_, rhs=xT2[:, c, :], start=(c == 0), stop=(c == 7)))
                for c in range(8):
                    l_ = wp[:, c, 512 + q * 128: 512 + q * 128 + 128]
                    fns.append(lambda e, l_=l_, c=c, BA=BA: e.matmul(BA[:, 256:260], lhsT=l_, rhs=xhT3[:, c, :], start=(c == 0), stop=(c == 7)))
                for c in range(8):
                    l_ = wp[:, c, 1024 + q * 128: 1024 + q * 128 + 128]
                    fns.append(lambda e, l_=l_, c=c, BA=BA: e.matmul(BA[:, 260:264], lhsT=l_, rhs=xhT3[:, c, :], start=(c == 0), stop=(c == 7)))
                P.auto("tensor", fns, reads=["wp", "xT2a", "xT2b", "xhT"], psum=[nBA])
                fns = []
                for c in range(8):
                    l_ = wp[:, c, 1024 + q * 128: 1024 + q * 128 + 128]
                    fns.append(lambda e, l_=l_, c=c, BB=BB: e.matmul(BB[:, 0:256], lhsT=l_, rhs=xT2[:, c, :], start=(c == 0), stop=(c == 7)))
                for c in range(8):
                    l_ = wp[:, c, q * 128: q * 128 + 128]
                    fns.append(lambda e, l_=l_, c=c, BB=BB: e.matmul(BB[:, 256:512], lhsT=l_, rhs=xT2[:, c, :], start=(c == 0), stop=(c == 7)))
                P.auto("tensor", fns, reads=["wp", "xT2a", "xT2b"], psum=[nBB])
                P.auto("scalar", lambda e, ccs=ccs, BA=BA: e.copy(out=ccs, in_=BA[:, 0:264]), writes=[nccs], psum=[nBA])
                P.auto("vector", lambda e, ccs=ccs, Ub=Ub, BB=BB: e.tensor_tensor(out=Ub[:, :, 2:130], in0=ccs[:, 0:256].rearrange("p (b t) -> p b t", b=2),
                                                          in1=BB[:, 0:256].rearrange("p (b t) -> p b t", b=2), op=ALU.mult),
                       reads=[nccs], writes=[nU], psum=[nBB])
                P.auto("vector", lambda e, ccs=ccs, Ub=Ub: e.tensor_tensor(out=Ub[:, :, 0:2], in0=ccs[:, 256:260].rearrange("p (b t) -> p b t", b=2),
                                                          in1=ccs[:, 260:264].rearrange("p (b t) -> p b t", b=2), op=ALU.mult),
                       reads=[nccs], writes=[nU2])
                P.auto("vector", lambda e, q=q, T1=T1, Ub=Ub: e.tensor_scalar(out=T1, in0=Ub[:, :, 0:128], scalar1=cws[:, q:q + 1], scalar2=None, op0=ALU.mult),
                       reads=[nU, nU2, "cws"], writes=[nT1])
                P.auto("vector", lambda e, q=q, T1=T1, Ub=Ub: e.scalar_tensor_tensor(out=T1, in0=Ub[:, :, 1:129], scalar=cws[:, 4 + q:5 + q], in1=T1, op0=ALU.mult, op1=ALU.add),
                       reads=[nU, nU2], writes=[nT1])
                P.auto("vector", lambda e, q=q, T1=T1, Ub=Ub: e.scalar_tensor_tensor(out=T1, in0=Ub[:, :, 2:130], scalar=cws[:, 8 + q:9 + q], in1=T1, op0=ALU.mult, op1=ALU.add),
                       reads=[nU, nU2], writes=[nT1])
                P.auto("vector", lambda e, q=q, T1=T1, BB=BB: e.tensor_tensor(out=Zb[:, q, :], in0=T1.rearrange("p b t -> p (b t)"), in1=BB[:, 256:512], op=ALU.mult),
                       reads=[nT1], writes=["Z%d" % q], psum=[nBB])
            for c8 in range(8):
                cp_ = c8 % 2
                BG, BY = PB[2 + 2 * cp_], PB[3 + 2 * cp_]
                nBG, nBY = "b%d" % (2 + 2 * cp_), "b%d" % (3 + 2 * cp_)
                g0, g1, m0, m1 = g0_[cp_], g1_[cp_], m0_[cp_], m1_[cp_]
                ng0, ng1, nm0, nm1 = "g0%d" % cp_, "g1%d" % cp_, "m0%d" % cp_, "m1%d" % cp_
                fns = []
                for c in range(8):
                    l_ = wp[:, c, 1536 + c8 * 128: 1536 + c8 * 128 + 128]
                    fns.append(lambda e, l_=l_, c=c, BG=BG: e.matmul(BG[:, 0:256], lhsT=l_, rhs=xT2[:, c, :], start=(c == 0), stop=(c == 7)))
                for c in range(8):
                    l_ = wp[:, c, 2560 + c8 * 128: 2560 + c8 * 128 + 128]
                    fns.append(lambda e, l_=l_, c=c, BG=BG: e.matmul(BG[:, 256:512], lhsT=l_, rhs=xT2[:, c, :], start=(c == 0), stop=(c == 7)))
                P.auto("tensor", fns, reads=["wp", "xT2a", "xT2b"], psum=[nBG])
                P.auto("scalar", lambda e, c8=c8, g0=g0, BG=BG: e.activation(out=g0, in_=BG[:, 0:256], func=AF.Sigmoid, bias=bgs[:, c8:c8 + 1]), reads=["bgs"], writes=[ng0], psum=[nBG])
                P.auto("scalar", lambda e, c8=c8, g1=g1, BG=BG: e.activation(out=g1, in_=BG[:, 256:512], func=AF.Sigmoid, bias=bgs[:, 8 + c8:9 + c8]), reads=["bgs"], writes=[ng1], psum=[nBG])
                fns = []
                for h in range(4):
                    l_ = woa[:, h, c8 * 128:(c8 + 1) * 128]
                    r_ = QO[:, h, r0:r0 + 256]
                    fns.append(lambda e, l_=l_, r_=r_, h=h, BY=BY: e.matmul(BY[:, 0:256], lhsT=l_, rhs=r_, start=(h == 0), stop=(h == 3)))
                for q in range(4):
                    l_ = woc[:, q, c8 * 128:(c8 + 1) * 128]
                    r_ = Zb[:, q, :]
                    fns.append(lambda e, l_=l_, r_=r_, q=q, BY=BY: e.matmul(BY[:, 256:512], lhsT=l_, rhs=r_, start=(q == 0), stop=(q == 3)))
                P.auto("tensor", fns, reads=["woa", "woc", "Z0", "Z1", "Z2", "Z3"], psum=[nBY])
                P.auto("vector", lambda e, m0=m0, g0=g0, BY=BY: e.tensor_tensor(out=m0, in0=g0, in1=BY[:, 0:256], op=ALU.mult), reads=[ng0], writes=[nm0], psum=[nBY])
                P.auto("vector", lambda e, m1=m1, g1=g1, BY=BY: e.tensor_tensor(out=m1, in0=g1, in1=BY[:, 256:512], op=ALU.mult), reads=[ng1], writes=[nm1], psum=[nBY])
                P.auto("vector", lambda e, c8=c8, m0=m0, m1=m1: e.tensor_tensor(out=MT[:, c8, :], in0=m0, in1=m1, op=ALU.add), reads=[nm0, nm1], writes=["MT%d" % c8])
            for blk in range(2):
                gb = tb * 2 + blk
                fns = []
                for half in range(2):
                    for c in range(8):
                        l_ = MT[:, c, blk * 128:(blk + 1) * 128]
                        r_ = wmx[:, c, half * 512:(half + 1) * 512]
                        fns.append(lambda e, l_=l_, r_=r_, c=c, half=half: e.matmul(PB[6 + half], lhsT=l_, rhs=r_, start=(c == 0), stop=(c == 7)))
                P.auto("tensor", fns, reads=["MT%d" % c_ for c_ in range(8)] + ["wmx"], psum=["b6", "b7"])
                P.auto("vector", lambda e, blk=blk: e.scalar_tensor_tensor(out=Rb[:, 0:512], in0=xres[:, blk, 0:512], scalar=ALPHA, in1=PB[6], op0=ALU.mult, op1=ALU.add),
                       reads=[n_xres], writes=["R"], psum=["b6"])
                P.auto("vector", lambda e, blk=blk: e.scalar_tensor_tensor(out=Rb[:, 512:1024], in0=xres[:, blk, 512:1024], scalar=ALPHA, in1=PB[7], op0=ALU.mult, op1=ALU.add),
                       reads=[n_xres], writes=["R"], psum=["b7"])
                layer_norm(Rb, "lng", "lnb", lng, lnb)
                finals_h1.append(P.adma("sync", h1f[gb * 128:(gb + 1) * 128, :], Rb, "h1w", reads=["R"]))
                fns = []
                for c in range(8):
                    o_ = PB[c // 4][:, (c % 4) * 128:(c % 4) * 128 + 128]
                    i_ = Rb[:, c * 128:(c + 1) * 128]
                    fns.append(lambda e, o_=o_, i_=i_: e.transpose(out=o_, in_=i_, identity=identf))
                P.auto("tensor", fns, reads=["R"], psum=["b0", "b1"], extra=[d_cf])
                P.auto("scalar", lambda e: e.copy(out=H1T[:, 0:4, :].rearrange("p a b -> p (a b)"), in_=PB[0]), writes=["H1Ta"], psum=["b0"])
                P.auto("vector", lambda e: e.tensor_copy(out=H1T[:, 4:8, :].rearrange("p a b -> p (a b)"), in_=PB[1]), writes=["H1Tb"], psum=["b1"])
                fns = []
                for c in range(8):
                    fns.append(lambda e, c=c: e.matmul(PB[0][:, 0:36], lhsT=H1T[:, c, :], rhs=wr[:, c, :], start=(c == 0), stop=(c == 7)))
                P.auto("tensor", fns, reads=["H1Ta", "H1Tb", "wr"], psum=["b0"])
                P.auto("vector", lambda e, gb=gb: e.tensor_tensor(out=LG[:, gb, :], in0=PB[0][:, 0:36], in1=rbias, op=ALU.add), reads=["rbias"], writes=["LG"], psum=["b0"])

        finals_h1 = []
        NTB = NQT * 2
        b_loads(0)
        for tb in range(NTB):
            b_tile(tb)
        tap("lg", LG, [P.lw["LG"]])
        tap("h1", h1f[0:256, :], finals_h1)
        if upto == "B":
            P.emit(finals + finals_h1)
            return nc, tap_out

        P.base_waits = [P.lw["LG"]] + finals_h1
        AR.reset(pers_mark)
        NB = NTB * 2
        gm = AR.alloc([128, 32], F32)
        gd = AR.alloc([128, 32, 4], F32)
        pen = AR.alloc([128, 32, 4], F32)
        ge = AR.alloc([128, 32, 4], F32)
        gs = AR.alloc([128, 32], F32)
        gw = AR.alloc([128, 32], F32)
        em = AR.alloc([128, 32, 32], F32)
        em2 = AR.alloc([128, 32, 32], F32)
        oh1 = AR.alloc([128, 32, 32], F32)
        oh2 = AR.alloc([128, 32, 32], F32)
        Aa = AR.alloc([128, 32, 32], F32)
        Tt = AR.alloc([128, 32, 32], F32)
        sc0 = AR.alloc([128, 32, 32], F32)
        sc1 = AR.alloc([128, 32, 32], F32)
        rank = AR.alloc([128, 32, 32], F32)
        tmpc = AR.alloc([128, 32, 32], F32)
        v1 = AR.alloc([128, 32], F32)
        v2 = AR.alloc([128, 32], F32)
        dv = AR.alloc([128, 32], F32)
        s12 = AR.alloc([128, 2, 32], F32)
        stt_c = AR.alloc([128, 16], F32)
        fl = lambda t: t.rearrange("p a b -> p (a b)")
        gl = LG[:, :, 0:4]
        el = LG[:, :, 4:36]
        bc3 = lambda t: t.unsqueeze(2).to_broadcast([128, 32, 32])
        V = "vector"
        P.auto(V, lambda e: e.tensor_reduce(out=gm, in_=gl, axis=AX.X, op=ALU.max), reads=["LG"], writes=["gm"])
        P.auto(V, lambda e: e.tensor_tensor(out=gd, in0=gl, in1=gm.unsqueeze(2).to_broadcast([128, 32, 4]), op=ALU.subtract), reads=["LG", "gm"], writes=["gd"])
        P.auto(V, lambda e: e.tensor_scalar(out=pen, in0=gd, scalar1=0.0, scalar2=NEG, op0=ALU.is_lt, op1=ALU.mult), reads=["gd"], writes=["pen"])
        P.auto("scalar", lambda e: e.activation(out=ge, in_=gd, func=AF.Exp), reads=["gd"], writes=["ge"])
        P.auto(V, lambda e: e.tensor_reduce(out=gs, in_=ge, axis=AX.X, op=ALU.add), reads=["ge"], writes=["gs"])
        P.auto(V, lambda e: e.reciprocal(out=gw, in_=gs), reads=["gs"], writes=["gw"])
        P.auto(V, lambda e: e.tensor_tensor(out=em.rearrange("p b (g k) -> p b g k", g=4), in0=el.rearrange("p b (g k) -> p b g k", g=4),
                                            in1=pen.unsqueeze(3).to_broadcast([128, 32, 4, 8]), op=ALU.add), reads=["LG", "pen"], writes=["em"])
        P.auto(V, lambda e: e.tensor_reduce(out=v1, in_=em, axis=AX.X, op=ALU.max), reads=["em"], writes=["v1"])
        P.auto(V, lambda e: e.tensor_tensor(out=oh1, in0=em, in1=bc3(v1), op=ALU.is_equal), reads=["em", "v1"], writes=["oh1"])
        P.auto(V, lambda e: e.scalar_tensor_tensor(out=fl(em2), in0=fl(oh1), scalar=NEG, in1=fl(em), op0=ALU.mult, op1=ALU.add), reads=["oh1", "em"], writes=["em2"])
        P.auto(V, lambda e: e.tensor_reduce(out=v2, in_=em2, axis=AX.X, op=ALU.max), reads=["em2"], writes=["v2"])
        P.auto(V, lambda e: e.tensor_tensor(out=oh2, in0=em2, in1=bc3(v2), op=ALU.is_equal), reads=["em2", "v2"], writes=["oh2"])
        P.auto(V, lambda e: e.tensor_tensor(out=dv, in0=v2, in1=v1, op=ALU.subtract), reads=["v1", "v2"], writes=["dv"])
        P.auto("scalar", lambda e: e.activation(out=dv, in_=dv, func=AF.Exp), reads=[], writes=["dv"])
        P.auto(V, lambda e: e.tensor_scalar(out=dv, in0=dv, scalar1=1.0, scalar2=None, op0=ALU.add), writes=["dv"])
        P.auto(V, lambda e: e.reciprocal(out=dv, in_=dv), writes=["dv"])
        P.auto(V, lambda e: e.tensor_tensor(out=PW[:, 0, :], in0=dv, in1=gw, op=ALU.mult), reads=["dv", "gw"], writes=["PW0"])
        P.auto(V, lambda e: e.tensor_tensor(out=PW[:, 1, :], in0=gw, in1=PW[:, 0, :], op=ALU.subtract), reads=["PW0", "gw"], writes=["PW1"])
        P.auto(V, lambda e: e.tensor_tensor(out=fl(Aa), in0=fl(oh1), in1=fl(oh2), op=ALU.add), reads=["oh1", "oh2"], writes=["Aa"])
        Af = fl(Aa)
        P.auto("tensor", [lambda e: e.matmul(PB[0], lhsT=trif, rhs=Af[:, 0:512], start=True, stop=True),
                          lambda e: e.matmul(PB[1], lhsT=trif, rhs=Af[:, 512:1024], start=True, stop=True),
                          lambda e: e.matmul(PB[2], lhsT=onesf, rhs=Af[:, 0:512], start=True, stop=True),
                          lambda e: e.matmul(PB[3], lhsT=onesf, rhs=Af[:, 512:1024], start=True, stop=True)],
               reads=["Aa"], psum=["b0", "b1", "b2", "b3"], extra=[d_cf])
        P.auto(V, lambda e: e.tensor_copy(out=fl(Tt)[:, 0:512], in_=PB[2]), writes=["Tt"], psum=["b2"])
        P.auto(V, lambda e: e.tensor_copy(out=fl(Tt)[:, 512:1024], in_=PB[3]), writes=["Tt"], psum=["b3"])
        cur, cname = Tt, "Tt"
        for si, sh in enumerate((1, 2, 4, 8, 16)):
            nxt, nname = (sc0, "sc0") if si % 2 == 0 else (sc1, "sc1")
            P.auto(V, lambda e, cur=cur, nxt=nxt, sh=sh: e.tensor_tensor(out=nxt[:, sh:32, :], in0=cur[:, sh:32, :], in1=cur[:, 0:32 - sh, :], op=ALU.add), reads=[cname], writes=[nname])
            P.auto(V, lambda e, cur=cur, nxt=nxt, sh=sh: e.tensor_copy(out=nxt[:, 0:sh, :], in_=cur[:, 0:sh, :]), reads=[cname], writes=[nname])
            cur, cname = nxt, nname
        P.auto(V, lambda e, cur=cur: e.tensor_tensor(out=fl(tmpc), in0=fl(cur), in1=fl(Tt), op=ALU.subtract), reads=[cname, "Tt"], writes=["tmpc"])
        P.auto(V, lambda e: e.tensor_tensor(out=fl(rank)[:, 0:512], in0=fl(tmpc)[:, 0:512], in1=PB[0], op=ALU.add), reads=["tmpc"], writes=["rank"], psum=["b0"])
        P.auto(V, lambda e: e.tensor_tensor(out=fl(rank)[:, 512:1024], in0=fl(tmpc)[:, 512:1024], in1=PB[1], op=ALU.add), reads=["tmpc"], writes=["rank"], psum=["b1"])
        P.auto(V, lambda e: e.tensor_scalar(out=fl(rank), in0=fl(rank), scalar1=float(CAP - 1), scalar2=None, op0=ALU.min), writes=["rank"])
        P.auto(V, lambda e: e.tensor_tensor(out=rank, in0=rank, in1=ecap.unsqueeze(1).to_broadcast([128, 32, 32]), op=ALU.add), writes=["rank"], extra=[d_cf])
        P.auto(V, lambda e: e.tensor_tensor(out=fl(tmpc), in0=fl(oh1), in1=fl(rank), op=ALU.mult), reads=["oh1", "rank"], writes=["tmpc"])
        P.auto(V, lambda e: e.tensor_reduce(out=s12[:, 0, :], in_=tmpc, axis=AX.X, op=ALU.add), reads=["tmpc"], writes=["s12a"])
        P.auto(V, lambda e: e.tensor_tensor(out=fl(tmpc), in0=fl(oh2), in1=fl(rank), op=ALU.mult), reads=["oh2", "rank"], writes=["tmpc"])
        P.auto(V, lambda e: e.tensor_reduce(out=s12[:, 1, :], in_=tmpc, axis=AX.X, op=ALU.add), reads=["tmpc"], writes=["s12b"])
        P.auto(V, lambda e: e.tensor_scalar(out=fl(s12), in0=fl(s12), scalar1=float(NSLOT - 1), scalar2=0.0, op0=ALU.min, op1=ALU.max), reads=["s12a", "s12b"], writes=["s12a", "s12b"])
        P.auto(V, lambda e: e.tensor_copy(out=SLI, in_=s12), reads=["s12a", "s12b"], writes=["SLI"])
        tap("sli", SLI, [P.lw["SLI"]], I32)
        tap("pw", PW, [P.lw["PW1"]])

        P.base_waits = [P.lw["SLI"], P.lw["PW1"], P.lw["PW0"]] + finals_h1
        AR.reset(pers_mark)
        stt_c = AR.alloc([128, 16], F32)
        wg = [AR.alloc([128, 8, 512], BF16) for _ in range(2)]
        wu = [AR.alloc([128, 8, 512], BF16) for _ in range(2)]
        wd = [AR.alloc([128, 4, 1024], BF16) for _ in range(2)]

        def e_loads_w(e_):
            i = e_ % 2
            P.adma("gpsimd", wg[i], w_eg[e_].rearrange("(c p) n -> p c n", p=128), "wg%d" % i, writes=["wg%d" % i])
            P.adma("gpsimd", wu[i], w_eu[e_].rearrange("(c p) n -> p c n", p=128), "wu%d" % i, writes=["wu%d" % i])
            P.adma("gpsimd", wd[i], w_ed[e_].rearrange("(c p) n -> p c n", p=128), "wd%d" % i, writes=["wd%d" % i])

        e_loads_w(0)
        hb = [AR.alloc([128, 1024], F32) for _ in range(2)]
        sc_toks = []
        for blk in range(NB):
            i = blk % 2
            P.adma("sync", hb[i], h1f[blk * 128:(blk + 1) * 128, :], "hb%d" % i, writes=["hb%d" % i])
            for k in range(2):
                off = SLI[:, k, blk:blk + 1].bitcast(U32)
                src = hb[i]
                sc_toks.append(P.acdma("gpsimd", lambda e, off=off, src=src: e.indirect_dma_start(
                    out=xs_d, out_offset=bass.IndirectOffsetOnAxis(ap=off, axis=0), in_=src, in_offset=None),
                    "sc%d_%d" % (i, k), reads=["hb%d" % i, "SLI"]))

        XE = [AR.alloc([128, 3, 1024], BF16) for _ in range(2)]
        XT = AR.alloc([128, 8, 384], BF16)
        sg = AR.alloc([128, 384], F32)
        HT = AR.alloc([128, 4, 384], BF16)
        YSb = [AR.alloc([128, 1024], F32) for _ in range(2)]
        NE = 32
        ys_toks = []

        def e_loads(e_):
            i = e_ % 2
            if e_ > 0:
                e_loads_w(e_)
            P.adma("sync", XE[i], xs_d[e_ * CAP:(e_ + 1) * CAP, :].rearrange("(b p) d -> p b d", p=128), "xe%d" % i, writes=["xe%d" % i], extra=sc_toks)

        def e_compute(e_):
            i = e_ % 2
            for pr in range(4):
                bank = pr % 2
                fns = []
                for cc in range(2):
                    c = 2 * pr + cc
                    for sb in range(3):
                        o_ = PBb[bank][:, cc * 384 + sb * 128: cc * 384 + sb * 128 + 128]
                        i_ = XE[i][:, sb, c * 128:(c + 1) * 128]
                        fns.append(lambda e, o_=o_, i_=i_: e.transpose(out=o_, in_=i_, identity=ident))
                P.auto("tensor", fns, reads=["xe%d" % i], psum=["b%d" % bank])
                dst = XT[:, 2 * pr:2 * pr + 2, :].rearrange("p a b -> p (a b)")
                if bank == 0:
                    P.auto("vector", lambda e, dst=dst: e.tensor_copy(out=dst, in_=PBb[0][:, 0:768]), writes=["XT%d" % pr], psum=["b0"])
                else:
                    P.auto("scalar", lambda e, dst=dst: e.copy(out=dst, in_=PBb[1][:, 0:768]), writes=["XT%d" % pr], psum=["b1"])
            xt_names = ["XT%d" % pr for pr in range(4)]
            for f in range(4):
                gb_ = 2 + 2 * (f % 2)
                ub_ = 3 + 2 * (f % 2)
                fns = []
                for c in range(8):
                    l_ = wg[i][:, c, f * 128:(f + 1) * 128]
                    fns.append(lambda e, l_=l_, c=c, gb_=gb_: e.matmul(PB[gb_][:, 0:384], lhsT=l_, rhs=XT[:, c, :], start=(c == 0), stop=(c == 7)))
                P.auto("tensor", fns, reads=["wg%d" % i] + xt_names, psum=["b%d" % gb_])
                fns = []
                for c in range(8):
                    l_ = wu[i][:, c, f * 128:(f + 1) * 128]
                    fns.append(lambda e, l_=l_, c=c, ub_=ub_: e.matmul(PB[ub_][:, 0:384], lhsT=l_, rhs=XT[:, c, :], start=(c == 0), stop=(c == 7)))
                P.auto("tensor", fns, reads=["wu%d" % i] + xt_names, psum=["b%d" % ub_])
                P.auto("scalar", lambda e, gb_=gb_: e.activation(out=sg, in_=PB[gb_][:, 0:384], func=AF.Silu), writes=["sg"], psum=["b%d" % gb_])
                P.auto("vector", lambda e, ub_=ub_, f=f: e.tensor_tensor(out=HT[:, f, :], in0=sg, in1=PB[ub_][:, 0:384], op=ALU.mult), reads=["sg"], writes=["HT%d" % f], psum=["b%d" % ub_])
            ht_names = ["HT%d" % f for f in range(4)]
            for sb in range(3):
                k = (e_ * 3 + sb) % 2
                fns = []
                for half in range(2):
                    for f in range(4):
                        l_ = HT[:, f, sb * 128:(sb + 1) * 128]
                        r_ = wd[i][:, f, half * 512:(half + 1) * 512]
                        fns.append(lambda e, l_=l_, r_=r_, f=f, half=half: e.matmul(PB[6 + half], lhsT=l_, rhs=r_, start=(f == 0), stop=(f == 3)))
                P.auto("tensor", fns, reads=["wd%d" % i] + ht_names, psum=["b6", "b7"])
                ysb = YSb[k]
                P.auto("scalar", lambda e, ysb=ysb: e.copy(out=ysb[:, 0:512], in_=PB[6]), writes=["ysa%d" % k], psum=["b6"])
                P.auto("vector", lambda e, ysb=ysb: e.tensor_copy(out=ysb[:, 512:1024], in_=PB[7]), writes=["ysb%d" % k], psum=["b7"])
                r0 = e_ * CAP + sb * 128
                ys_toks.append(P.adma("sync", ys_d[r0:r0 + 128, :], ysb, "ysw%d" % k, reads=["ysa%d" % k, "ysb%d" % k]))

        e_loads(0)
        for e_ in range(NE):
            if e_ + 1 < NE:
                e_loads(e_ + 1)
            e_compute(e_)

        Y1_ = [AR.alloc([128, 1024], F32) for _ in range(2)]
        Y2_ = [AR.alloc([128, 1024], F32) for _ in range(2)]
        hc_ = [AR.alloc([128, 1024], F32) for _ in range(2)]
        R2_ = [AR.alloc([128, 1024], F32) for _ in range(2)]
        lng2 = AR.alloc([128, 1024], F32)
        lnb2 = AR.alloc([128, 1024], F32)
        P.adma("sync", lng2, ln2[0:1, :].to_broadcast([128, 1024]), "lng2", writes=["lng2"])
        P.adma("sync", lnb2, ln2[1:2, :].to_broadcast([128, 1024]), "lnb2", writes=["lnb2"])

        def c_loads(blk):
            pb = blk % 2
            P.adma("sync", hc_[pb], h1f[blk * 128:(blk + 1) * 128, :], "hc%d" % pb, writes=["hc%d" % pb])
            for k, yb in ((0, Y1_[pb]), (1, Y2_[pb])):
                off = SLI[:, k, blk:blk + 1].bitcast(U32)
                P.acdma("gpsimd", lambda e, off=off, yb=yb: e.indirect_dma_start(
                    out=yb, out_offset=None, in_=ys_d, in_offset=bass.IndirectOffsetOnAxis(ap=off, axis=0)),
                    "yg%d%d" % (k, pb), reads=["SLI"], writes=["Y%d%d" % (k, pb)], extra=ys_toks)

        def c_block(blk):
            pb = blk % 2
            if blk + 1 < NB:
                c_loads(blk + 1)
            Y1, Y2, hc, R2 = Y1_[pb], Y2_[pb], hc_[pb], R2_[pb]
            rn = "R2%d" % pb
            P.auto("scalar", lambda e: e.mul(out=R2, in_=hc, mul=ALPHA), reads=["hc%d" % pb], writes=[rn])
            P.auto(V, lambda e: e.scalar_tensor_tensor(out=R2, in0=Y1, scalar=PW[:, 0, blk:blk + 1], in1=R2, op0=ALU.mult, op1=ALU.add), reads=["Y0%d" % pb, "PW0"], writes=[rn])
            P.auto(V, lambda e: e.scalar_tensor_tensor(out=R2, in0=Y2, scalar=PW[:, 1, blk:blk + 1], in1=R2, op0=ALU.mult, op1=ALU.add), reads=["Y1%d" % pb, "PW1"], writes=[rn])
            layer_norm(R2, "lng2", "lnb2", lng2, lnb2, stt_c, rn)
            finals.append(P.adma("sync", out[blk * 128:(blk + 1) * 128, :], R2, "outw%d" % pb, reads=[rn]))

        c_loads(0)
        for blk in range(NB):
            c_block(blk)

        P.emit(finals)
    return nc, tap_out


def _consts(j):
    ident = np.eye(128, dtype=np.float32)
    rot = np.zeros((128, 128), np.float32)
    for m in range(128):
        if (m % 64) < 32:
            rot[m + 32, m] = -1.0
        else:
            rot[m - 32, m] = 1.0
    ones = np.ones((128, 128), np.float32)
    kk = np.arange(128)[:, None, None]
    r = np.arange(8)[None, :, None]
    qq = np.arange(512)[None, None, :]
    mask = ((r * 128 + kk) <= ((2 * (qq // 128) + j) * 128 + (qq % 128))).astype(np.float32)
    cbf = np.concatenate([ident, rot, ones, mask.reshape(128, 4096)], axis=1)
    cf = np.zeros((128, 512), np.float32)
    cf[:, 0:128] = ident
    cf[:, 128:256] = 1.0
    cf[:, 256:384] = (np.arange(128)[:, None] < np.arange(128)[None, :]).astype(np.float32)
    inv_freq = (np.float32(10000.0) ** (-np.arange(0, 64, 2, dtype=np.float32) / np.float32(64))).astype(np.float32)
    p = np.arange(128)
    cf[:, 384] = (inv_freq[(p % 64) % 32].astype(np.float64) / (2.0 * np.pi)).astype(np.float32)
    cf[:, 392:424] = (np.arange(32) * CAP)[None, :]
    cf[:, 424] = -0.5
    cf[:, 425] = 1e-5
    return np.ascontiguousarray(cbf), cf


def make_core_inputs(inp, c):
    b, j = c // 2, c % 2
    x = inp["x"]
    xb_ = x[b]
    blocks = xb_.reshape(64, 128, D)
    xo = np.ascontiguousarray(blocks[j::2].reshape(NOWN, D))
    xh = np.zeros((32, 2, D), np.float32)
    for m in range(32):
        blk = 2 * m + j
        if blk > 0:
            xh[m] = xb_[blk * 128 - 2: blk * 128]
    pos = np.asarray(inp["positions"][b], dtype=np.int32)
    poso = np.ascontiguousarray(pos.reshape(64, 128)[j::2].reshape(1, NOWN))
    cbf, cf = _consts(j)
    f = lambda a: np.ascontiguousarray(np.asarray(a, dtype=np.float32))
    return {
        "xf": f(xb_), "xo": xo, "xh": f(xh.reshape(64, D)), "posf": np.ascontiguousarray(pos.reshape(1, S)), "poso": poso,
        "w_in": f(inp["w_in"][0]), "b_gate": f(inp["b_gate"][0].reshape(1, 2048)),
        "lam_in": f(np.concatenate([inp["lambda_q1"][0], inp["lambda_k1"][0], inp["lambda_q2"][0], inp["lambda_k2"][0]]).reshape(1, 256)),
        "subln_g": f(inp["subln_g"][0].reshape(1, 128)), "w_o_att": f(inp["w_o_att"][0]), "conv_w": f(inp["conv_w"][0]),
        "w_o_conv": f(inp["w_o_conv"][0]), "w_mix": f(inp["w_mix_out"][0]),
        "ln1": f(np.stack([inp["ln1_g"][0], inp["ln1_b"][0]])), "ln2": f(np.stack([inp["ln2_g"][0], inp["ln2_b"][0]])),
        "w_rt": f(np.concatenate([inp["w_router_group"][0], inp["w_router_expert"][0]], axis=1)),
        "b_rt": f(np.concatenate([inp["b_router_group"][0], inp["b_router_expert"][0]]).reshape(1, 36)),
        "w_eg": f(inp["w_exp_gate"][0]), "w_eu": f(inp["w_exp_up"][0]), "w_ed": f(inp["w_exp_down"][0]),
        "cbf": cbf, "cf32": cf,
    }


def kernel(**inputs):
    inp = {k: np.asarray(v) for k, v in inputs.items()}
    nc, _ = build()
    in_maps = [make_core_inputs(inp, c) for c in range(8)]
    res = run_bass_kernel_spmd(nc, in_maps, core_ids=list(range(8)))
    outp = np.zeros((4, S, D), np.float32)
    for c in range(8):
        b, j = c // 2, c % 2
        o = np.asarray(res.results[c]["out"]).reshape(32, 128, D)
        outp[b].reshape(64, 128, D)[j::2] = o
    return outp
```

```python
import math
import numpy as np
import ml_dtypes
from contextlib import ExitStack
import concourse.bass as bass
import concourse.mybir as mybir
from concourse.bass_utils import run_bass_kernel_spmd

F32 = mybir.dt.float32
BF16 = mybir.dt.bfloat16
I32 = mybir.dt.int32
U32 = mybir.dt.uint32
AF = mybir.ActivationFunctionType
ALU = mybir.AluOpType
AX = mybir.AxisListType
ENGS = ["tensor", "vector", "scalar", "gpsimd", "sync"]

S = 8192
D = 1024
NOWN = 4096
CAP = 384
NSLOT = 32 * CAP
ALPHA = 2.0 ** 0.25
LAMBDA_INIT = 0.8 - 0.6 * math.exp(0.0)
TWO_PI = 2.0 * math.pi
NEG = -1.0e30


class Prog:
    def __init__(self, nc, es):
        self.nc = nc
        self.es = es
        self.ops = {e: [] for e in ENGS}
        self.cnt = {e: 0 for e in ENGS}
        self.esem = {e: es.enter_context(nc.semaphore("es_" + e)) for e in ENGS}
        self.dsem = {}
        self.dcnt = {}
        self.waited = {e: {} for e in ENGS}
        self.base_waits = []

    def _waits(self, eng, waits):
        best = {}
        for w in list(waits) + list(self.base_waits):
            if w is None:
                continue
            sem, val, key = w
            if key not in best or best[key][1] < val:
                best[key] = (sem, val)
        out = []
        for key, (sem, val) in best.items():
            if self.waited[eng].get(key, 0) >= val:
                continue
            self.waited[eng][key] = val
            out.append((sem, val))
        return out

    def op(self, eng, fn, waits=(), signal=True):
        ws = self._waits(eng, waits)
        inc = None
        tok = None
        if signal:
            self.cnt[eng] += 1
            inc = (self.esem[eng], 1)
            tok = (self.esem[eng], self.cnt[eng], "e_" + eng)
        self.ops[eng].append((fn, ws, inc))
        return tok

    def _dsem(self, sem):
        if sem not in self.dsem:
            self.dsem[sem] = self.es.enter_context(self.nc.semaphore("ds_" + sem))
            self.dcnt[sem] = 0
        return self.dsem[sem]

    def dma(self, q, out, in_, sem, waits=(), **kw):
        return self.cdma(q, lambda e: e.dma_start(out=out, in_=in_, **kw), sem, waits)

    def cdma(self, q, fn, sem, waits=()):
        s = self._dsem(sem)
        ws = self._waits(q, waits)
        self.dcnt[sem] += 16
        self.ops[q].append((fn, ws, (s, 16)))
        return (s, self.dcnt[sem], "d_" + sem)

    def _auto_waits(self, reads, writes, psum):
        if not hasattr(self, "lw"):
            self.lw, self.rd, self.pa = {}, {}, {}
        ws = []
        for r in reads:
            ws.append(self.lw.get(r))
        for w in writes:
            ws.append(self.lw.get(w))
            ws.extend(self.rd.get(w, []))
        for p in psum:
            ws.append(self.pa.get(p))
        return ws

    def _auto_done(self, tok, reads, writes, psum):
        for r in reads:
            self.rd.setdefault(r, []).append(tok)
        for w in writes:
            self.lw[w] = tok
            self.rd[w] = []
        for p in psum:
            self.pa[p] = tok

    def auto(self, eng, fns, reads=(), writes=(), psum=(), extra=()):
        if not isinstance(fns, (list, tuple)):
            fns = [fns]
        ws = self._auto_waits(reads, writes, psum) + list(extra)
        tok = None
        for k, fn in enumerate(fns):
            tok = self.op(eng, fn, waits=ws if k == 0 else (), signal=(k == len(fns) - 1))
        self._auto_done(tok, reads, writes, psum)
        return tok

    def adma(self, q, out, in_, sem, reads=(), writes=(), extra=(), **kw):
        ws = self._auto_waits(reads, writes, ()) + list(extra)
        tok = self.dma(q, out, in_, sem, waits=ws, **kw)
        self._auto_done(tok, reads, writes, ())
        return tok

    def acdma(self, q, fn, sem, reads=(), writes=(), extra=()):
        ws = self._auto_waits(reads, writes, ()) + list(extra)
        tok = self.cdma(q, fn, sem, waits=ws)
        self._auto_done(tok, reads, writes, ())
        return tok

    def emit(self, final_waits):
        nc = self.nc
        with nc.Block() as block:
            def mk(eng):
                def body(e):
                    for fn, ws, inc in self.ops[eng]:
                        for sem, val in ws:
                            e.wait_ge(sem, val)
                        ins = fn(e)
                        if inc is not None:
                            ins.then_inc(inc[0], inc[1])
                    if eng == "sync":
                        for w in final_waits:
                            if w is not None:
                                e.wait_ge(w[0], w[1])
                return body
            block.tensor(mk("tensor"))
            block.vector(mk("vector"))
            block.scalar(mk("scalar"))
            block.gpsimd(mk("gpsimd"))
            block.sync(mk("sync"))


class Arena:
    def __init__(self, t, nwords):
        self.t = t
        self.n = nwords
        self.top = 0

    def mark(self):
        return self.top

    def reset(self, m):
        self.top = m

    def alloc(self, shape, dt):
        per = 1
        for s_ in shape[1:]:
            per *= s_
        if dt == BF16:
            words = (per + 1) // 2
        else:
            words = per
        words = (words + 7) // 8 * 8
        a = self.top
        self.top += words
        assert self.top <= self.n, ("arena overflow", self.top, self.n)
        v = self.t[:, a:a + words]
        if dt == BF16:
            v = v.bitcast(BF16)[:, 0:per]
        elif dt == I32:
            v = v.bitcast(I32)[:, 0:per]
        else:
            v = v[:, 0:per]
        if len(shape) == 3:
            v = v.rearrange("p (a b) -> p a b", a=shape[1])
        elif len(shape) == 4:
            v = v.rearrange("p (a b c) -> p a b c", a=shape[1], b=shape[2])
        if shape[0] != 128:
            v = v[0:shape[0]]
        return v


def build(upto="all", taps=(), NQT=8):
    nc = bass.Bass("TRN2", target_bir_lowering=False)
    din = lambda name, shape, dt: nc.dram_tensor(name, shape, dt, kind="ExternalInput").ap()
    xf = din("xf", [S, D], F32)
    xo = din("xo", [NOWN, D], F32)
    xh = din("xh", [64, D], F32)
    posf = din("posf", [1, S], I32)
    poso = din("poso", [1, NOWN], I32)
    w_in = din("w_in", [D, 5120], F32)
    b_gate = din("b_gate", [1, 2048], F32)
    lam_in = din("lam_in", [1, 256], F32)
    subln_g = din("subln_g", [1, 128], F32)
    w_o_att = din("w_o_att", [512, D], F32)
    conv_w = din("conv_w", [3, 512], F32)
    w_o_conv = din("w_o_conv", [512, D], F32)
    w_mix = din("w_mix", [D, D], F32)
    ln1 = din("ln1", [2, D], F32)
    ln2 = din("ln2", [2, D], F32)
    w_rt = din("w_rt", [D, 36], F32)
    b_rt = din("b_rt", [1, 36], F32)
    w_eg = din("w_eg", [32, D, 512], F32)
    w_eu = din("w_eu", [32, D, 512], F32)
    w_ed = din("w_ed", [32, 512, D], F32)
    cbf = din("cbf", [128, 384 + 4096], F32)
    cf32 = din("cf32", [128, 512], F32)
    out = nc.dram_tensor("out", [NOWN, D], F32, kind="ExternalOutput").ap()
    h1f = nc.dram_tensor("h1f", [NOWN, D], F32, kind="Internal").ap()
    xs_d = nc.dram_tensor("xs_d", [NSLOT, D], BF16, kind="Internal").ap()
    ys_d = nc.dram_tensor("ys_d", [NSLOT, D], F32, kind="Internal").ap()
    tap_out = {}

    with ExitStack() as es:
        P = Prog(nc, es)
        NW = 51 * 1024
        arena_t = es.enter_context(nc.sbuf_tensor("arena", [128, NW], F32))
        AR = Arena(arena_t, NW)
        psum = es.enter_context(nc.psum_tensor("psum", [128, 8, 512], F32))
        PB = [psum[:, i, :] for i in range(8)]
        PBb = [psum[:, i, :].bitcast(BF16) for i in range(8)]
        finals = []

        def tap(name, ap, waits, dt=F32):
            if name not in taps:
                return
            shp = list(ap.shape)
            o = nc.dram_tensor("tap_" + name, shp, dt, kind="ExternalOutput").ap()
            tap_out[name] = shp
            finals.append(P.dma("sync", o, ap, "tap_" + name, waits=waits))

        cb_t = AR.alloc([128, 384], BF16)
        ident = cb_t[:, 0:128]
        rotm = cb_t[:, 128:256]
        onesb = cb_t[:, 256:384]
        cf_t = AR.alloc([128, 512], F32)
        identf = cf_t[:, 0:128]
        onesf = cf_t[:, 128:256]
        trif = cf_t[:, 256:384]
        invf = cf_t[:, 384:385]
        ecap = cf_t[:, 392:424]
        mhalf = cf_t[:, 424:425]
        epsc = cf_t[:, 425:426]
        d_cb = P.dma("gpsimd", cb_t, cbf[:, 0:384], "cb")
        d_cf = P.dma("sync", cf_t, cf32, "cf")
        QO = AR.alloc([128, 4, NOWN], BF16)
        small = AR.alloc([128, 64], F32)
        neglam = small[:, 0:1]
        gsc = small[:, 1:2]
        lamv = AR.alloc([128, 256], F32)
        slg = AR.alloc([128, 128], F32)
        d_lam = P.dma("sync", lamv, lam_in.to_broadcast([128, 256]), "lam")
        d_slg = P.dma("sync", slg[:, 0:1], subln_g.rearrange("o p -> p o"), "slg", allow_slow_non_contiguous=True)
        lt = small[:, 8:10]
        t_l1 = P.op("vector", lambda e: e.tensor_tensor(out=lamv[:, 0:64], in0=lamv[:, 0:64], in1=lamv[:, 64:128], op=ALU.mult), waits=[d_lam])
        t_l2 = P.op("vector", lambda e: e.tensor_tensor(out=lamv[:, 128:192], in0=lamv[:, 128:192], in1=lamv[:, 192:256], op=ALU.mult), waits=[d_lam])
        t_l3 = P.op("vector", lambda e: e.tensor_reduce(out=lt[:, 0:1], in_=lamv[:, 0:64], axis=AX.X, op=ALU.add), waits=[t_l1])
        t_l4 = P.op("vector", lambda e: e.tensor_reduce(out=lt[:, 1:2], in_=lamv[:, 128:192], axis=AX.X, op=ALU.add), waits=[t_l2])
        t_l5 = P.op("scalar", lambda e: e.activation(out=small[:, 10:12], in_=lt, func=AF.Exp), waits=[t_l3, t_l4])
        t_l6 = P.op("vector", lambda e: e.tensor_tensor(out=small[:, 12:13], in0=small[:, 11:12], in1=small[:, 10:11], op=ALU.subtract), waits=[t_l5])
        t_l7 = P.op("vector", lambda e: e.tensor_scalar(out=neglam, in0=small[:, 12:13], scalar1=-LAMBDA_INIT, scalar2=None, op0=ALU.add), waits=[t_l6])
        t_g = P.op("vector", lambda e: e.tensor_scalar(out=gsc, in0=slg[:, 0:1], scalar1=1.0 - LAMBDA_INIT, scalar2=None, op0=ALU.mult), waits=[d_slg])
        t_consts = [d_cb, d_cf, t_l7, t_g]
        LG = AR.alloc([128, 32, 36], F32)
        PW = AR.alloc([128, 2, 32], F32)
        SLI = AR.alloc([128, 2, 32], I32)
        pers_mark = AR.mark()

        KT = AR.alloc([128, 2, S], BF16)
        maskt_t = AR.alloc([128, 4096], BF16)
        maskt = maskt_t.rearrange("p (r q) -> p r q", r=8)
        d_mask = P.dma("gpsimd", maskt_t, cbf[:, 384:384 + 4096], "mask", max_dma_last_dim=4096)
        VV = AR.alloc([128, 64, 256], BF16)
        wq = AR.alloc([128, 8, 512], BF16)
        wkv = AR.alloc([128, 8, 512], BF16)
        XBt = [AR.alloc([128, 4, D], BF16) for _ in range(2)]
        XTt = [AR.alloc([128, 8, 512], BF16) for _ in range(2)]
        post2 = [AR.alloc([128, 512], F32) for _ in range(2)]
        tq = AR.alloc([128, 512], F32)
        ki = AR.alloc([128, 512], I32)
        cst2 = [AR.alloc([128, 512], F32) for _ in range(2)]
        snt2 = [AR.alloc([128, 512], F32) for _ in range(2)]
        qsb = [AR.alloc([128, 512], BF16) for _ in range(2)]
        ra = [AR.alloc([128, 512], F32) for _ in range(2)]
        rb = [AR.alloc([128, 512], F32) for _ in range(2)]
        PT = [AR.alloc([128, 2, 512], BF16) for _ in range(3)]
        EE = [AR.alloc([128, 512], F32) for _ in range(4)]

        st = {"xb_free": [None, None], "xt_free": [None, None], "tp_free": [None, None],
              "kp_free": [[], []], "rp_free": [None, None], "vp_free": [None, None],
              "tab_free": [[], []], "qs_free": [None, None], "ra_free": [None, None],
              "n_kp": 0, "n_tp": 0, "n_vp": 0, "n_x": 0, "n_tab": 0, "last_sin": None}

        def load_w(dst, src_cols, sem, waits):
            return P.dma("gpsimd", dst, src_cols.rearrange("(c p) n -> p c n", p=128), sem, waits=waits)

        def tables_dma(pos_src, t0):
            ti = st["n_tab"] % 2
            st["n_tab"] += 1
            w0 = list(st["tab_free"][ti])
            d = P.dma("gpsimd", post2[ti], pos_src[0:1, t0:t0 + 512].to_broadcast([128, 512]), "pos%d" % ti, waits=w0)
            return ti, d, w0

        def tables(pos_src, t0):
            return tables_compute(*tables_dma(pos_src, t0))

        def tables_compute(ti, d, w0):
            post = post2[ti]
            prev = [st["last_sin"]]
            for dst, add in ((snt2[ti], 0.0), (cst2[ti], 0.25)):
                a = P.op("vector", lambda e, add=add: e.tensor_scalar(out=tq, in0=post, scalar1=invf, scalar2=add, op0=ALU.mult, op1=ALU.add), waits=[d, d_cf] + prev)
                b = P.op("vector", lambda e: e.tensor_copy(out=ki, in_=tq), waits=[a])
                c = P.op("vector", lambda e: e.tensor_tensor(out=tq, in0=tq, in1=ki, op=ALU.subtract), waits=[b])
                s_ = P.op("scalar", lambda e, dst=dst: e.activation(out=dst, in_=tq, func=AF.Sin, scale=TWO_PI), waits=[c] + w0)
                prev = [s_]
            st["last_sin"] = prev[0]
            return prev[0], ti

        def load_x_tile(src, row0):
            i = st["n_x"] % 2
            st["n_x"] += 1
            xb = XBt[i]
            d = P.dma("gpsimd", xb, src[row0:row0 + 512, :].rearrange("(b p) d -> p b d", p=128),
                      "xb%d" % i, waits=[st["xb_free"][i]])
            return i, d

        def transpose_tile(i, dx):
            xb = XBt[i]
            xt = XTt[i]
            evs = []
            last_t = None
            for g in range(4):
                bk = st["n_tp"] % 2
                st["n_tp"] += 1
                for cc in range(2):
                    c = 2 * g + cc
                    for blk in range(4):
                        last = (cc == 1 and blk == 3)
                        o_ = PBb[bk][:, cc * 512 + blk * 128: cc * 512 + blk * 128 + 128]
                        i_ = xb[:, blk, c * 128:(c + 1) * 128]
                        tk = P.op("tensor", lambda e, o_=o_, i_=i_: e.transpose(out=o_, in_=i_, identity=ident),
                                  waits=[dx, d_cb, st["tp_free"][bk]], signal=last)
                        if last:
                            last_t = tk
                dst = xt[:, 2 * g:2 * g + 2, :].rearrange("p a b -> p (a b)")
                src = PBb[bk]
                if g % 2 == 0:
                    ev = P.op("vector", lambda e, dst=dst, src=src: e.tensor_copy(out=dst, in_=src), waits=[last_t, st["xt_free"][i]])
                else:
                    ev = P.op("scalar", lambda e, dst=dst, src=src: e.copy(out=dst, in_=src), waits=[last_t, st["xt_free"][i]])
                st["tp_free"][bk] = ev
                evs.append(ev)
            st["xb_free"][i] = last_t
            return evs

        def proj_part1(xt, evs, wt, wcol, tab_tok, ti, extra_w):
            kb = st["n_kp"] % 2
            st["n_kp"] += 1
            kp = PB[2 + kb]
            q_ = qsb[kb]
            ra_ = ra[kb]
            cs_ = cst2[ti]
            mm = None
            for c in range(8):
                l_ = wt[:, c, wcol:wcol + 128]
                r_ = xt[:, c, :]
                mm = P.op("tensor", lambda e, l_=l_, r_=r_, c=c: e.matmul(kp, lhsT=l_, rhs=r_, start=(c == 0), stop=(c == 7)),
                          waits=evs + st["kp_free"][kb] + extra_w, signal=(c == 7))
            cp = P.op("scalar", lambda e: e.copy(out=q_, in_=kp), waits=[mm, st["qs_free"][kb]])
            a = P.op("vector", lambda e: e.tensor_tensor(out=ra_, in0=kp, in1=cs_, op=ALU.mult), waits=[mm, cp, tab_tok, st["ra_free"][kb]])
            st["kp_free"][kb] = [a, cp]
            return {"kb": kb, "cp": cp, "a": a, "tab": tab_tok, "ti": ti, "mm": mm}

        def proj_part2(cx, dst):
            kb = cx["kb"]
            rp = PB[4 + kb]
            q_ = qsb[kb]
            ra_ = ra[kb]
            rb_ = rb[kb]
            sn_ = snt2[cx["ti"]]
            rm = P.op("tensor", lambda e: e.matmul(rp, lhsT=rotm, rhs=q_, start=True, stop=True), waits=[cx["cp"], d_cb, st["rp_free"][kb]])
            st["qs_free"][kb] = rm
            b = P.op("vector", lambda e: e.tensor_tensor(out=rb_, in0=rp, in1=sn_, op=ALU.mult), waits=[rm, cx["tab"], st["ra_free"][kb]])
            st["rp_free"][kb] = b
            f = P.op("vector", lambda e: e.tensor_tensor(out=dst, in0=ra_, in1=rb_, op=ALU.add), waits=[cx["a"], b])
            st["ra_free"][kb] = f
            return f, b, rm

        pre = {}

        def prefetch(key, src, pos_src, T):
            i, dx = load_x_tile(src, T * 512)
            tab, ti = tables(pos_src, T * 512)
            pre[key] = (i, dx, tab, ti)

        def prefetch_dma(key, src, pos_src, T):
            i, dx = load_x_tile(src, T * 512)
            return (key, i, dx, tables_dma(pos_src, T * 512))

        def prefetch_compute(pf):
            key, i, dx, td = pf
            tab, ti = tables_compute(*td)
            pre[key] = (i, dx, tab, ti)

        def a_tile(T, dwk, dwv, last, nxt):
            i, dx, tab, ti = pre.pop(("a", T))
            pf = prefetch_dma(*nxt) if nxt is not None else None
            evs = transpose_tile(i, dx)
            cxs = [proj_part1(XTt[i], evs, wkv, hl * 128, tab, ti, [dwk]) for hl in range(2)]
            if pf is not None:
                prefetch_compute(pf)
            mm = None
            for blk in range(4):
                vb = st["n_vp"] % 2
                st["n_vp"] += 1
                vp = PB[6 + vb][:, 0:256]
                for c in range(8):
                    l_ = XTt[i][:, c, blk * 128:(blk + 1) * 128]
                    r_ = wkv[:, c, 256:512]
                    mm = P.op("tensor", lambda e, l_=l_, r_=r_, c=c, vp=vp: e.matmul(vp, lhsT=l_, rhs=r_, start=(c == 0), stop=(c == 7)),
                              waits=evs + [dwv, st["vp_free"][vb]], signal=(c == 7))
                o_ = VV[:, T * 4 + blk, :]
                vts = P.op("scalar", lambda e, o_=o_, vp=vp: e.copy(out=o_, in_=vp), waits=[mm])
                st["vp_free"][vb] = vts
                last.append(vts)
            st["xt_free"][i] = mm
            tabfree = []
            for hl in range(2):
                f, b, rm = proj_part2(cxs[hl], KT[:, hl, T * 512:(T + 1) * 512])
                tabfree.append(b)
                last.append(f)
            st["tab_free"][ti] = tabfree

        def phase_A(hp, then_q):
            dwk = load_w(wkv[:, :, 0:256], w_in[:, 512 + hp * 256: 512 + hp * 256 + 256], "wk", [])
            dwv = load_w(wkv[:, :, 256:512], w_in[:, 1024 + hp * 256: 1024 + hp * 256 + 256], "wv", [])
            last = []
            prefetch(("a", 0), xf, posf, 0)
            for T in range(16):
                if T + 1 < 16:
                    nxt = (("a", T + 1), xf, posf, T + 1)
                elif then_q:
                    nxt = (("q", 0), xo, poso, 0)
                else:
                    nxt = None
                a_tile(T, dwk, dwv, last, nxt)
            return last

        def q_tile(T, dwq, last):
            i, dx, tab, ti = pre.pop(("q", T))
            pf = prefetch_dma(("q", T + 1), xo, poso, T + 1) if T + 1 < NQT else None
            evs = transpose_tile(i, dx)
            tabfree = []
            rm = None
            cxs = {}
            cxs[0] = proj_part1(XTt[i], evs, wq, 0, tab, ti, [dwq])
            if pf is not None:
                prefetch_compute(pf)
            for h in range(4):
                if h + 1 < 4:
                    cxs[h + 1] = proj_part1(XTt[i], evs, wq, (h + 1) * 128, tab, ti, [dwq])
                f, b, rm = proj_part2(cxs[h], QO[:, h, T * 512:(T + 1) * 512])
                tabfree.append(b)
                last.append(f)
            st["tab_free"][ti] = tabfree
            st["xt_free"][i] = rm

        def phase_Q():
            dwq = load_w(wq, w_in[:, 0:512], "wq", [])
            last = []
            for T in range(NQT):
                q_tile(T, dwq, last)
            return last

        ACC0 = ra[0]
        att = {"a0": None, "accL_free": None, "l6_free": None, "s_free": [None, None], "pt_free": [[], [], []], "acc_free": None, "n_s": 0, "n_pt": 0, "ee_free": None, "pending": None}

        def att_unit(i, hl, h):
            nkb = 8 * i + 8
            qk_tok = {}
            qrange = slice(i * 512, (i + 1) * 512)

            def issue_qk(kb):
                s = kb % 2
                krange = slice(kb * 128, (kb + 1) * 128)
                P.op("tensor", lambda e: e.matmul(psum[:, 2 * s, :], lhsT=KT[0:64, hl, krange], rhs=QO[0:64, h, qrange], start=True, stop=True),
                     waits=[att["s_free"][s]], signal=False)
                t = P.op("tensor", lambda e: e.matmul(psum[:, 2 * s + 1, :], lhsT=KT[64:128, hl, krange], rhs=QO[64:128, h, qrange], start=True, stop=True))
                qk_tok[kb] = (t, s)

            def exp_step(kb):
                t, s = qk_tok[kb]
                pi = att["n_pt"] % 3
                att["n_pt"] += 1
                pt = PT[pi]
                ex = P.op("scalar", lambda e: e.activation(out=pt, in_=psum[:, 2 * s:2 * s + 2, :], func=AF.Exp, scale=0.125), waits=[t] + att["pt_free"][pi])
                att["s_free"][s] = ex
                return ex, pt, pi

            def pv_step(kb, ex, pt, pi):
                pv_w = ex
                if kb >= 8 * i:
                    r = kb - 8 * i
                    pv_w = P.op("vector", lambda e: e.tensor_tensor(out=pt, in0=pt, in1=maskt[:, r, :].unsqueeze(1).to_broadcast([128, 2, 512]), op=ALU.mult),
                                waits=[ex, d_mask])
                s0 = (kb == 0)
                s1 = (kb == nkb - 1)
                w0 = [pv_w, att["acc_free"]] if kb == 0 else [pv_w]
                vv = VV[:, kb, hl * 128:(hl + 1) * 128]
                P.op("tensor", lambda e: e.matmul(PB[4], lhsT=vv, rhs=pt[:, 0, :], start=s0, stop=s1), waits=w0, signal=False)
                P.op("tensor", lambda e: e.matmul(PB[5], lhsT=vv, rhs=pt[:, 1, :], start=s0, stop=s1), signal=False)
                pv = P.op("tensor", lambda e: e.matmul(PB[7], lhsT=onesb, rhs=pt[:, 1, :], start=s0, stop=s1))
                if kb == 0:
                    a0 = P.op("vector", lambda e: e.tensor_copy(out=ACC0, in_=pt[:, 0, :]), waits=[pv_w, att["accL_free"]])
                else:
                    a0 = P.op("vector", lambda e: e.tensor_tensor(out=ACC0, in0=ACC0, in1=pt[:, 0, :], op=ALU.add), waits=[pv_w, att["a0"]])
                att["a0"] = a0
                att["pt_free"][pi] = [pv, a0]
                return pv

            issue_qk(0)
            issue_qk(1)
            pv = None
            for kb in range(nkb):
                ex, pt, pi = exp_step(kb)
                if kb + 2 < nkb:
                    issue_qk(kb + 2)
                pv = pv_step(kb, ex, pt, pi)
                if kb == 2 and att["pending"] is not None:
                    att["pending"]()
                    att["pending"] = None
            wfree = [att["ee_free"]]
            lsum = P.op("tensor", lambda e: e.matmul(PB[6], lhsT=onesf, rhs=ACC0, start=True, stop=True), waits=[att["a0"], att["acc_free"], att["l6_free"], d_cf])
            att["accL_free"] = lsum
            e0 = P.op("vector", lambda e: e.reciprocal(out=EE[0], in_=PB[6]), waits=[pv, lsum] + wfree)
            e1 = P.op("vector", lambda e: e.reciprocal(out=EE[1], in_=PB[7]), waits=[pv, lsum] + wfree)
            e2 = P.op("vector", lambda e: e.tensor_tensor(out=EE[0], in0=PB[4], in1=EE[0], op=ALU.mult), waits=[e0])
            e3 = P.op("vector", lambda e: e.tensor_tensor(out=EE[1], in0=PB[5], in1=EE[1], op=ALU.mult), waits=[e1])
            att["acc_free"] = e3
            e4 = P.op("vector", lambda e: e.scalar_tensor_tensor(out=EE[2], in0=EE[1], scalar=neglam, in1=EE[0], op0=ALU.mult, op1=ALU.add), waits=[e2, e3, t_l7] + wfree)
            e5 = P.op("gpsimd", lambda e: e.tensor_tensor(out=EE[3], in0=EE[2], in1=EE[2], op=ALU.mult), waits=[e4] + wfree)

            def finish():
                ss = P.op("tensor", lambda e: e.matmul(PB[6], lhsT=onesf, rhs=EE[3], start=True, stop=True), waits=[e5, e3, d_cf, att["l6_free"]])
                e6 = P.op("scalar", lambda e: e.activation(out=EE[3], in_=PB[6], func=AF.Ln, scale=1.0 / 128.0, bias=epsc), waits=[ss])
                att["l6_free"] = e6
                e7 = P.op("scalar", lambda e: e.activation(out=EE[3], in_=EE[3], func=AF.Exp, scale=-0.5), waits=[e6])
                e8 = P.op("vector", lambda e: e.tensor_tensor(out=EE[2], in0=EE[2], in1=EE[3], op=ALU.mult), waits=[e7])
                e9 = P.op("vector", lambda e: e.tensor_scalar(out=QO[:, h, qrange], in0=EE[2], scalar1=gsc, scalar2=None, op0=ALU.mult), waits=[e8, t_g])
                att["ee_free"] = e9
                att["last"] = e9
            att["pending"] = finish

        def attention(hp):
            for i in range(NQT):
                for hl in range(2):
                    att_unit(i, hl, 2 * hp + hl)
            att["pending"]()
            att["pending"] = None
            return att["last"]

        att_last = None
        if upto == "consts":
            tap("small", small, t_consts)
            P.emit(finals)
            return nc, tap_out
        for hp in range(2):
            kvt = phase_A(hp, hp == 0)
            if upto == "A":
                tap("kt", KT[:, :, 0:2048], kvt, BF16)
                tap("vv", VV[:, 0:8, :], kvt, BF16)
                P.emit(finals)
                return nc, tap_out
            qtoks = phase_Q() if hp == 0 else []
            P.base_waits = [t for t in kvt + qtoks if t is not None] + t_consts
            att_last = attention(hp)
            P.base_waits = [att_last]
            if upto == "att0":
                break
        tap("qo", QO, [att_last], BF16)
        tap("kt", KT[:, :, 0:2048], [att_last], BF16)
        tap("vv", VV[:, 0:8, :], [att_last], BF16)

        if upto in ("att0", "att"):
            P.emit(finals)
            return nc, tap_out

        AR.reset(pers_mark)
        wp = AR.alloc([128, 8, 3584], BF16)
        woa = AR.alloc([128, 4, 1024], BF16)
        woc = AR.alloc([128, 4, 1024], BF16)
        wmx = AR.alloc([128, 8, 1024], BF16)
        xb2_ = [AR.alloc([128, 2, 1024], BF16) for _ in range(2)]
        xhb_ = [AR.alloc([128, 1024], BF16) for _ in range(2)]
        xres_ = [AR.alloc([128, 2, 1024], F32) for _ in range(2)]
        xT2 = AR.alloc([128, 8, 256], BF16)
        xhT = AR.alloc([128, 32], BF16)
        ccs_ = [AR.alloc([128, 264], F32) for _ in range(2)]
        Ub_ = [AR.alloc([128, 2, 130], F32) for _ in range(2)]
        T1_ = [AR.alloc([128, 2, 128], F32) for _ in range(2)]
        Zb = AR.alloc([128, 4, 256], BF16)
        g0_ = [AR.alloc([128, 256], F32) for _ in range(2)]
        g1_ = [AR.alloc([128, 256], F32) for _ in range(2)]
        m0_ = [AR.alloc([128, 256], F32) for _ in range(2)]
        m1_ = [AR.alloc([128, 256], F32) for _ in range(2)]
        MT = AR.alloc([128, 8, 256], BF16)
        Rb = AR.alloc([128, 1024], F32)
        lng = AR.alloc([128, 1024], F32)
        lnb = AR.alloc([128, 1024], F32)
        H1T = AR.alloc([128, 8, 128], F32)
        stt_b = AR.alloc([128, 16], F32)
        bgs = AR.alloc([128, 16], F32)
        cws = AR.alloc([128, 12], F32)
        rbias = AR.alloc([128, 36], F32)
        wr = AR.alloc([128, 8, 36], F32)
        for i7 in range(7):
            P.adma("gpsimd", wp[:, :, i7 * 512:(i7 + 1) * 512], w_in[:, 1536 + i7 * 512:1536 + (i7 + 1) * 512].rearrange("(c p) n -> p c n", p=128), "wp", writes=["wp"])
        P.adma("gpsimd", woa, w_o_att.rearrange("(c p) n -> p c n", p=128), "woa", writes=["woa"])
        P.adma("gpsimd", woc, w_o_conv.rearrange("(c p) n -> p c n", p=128), "woc", writes=["woc"])
        P.adma("gpsimd", wmx, w_mix.rearrange("(c p) n -> p c n", p=128), "wmx", writes=["wmx"])
        P.adma("sync", wr, w_rt.rearrange("(c p) n -> p c n", p=128), "wr", writes=["wr"])
        P.adma("sync", bgs, b_gate.rearrange("o (q p) -> p (o q)", p=128), "bgs", writes=["bgs"], allow_slow_non_contiguous=True)
        P.adma("sync", cws.rearrange("p (k q) -> p k q", k=3), conv_w.rearrange("k (q p) -> p k q", p=128), "cws", writes=["cws"], allow_slow_non_contiguous=True)
        P.adma("sync", rbias, b_rt.to_broadcast([128, 36]), "rbias", writes=["rbias"])
        P.adma("sync", lng, ln1[0:1, :].to_broadcast([128, 1024]), "lng", writes=["lng"])
        P.adma("sync", lnb, ln1[1:2, :].to_broadcast([128, 1024]), "lnb", writes=["lnb"])

        def layer_norm(buf, gname, bname, gt, bt, stt=None, rn="R"):
            stt = stt_b if stt is None else stt
            mv = stt[:, 12:14]
            ve = stt[:, 14:15]
            rs = stt[:, 15:16]
            P.auto("vector", lambda e: e.bn_stats(out=stt[:, 0:6], in_=buf[:, 0:512]), reads=[rn], writes=["stt"])
            P.auto("vector", lambda e: e.bn_stats(out=stt[:, 6:12], in_=buf[:, 512:1024]), reads=[rn], writes=["stt2"])
            P.auto("vector", lambda e: e.bn_aggr(out=mv, in_=stt[:, 0:12]), reads=["stt", "stt2"], writes=["mv"])
            P.auto("vector", lambda e: e.tensor_scalar(out=ve, in0=mv[:, 1:2], scalar1=1e-5, scalar2=None, op0=ALU.add), reads=["mv"], writes=["ve"])
            P.auto("gpsimd", lambda e: e.tensor_tensor(out=rs, in0=ve, in1=mhalf, op=ALU.pow), reads=["ve"], writes=["rs"], extra=[d_cf])
            P.auto("vector", lambda e: e.tensor_scalar(out=buf, in0=buf, scalar1=mv[:, 0:1], scalar2=rs, op0=ALU.subtract, op1=ALU.mult), reads=["mv", "rs"], writes=[rn])
            P.auto("vector", lambda e: e.tensor_tensor(out=buf, in0=buf, in1=gt, op=ALU.mult), reads=[gname], writes=[rn])
            return P.auto("gpsimd", lambda e: e.tensor_tensor(out=buf, in0=buf, in1=bt, op=ALU.add), reads=[bname], writes=[rn])

        def b_loads(tb):
            r0 = tb * 256
            pb = tb % 2
            P.adma("gpsimd", xb2_[pb], xo[r0:r0 + 256, :].rearrange("(b p) d -> p b d", p=128), "xb2%d" % pb, writes=["xb2%d" % pb])
            P.adma("gpsimd", xhb_[pb][0:4, :], xh[tb * 4:tb * 4 + 4, :], "xhb%d" % pb, writes=["xhb%d" % pb])
            P.adma("sync", xres_[pb], xo[r0:r0 + 256, :].rearrange("(b p) d -> p b d", p=128), "xres%d" % pb, writes=["xres%d" % pb])

        def b_tile(tb):
            r0 = tb * 256
            pb = tb % 2
            xb2, xhb, xres = xb2_[pb], xhb_[pb], xres_[pb]
            n_xb2, n_xhb, n_xres = "xb2%d" % pb, "xhb%d" % pb, "xres%d" % pb
            if tb + 1 < NTB:
                b_loads(tb + 1)
            for half in range(2):
                fns = []
                for cc in range(4):
                    c = half * 4 + cc
                    for blk in range(2):
                        o_ = PBb[half][:, cc * 256 + blk * 128: cc * 256 + blk * 128 + 128]
                        i_ = xb2[:, blk, c * 128:(c + 1) * 128]
                        fns.append(lambda e, o_=o_, i_=i_: e.transpose(out=o_, in_=i_, identity=ident))
                P.auto("tensor", fns, reads=[n_xb2], psum=["b%d" % half], extra=[d_cb])
                dst = xT2[:, half * 4:half * 4 + 4, :].rearrange("p a b -> p (a b)")
                if half == 0:
                    P.auto("vector", lambda e, dst=dst: e.tensor_copy(out=dst, in_=PBb[0]), writes=["xT2a"], psum=["b0"])
                else:
                    P.auto("scalar", lambda e, dst=dst: e.copy(out=dst, in_=PBb[1]), writes=["xT2b"], psum=["b1"])
            fns = []
            for c in range(8):
                o_ = PBb[6][:, c * 4:(c + 1) * 4]
                i_ = xhb[0:4, c * 128:(c + 1) * 128]
                fns.append(lambda e, o_=o_, i_=i_: e.transpose(out=o_, in_=i_, identity=ident[0:4, 0:4]))
            P.auto("tensor", fns, reads=[n_xhb], psum=["b6"], extra=[d_cb])
            P.auto("vector", lambda e: e.tensor_copy(out=xhT, in_=PBb[6][:, 0:32]), writes=["xhT"], psum=["b6"])
            xhT3 = xhT.rearrange("p (c k) -> p c k", c=8)
            for q in range(4):
                qp = q % 2
                BA, BB = PB[2 + 2 * qp], PB[3 + 2 * qp]
                nBA, nBB = "b%d" % (2 + 2 * qp), "b%d" % (3 + 2 * qp)
                ccs, Ub, T1 = ccs_[qp], Ub_[qp], T1_[qp]
                nccs, nU, nU2, nT1 = "ccs%d" % qp, "U%d" % qp, "U2%d" % qp, "T1%d" % qp
                fns = []
                for c in range(8):
                    l_ = wp[:, c, 512 + q * 128: 512 + q * 128 + 128]
                    fns.append(lambda e, l_=l_, c=c, BA=BA: e.matmul(BA[:, 0:256], lhsT=l_, rhs=xT2[:, c, :], start=(c == 0), stop=(c == 7)))
                for c in range(8):
                    l_ = wp[:, c, 512 + q * 128: 512 + q * 128 + 128]
                    fns.append(lambda e, l_=l_, c=c, BA=BA: e.matmul(BA[:, 256:260], lhsT=l_, rhs=xhT3[:, c, :], start=(c == 0), stop=(c == 7)))
                for c in range(8):
                    l_ = wp[:, c, 1024 + q * 128: 1024 + q * 128 + 128]
                    fns.append(lambda e, l_=l_, c=c, BA=BA: e.matmul(BA[:, 260:264], lhsT=l_, rhs=xhT3[:, c, :], start=(c == 0), stop=(c == 7)))
                P.auto("tensor", fns, reads=["wp", "xT2a", "xT2b", "xhT"], psum=[nBA])
                fns = []
                for c in range(8):
                    l_ = wp[:, c, 1024 + q * 128: 1024 + q * 128 + 128]
                    fns.append(lambda e, l_=l_, c=c, BB=BB: e.matmul(BB[:, 0:256], lhsT=l_, rhs=xT2[:, c, :], start=(c == 0), stop=(c == 7)))
                for c in range(8):
                    l_ = wp[:, c, q * 128: q * 128 + 128]
                    fns.append(lambda e, l_=l_, c=c, BB=BB: e.matmul(BB[:, 256:512], lhsT=l_, rhs=xT2[:, c, :], start=(c == 0), stop=(c == 7)))
                P.auto("tensor", fns, reads=["wp", "xT2a", "xT2b"], psum=[nBB])
                P.auto("scalar", lambda e, ccs=ccs, BA=BA: e.copy(out=ccs, in_=BA[:, 0:264]), writes=[nccs], psum=[nBA])
                P.auto("vector", lambda e, ccs=ccs, Ub=Ub, BB=BB: e.tensor_tensor(out=Ub[:, :, 2:130], in0=ccs[:, 0:256].rearrange("p (b t) -> p b t", b=2),
                                                          in1=BB[:, 0:256].rearrange("p (b t) -> p b t", b=2), op=ALU.mult),
                       reads=[nccs], writes=[nU], psum=[nBB])
                P.auto("vector", lambda e, ccs=ccs, Ub=Ub: e.tensor_tensor(out=Ub[:, :, 0:2], in0=ccs[:, 256:260].rearrange("p (b t) -> p b t", b=2),
                                                          in1=ccs[:, 260:264].rearrange("p (b t) -> p b t", b=2), op=ALU.mult),
                       reads=[nccs], writes=[nU2])
                P.auto("vector", lambda e, q=q, T1=T1, Ub=Ub: e.tensor_scalar(out=T1, in0=Ub[:, :, 0:128], scalar1=cws[:, q:q + 1], scalar2=None, op0=ALU.mult),
                       reads=[nU, nU2, "cws"], writes=[nT1])
                P.auto("vector", lambda e, q=q, T1=T1, Ub=Ub: e.scalar_tensor_tensor(out=T1, in0=Ub[:, :, 1:129], scalar=cws[:, 4 + q:5 + q], in1=T1, op0=ALU.mult, op1=ALU.add),
                       reads=[nU, nU2], writes=[nT1])
                P.auto("vector", lambda e, q=q, T1=T1, Ub=Ub: e.scalar_tensor_tensor(out=T1, in0=Ub[:, :, 2:130], scalar=cws[:, 8 + q:9 + q], in1=T1, op0=ALU.mult, op1=ALU.add),
                       reads=[nU, nU2], writes=[nT1])
                P.auto("vector", lambda e, q=q, T1=T1, BB=BB: e.tensor_tensor(out=Zb[:, q, :], in0=T1.rearrange("p b t -> p (b t)"), in1=BB[:, 256:512], op=ALU.mult),
                       reads=[nT1], writes=["Z%d" % q], psum=[nBB])
            for c8 in range(8):
                cp_ = c8 % 2
                BG, BY = PB[2 + 2 * cp_], PB[3 + 2 * cp_]
                nBG, nBY = "b%d" % (2 + 2 * cp_), "b%d" % (3 + 2 * cp_)
                g0, g1, m0, m1 = g0_[cp_], g1_[cp_], m0_[cp_], m1_[cp_]
                ng0, ng1, nm0, nm1 = "g0%d" % cp_, "g1%d" % cp_, "m0%d" % cp_, "m1%d" % cp_
                fns = []
                for c in range(8):
                    l_ = wp[:, c, 1536 + c8 * 128: 1536 + c8 * 128 + 128]
                    fns.append(lambda e, l_=l_, c=c, BG=BG: e.matmul(BG[:, 0:256], lhsT=l_, rhs=xT2[:, c, :], start=(c == 0), stop=(c == 7)))
                for c in range(8):
                    l_ = wp[:, c, 2560 + c8 * 128: 2560 + c8 * 128 + 128]
                    fns.append(lambda e, l_=l_, c=c, BG=BG: e.matmul(BG[:, 256:512], lhsT=l_, rhs=xT2[:, c, :], start=(c == 0), stop=(c == 7)))
                P.auto("tensor", fns, reads=["wp", "xT2a", "xT2b"], psum=[nBG])
                P.auto("scalar", lambda e, c8=c8, g0=g0, BG=BG: e.activation(out=g0, in_=BG[:, 0:256], func=AF.Sigmoid, bias=bgs[:, c8:c8 + 1]), reads=["bgs"], writes=[ng0], psum=[nBG])
                P.auto("scalar", lambda e, c8=c8, g1=g1, BG=BG: e.activation(out=g1, in_=BG[:, 256:512], func=AF.Sigmoid, bias=bgs[:, 8 + c8:9 + c8]), reads=["bgs"], writes=[ng1], psum=[nBG])
                fns = []
                for h in range(4):
                    l_ = woa[:, h, c8 * 128:(c8 + 1) * 128]
                    r_ = QO[:, h, r0:r0 + 256]
                    fns.append(lambda e, l_=l_, r_=r_, h=h, BY=BY: e.matmul(BY[:, 0:256], lhsT=l_, rhs=r_, start=(h == 0), stop=(h == 3)))
                for q in range(4):
                    l_ = woc[:, q, c8 * 128:(c8 + 1) * 128]
                    r_ = Zb[:, q, :]
                    fns.append(lambda e, l_=l_, r_=r_, q=q, BY=BY: e.matmul(BY[:, 256:512], lhsT=l_, rhs=r_, start=(q == 0), stop=(q == 3)))
                P.auto("tensor", fns, reads=["woa", "woc", "Z0", "Z1", "Z2", "Z3"], psum=[nBY])
                P.auto("vector", lambda e, m0=m0, g0=g0, BY=BY: e.tensor_tensor(out=m0, in0=g0, in1=BY[:, 0:256], op=ALU.mult), reads=[ng0], writes=[nm0], psum=[nBY])
                P.auto("vector", lambda e, m1=m1, g1=g1, BY=BY: e.tensor_tensor(out=m1, in0=g1, in1=BY[:, 256:512], op=ALU.mult), reads=[ng1], writes=[nm1], psum=[nBY])
                P.auto("vector", lambda e, c8=c8, m0=m0, m1=m1: e.tensor_tensor(out=MT[:, c8, :], in0=m0, in1=m1, op=ALU.add), reads=[nm0, nm1], writes=["MT%d" % c8])
            for blk in range(2):
                gb = tb * 2 + blk
                fns = []
                for half in range(2):
                    for c in range(8):
                        l_ = MT[:, c, blk * 128:(blk + 1) * 128]
                        r_ = wmx[:, c, half * 512:(half + 1) * 512]
                        fns.append(lambda e, l_=l_, r_=r_, c=c, half=half: e.matmul(PB[6 + half], lhsT=l_, rhs=r_, start=(c == 0), stop=(c == 7)))
                P.auto("tensor", fns, reads=["MT%d" % c_ for c_ in range(8)] + ["wmx"], psum=["b6", "b7"])
                P.auto("vector", lambda e, blk=blk: e.scalar_tensor_tensor(out=Rb[:, 0:512], in0=xres[:, blk, 0:512], scalar=ALPHA, in1=PB[6], op0=ALU.mult, op1=ALU.add),
                       reads=[n_xres], writes=["R"], psum=["b6"])
                P.auto("vector", lambda e, blk=blk: e.scalar_tensor_tensor(out=Rb[:, 512:1024], in0=xres[:, blk, 512:1024], scalar=ALPHA, in1=PB[7], op0=ALU.mult, op1=ALU.add),
                       reads=[n_xres], writes=["R"], psum=["b7"])
                layer_norm(Rb, "lng", "lnb", lng, lnb)
                finals_h1.append(P.adma("sync", h1f[gb * 128:(gb + 1) * 128, :], Rb, "h1w", reads=["R"]))
                fns = []
                for c in range(8):
                    o_ = PB[c // 4][:, (c % 4) * 128:(c % 4) * 128 + 128]
                    i_ = Rb[:, c * 128:(c + 1) * 128]
                    fns.append(lambda e, o_=o_, i_=i_: e.transpose(out=o_, in_=i_, identity=identf))
                P.auto("tensor", fns, reads=["R"], psum=["b0", "b1"], extra=[d_cf])
                P.auto("scalar", lambda e: e.copy(out=H1T[:, 0:4, :].rearrange("p a b -> p (a b)"), in_=PB[0]), writes=["H1Ta"], psum=["b0"])
                P.auto("vector", lambda e: e.tensor_copy(out=H1T[:, 4:8, :].rearrange("p a b -> p (a b)"), in_=PB[1]), writes=["H1Tb"], psum=["b1"])
                fns = []
                for c in range(8):
                    fns.append(lambda e, c=c: e.matmul(PB[0][:, 0:36], lhsT=H1T[:, c, :], rhs=wr[:, c, :], start=(c == 0), stop=(c == 7)))
                P.auto("tensor", fns, reads=["H1Ta", "H1Tb", "wr"], psum=["b0"])
                P.auto("vector", lambda e, gb=gb: e.tensor_tensor(out=LG[:, gb, :], in0=PB[0][:, 0:36], in1=rbias, op=ALU.add), reads=["rbias"], writes=["LG"], psum=["b0"])

        finals_h1 = []
        NTB = NQT * 2
        b_loads(0)
        for tb in range(NTB):
            b_tile(tb)
        tap("lg", LG, [P.lw["LG"]])
        tap("h1", h1f[0:256, :], finals_h1)
        if upto == "B":
            P.emit(finals + finals_h1)
            return nc, tap_out

        P.base_waits = [P.lw["LG"]] + finals_h1
        AR.reset(pers_mark)
        NB = NTB * 2
        gm = AR.alloc([128, 32], F32)
        gd = AR.alloc([128, 32, 4], F32)
        pen = AR.alloc([128, 32, 4], F32)
        ge = AR.alloc([128, 32, 4], F32)
        gs = AR.alloc([128, 32], F32)
        gw = AR.alloc([128, 32], F32)
        em = AR.alloc([128, 32, 32], F32)
        em2 = AR.alloc([128, 32, 32], F32)
        oh1 = AR.alloc([128, 32, 32], F32)
        oh2 = AR.alloc([128, 32, 32], F32)
        Aa = AR.alloc([128, 32, 32], F32)
        Tt = AR.alloc([128, 32, 32], F32)
        sc0 = AR.alloc([128, 32, 32], F32)
        sc1 = AR.alloc([128, 32, 32], F32)
        rank = AR.alloc([128, 32, 32], F32)
        tmpc = AR.alloc([128, 32, 32], F32)
        v1 = AR.alloc([128, 32], F32)
        v2 = AR.alloc([128, 32], F32)
        dv = AR.alloc([128, 32], F32)
        s12 = AR.alloc([128, 2, 32], F32)
        stt_c = AR.alloc([128, 16], F32)
        fl = lambda t: t.rearrange("p a b -> p (a b)")
        gl = LG[:, :, 0:4]
        el = LG[:, :, 4:36]
        bc3 = lambda t: t.unsqueeze(2).to_broadcast([128, 32, 32])
        V = "vector"
        P.auto(V, lambda e: e.tensor_reduce(out=gm, in_=gl, axis=AX.X, op=ALU.max), reads=["LG"], writes=["gm"])
        P.auto(V, lambda e: e.tensor_tensor(out=gd, in0=gl, in1=gm.unsqueeze(2).to_broadcast([128, 32, 4]), op=ALU.subtract), reads=["LG", "gm"], writes=["gd"])
        P.auto(V, lambda e: e.tensor_scalar(out=pen, in0=gd, scalar1=0.0, scalar2=NEG, op0=ALU.is_lt, op1=ALU.mult), reads=["gd"], writes=["pen"])
        P.auto("scalar", lambda e: e.activation(out=ge, in_=gd, func=AF.Exp), reads=["gd"], writes=["ge"])
        P.auto(V, lambda e: e.tensor_reduce(out=gs, in_=ge, axis=AX.X, op=ALU.add), reads=["ge"], writes=["gs"])
        P.auto(V, lambda e: e.reciprocal(out=gw, in_=gs), reads=["gs"], writes=["gw"])
        P.auto(V, lambda e: e.tensor_tensor(out=em.rearrange("p b (g k) -> p b g k", g=4), in0=el.rearrange("p b (g k) -> p b g k", g=4),
                                            in1=pen.unsqueeze(3).to_broadcast([128, 32, 4, 8]), op=ALU.add), reads=["LG", "pen"], writes=["em"])
        P.auto(V, lambda e: e.tensor_reduce(out=v1, in_=em, axis=AX.X, op=ALU.max), reads=["em"], writes=["v1"])
        P.auto(V, lambda e: e.tensor_tensor(out=oh1, in0=em, in1=bc3(v1), op=ALU.is_equal), reads=["em", "v1"], writes=["oh1"])
        P.auto(V, lambda e: e.scalar_tensor_tensor(out=fl(em2), in0=fl(oh1), scalar=NEG, in1=fl(em), op0=ALU.mult, op1=ALU.add), reads=["oh1", "em"], writes=["em2"])
        P.auto(V, lambda e: e.tensor_reduce(out=v2, in_=em2, axis=AX.X, op=ALU.max), reads=["em2"], writes=["v2"])
        P.auto(V, lambda e: e.tensor_tensor(out=oh2, in0=em2, in1=bc3(v2), op=ALU.is_equal), reads=["em2", "v2"], writes=["oh2"])
        P.auto(V, lambda e: e.tensor_tensor(out=dv, in0=v2, in1=v1, op=ALU.subtract), reads=["v1", "v2"], writes=["dv"])
        P.auto("scalar", lambda e: e.activation(out=dv, in_=dv, func=AF.Exp), reads=[], writes=["dv"])
        P.auto(V, lambda e: e.tensor_scalar(out=dv, in0=dv, scalar1=1.0, scalar2=None, op0=ALU.add), writes=["dv"])
        P.auto(V, lambda e: e.reciprocal(out=dv, in_=dv), writes=["dv"])
        P.auto(V, lambda e: e.tensor_tensor(out=PW[:, 0, :], in0=dv, in1=gw, op=ALU.mult), reads=["dv", "gw"], writes=["PW0"])
        P.auto(V, lambda e: e.tensor_tensor(out=PW[:, 1, :], in0=gw, in1=PW[:, 0, :], op=ALU.subtract), reads=["PW0", "gw"], writes=["PW1"])
        P.auto(V, lambda e: e.tensor_tensor(out=fl(Aa), in0=fl(oh1), in1=fl(oh2), op=ALU.add), reads=["oh1", "oh2"], writes=["Aa"])
        Af = fl(Aa)
        P.auto("tensor", [lambda e: e.matmul(PB[0], lhsT=trif, rhs=Af[:, 0:512], start=True, stop=True),
                          lambda e: e.matmul(PB[1], lhsT=trif, rhs=Af[:, 512:1024], start=True, stop=True),
                          lambda e: e.matmul(PB[2], lhsT=onesf, rhs=Af[:, 0:512], start=True, stop=True),
                          lambda e: e.matmul(PB[3], lhsT=onesf, rhs=Af[:, 512:1024], start=True, stop=True)],
               reads=["Aa"], psum=["b0", "b1", "b2", "b3"], extra=[d_cf])
        P.auto(V, lambda e: e.tensor_copy(out=fl(Tt)[:, 0:512], in_=PB[2]), writes=["Tt"], psum=["b2"])
        P.auto(V, lambda e: e.tensor_copy(out=fl(Tt)[:, 512:1024], in_=PB[3]), writes=["Tt"], psum=["b3"])
        cur, cname = Tt, "Tt"
        for si, sh in enumerate((1, 2, 4, 8, 16)):
            nxt, nname = (sc0, "sc0") if si % 2 == 0 else (sc1, "sc1")
            P.auto(V, lambda e, cur=cur, nxt=nxt, sh=sh: e.tensor_tensor(out=nxt[:, sh:32, :], in0=cur[:, sh:32, :], in1=cur[:, 0:32 - sh, :], op=ALU.add), reads=[cname], writes=[nname])
            P.auto(V, lambda e, cur=cur, nxt=nxt, sh=sh: e.tensor_copy(out=nxt[:, 0:sh, :], in_=cur[:, 0:sh, :]), reads=[cname], writes=[nname])
            cur, cname = nxt, nname
        P.auto(V, lambda e, cur=cur: e.tensor_tensor(out=fl(tmpc), in0=fl(cur), in1=fl(Tt), op=ALU.subtract), reads=[cname, "Tt"], writes=["tmpc"])
        P.auto(V, lambda e: e.tensor_tensor(out=fl(rank)[:, 0:512], in0=fl(tmpc)[:, 0:512], in1=PB[0], op=ALU.add), reads=["tmpc"], writes=["rank"], psum=["b0"])
        P.auto(V, lambda e: e.tensor_tensor(out=fl(rank)[:, 512:1024], in0=fl(tmpc)[:, 512:1024], in1=PB[1], op=ALU.add), reads=["tmpc"], writes=["rank"], psum=["b1"])
        P.auto(V, lambda e: e.tensor_scalar(out=fl(rank), in0=fl(rank), scalar1=float(CAP - 1), scalar2=None, op0=ALU.min), writes=["rank"])
        P.auto(V, lambda e: e.tensor_tensor(out=rank, in0=rank, in1=ecap.unsqueeze(1).to_broadcast([128, 32, 32]), op=ALU.add), writes=["rank"], extra=[d_cf])
        P.auto(V, lambda e: e.tensor_tensor(out=fl(tmpc), in0=fl(oh1), in1=fl(rank), op=ALU.mult), reads=["oh1", "rank"], writes=["tmpc"])
        P.auto(V, lambda e: e.tensor_reduce(out=s12[:, 0, :], in_=tmpc, axis=AX.X, op=ALU.add), reads=["tmpc"], writes=["s12a"])
        P.auto(V, lambda e: e.tensor_tensor(out=fl(tmpc), in0=fl(oh2), in1=fl(rank), op=ALU.mult), reads=["oh2", "rank"], writes=["tmpc"])
        P.auto(V, lambda e: e.tensor_reduce(out=s12[:, 1, :], in_=tmpc, axis=AX.X, op=ALU.add), reads=["tmpc"], writes=["s12b"])
        P.auto(V, lambda e: e.tensor_scalar(out=fl(s12), in0=fl(s12), scalar1=float(NSLOT - 1), scalar2=0.0, op0=ALU.min, op1=ALU.max), reads=["s12a", "s12b"], writes=["s12a", "s12b"])
        P.auto(V, lambda e: e.tensor_copy(out=SLI, in_=s12), reads=["s12a", "s12b"], writes=["SLI"])
        tap("sli", SLI, [P.lw["SLI"]], I32)
        tap("pw", PW, [P.lw["PW1"]])

        P.base_waits = [P.lw["SLI"], P.lw["PW1"], P.lw["PW0"]] + finals_h1
        AR.reset(pers_mark)
        stt_c = AR.alloc([128, 16], F32)
        wg = [AR.alloc([128, 8, 512], BF16) for _ in range(2)]
        wu = [AR.alloc([128, 8, 512], BF16) for _ in range(2)]
        wd = [AR.alloc([128, 4, 1024], BF16) for _ in range(2)]

        def e_loads_w(e_):
            i = e_ % 2
            P.adma("gpsimd", wg[i], w_eg[e_].rearrange("(c p) n -> p c n", p=128), "wg%d" % i, writes=["wg%d" % i])
            P.adma("gpsimd", wu[i], w_eu[e_].rearrange("(c p) n -> p c n", p=128), "wu%d" % i, writes=["wu%d" % i])
            P.adma("gpsimd", wd[i], w_ed[e_].rearrange("(c p) n -> p c n", p=128), "wd%d" % i, writes=["wd%d" % i])

        e_loads_w(0)
        hb = [AR.alloc([128, 1024], F32) for _ in range(2)]
        sc_toks = []
        for blk in range(NB):
            i = blk % 2
            P.adma("sync", hb[i], h1f[blk * 128:(blk + 1) * 128, :], "hb%d" % i, writes=["hb%d" % i])
            for k in range(2):
                off = SLI[:, k, blk:blk + 1].bitcast(U32)
                src = hb[i]
                sc_toks.append(P.acdma("gpsimd", lambda e, off=off, src=src: e.indirect_dma_start(
                    out=xs_d, out_offset=bass.IndirectOffsetOnAxis(ap=off, axis=0), in_=src, in_offset=None),
                    "sc%d_%d" % (i, k), reads=["hb%d" % i, "SLI"]))

        XE = [AR.alloc([128, 3, 1024], BF16) for _ in range(2)]
        XT = AR.alloc([128, 8, 384], BF16)
        sg = AR.alloc([128, 384], F32)
        HT = AR.alloc([128, 4, 384], BF16)
        YSb = [AR.alloc([128, 1024], F32) for _ in range(2)]
        NE = 32
        ys_toks = []

        def e_loads(e_):
            i = e_ % 2
            if e_ > 0:
                e_loads_w(e_)
            P.adma("sync", XE[i], xs_d[e_ * CAP:(e_ + 1) * CAP, :].rearrange("(b p) d -> p b d", p=128), "xe%d" % i, writes=["xe%d" % i], extra=sc_toks)

        def e_compute(e_):
            i = e_ % 2
            for pr in range(4):
                bank = pr % 2
                fns = []
                for cc in range(2):
                    c = 2 * pr + cc
                    for sb in range(3):
                        o_ = PBb[bank][:, cc * 384 + sb * 128: cc * 384 + sb * 128 + 128]
                        i_ = XE[i][:, sb, c * 128:(c + 1) * 128]
                        fns.append(lambda e, o_=o_, i_=i_: e.transpose(out=o_, in_=i_, identity=ident))
                P.auto("tensor", fns, reads=["xe%d" % i], psum=["b%d" % bank])
                dst = XT[:, 2 * pr:2 * pr + 2, :].rearrange("p a b -> p (a b)")
                if bank == 0:
                    P.auto("vector", lambda e, dst=dst: e.tensor_copy(out=dst, in_=PBb[0][:, 0:768]), writes=["XT%d" % pr], psum=["b0"])
                else:
                    P.auto("scalar", lambda e, dst=dst: e.copy(out=dst, in_=PBb[1][:, 0:768]), writes=["XT%d" % pr], psum=["b1"])
            xt_names = ["XT%d" % pr for pr in range(4)]
            for f in range(4):
                gb_ = 2 + 2 * (f % 2)
                ub_ = 3 + 2 * (f % 2)
                fns = []
                for c in range(8):
                    l_ = wg[i][:, c, f * 128:(f + 1) * 128]
                    fns.append(lambda e, l_=l_, c=c, gb_=gb_: e.matmul(PB[gb_][:, 0:384], lhsT=l_, rhs=XT[:, c, :], start=(c == 0), stop=(c == 7)))
                P.auto("tensor", fns, reads=["wg%d" % i] + xt_names, psum=["b%d" % gb_])
                fns = []
                for c in range(8):
                    l_ = wu[i][:, c, f * 128:(f + 1) * 128]
                    fns.append(lambda e, l_=l_, c=c, ub_=ub_: e.matmul(PB[ub_][:, 0:384], lhsT=l_, rhs=XT[:, c, :], start=(c == 0), stop=(c == 7)))
                P.auto("tensor", fns, reads=["wu%d" % i] + xt_names, psum=["b%d" % ub_])
                P.auto("scalar", lambda e, gb_=gb_: e.activation(out=sg, in_=PB[gb_][:, 0:384], func=AF.Silu), writes=["sg"], psum=["b%d" % gb_])
                P.auto("vector", lambda e, ub_=ub_, f=f: e.tensor_tensor(out=HT[:, f, :], in0=sg, in1=PB[ub_][:, 0:384], op=ALU.mult), reads=["sg"], writes=["HT%d" % f], psum=["b%d" % ub_])
            ht_names = ["HT%d" % f for f in range(4)]
            for sb in range(3):
                k = (e_ * 3 + sb) % 2
                fns = []
                for half in range(2):
                    for f in range(4):
                        l_ = HT[:, f, sb * 128:(sb + 1) * 128]
                        r_ = wd[i][:, f, half * 512:(half + 1) * 512]
                        fns.append(lambda e, l_=l_, r_=r_, f=f, half=half: e.matmul(PB[6 + half], lhsT=l_, rhs=r_, start=(f == 0), stop=(f == 3)))
                P.auto("tensor", fns, reads=["wd%d" % i] + ht_names, psum=["b6", "b7"])
                ysb = YSb[k]
                P.auto("scalar", lambda e, ysb=ysb: e.copy(out=ysb[:, 0:512], in_=PB[6]), writes=["ysa%d" % k], psum=["b6"])
                P.auto("vector", lambda e, ysb=ysb: e.tensor_copy(out=ysb[:, 512:1024], in_=PB[7]), writes=["ysb%d" % k], psum=["b7"])
                r0 = e_ * CAP + sb * 128
                ys_toks.append(P.adma("sync", ys_d[r0:r0 + 128, :], ysb, "ysw%d" % k, reads=["ysa%d" % k, "ysb%d" % k]))

        e_loads(0)
        for e_ in range(NE):
            if e_ + 1 < NE:
                e_loads(e_ + 1)
            e_compute(e_)

        Y1_ = [AR.alloc([128, 1024], F32) for _ in range(2)]
        Y2_ = [AR.alloc([128, 1024], F32) for _ in range(2)]
        hc_ = [AR.alloc([128, 1024], F32) for _ in range(2)]
        R2_ = [AR.alloc([128, 1024], F32) for _ in range(2)]
        lng2 = AR.alloc([128, 1024], F32)
        lnb2 = AR.alloc([128, 1024], F32)
        P.adma("sync", lng2, ln2[0:1, :].to_broadcast([128, 1024]), "lng2", writes=["lng2"])
        P.adma("sync", lnb2, ln2[1:2, :].to_broadcast([128, 1024]), "lnb2", writes=["lnb2"])

        def c_loads(blk):
            pb = blk % 2
            P.adma("sync", hc_[pb], h1f[blk * 128:(blk + 1) * 128, :], "hc%d" % pb, writes=["hc%d" % pb])
            for k, yb in ((0, Y1_[pb]), (1, Y2_[pb])):
                off = SLI[:, k, blk:blk + 1].bitcast(U32)
                P.acdma("gpsimd", lambda e, off=off, yb=yb: e.indirect_dma_start(
                    out=yb, out_offset=None, in_=ys_d, in_offset=bass.IndirectOffsetOnAxis(ap=off, axis=0)),
                    "yg%d%d" % (k, pb), reads=["SLI"], writes=["Y%d%d" % (k, pb)], extra=ys_toks)

        def c_block(blk):
            pb = blk % 2
            if blk + 1 < NB:
                c_loads(blk + 1)
            Y1, Y2, hc, R2 = Y1_[pb], Y2_[pb], hc_[pb], R2_[pb]
            rn = "R2%d" % pb
            P.auto("scalar", lambda e: e.mul(out=R2, in_=hc, mul=ALPHA), reads=["hc%d" % pb], writes=[rn])
            P.auto(V, lambda e: e.scalar_tensor_tensor(out=R2, in0=Y1, scalar=PW[:, 0, blk:blk + 1], in1=R2, op0=ALU.mult, op1=ALU.add), reads=["Y0%d" % pb, "PW0"], writes=[rn])
            P.auto(V, lambda e: e.scalar_tensor_tensor(out=R2, in0=Y2, scalar=PW[:, 1, blk:blk + 1], in1=R2, op0=ALU.mult, op1=ALU.add), reads=["Y1%d" % pb, "PW1"], writes=[rn])
            layer_norm(R2, "lng2", "lnb2", lng2, lnb2, stt_c, rn)
            finals.append(P.adma("sync", out[blk * 128:(blk + 1) * 128, :], R2, "outw%d" % pb, reads=[rn]))

        c_loads(0)
        for blk in range(NB):
            c_block(blk)

        P.emit(finals)
    return nc, tap_out


def _consts(j):
    ident = np.eye(128, dtype=np.float32)
    rot = np.zeros((128, 128), np.float32)
    for m in range(128):
        if (m % 64) < 32:
            rot[m + 32, m] = -1.0
        else:
            rot[m - 32, m] = 1.0
    ones = np.ones((128, 128), np.float32)
    kk = np.arange(128)[:, None, None]
    r = np.arange(8)[None, :, None]
    qq = np.arange(512)[None, None, :]
    mask = ((r * 128 + kk) <= ((2 * (qq // 128) + j) * 128 + (qq % 128))).astype(np.float32)
    cbf = np.concatenate([ident, rot, ones, mask.reshape(128, 4096)], axis=1)
    cf = np.zeros((128, 512), np.float32)
    cf[:, 0:128] = ident
    cf[:, 128:256] = 1.0
    cf[:, 256:384] = (np.arange(128)[:, None] < np.arange(128)[None, :]).astype(np.float32)
    inv_freq = (np.float32(10000.0) ** (-np.arange(0, 64, 2, dtype=np.float32) / np.float32(64))).astype(np.float32)
    p = np.arange(128)
    cf[:, 384] = (inv_freq[(p % 64) % 32].astype(np.float64) / (2.0 * np.pi)).astype(np.float32)
    cf[:, 392:424] = (np.arange(32) * CAP)[None, :]
    cf[:, 424] = -0.5
    cf[:, 425] = 1e-5
    return np.ascontiguousarray(cbf), cf


def make_core_inputs(inp, c):
    b, j = c // 2, c % 2
    x = inp["x"]
    xb_ = x[b]
    blocks = xb_.reshape(64, 128, D)
    xo = np.ascontiguousarray(blocks[j::2].reshape(NOWN, D))
    xh = np.zeros((32, 2, D), np.float32)
    for m in range(32):
        blk = 2 * m + j
        if blk > 0:
            xh[m] = xb_[blk * 128 - 2: blk * 128]
    pos = np.asarray(inp["positions"][b], dtype=np.int32)
    poso = np.ascontiguousarray(pos.reshape(64, 128)[j::2].reshape(1, NOWN))
    cbf, cf = _consts(j)
    f = lambda a: np.ascontiguousarray(np.asarray(a, dtype=np.float32))
    return {
        "xf": f(xb_), "xo": xo, "xh": f(xh.reshape(64, D)), "posf": np.ascontiguousarray(pos.reshape(1, S)), "poso": poso,
        "w_in": f(inp["w_in"][0]), "b_gate": f(inp["b_gate"][0].reshape(1, 2048)),
        "lam_in": f(np.concatenate([inp["lambda_q1"][0], inp["lambda_k1"][0], inp["lambda_q2"][0], inp["lambda_k2"][0]]).reshape(1, 256)),
        "subln_g": f(inp["subln_g"][0].reshape(1, 128)), "w_o_att": f(inp["w_o_att"][0]), "conv_w": f(inp["conv_w"][0]),
        "w_o_conv": f(inp["w_o_conv"][0]), "w_mix": f(inp["w_mix_out"][0]),
        "ln1": f(np.stack([inp["ln1_g"][0], inp["ln1_b"][0]])), "ln2": f(np.stack([inp["ln2_g"][0], inp["ln2_b"][0]])),
        "w_rt": f(np.concatenate([inp["w_router_group"][0], inp["w_router_expert"][0]], axis=1)),
        "b_rt": f(np.concatenate([inp["b_router_group"][0], inp["b_router_expert"][0]]).reshape(1, 36)),
        "w_eg": f(inp["w_exp_gate"][0]), "w_eu": f(inp["w_exp_up"][0]), "w_ed": f(inp["w_exp_down"][0]),
        "cbf": cbf, "cf32": cf,
    }


def kernel(**inputs):
    inp = {k: np.asarray(v) for k, v in inputs.items()}
    nc, _ = build()
    in_maps = [make_core_inputs(inp, c) for c in range(8)]
    res = run_bass_kernel_spmd(nc, in_maps, core_ids=list(range(8)))
    outp = np.zeros((4, S, D), np.float32)
    for c in range(8):
        b, j = c // 2, c % 2
        o = np.asarray(res.results[c]["out"]).reshape(32, 128, D)
        outp[b].reshape(64, 128, D)[j::2] = o
    return outp
```

```python
import math
import numpy as np
import ml_dtypes
from contextlib import ExitStack
import concourse.bass as bass
import concourse.mybir as mybir
from concourse.bass_utils import run_bass_kernel_spmd

F32 = mybir.dt.float32
BF16 = mybir.dt.bfloat16
I32 = mybir.dt.int32
U32 = mybir.dt.uint32
AF = mybir.ActivationFunctionType
ALU = mybir.AluOpType
AX = mybir.AxisListType
ENGS = ["tensor", "vector", "scalar", "gpsimd", "sync"]

S = 8192
D = 1024
NOWN = 4096
CAP = 384
NSLOT = 32 * CAP
ALPHA = 2.0 ** 0.25
LAMBDA_INIT = 0.8 - 0.6 * math.exp(0.0)
TWO_PI = 2.0 * math.pi
NEG = -1.0e30


class Prog:
    def __init__(self, nc, es):
        self.nc = nc
        self.es = es
        self.ops = {e: [] for e in ENGS}
        self.cnt = {e: 0 for e in ENGS}
        self.esem = {e: es.enter_context(nc.semaphore("es_" + e)) for e in ENGS}
        self.dsem = {}
        self.dcnt = {}
        self.waited = {e: {} for e in ENGS}
        self.base_waits = []

    def _waits(self, eng, waits):
        best = {}
        for w in list(waits) + list(self.base_waits):
            if w is None:
                continue
            sem, val, key = w
            if key not in best or best[key][1] < val:
                best[key] = (sem, val)
        out = []
        for key, (sem, val) in best.items():
            if self.waited[eng].get(key, 0) >= val:
                continue
            self.waited[eng][key] = val
            out.append((sem, val))
        return out

    def op(self, eng, fn, waits=(), signal=True):
        ws = self._waits(eng, waits)
        inc = None
        tok = None
        if signal:
            self.cnt[eng] += 1
            inc = (self.esem[eng], 1)
            tok = (self.esem[eng], self.cnt[eng], "e_" + eng)
        self.ops[eng].append((fn, ws, inc))
        return tok

    def _dsem(self, sem):
        if sem not in self.dsem:
            self.dsem[sem] = self.es.enter_context(self.nc.semaphore("ds_" + sem))
            self.dcnt[sem] = 0
        return self.dsem[sem]

    def dma(self, q, out, in_, sem, waits=(), **kw):
        return self.cdma(q, lambda e: e.dma_start(out=out, in_=in_, **kw), sem, waits)

    def cdma(self, q, fn, sem, waits=()):
        s = self._dsem(sem)
        ws = self._waits(q, waits)
        self.dcnt[sem] += 16
        self.ops[q].append((fn, ws, (s, 16)))
        return (s, self.dcnt[sem], "d_" + sem)

    def _auto_waits(self, reads, writes, psum):
        if not hasattr(self, "lw"):
            self.lw, self.rd, self.pa = {}, {}, {}
        ws = []
        for r in reads:
            ws.append(self.lw.get(r))
        for w in writes:
            ws.append(self.lw.get(w))
            ws.extend(self.rd.get(w, []))
        for p in psum:
            ws.append(self.pa.get(p))
        return ws

    def _auto_done(self, tok, reads, writes, psum):
        for r in reads:
            self.rd.setdefault(r, []).append(tok)
        for w in writes:
            self.lw[w] = tok
            self.rd[w] = []
        for p in psum:
            self.pa[p] = tok

    def auto(self, eng, fns, reads=(), writes=(), psum=(), extra=()):
        if not isinstance(fns, (list, tuple)):
            fns = [fns]
        ws = self._auto_waits(reads, writes, psum) + list(extra)
        tok = None
        for k, fn in enumerate(fns):
            tok = self.op(eng, fn, waits=ws if k == 0 else (), signal=(k == len(fns) - 1))
        self._auto_done(tok, reads, writes, psum)
        return tok

    def adma(self, q, out, in_, sem, reads=(), writes=(), extra=(), **kw):
        ws = self._auto_waits(reads, writes, ()) + list(extra)
        tok = self.dma(q, out, in_, sem, waits=ws, **kw)
        self._auto_done(tok, reads, writes, ())
        return tok

    def acdma(self, q, fn, sem, reads=(), writes=(), extra=()):
        ws = self._auto_waits(reads, writes, ()) + list(extra)
        tok = self.cdma(q, fn, sem, waits=ws)
        self._auto_done(tok, reads, writes, ())
        return tok

    def emit(self, final_waits):
        nc = self.nc
        with nc.Block() as block:
            def mk(eng):
                def body(e):
                    for fn, ws, inc in self.ops[eng]:
                        for sem, val in ws:
                            e.wait_ge(sem, val)
                        ins = fn(e)
                        if inc is not None:
                            ins.then_inc(inc[0], inc[1])
                    if eng == "sync":
                        for w in final_waits:
                            if w is not None:
                                e.wait_ge(w[0], w[1])
                return body
            block.tensor(mk("tensor"))
            block.vector(mk("vector"))
            block.scalar(mk("scalar"))
            block.gpsimd(mk("gpsimd"))
            block.sync(mk("sync"))


class Arena:
    def __init__(self, t, nwords):
        self.t = t
        self.n = nwords
        self.top = 0

    def mark(self):
        return self.top

    def reset(self, m):
        self.top = m

    def alloc(self, shape, dt):
        per = 1
        for s_ in shape[1:]:
            per *= s_
        if dt == BF16:
            words = (per + 1) // 2
        else:
            words = per
        words = (words + 7) // 8 * 8
        a = self.top
        self.top += words
        assert self.top <= self.n, ("arena overflow", self.top, self.n)
        v = self.t[:, a:a + words]
        if dt == BF16:
            v = v.bitcast(BF16)[:, 0:per]
        elif dt == I32:
            v = v.bitcast(I32)[:, 0:per]
        else:
            v = v[:, 0:per]
        if len(shape) == 3:
            v = v.rearrange("p (a b) -> p a b", a=shape[1])
        elif len(shape) == 4:
            v = v.rearrange("p (a b c) -> p a b c", a=shape[1], b=shape[2])
        if shape[0] != 128:
            v = v[0:shape[0]]
        return v


def build(upto="all", taps=(), NQT=8):
    nc = bass.Bass("TRN2", target_bir_lowering=False)
    din = lambda name, shape, dt: nc.dram_tensor(name, shape, dt, kind="ExternalInput").ap()
    xf = din("xf", [S, D], F32)
    xo = din("xo", [NOWN, D], F32)
    xh = din("xh", [64, D], F32)
    posf = din("posf", [1, S], I32)
    poso = din("poso", [1, NOWN], I32)
    w_in = din("w_in", [D, 5120], F32)
    b_gate = din("b_gate", [1, 2048], F32)
    lam_in = din("lam_in", [1, 256], F32)
    subln_g = din("subln_g", [1, 128], F32)
    w_o_att = din("w_o_att", [512, D], F32)
    conv_w = din("conv_w", [3, 512], F32)
    w_o_conv = din("w_o_conv", [512, D], F32)
    w_mix = din("w_mix", [D, D], F32)
    ln1 = din("ln1", [2, D], F32)
    ln2 = din("ln2", [2, D], F32)
    w_rt = din("w_rt", [D, 36], F32)
    b_rt = din("b_rt", [1, 36], F32)
    w_eg = din("w_eg", [32, D, 512], F32)
    w_eu = din("w_eu", [32, D, 512], F32)
    w_ed = din("w_ed", [32, 512, D], F32)
    cbf = din("cbf", [128, 384 + 4096], F32)
    cf32 = din("cf32", [128, 512], F32)
    out = nc.dram_tensor("out", [NOWN, D], F32, kind="ExternalOutput").ap()
    h1f = nc.dram_tensor("h1f", [NOWN, D], F32, kind="Internal").ap()
    xs_d = nc.dram_tensor("xs_d", [NSLOT, D], BF16, kind="Internal").ap()
    ys_d = nc.dram_tensor("ys_d", [NSLOT, D], F32, kind="Internal").ap()
    tap_out = {}

    with ExitStack() as es:
        P = Prog(nc, es)
        NW = 51 * 1024
        arena_t = es.enter_context(nc.sbuf_tensor("arena", [128, NW], F32))
        AR = Arena(arena_t, NW)
        psum = es.enter_context(nc.psum_tensor("psum", [128, 8, 512], F32))
        PB = [psum[:, i, :] for i in range(8)]
        PBb = [psum[:, i, :].bitcast(BF16) for i in range(8)]
        finals = []

        def tap(name, ap, waits, dt=F32):
            if name not in taps:
                return
            shp = list(ap.shape)
            o = nc.dram_tensor("tap_" + name, shp, dt, kind="ExternalOutput").ap()
            tap_out[name] = shp
            finals.append(P.dma("sync", o, ap, "tap_" + name, waits=waits))

        cb_t = AR.alloc([128, 384], BF16)
        ident = cb_t[:, 0:128]
        rotm = cb_t[:, 128:256]
        onesb = cb_t[:, 256:384]
        cf_t = AR.alloc([128, 512], F32)
        identf = cf_t[:, 0:128]
        onesf = cf_t[:, 128:256]
        trif = cf_t[:, 256:384]
        invf = cf_t[:, 384:385]
        ecap = cf_t[:, 392:424]
        mhalf = cf_t[:, 424:425]
        epsc = cf_t[:, 425:426]
        d_cb = P.dma("gpsimd", cb_t, cbf[:, 0:384], "cb")
        d_cf = P.dma("sync", cf_t, cf32, "cf")
        QO = AR.alloc([128, 4, NOWN], BF16)
        small = AR.alloc([128, 64], F32)
        neglam = small[:, 0:1]
        gsc = small[:, 1:2]
        lamv = AR.alloc([128, 256], F32)
        slg = AR.alloc([128, 128], F32)
        d_lam = P.dma("sync", lamv, lam_in.to_broadcast([128, 256]), "lam")
        d_slg = P.dma("sync", slg[:, 0:1], subln_g.rearrange("o p -> p o"), "slg", allow_slow_non_contiguous=True)
        lt = small[:, 8:10]
        t_l1 = P.op("vector", lambda e: e.tensor_tensor(out=lamv[:, 0:64], in0=lamv[:, 0:64], in1=lamv[:, 64:128], op=ALU.mult), waits=[d_lam])
        t_l2 = P.op("vector", lambda e: e.tensor_tensor(out=lamv[:, 128:192], in0=lamv[:, 128:192], in1=lamv[:, 192:256], op=ALU.mult), waits=[d_lam])
        t_l3 = P.op("vector", lambda e: e.tensor_reduce(out=lt[:, 0:1], in_=lamv[:, 0:64], axis=AX.X, op=ALU.add), waits=[t_l1])
        t_l4 = P.op("vector", lambda e: e.tensor_reduce(out=lt[:, 1:2], in_=lamv[:, 128:192], axis=AX.X, op=ALU.add), waits=[t_l2])
        t_l5 = P.op("scalar", lambda e: e.activation(out=small[:, 10:12], in_=lt, func=AF.Exp), waits=[t_l3, t_l4])
        t_l6 = P.op("vector", lambda e: e.tensor_tensor(out=small[:, 12:13], in0=small[:, 11:12], in1=small[:, 10:11], op=ALU.subtract), waits=[t_l5])
        t_l7 = P.op("vector", lambda e: e.tensor_scalar(out=neglam, in0=small[:, 12:13], scalar1=-LAMBDA_INIT, scalar2=None, op0=ALU.add), waits=[t_l6])
        t_g = P.op("vector", lambda e: e.tensor_scalar(out=gsc, in0=slg[:, 0:1], scalar1=1.0 - LAMBDA_INIT, scalar2=None, op0=ALU.mult), waits=[d_slg])
        t_consts = [d_cb, d_cf, t_l7, t_g]
        LG = AR.alloc([128, 32, 36], F32)
        PW = AR.alloc([128, 2, 32], F32)
        SLI = AR.alloc([128, 2, 32], I32)
        pers_mark = AR.mark()

        KT = AR.alloc([128, 2, S], BF16)
        maskt_t = AR.alloc([128, 4096], BF16)
        maskt = maskt_t.rearrange("p (r q) -> p r q", r=8)
        d_mask = P.dma("gpsimd", maskt_t, cbf[:, 384:384 + 4096], "mask", max_dma_last_dim=4096)
        VV = AR.alloc([128, 64, 256], BF16)
        wq = AR.alloc([128, 8, 512], BF16)
        wkv = AR.alloc([128, 8, 512], BF16)
        XBt = [AR.alloc([128, 4, D], BF16) for _ in range(2)]
        XTt = [AR.alloc([128, 8, 512], BF16) for _ in range(2)]
        post2 = [AR.alloc([128, 512], F32) for _ in range(2)]
        tq = AR.alloc([128, 512], F32)
        ki = AR.alloc([128, 512], I32)
        cst2 = [AR.alloc([128, 512], F32) for _ in range(2)]
        snt2 = [AR.alloc([128, 512], F32) for _ in range(2)]
        qsb = [AR.alloc([128, 512], BF16) for _ in range(2)]
        ra = [AR.alloc([128, 512], F32) for _ in range(2)]
        rb = [AR.alloc([128, 512], F32) for _ in range(2)]
        PT = [AR.alloc([128, 2, 512], BF16) for _ in range(3)]
        EE = [AR.alloc([128, 512], F32) for _ in range(4)]

        st = {"xb_free": [None, None], "xt_free": [None, None], "tp_free": [None, None],
              "kp_free": [[], []], "rp_free": [None, None], "vp_free": [None, None],
              "tab_free": [[], []], "qs_free": [None, None], "ra_free": [None, None],
              "n_kp": 0, "n_tp": 0, "n_vp": 0, "n_x": 0, "n_tab": 0, "last_sin": None}

        def load_w(dst, src_cols, sem, waits):
            return P.dma("gpsimd", dst, src_cols.rearrange("(c p) n -> p c n", p=128), sem, waits=waits)

        def tables_dma(pos_src, t0):
            ti = st["n_tab"] % 2
            st["n_tab"] += 1
            w0 = list(st["tab_free"][ti])
            d = P.dma("gpsimd", post2[ti], pos_src[0:1, t0:t0 + 512].to_broadcast([128, 512]), "pos%d" % ti, waits=w0)
            return ti, d, w0

        def tables(pos_src, t0):
            return tables_compute(*tables_dma(pos_src, t0))

        def tables_compute(ti, d, w0):
            post = post2[ti]
            prev = [st["last_sin"]]
            for dst, add in ((snt2[ti], 0.0), (cst2[ti], 0.25)):
                a = P.op("vector", lambda e, add=add: e.tensor_scalar(out=tq, in0=post, scalar1=invf, scalar2=add, op0=ALU.mult, op1=ALU.add), waits=[d, d_cf] + prev)
                b = P.op("vector", lambda e: e.tensor_copy(out=ki, in_=tq), waits=[a])
                c = P.op("vector", lambda e: e.tensor_tensor(out=tq, in0=tq, in1=ki, op=ALU.subtract), waits=[b])
                s_ = P.op("scalar", lambda e, dst=dst: e.activation(out=dst, in_=tq, func=AF.Sin, scale=TWO_PI), waits=[c] + w0)
                prev = [s_]
            st["last_sin"] = prev[0]
            return prev[0], ti

        def load_x_tile(src, row0):
            i = st["n_x"] % 2
            st["n_x"] += 1
            xb = XBt[i]
            d = P.dma("gpsimd", xb, src[row0:row0 + 512, :].rearrange("(b p) d -> p b d", p=128),
                      "xb%d" % i, waits=[st["xb_free"][i]])
            return i, d

        def transpose_tile(i, dx):
            xb = XBt[i]
            xt = XTt[i]
            evs = []
            last_t = None
            for g in range(4):
                bk = st["n_tp"] % 2
                st["n_tp"] += 1
                for cc in range(2):
                    c = 2 * g + cc
                    for blk in range(4):
                        last = (cc == 1 and blk == 3)
                        o_ = PBb[bk][:, cc * 512 + blk * 128: cc * 512 + blk * 128 + 128]
                        i_ = xb[:, blk, c * 128:(c + 1) * 128]
                        tk = P.op("tensor", lambda e, o_=o_, i_=i_: e.transpose(out=o_, in_=i_, identity=ident),
                                  waits=[dx, d_cb, st["tp_free"][bk]], signal=last)
                        if last:
                            last_t = tk
                dst = xt[:, 2 * g:2 * g + 2, :].rearrange("p a b -> p (a b)")
                src = PBb[bk]
                if g % 2 == 0:
                    ev = P.op("vector", lambda e, dst=dst, src=src: e.tensor_copy(out=dst, in_=src), waits=[last_t, st["xt_free"][i]])
                else:
                    ev = P.op("scalar", lambda e, dst=dst, src=src: e.copy(out=dst, in_=src), waits=[last_t, st["xt_free"][i]])
                st["tp_free"][bk] = ev
                evs.append(ev)
            st["xb_free"][i] = last_t
            return evs

        def proj_part1(xt, evs, wt, wcol, tab_tok, ti, extra_w):
            kb = st["n_kp"] % 2
            st["n_kp"] += 1
            kp = PB[2 + kb]
            q_ = qsb[kb]
            ra_ = ra[kb]
            cs_ = cst2[ti]
            mm = None
            for c in range(8):
                l_ = wt[:, c, wcol:wcol + 128]
                r_ = xt[:, c, :]
                mm = P.op("tensor", lambda e, l_=l_, r_=r_, c=c: e.matmul(kp, lhsT=l_, rhs=r_, start=(c == 0), stop=(c == 7)),
                          waits=evs + st["kp_free"][kb] + extra_w, signal=(c == 7))
            cp = P.op("scalar", lambda e: e.copy(out=q_, in_=kp), waits=[mm, st["qs_free"][kb]])
            a = P.op("vector", lambda e: e.tensor_tensor(out=ra_, in0=kp, in1=cs_, op=ALU.mult), waits=[mm, cp, tab_tok, st["ra_free"][kb]])
            st["kp_free"][kb] = [a, cp]
            return {"kb": kb, "cp": cp, "a": a, "tab": tab_tok, "ti": ti, "mm": mm}

        def proj_part2(cx, dst):
            kb = cx["kb"]
            rp = PB[4 + kb]
            q_ = qsb[kb]
            ra_ = ra[kb]
            rb_ = rb[kb]
            sn_ = snt2[cx["ti"]]
            rm = P.op("tensor", lambda e: e.matmul(rp, lhsT=rotm, rhs=q_, start=True, stop=True), waits=[cx["cp"], d_cb, st["rp_free"][kb]])
            st["qs_free"][kb] = rm
            b = P.op("vector", lambda e: e.tensor_tensor(out=rb_, in0=rp, in1=sn_, op=ALU.mult), waits=[rm, cx["tab"], st["ra_free"][kb]])
            st["rp_free"][kb] = b
            f = P.op("vector", lambda e: e.tensor_tensor(out=dst, in0=ra_, in1=rb_, op=ALU.add), waits=[cx["a"], b])
            st["ra_free"][kb] = f
            return f, b, rm

        pre = {}

        def prefetch(key, src, pos_src, T):
            i, dx = load_x_tile(src, T * 512)
            tab, ti = tables(pos_src, T * 512)
            pre[key] = (i, dx, tab, ti)

        def prefetch_dma(key, src, pos_src, T):
            i, dx = load_x_tile(src, T * 512)
            return (key, i, dx, tables_dma(pos_src, T * 512))

        def prefetch_compute(pf):
            key, i, dx, td = pf
            tab, ti = tables_compute(*td)
            pre[key] = (i, dx, tab, ti)

        def a_tile(T, dwk, dwv, last, nxt):
            i, dx, tab, ti = pre.pop(("a", T))
            pf = prefetch_dma(*nxt) if nxt is not None else None
            evs = transpose_tile(i, dx)
            cxs = [proj_part1(XTt[i], evs, wkv, hl * 128, tab, ti, [dwk]) for hl in range(2)]
            if pf is not None:
                prefetch_compute(pf)
            mm = None
            for blk in range(4):
                vb = st["n_vp"] % 2
                st["n_vp"] += 1
                vp = PB[6 + vb][:, 0:256]
                for c in range(8):
                    l_ = XTt[i][:, c, blk * 128:(blk + 1) * 128]
                    r_ = wkv[:, c, 256:512]
                    mm = P.op("tensor", lambda e, l_=l_, r_=r_, c=c, vp=vp: e.matmul(vp, lhsT=l_, rhs=r_, start=(c == 0), stop=(c == 7)),
                              waits=evs + [dwv, st["vp_free"][vb]], signal=(c == 7))
                o_ = VV[:, T * 4 + blk, :]
                vts = P.op("scalar", lambda e, o_=o_, vp=vp: e.copy(out=o_, in_=vp), waits=[mm])
                st["vp_free"][vb] = vts
                last.append(vts)
            st["xt_free"][i] = mm
            tabfree = []
            for hl in range(2):
                f, b, rm = proj_part2(cxs[hl], KT[:, hl, T * 512:(T + 1) * 512])
                tabfree.append(b)
                last.append(f)
            st["tab_free"][ti] = tabfree

        def phase_A(hp, then_q):
            dwk = load_w(wkv[:, :, 0:256], w_in[:, 512 + hp * 256: 512 + hp * 256 + 256], "wk", [])
            dwv = load_w(wkv[:, :, 256:512], w_in[:, 1024 + hp * 256: 1024 + hp * 256 + 256], "wv", [])
            last = []
            prefetch(("a", 0), xf, posf, 0)
            for T in range(16):
                if T + 1 < 16:
                    nxt = (("a", T + 1), xf, posf, T + 1)
                elif then_q:
                    nxt = (("q", 0), xo, poso, 0)
                else:
                    nxt = None
                a_tile(T, dwk, dwv, last, nxt)
            return last

        def q_tile(T, dwq, last):
            i, dx, tab, ti = pre.pop(("q", T))
            pf = prefetch_dma(("q", T + 1), xo, poso, T + 1) if T + 1 < NQT else None
            evs = transpose_tile(i, dx)
            tabfree = []
            rm = None
            cxs = {}
            cxs[0] = proj_part1(XTt[i], evs, wq, 0, tab, ti, [dwq])
            if pf is not None:
                prefetch_compute(pf)
            for h in range(4):
                if h + 1 < 4:
                    cxs[h + 1] = proj_part1(XTt[i], evs, wq, (h + 1) * 128, tab, ti, [dwq])
                f, b, rm = proj_part2(cxs[h], QO[:, h, T * 512:(T + 1) * 512])
                tabfree.append(b)
                last.append(f)
            st["tab_free"][ti] = tabfree
            st["xt_free"][i] = rm

        def phase_Q():
            dwq = load_w(wq, w_in[:, 0:512], "wq", [])
            last = []
            for T in range(NQT):
                q_tile(T, dwq, last)
            return last

        ACC0 = ra[0]
        att = {"a0": None, "accL_free": None, "l6_free": None, "s_free": [None, None], "pt_free": [[], [], []], "acc_free": None, "n_s": 0, "n_pt": 0, "ee_free": None, "pending": None}

        def att_unit(i, hl, h):
            nkb = 8 * i + 8
            qk_tok = {}
            qrange = slice(i * 512, (i + 1) * 512)

            def col0(kb):
                r = kb - 8 * i
                return 128 * (r // 2) if r > 0 else 0

            def issue_qk(kb):
                s = kb % 2
                c0 = col0(kb)
                krange = slice(kb * 128, (kb + 1) * 128)
                qr = slice(i * 512 + c0, (i + 1) * 512)
                P.op("tensor", lambda e: e.matmul(psum[:, 2 * s, c0:512], lhsT=KT[0:64, hl, krange], rhs=QO[0:64, h, qr], start=True, stop=True),
                     waits=[att["s_free"][s]], signal=False)
                t = P.op("tensor", lambda e: e.matmul(psum[:, 2 * s + 1, c0:512], lhsT=KT[64:128, hl, krange], rhs=QO[64:128, h, qr], start=True, stop=True))
                qk_tok[kb] = (t, s)

            def exp_step(kb):
                t, s = qk_tok[kb]
                c0 = col0(kb)
                pi = att["n_pt"] % 3
                att["n_pt"] += 1
                pt = PT[pi]
                ex = P.op("scalar", lambda e: e.activation(out=pt[:, :, c0:512], in_=psum[:, 2 * s:2 * s + 2, c0:512], func=AF.Exp, scale=0.125), waits=[t] + att["pt_free"][pi])
                att["s_free"][s] = ex
                return ex, pt, pi

            def pv_step(kb, ex, pt, pi):
                pv_w = ex
                c0 = col0(kb)
                if kb >= 8 * i:
                    r = kb - 8 * i
                    pv_w = P.op("vector", lambda e: e.tensor_tensor(out=pt[:, :, c0:512], in0=pt[:, :, c0:512],
                                                                      in1=maskt[:, r, c0:512].unsqueeze(1).to_broadcast([128, 2, 512 - c0]), op=ALU.mult),
                                waits=[ex, d_mask])
                s0 = (kb == 0)
                s1 = (kb == nkb - 1)
                w0 = [pv_w, att["acc_free"]] if kb == 0 else [pv_w]
                vv = VV[:, kb, hl * 128:(hl + 1) * 128]
                P.op("tensor", lambda e: e.matmul(PB[4][:, c0:512], lhsT=vv, rhs=pt[:, 0, c0:512], start=s0, stop=s1), waits=w0, signal=False)
                P.op("tensor", lambda e: e.matmul(PB[5][:, c0:512], lhsT=vv, rhs=pt[:, 1, c0:512], start=s0, stop=s1), signal=False)
                pv = P.op("tensor", lambda e: e.matmul(PB[7][:, c0:512], lhsT=onesb, rhs=pt[:, 1, c0:512], start=s0, stop=s1))
                if kb == 0:
                    a0 = P.op("vector", lambda e: e.tensor_copy(out=ACC0, in_=pt[:, 0, :]), waits=[pv_w, att["accL_free"]])
                else:
                    a0 = P.op("vector", lambda e: e.tensor_tensor(out=ACC0[:, c0:512], in0=ACC0[:, c0:512], in1=pt[:, 0, c0:512], op=ALU.add), waits=[pv_w, att["a0"]])
                att["a0"] = a0
                att["pt_free"][pi] = [pv, a0]
                return pv

            issue_qk(0)
            issue_qk(1)
            pv = None
            for kb in range(nkb):
                ex, pt, pi = exp_step(kb)
                if kb + 2 < nkb:
                    issue_qk(kb + 2)
                pv = pv_step(kb, ex, pt, pi)
                if kb == 2 and att["pending"] is not None:
                    att["pending"]()
                    att["pending"] = None
            wfree = [att["ee_free"]]
            lsum = P.op("tensor", lambda e: e.matmul(PB[6], lhsT=onesf, rhs=ACC0, start=True, stop=True), waits=[att["a0"], att["acc_free"], att["l6_free"], d_cf])
            att["accL_free"] = lsum
            e0 = P.op("vector", lambda e: e.reciprocal(out=EE[0], in_=PB[6]), waits=[pv, lsum] + wfree)
            e1 = P.op("vector", lambda e: e.reciprocal(out=EE[1], in_=PB[7]), waits=[pv, lsum] + wfree)
            e2 = P.op("vector", lambda e: e.tensor_tensor(out=EE[0], in0=PB[4], in1=EE[0], op=ALU.mult), waits=[e0])
            e3 = P.op("vector", lambda e: e.tensor_tensor(out=EE[1], in0=PB[5], in1=EE[1], op=ALU.mult), waits=[e1])
            att["acc_free"] = e3
            e4 = P.op("vector", lambda e: e.scalar_tensor_tensor(out=EE[2], in0=EE[1], scalar=neglam, in1=EE[0], op0=ALU.mult, op1=ALU.add), waits=[e2, e3, t_l7] + wfree)
            e5 = P.op("gpsimd", lambda e: e.tensor_tensor(out=EE[3], in0=EE[2], in1=EE[2], op=ALU.mult), waits=[e4] + wfree)

            def finish():
                ss = P.op("tensor", lambda e: e.matmul(PB[6], lhsT=onesf, rhs=EE[3], start=True, stop=True), waits=[e5, e3, d_cf, att["l6_free"]])
                e6 = P.op("scalar", lambda e: e.activation(out=EE[3], in_=PB[6], func=AF.Ln, scale=1.0 / 128.0, bias=epsc), waits=[ss])
                att["l6_free"] = e6
                e7 = P.op("scalar", lambda e: e.activation(out=EE[3], in_=EE[3], func=AF.Exp, scale=-0.5), waits=[e6])
                e8 = P.op("vector", lambda e: e.tensor_tensor(out=EE[2], in0=EE[2], in1=EE[3], op=ALU.mult), waits=[e7])
                e9 = P.op("vector", lambda e: e.tensor_scalar(out=QO[:, h, qrange], in0=EE[2], scalar1=gsc, scalar2=None, op0=ALU.mult), waits=[e8, t_g])
                att["ee_free"] = e9
                att["last"] = e9
            att["pending"] = finish

        def attention(hp):
            for i in range(NQT):
                for hl in range(2):
                    att_unit(i, hl, 2 * hp + hl)
            att["pending"]()
            att["pending"] = None
            return att["last"]

        att_last = None
        if upto == "consts":
            tap("small", small, t_consts)
            P.emit(finals)
            return nc, tap_out
        for hp in range(2):
            kvt = phase_A(hp, hp == 0)
            if upto == "A":
                tap("kt", KT[:, :, 0:2048], kvt, BF16)
                tap("vv", VV[:, 0:8, :], kvt, BF16)
                P.emit(finals)
                return nc, tap_out
            qtoks = phase_Q() if hp == 0 else []
            P.base_waits = [t for t in kvt + qtoks if t is not None] + t_consts
            att_last = attention(hp)
            P.base_waits = [att_last]
            if upto == "att0":
                break
        tap("qo", QO, [att_last], BF16)
        tap("kt", KT[:, :, 0:2048], [att_last], BF16)
        tap("vv", VV[:, 0:8, :], [att_last], BF16)

        if upto in ("att0", "att"):
            P.emit(finals)
            return nc, tap_out

        AR.reset(pers_mark)
        wp = AR.alloc([128, 8, 3584], BF16)
        woa = AR.alloc([128, 4, 1024], BF16)
        woc = AR.alloc([128, 4, 1024], BF16)
        wmx = AR.alloc([128, 8, 1024], BF16)
        xb2_ = [AR.alloc([128, 2, 1024], BF16) for _ in range(2)]
        xhb_ = [AR.alloc([128, 1024], BF16) for _ in range(2)]
        xres_ = [AR.alloc([128, 2, 1024], F32) for _ in range(2)]
        xT2 = AR.alloc([128, 8, 256], BF16)
        xhT = AR.alloc([128, 32], BF16)
        ccs_ = [AR.alloc([128, 264], F32) for _ in range(2)]
        Ub_ = [AR.alloc([128, 2, 130], F32) for _ in range(2)]
        T1_ = [AR.alloc([128, 2, 128], F32) for _ in range(2)]
        Zb = AR.alloc([128, 4, 256], BF16)
        g0_ = [AR.alloc([128, 256], F32) for _ in range(2)]
        g1_ = [AR.alloc([128, 256], F32) for _ in range(2)]
        m0_ = [AR.alloc([128, 256], F32) for _ in range(2)]
        m1_ = [AR.alloc([128, 256], F32) for _ in range(2)]
        MT = AR.alloc([128, 8, 256], BF16)
        Rb_ = [AR.alloc([128, 1024], F32) for _ in range(2)]
        lng = AR.alloc([128, 1024], F32)
        lnb = AR.alloc([128, 1024], F32)
        H1T = AR.alloc([128, 8, 128], F32)
        stt_b = AR.alloc([128, 16], F32)
        bgs = AR.alloc([128, 16], F32)
        cws = AR.alloc([128, 12], F32)
        rbias = AR.alloc([128, 36], F32)
        wr = AR.alloc([128, 8, 36], F32)
        for i7 in range(7):
            P.adma("gpsimd", wp[:, :, i7 * 512:(i7 + 1) * 512], w_in[:, 1536 + i7 * 512:1536 + (i7 + 1) * 512].rearrange("(c p) n -> p c n", p=128), "wp", writes=["wp"])
        P.adma("gpsimd", woa, w_o_att.rearrange("(c p) n -> p c n", p=128), "woa", writes=["woa"])
        P.adma("gpsimd", woc, w_o_conv.rearrange("(c p) n -> p c n", p=128), "woc", writes=["woc"])
        P.adma("gpsimd", wmx, w_mix.rearrange("(c p) n -> p c n", p=128), "wmx", writes=["wmx"])
        P.adma("sync", wr, w_rt.rearrange("(c p) n -> p c n", p=128), "wr", writes=["wr"])
        P.adma("sync", bgs, b_gate.rearrange("o (q p) -> p (o q)", p=128), "bgs", writes=["bgs"], allow_slow_non_contiguous=True)
        P.adma("sync", cws.rearrange("p (k q) -> p k q", k=3), conv_w.rearrange("k (q p) -> p k q", p=128), "cws", writes=["cws"], allow_slow_non_contiguous=True)
        P.adma("sync", rbias, b_rt.to_broadcast([128, 36]), "rbias", writes=["rbias"])
        P.adma("sync", lng, ln1[0:1, :].to_broadcast([128, 1024]), "lng", writes=["lng"])
        P.adma("sync", lnb, ln1[1:2, :].to_broadcast([128, 1024]), "lnb", writes=["lnb"])

        def layer_norm(buf, gname, bname, gt, bt, stt=None, rn="R"):
            stt = stt_b if stt is None else stt
            mv = stt[:, 12:14]
            ve = stt[:, 14:15]
            rs = stt[:, 15:16]
            P.auto("vector", lambda e: e.bn_stats(out=stt[:, 0:6], in_=buf[:, 0:512]), reads=[rn], writes=["stt"])
            P.auto("vector", lambda e: e.bn_stats(out=stt[:, 6:12], in_=buf[:, 512:1024]), reads=[rn], writes=["stt2"])
            P.auto("vector", lambda e: e.bn_aggr(out=mv, in_=stt[:, 0:12]), reads=["stt", "stt2"], writes=["mv"])
            P.auto("vector", lambda e: e.tensor_scalar(out=ve, in0=mv[:, 1:2], scalar1=1e-5, scalar2=None, op0=ALU.add), reads=["mv"], writes=["ve"])
            P.auto("gpsimd", lambda e: e.tensor_tensor(out=rs, in0=ve, in1=mhalf, op=ALU.pow), reads=["ve"], writes=["rs"], extra=[d_cf])
            P.auto("vector", lambda e: e.tensor_scalar(out=buf, in0=buf, scalar1=mv[:, 0:1], scalar2=rs, op0=ALU.subtract, op1=ALU.mult), reads=["mv", "rs"], writes=[rn])
            P.auto("vector", lambda e: e.tensor_tensor(out=buf, in0=buf, in1=gt, op=ALU.mult), reads=[gname], writes=[rn])
            return P.auto("gpsimd", lambda e: e.tensor_tensor(out=buf, in0=buf, in1=bt, op=ALU.add), reads=[bname], writes=[rn])

        def b_loads(tb):
            r0 = tb * 256
            pb = tb % 2
            P.adma("gpsimd", xb2_[pb], xo[r0:r0 + 256, :].rearrange("(b p) d -> p b d", p=128), "xb2%d" % pb, writes=["xb2%d" % pb])
            P.adma("gpsimd", xhb_[pb][0:4, :], xh[tb * 4:tb * 4 + 4, :], "xhb%d" % pb, writes=["xhb%d" % pb])
            P.adma("sync", xres_[pb], xo[r0:r0 + 256, :].rearrange("(b p) d -> p b d", p=128), "xres%d" % pb, writes=["xres%d" % pb])

        def b_tile(tb):
            r0 = tb * 256
            pb = tb % 2
            xb2, xhb, xres = xb2_[pb], xhb_[pb], xres_[pb]
            n_xb2, n_xhb, n_xres = "xb2%d" % pb, "xhb%d" % pb, "xres%d" % pb
            if tb + 1 < NTB:
                b_loads(tb + 1)
            for half in range(2):
                fns = []
                for cc in range(4):
                    c = half * 4 + cc
                    for blk in range(2):
                        o_ = PBb[half][:, cc * 256 + blk * 128: cc * 256 + blk * 128 + 128]
                        i_ = xb2[:, blk, c * 128:(c + 1) * 128]
                        fns.append(lambda e, o_=o_, i_=i_: e.transpose(out=o_, in_=i_, identity=ident))
                P.auto("tensor", fns, reads=[n_xb2], psum=["b%d" % half], extra=[d_cb])
                dst = xT2[:, half * 4:half * 4 + 4, :].rearrange("p a b -> p (a b)")
                if half == 0:
                    P.auto("vector", lambda e, dst=dst: e.tensor_copy(out=dst, in_=PBb[0]), writes=["xT2a"], psum=["b0"])
                else:
                    P.auto("scalar", lambda e, dst=dst: e.copy(out=dst, in_=PBb[1]), writes=["xT2b"], psum=["b1"])
            fns = []
            for c in range(8):
                o_ = PBb[6][:, c * 4:(c + 1) * 4]
                i_ = xhb[0:4, c * 128:(c + 1) * 128]
                fns.append(lambda e, o_=o_, i_=i_: e.transpose(out=o_, in_=i_, identity=ident[0:4, 0:4]))
            P.auto("tensor", fns, reads=[n_xhb], psum=["b6"], extra=[d_cb])
            P.auto("vector", lambda e: e.tensor_copy(out=xhT, in_=PBb[6][:, 0:32]), writes=["xhT"], psum=["b6"])
            xhT3 = xhT.rearrange("p (c k) -> p c k", c=8)
            for q in range(4):
                qp = q % 2
                BA, BB = PB[2 + 2 * qp], PB[3 + 2 * qp]
                nBA, nBB = "b%d" % (2 + 2 * qp), "b%d" % (3 + 2 * qp)
                ccs, Ub, T1 = ccs_[qp], Ub_[qp], T1_[qp]
                nccs, nU, nU2, nT1 = "ccs%d" % qp, "U%d" % qp, "U2%d" % qp, "T1%d" % qp
                fns = []
                for c in range(8):
                    l_ = wp[:, c, 512 + q * 128: 512 + q * 128 + 128]
                    fns.append(lambda e, l_=l_, c=c, BA=BA: e.matmul(BA[:, 0:256], lhsT=l_, rhs=xT2[:, c, :], start=(c == 0), stop=(c == 7)))
                for c in range(8):
                    l_ = wp[:, c, 512 + q * 128: 512 + q * 128 + 128]
                    fns.append(lambda e, l_=l_, c=c, BA=BA: e.matmul(BA[:, 256:260], lhsT=l_, rhs=xhT3[:, c, :], start=(c == 0), stop=(c == 7)))
                for c in range(8):
                    l_ = wp[:, c, 1024 + q * 128: 1024 + q * 128 + 128]
                    fns.append(lambda e, l_=l_, c=c, BA=BA: e.matmul(BA[:, 260:264], lhsT=l_, rhs=xhT3[:, c, :], start=(c == 0), stop=(c == 7)))
                P.auto("tensor", fns, reads=["wp", "xT2a", "xT2b", "xhT"], psum=[nBA])
                fns = []
                for c in range(8):
                    l_ = wp[:, c, 1024 + q * 128: 1024 + q * 128 + 128]
                    fns.append(lambda e, l_=l_, c=c, BB=BB: e.matmul(BB[:, 0:256], lhsT=l_, rhs=xT2[:, c, :], start=(c == 0), stop=(c == 7)))
                for c in range(8):
                    l_ = wp[:, c, q * 128: q * 128 + 128]
                    fns.append(lambda e, l_=l_, c=c, BB=BB: e.matmul(BB[:, 256:512], lhsT=l_, rhs=xT2[:, c, :], start=(c == 0), stop=(c == 7)))
                P.auto("tensor", fns, reads=["wp", "xT2a", "xT2b"], psum=[nBB])
                P.auto("scalar", lambda e, ccs=ccs, BA=BA: e.copy(out=ccs, in_=BA[:, 0:264]), writes=[nccs], psum=[nBA])
                P.auto("vector", lambda e, ccs=ccs, Ub=Ub, BB=BB: e.tensor_tensor(out=Ub[:, :, 2:130], in0=ccs[:, 0:256].rearrange("p (b t) -> p b t", b=2),
                                                          in1=BB[:, 0:256].rearrange("p (b t) -> p b t", b=2), op=ALU.mult),
                       reads=[nccs], writes=[nU], psum=[nBB])
                P.auto("vector", lambda e, ccs=ccs, Ub=Ub: e.tensor_tensor(out=Ub[:, :, 0:2], in0=ccs[:, 256:260].rearrange("p (b t) -> p b t", b=2),
                                                          in1=ccs[:, 260:264].rearrange("p (b t) -> p b t", b=2), op=ALU.mult),
                       reads=[nccs], writes=[nU2])
                P.auto("vector", lambda e, q=q, T1=T1, Ub=Ub: e.tensor_scalar(out=T1, in0=Ub[:, :, 0:128], scalar1=cws[:, q:q + 1], scalar2=None, op0=ALU.mult),
                       reads=[nU, nU2, "cws"], writes=[nT1])
                P.auto("vector", lambda e, q=q, T1=T1, Ub=Ub: e.scalar_tensor_tensor(out=T1, in0=Ub[:, :, 1:129], scalar=cws[:, 4 + q:5 + q], in1=T1, op0=ALU.mult, op1=ALU.add),
                       reads=[nU, nU2], writes=[nT1])
                P.auto("vector", lambda e, q=q, T1=T1, Ub=Ub: e.scalar_tensor_tensor(out=T1, in0=Ub[:, :, 2:130], scalar=cws[:, 8 + q:9 + q], in1=T1, op0=ALU.mult, op1=ALU.add),
                       reads=[nU, nU2], writes=[nT1])
                P.auto("vector", lambda e, q=q, T1=T1, BB=BB: e.tensor_tensor(out=Zb[:, q, :], in0=T1.rearrange("p b t -> p (b t)"), in1=BB[:, 256:512], op=ALU.mult),
                       reads=[nT1], writes=["Z%d" % q], psum=[nBB])
            flush_routers()
            for c8 in range(8):
                cp_ = c8 % 2
                BG, BY = PB[2 + 2 * cp_], PB[3 + 2 * cp_]
                nBG, nBY = "b%d" % (2 + 2 * cp_), "b%d" % (3 + 2 * cp_)
                g0, g1, m0, m1 = g0_[cp_], g1_[cp_], m0_[cp_], m1_[cp_]
                ng0, ng1, nm0, nm1 = "g0%d" % cp_, "g1%d" % cp_, "m0%d" % cp_, "m1%d" % cp_
                fns = []
                for c in range(8):
                    l_ = wp[:, c, 1536 + c8 * 128: 1536 + c8 * 128 + 128]
                    fns.append(lambda e, l_=l_, c=c, BG=BG: e.matmul(BG[:, 0:256], lhsT=l_, rhs=xT2[:, c, :], start=(c == 0), stop=(c == 7)))
                for c in range(8):
                    l_ = wp[:, c, 2560 + c8 * 128: 2560 + c8 * 128 + 128]
                    fns.append(lambda e, l_=l_, c=c, BG=BG: e.matmul(BG[:, 256:512], lhsT=l_, rhs=xT2[:, c, :], start=(c == 0), stop=(c == 7)))
                P.auto("tensor", fns, reads=["wp", "xT2a", "xT2b"], psum=[nBG])
                P.auto("scalar", lambda e, c8=c8, g0=g0, BG=BG: e.activation(out=g0, in_=BG[:, 0:256], func=AF.Sigmoid, bias=bgs[:, c8:c8 + 1]), reads=["bgs"], writes=[ng0], psum=[nBG])
                P.auto("scalar", lambda e, c8=c8, g1=g1, BG=BG: e.activation(out=g1, in_=BG[:, 256:512], func=AF.Sigmoid, bias=bgs[:, 8 + c8:9 + c8]), reads=["bgs"], writes=[ng1], psum=[nBG])
                fns = []
                for h in range(4):
                    l_ = woa[:, h, c8 * 128:(c8 + 1) * 128]
                    r_ = QO[:, h, r0:r0 + 256]
                    fns.append(lambda e, l_=l_, r_=r_, h=h, BY=BY: e.matmul(BY[:, 0:256], lhsT=l_, rhs=r_, start=(h == 0), stop=(h == 3)))
                for q in range(4):
                    l_ = woc[:, q, c8 * 128:(c8 + 1) * 128]
                    r_ = Zb[:, q, :]
                    fns.append(lambda e, l_=l_, r_=r_, q=q, BY=BY: e.matmul(BY[:, 256:512], lhsT=l_, rhs=r_, start=(q == 0), stop=(q == 3)))
                P.auto("tensor", fns, reads=["woa", "woc", "Z0", "Z1", "Z2", "Z3"], psum=[nBY])
                P.auto("vector", lambda e, m0=m0, g0=g0, BY=BY: e.tensor_tensor(out=m0, in0=g0, in1=BY[:, 0:256], op=ALU.mult), reads=[ng0], writes=[nm0], psum=[nBY])
                P.auto("vector", lambda e, m1=m1, g1=g1, BY=BY: e.tensor_tensor(out=m1, in0=g1, in1=BY[:, 256:512], op=ALU.mult), reads=[ng1], writes=[nm1], psum=[nBY])
                P.auto("vector", lambda e, c8=c8, m0=m0, m1=m1: e.tensor_tensor(out=MT[:, c8, :], in0=m0, in1=m1, op=ALU.add), reads=[nm0, nm1], writes=["MT%d" % c8])
            for blk in range(2):
                gb = tb * 2 + blk
                Rb = Rb_[blk]
                rn = "R%d" % blk
                fns = []
                for half in range(2):
                    for c in range(8):
                        l_ = MT[:, c, blk * 128:(blk + 1) * 128]
                        r_ = wmx[:, c, half * 512:(half + 1) * 512]
                        fns.append(lambda e, l_=l_, r_=r_, c=c, half=half: e.matmul(PB[6 + half], lhsT=l_, rhs=r_, start=(c == 0), stop=(c == 7)))
                P.auto("tensor", fns, reads=["MT%d" % c_ for c_ in range(8)] + ["wmx"], psum=["b6", "b7"])
                P.auto("vector", lambda e, blk=blk, Rb=Rb: e.scalar_tensor_tensor(out=Rb[:, 0:512], in0=xres[:, blk, 0:512], scalar=ALPHA, in1=PB[6], op0=ALU.mult, op1=ALU.add),
                       reads=[n_xres], writes=[rn], psum=["b6"])
                P.auto("vector", lambda e, blk=blk, Rb=Rb: e.scalar_tensor_tensor(out=Rb[:, 512:1024], in0=xres[:, blk, 512:1024], scalar=ALPHA, in1=PB[7], op0=ALU.mult, op1=ALU.add),
                       reads=[n_xres], writes=[rn], psum=["b7"])
                layer_norm(Rb, "lng", "lnb", lng, lnb, None, rn)
                finals_h1.append(P.adma("sync", h1f[gb * 128:(gb + 1) * 128, :], Rb, "h1w%d" % blk, reads=[rn]))
                pend_r.append(make_router(Rb, rn, gb))

        def make_router(Rb, rn, gb):
            def run():
                fns = []
                for c in range(8):
                    o_ = PB[c // 4][:, (c % 4) * 128:(c % 4) * 128 + 128]
                    i_ = Rb[:, c * 128:(c + 1) * 128]
                    fns.append(lambda e, o_=o_, i_=i_: e.transpose(out=o_, in_=i_, identity=identf))
                P.auto("tensor", fns, reads=[rn], psum=["b0", "b1"], extra=[d_cf])
                P.auto("scalar", lambda e: e.copy(out=H1T[:, 0:4, :].rearrange("p a b -> p (a b)"), in_=PB[0]), writes=["H1Ta"], psum=["b0"])
                P.auto("vector", lambda e: e.tensor_copy(out=H1T[:, 4:8, :].rearrange("p a b -> p (a b)"), in_=PB[1]), writes=["H1Tb"], psum=["b1"])
                fns = []
                for c in range(8):
                    fns.append(lambda e, c=c: e.matmul(PB[0][:, 0:36], lhsT=H1T[:, c, :], rhs=wr[:, c, :], start=(c == 0), stop=(c == 7)))
                P.auto("tensor", fns, reads=["H1Ta", "H1Tb", "wr"], psum=["b0"])
                P.auto("vector", lambda e: e.tensor_tensor(out=LG[:, gb, :], in0=PB[0][:, 0:36], in1=rbias, op=ALU.add), reads=["rbias"], writes=["LG"], psum=["b0"])
            return run

        pend_r = []

        def flush_routers():
            while pend_r:
                pend_r.pop(0)()

        finals_h1 = []
        NTB = NQT * 2
        b_loads(0)
        for tb in range(NTB):
            b_tile(tb)
        flush_routers()
        tap("lg", LG, [P.lw["LG"]])
        tap("h1", h1f[0:256, :], finals_h1)
        if upto == "B":
            P.emit(finals + finals_h1)
            return nc, tap_out

        P.base_waits = [P.lw["LG"]] + finals_h1
        AR.reset(pers_mark)
        NB = NTB * 2
        gm = AR.alloc([128, 32], F32)
        gd = AR.alloc([128, 32, 4], F32)
        pen = AR.alloc([128, 32, 4], F32)
        ge = AR.alloc([128, 32, 4], F32)
        gs = AR.alloc([128, 32], F32)
        gw = AR.alloc([128, 32], F32)
        em = AR.alloc([128, 32, 32], F32)
        em2 = AR.alloc([128, 32, 32], F32)
        oh1 = AR.alloc([128, 32, 32], F32)
        oh2 = AR.alloc([128, 32, 32], F32)
        Aa = AR.alloc([128, 32, 32], F32)
        Tt = AR.alloc([128, 32, 32], F32)
        sc0 = AR.alloc([128, 32, 32], F32)
        sc1 = AR.alloc([128, 32, 32], F32)
        rank = AR.alloc([128, 32, 32], F32)
        tmpc = AR.alloc([128, 32, 32], F32)
        v1 = AR.alloc([128, 32], F32)
        v2 = AR.alloc([128, 32], F32)
        dv = AR.alloc([128, 32], F32)
        s12 = AR.alloc([128, 2, 32], F32)
        stt_c = AR.alloc([128, 16], F32)
        fl = lambda t: t.rearrange("p a b -> p (a b)")
        gl = LG[:, :, 0:4]
        el = LG[:, :, 4:36]
        bc3 = lambda t: t.unsqueeze(2).to_broadcast([128, 32, 32])
        V = "vector"
        P.auto(V, lambda e: e.tensor_reduce(out=gm, in_=gl, axis=AX.X, op=ALU.max), reads=["LG"], writes=["gm"])
        P.auto(V, lambda e: e.tensor_tensor(out=gd, in0=gl, in1=gm.unsqueeze(2).to_broadcast([128, 32, 4]), op=ALU.subtract), reads=["LG", "gm"], writes=["gd"])
        P.auto(V, lambda e: e.tensor_scalar(out=pen, in0=gd, scalar1=0.0, scalar2=NEG, op0=ALU.is_lt, op1=ALU.mult), reads=["gd"], writes=["pen"])
        P.auto("scalar", lambda e: e.activation(out=ge, in_=gd, func=AF.Exp), reads=["gd"], writes=["ge"])
        P.auto(V, lambda e: e.tensor_reduce(out=gs, in_=ge, axis=AX.X, op=ALU.add), reads=["ge"], writes=["gs"])
        P.auto(V, lambda e: e.reciprocal(out=gw, in_=gs), reads=["gs"], writes=["gw"])
        P.auto(V, lambda e: e.tensor_tensor(out=em.rearrange("p b (g k) -> p b g k", g=4), in0=el.rearrange("p b (g k) -> p b g k", g=4),
                                            in1=pen.unsqueeze(3).to_broadcast([128, 32, 4, 8]), op=ALU.add), reads=["LG", "pen"], writes=["em"])
        P.auto(V, lambda e: e.tensor_reduce(out=v1, in_=em, axis=AX.X, op=ALU.max), reads=["em"], writes=["v1"])
        P.auto(V, lambda e: e.tensor_tensor(out=oh1, in0=em, in1=bc3(v1), op=ALU.is_equal), reads=["em", "v1"], writes=["oh1"])
        P.auto(V, lambda e: e.scalar_tensor_tensor(out=fl(em2), in0=fl(oh1), scalar=NEG, in1=fl(em), op0=ALU.mult, op1=ALU.add), reads=["oh1", "em"], writes=["em2"])
        P.auto(V, lambda e: e.tensor_reduce(out=v2, in_=em2, axis=AX.X, op=ALU.max), reads=["em2"], writes=["v2"])
        P.auto(V, lambda e: e.tensor_tensor(out=oh2, in0=em2, in1=bc3(v2), op=ALU.is_equal), reads=["em2", "v2"], writes=["oh2"])
        P.auto(V, lambda e: e.tensor_tensor(out=dv, in0=v2, in1=v1, op=ALU.subtract), reads=["v1", "v2"], writes=["dv"])
        P.auto("scalar", lambda e: e.activation(out=dv, in_=dv, func=AF.Exp), reads=[], writes=["dv"])
        P.auto(V, lambda e: e.tensor_scalar(out=dv, in0=dv, scalar1=1.0, scalar2=None, op0=ALU.add), writes=["dv"])
        P.auto(V, lambda e: e.reciprocal(out=dv, in_=dv), writes=["dv"])
        P.auto(V, lambda e: e.tensor_tensor(out=PW[:, 0, :], in0=dv, in1=gw, op=ALU.mult), reads=["dv", "gw"], writes=["PW0"])
        P.auto(V, lambda e: e.tensor_tensor(out=PW[:, 1, :], in0=gw, in1=PW[:, 0, :], op=ALU.subtract), reads=["PW0", "gw"], writes=["PW1"])
        P.auto(V, lambda e: e.tensor_tensor(out=fl(Aa), in0=fl(oh1), in1=fl(oh2), op=ALU.add), reads=["oh1", "oh2"], writes=["Aa"])
        Af = fl(Aa)
        P.auto("tensor", [lambda e: e.matmul(PB[0], lhsT=trif, rhs=Af[:, 0:512], start=True, stop=True),
                          lambda e: e.matmul(PB[1], lhsT=trif, rhs=Af[:, 512:1024], start=True, stop=True),
                          lambda e: e.matmul(PB[2], lhsT=onesf, rhs=Af[:, 0:512], start=True, stop=True),
                          lambda e: e.matmul(PB[3], lhsT=onesf, rhs=Af[:, 512:1024], start=True, stop=True)],
               reads=["Aa"], psum=["b0", "b1", "b2", "b3"], extra=[d_cf])
        P.auto(V, lambda e: e.tensor_copy(out=fl(Tt)[:, 0:512], in_=PB[2]), writes=["Tt"], psum=["b2"])
        P.auto(V, lambda e: e.tensor_copy(out=fl(Tt)[:, 512:1024], in_=PB[3]), writes=["Tt"], psum=["b3"])
        cur, cname = Tt, "Tt"
        for si, sh in enumerate((1, 2, 4, 8, 16)):
            nxt, nname = (sc0, "sc0") if si % 2 == 0 else (sc1, "sc1")
            P.auto(V, lambda e, cur=cur, nxt=nxt, sh=sh: e.tensor_tensor(out=nxt[:, sh:32, :], in0=cur[:, sh:32, :], in1=cur[:, 0:32 - sh, :], op=ALU.add), reads=[cname], writes=[nname])
            P.auto(V, lambda e, cur=cur, nxt=nxt, sh=sh: e.tensor_copy(out=nxt[:, 0:sh, :], in_=cur[:, 0:sh, :]), reads=[cname], writes=[nname])
            cur, cname = nxt, nname
        P.auto(V, lambda e, cur=cur: e.tensor_tensor(out=fl(tmpc), in0=fl(cur), in1=fl(Tt), op=ALU.subtract), reads=[cname, "Tt"], writes=["tmpc"])
        P.auto(V, lambda e: e.tensor_tensor(out=fl(rank)[:, 0:512], in0=fl(tmpc)[:, 0:512], in1=PB[0], op=ALU.add), reads=["tmpc"], writes=["rank"], psum=["b0"])
        P.auto(V, lambda e: e.tensor_tensor(out=fl(rank)[:, 512:1024], in0=fl(tmpc)[:, 512:1024], in1=PB[1], op=ALU.add), reads=["tmpc"], writes=["rank"], psum=["b1"])
        P.auto(V, lambda e: e.tensor_scalar(out=fl(rank), in0=fl(rank), scalar1=float(CAP - 1), scalar2=None, op0=ALU.min), writes=["rank"])
        P.auto(V, lambda e: e.tensor_tensor(out=rank, in0=rank, in1=ecap.unsqueeze(1).to_broadcast([128, 32, 32]), op=ALU.add), writes=["rank"], extra=[d_cf])
        P.auto(V, lambda e: e.tensor_tensor(out=fl(tmpc), in0=fl(oh1), in1=fl(rank), op=ALU.mult), reads=["oh1", "rank"], writes=["tmpc"])
        P.auto(V, lambda e: e.tensor_reduce(out=s12[:, 0, :], in_=tmpc, axis=AX.X, op=ALU.add), reads=["tmpc"], writes=["s12a"])
        P.auto(V, lambda e: e.tensor_tensor(out=fl(tmpc), in0=fl(oh2), in1=fl(rank), op=ALU.mult), reads=["oh2", "rank"], writes=["tmpc"])
        P.auto(V, lambda e: e.tensor_reduce(out=s12[:, 1, :], in_=tmpc, axis=AX.X, op=ALU.add), reads=["tmpc"], writes=["s12b"])
        P.auto(V, lambda e: e.tensor_scalar(out=fl(s12), in0=fl(s12), scalar1=float(NSLOT - 1), scalar2=0.0, op0=ALU.min, op1=ALU.max), reads=["s12a", "s12b"], writes=["s12a", "s12b"])
        P.auto(V, lambda e: e.tensor_copy(out=SLI, in_=s12), reads=["s12a", "s12b"], writes=["SLI"])
        tap("sli", SLI, [P.lw["SLI"]], I32)
        tap("pw", PW, [P.lw["PW1"]])

        P.base_waits = [P.lw["SLI"], P.lw["PW1"], P.lw["PW0"]] + finals_h1
        AR.reset(pers_mark)
        stt_c = AR.alloc([128, 16], F32)
        wg = [AR.alloc([128, 8, 512], BF16) for _ in range(2)]
        wu = [AR.alloc([128, 8, 512], BF16) for _ in range(2)]
        wd = [AR.alloc([128, 4, 1024], BF16) for _ in range(2)]

        def e_loads_w(e_):
            i = e_ % 2
            P.adma("gpsimd", wg[i], w_eg[e_].rearrange("(c p) n -> p c n", p=128), "wg%d" % i, writes=["wg%d" % i])
            P.adma("gpsimd", wu[i], w_eu[e_].rearrange("(c p) n -> p c n", p=128), "wu%d" % i, writes=["wu%d" % i])
            P.adma("gpsimd", wd[i], w_ed[e_].rearrange("(c p) n -> p c n", p=128), "wd%d" % i, writes=["wd%d" % i])

        e_loads_w(0)
        hb = [AR.alloc([128, 1024], F32) for _ in range(2)]
        sc_toks = []
        for blk in range(NB):
            i = blk % 2
            P.adma("sync", hb[i], h1f[blk * 128:(blk + 1) * 128, :], "hb%d" % i, writes=["hb%d" % i])
            for k in range(2):
                off = SLI[:, k, blk:blk + 1].bitcast(U32)
                src = hb[i]
                sc_toks.append(P.acdma("gpsimd", lambda e, off=off, src=src: e.indirect_dma_start(
                    out=xs_d, out_offset=bass.IndirectOffsetOnAxis(ap=off, axis=0), in_=src, in_offset=None),
                    "sc%d_%d" % (i, k), reads=["hb%d" % i, "SLI"]))

        XE = [AR.alloc([128, 3, 1024], BF16) for _ in range(2)]
        XT = AR.alloc([128, 8, 384], BF16)
        sg = AR.alloc([128, 384], F32)
        HT = AR.alloc([128, 4, 384], BF16)
        YSb = [AR.alloc([128, 1024], F32) for _ in range(2)]
        NE = 32
        ys_toks = []

        def e_loads(e_):
            i = e_ % 2
            if e_ > 0:
                e_loads_w(e_)
            P.adma("sync", XE[i], xs_d[e_ * CAP:(e_ + 1) * CAP, :].rearrange("(b p) d -> p b d", p=128), "xe%d" % i, writes=["xe%d" % i], extra=sc_toks)

        def e_compute(e_):
            i = e_ % 2
            for pr in range(4):
                bank = pr % 2
                fns = []
                for cc in range(2):
                    c = 2 * pr + cc
                    for sb in range(3):
                        o_ = PBb[bank][:, cc * 384 + sb * 128: cc * 384 + sb * 128 + 128]
                        i_ = XE[i][:, sb, c * 128:(c + 1) * 128]
                        fns.append(lambda e, o_=o_, i_=i_: e.transpose(out=o_, in_=i_, identity=ident))
                P.auto("tensor", fns, reads=["xe%d" % i], psum=["b%d" % bank])
                dst = XT[:, 2 * pr:2 * pr + 2, :].rearrange("p a b -> p (a b)")
                if bank == 0:
                    P.auto("vector", lambda e, dst=dst: e.tensor_copy(out=dst, in_=PBb[0][:, 0:768]), writes=["XT%d" % pr], psum=["b0"])
                else:
                    P.auto("scalar", lambda e, dst=dst: e.copy(out=dst, in_=PBb[1][:, 0:768]), writes=["XT%d" % pr], psum=["b1"])
            xt_names = ["XT%d" % pr for pr in range(4)]
            for f in range(4):
                gb_ = 2 + 2 * (f % 2)
                ub_ = 3 + 2 * (f % 2)
                fns = []
                for c in range(8):
                    l_ = wg[i][:, c, f * 128:(f + 1) * 128]
                    fns.append(lambda e, l_=l_, c=c, gb_=gb_: e.matmul(PB[gb_][:, 0:384], lhsT=l_, rhs=XT[:, c, :], start=(c == 0), stop=(c == 7)))
                P.auto("tensor", fns, reads=["wg%d" % i] + xt_names, psum=["b%d" % gb_])
                fns = []
                for c in range(8):
                    l_ = wu[i][:, c, f * 128:(f + 1) * 128]
                    fns.append(lambda e, l_=l_, c=c, ub_=ub_: e.matmul(PB[ub_][:, 0:384], lhsT=l_, rhs=XT[:, c, :], start=(c == 0), stop=(c == 7)))
                P.auto("tensor", fns, reads=["wu%d" % i] + xt_names, psum=["b%d" % ub_])
                P.auto("scalar", lambda e, gb_=gb_: e.activation(out=sg, in_=PB[gb_][:, 0:384], func=AF.Silu), writes=["sg"], psum=["b%d" % gb_])
                P.auto("vector", lambda e, ub_=ub_, f=f: e.tensor_tensor(out=HT[:, f, :], in0=sg, in1=PB[ub_][:, 0:384], op=ALU.mult), reads=["sg"], writes=["HT%d" % f], psum=["b%d" % ub_])
            ht_names = ["HT%d" % f for f in range(4)]
            for sb in range(3):
                k = (e_ * 3 + sb) % 2
                fns = []
                for half in range(2):
                    for f in range(4):
                        l_ = HT[:, f, sb * 128:(sb + 1) * 128]
                        r_ = wd[i][:, f, half * 512:(half + 1) * 512]
                        fns.append(lambda e, l_=l_, r_=r_, f=f, half=half: e.matmul(PB[6 + half], lhsT=l_, rhs=r_, start=(f == 0), stop=(f == 3)))
                P.auto("tensor", fns, reads=["wd%d" % i] + ht_names, psum=["b6", "b7"])
                ysb = YSb[k]
                P.auto("scalar", lambda e, ysb=ysb: e.copy(out=ysb[:, 0:512], in_=PB[6]), writes=["ysa%d" % k], psum=["b6"])
                P.auto("vector", lambda e, ysb=ysb: e.tensor_copy(out=ysb[:, 512:1024], in_=PB[7]), writes=["ysb%d" % k], psum=["b7"])
                r0 = e_ * CAP + sb * 128
                ys_toks.append(P.adma("sync", ys_d[r0:r0 + 128, :], ysb, "ysw%d" % k, reads=["ysa%d" % k, "ysb%d" % k]))

        e_loads(0)
        for e_ in range(NE):
            if e_ + 1 < NE:
                e_loads(e_ + 1)
            e_compute(e_)

        Y1_ = [AR.alloc([128, 1024], F32) for _ in range(2)]
        Y2_ = [AR.alloc([128, 1024], F32) for _ in range(2)]
        hc_ = [AR.alloc([128, 1024], F32) for _ in range(2)]
        R2_ = [AR.alloc([128, 1024], F32) for _ in range(2)]
        lng2 = AR.alloc([128, 1024], F32)
        lnb2 = AR.alloc([128, 1024], F32)
        P.adma("sync", lng2, ln2[0:1, :].to_broadcast([128, 1024]), "lng2", writes=["lng2"])
        P.adma("sync", lnb2, ln2[1:2, :].to_broadcast([128, 1024]), "lnb2", writes=["lnb2"])

        def c_loads(blk):
            pb = blk % 2
            P.adma("sync", hc_[pb], h1f[blk * 128:(blk + 1) * 128, :], "hc%d" % pb, writes=["hc%d" % pb])
            for k, yb in ((0, Y1_[pb]), (1, Y2_[pb])):
                off = SLI[:, k, blk:blk + 1].bitcast(U32)
                P.acdma("gpsimd", lambda e, off=off, yb=yb: e.indirect_dma_start(
                    out=yb, out_offset=None, in_=ys_d, in_offset=bass.IndirectOffsetOnAxis(ap=off, axis=0)),
                    "yg%d%d" % (k, pb), reads=["SLI"], writes=["Y%d%d" % (k, pb)], extra=ys_toks)

        def c_block(blk):
            pb = blk % 2
            if blk + 1 < NB:
                c_loads(blk + 1)
            Y1, Y2, hc, R2 = Y1_[pb], Y2_[pb], hc_[pb], R2_[pb]
            rn = "R2%d" % pb
            P.auto("scalar", lambda e: e.mul(out=R2, in_=hc, mul=ALPHA), reads=["hc%d" % pb], writes=[rn])
            P.auto(V, lambda e: e.scalar_tensor_tensor(out=R2, in0=Y1, scalar=PW[:, 0, blk:blk + 1], in1=R2, op0=ALU.mult, op1=ALU.add), reads=["Y0%d" % pb, "PW0"], writes=[rn])
            P.auto(V, lambda e: e.scalar_tensor_tensor(out=R2, in0=Y2, scalar=PW[:, 1, blk:blk + 1], in1=R2, op0=ALU.mult, op1=ALU.add), reads=["Y1%d" % pb, "PW1"], writes=[rn])
            layer_norm(R2, "lng2", "lnb2", lng2, lnb2, stt_c, rn)
            finals.append(P.adma("sync", out[blk * 128:(blk + 1) * 128, :], R2, "outw%d" % pb, reads=[rn]))

        c_loads(0)
        for blk in range(NB):
            c_block(blk)

        P.emit(finals)
    return nc, tap_out


def _consts(j):
    ident = np.eye(128, dtype=np.float32)
    rot = np.zeros((128, 128), np.float32)
    for m in range(128):
        if (m % 64) < 32:
            rot[m + 32, m] = -1.0
        else:
            rot[m - 32, m] = 1.0
    ones = np.ones((128, 128), np.float32)
    kk = np.arange(128)[:, None, None]
    r = np.arange(8)[None, :, None]
    qq = np.arange(512)[None, None, :]
    mask = ((r * 128 + kk) <= ((2 * (qq // 128) + j) * 128 + (qq % 128))).astype(np.float32)
    cbf = np.concatenate([ident, rot, ones, mask.reshape(128, 4096)], axis=1)
    cf = np.zeros((128, 512), np.float32)
    cf[:, 0:128] = ident
    cf[:, 128:256] = 1.0
    cf[:, 256:384] = (np.arange(128)[:, None] < np.arange(128)[None, :]).astype(np.float32)
    inv_freq = (np.float32(10000.0) ** (-np.arange(0, 64, 2, dtype=np.float32) / np.float32(64))).astype(np.float32)
    p = np.arange(128)
    cf[:, 384] = (inv_freq[(p % 64) % 32].astype(np.float64) / (2.0 * np.pi)).astype(np.float32)
    cf[:, 392:424] = (np.arange(32) * CAP)[None, :]
    cf[:, 424] = -0.5
    cf[:, 425] = 1e-5
    return np.ascontiguousarray(cbf), cf


def make_core_inputs(inp, c):
    b, j = c // 2, c % 2
    x = inp["x"]
    xb_ = x[b]
    blocks = xb_.reshape(64, 128, D)
    xo = np.ascontiguousarray(blocks[j::2].reshape(NOWN, D))
    xh = np.zeros((32, 2, D), np.float32)
    for m in range(32):
        blk = 2 * m + j
        if blk > 0:
            xh[m] = xb_[blk * 128 - 2: blk * 128]
    pos = np.asarray(inp["positions"][b], dtype=np.int32)
    poso = np.ascontiguousarray(pos.reshape(64, 128)[j::2].reshape(1, NOWN))
    cbf, cf = _consts(j)
    f = lambda a: np.ascontiguousarray(np.asarray(a, dtype=np.float32))
    return {
        "xf": f(xb_), "xo": xo, "xh": f(xh.reshape(64, D)), "posf": np.ascontiguousarray(pos.reshape(1, S)), "poso": poso,
        "w_in": f(inp["w_in"][0]), "b_gate": f(inp["b_gate"][0].reshape(1, 2048)),
        "lam_in": f(np.concatenate([inp["lambda_q1"][0], inp["lambda_k1"][0], inp["lambda_q2"][0], inp["lambda_k2"][0]]).reshape(1, 256)),
        "subln_g": f(inp["subln_g"][0].reshape(1, 128)), "w_o_att": f(inp["w_o_att"][0]), "conv_w": f(inp["conv_w"][0]),
        "w_o_conv": f(inp["w_o_conv"][0]), "w_mix": f(inp["w_mix_out"][0]),
        "ln1": f(np.stack([inp["ln1_g"][0], inp["ln1_b"][0]])), "ln2": f(np.stack([inp["ln2_g"][0], inp["ln2_b"][0]])),
        "w_rt": f(np.concatenate([inp["w_router_group"][0], inp["w_router_expert"][0]], axis=1)),
        "b_rt": f(np.concatenate([inp["b_router_group"][0], inp["b_router_expert"][0]]).reshape(1, 36)),
        "w_eg": f(inp["w_exp_gate"][0]), "w_eu": f(inp["w_exp_up"][0]), "w_ed": f(inp["w_exp_down"][0]),
        "cbf": cbf, "cf32": cf,
    }


def kernel(**inputs):
    inp = {k: np.asarray(v) for k, v in inputs.items()}
    nc, _ = build()
    in_maps = [make_core_inputs(inp, c) for c in range(8)]
    res = run_bass_kernel_spmd(nc, in_maps, core_ids=list(range(8)))
    outp = np.zeros((4, S, D), np.float32)
    for c in range(8):
        b, j = c // 2, c % 2
        o = np.asarray(res.results[c]["out"]).reshape(32, 128, D)
        outp[b].reshape(64, 128, D)[j::2] = o
    return outp
```

```python
import math
import numpy as np
import ml_dtypes
from contextlib import ExitStack
import concourse.bass as bass
import concourse.mybir as mybir
from concourse.bass_utils import run_bass_kernel_spmd

F32 = mybir.dt.float32
BF16 = mybir.dt.bfloat16
I32 = mybir.dt.int32
U32 = mybir.dt.uint32
AF = mybir.ActivationFunctionType
ALU = mybir.AluOpType
AX = mybir.AxisListType
ENGS = ["tensor", "vector", "scalar", "gpsimd", "sync"]

S = 8192
D = 1024
NOWN = 4096
CAP = 384
NSLOT = 32 * CAP
ALPHA = 2.0 ** 0.25
LAMBDA_INIT = 0.8 - 0.6 * math.exp(0.0)
TWO_PI = 2.0 * math.pi
NEG = -1.0e30


class Prog:
    def __init__(self, nc, es):
        self.nc = nc
        self.es = es
        self.ops = {e: [] for e in ENGS}
        self.cnt = {e: 0 for e in ENGS}
        self.esem = {e: es.enter_context(nc.semaphore("es_" + e)) for e in ENGS}
        self.dsem = {}
        self.dcnt = {}
        self.waited = {e: {} for e in ENGS}
        self.base_waits = []

    def _waits(self, eng, waits):
        best = {}
        for w in list(waits) + list(self.base_waits):
            if w is None:
                continue
            sem, val, key = w
            if key not in best or best[key][1] < val:
                best[key] = (sem, val)
        out = []
        for key, (sem, val) in best.items():
            if self.waited[eng].get(key, 0) >= val:
                continue
            self.waited[eng][key] = val
            out.append((sem, val))
        return out

    def op(self, eng, fn, waits=(), signal=True):
        ws = self._waits(eng, waits)
        inc = None
        tok = None
        if signal:
            self.cnt[eng] += 1
            inc = (self.esem[eng], 1)
            tok = (self.esem[eng], self.cnt[eng], "e_" + eng)
        self.ops[eng].append((fn, ws, inc))
        return tok

    def _dsem(self, sem):
        if sem not in self.dsem:
            self.dsem[sem] = self.es.enter_context(self.nc.semaphore("ds_" + sem))
            self.dcnt[sem] = 0
        return self.dsem[sem]

    def dma(self, q, out, in_, sem, waits=(), **kw):
        return self.cdma(q, lambda e: e.dma_start(out=out, in_=in_, **kw), sem, waits)

    def cdma(self, q, fn, sem, waits=()):
        s = self._dsem(sem)
        ws = self._waits(q, waits)
        self.dcnt[sem] += 16
        self.ops[q].append((fn, ws, (s, 16)))
        return (s, self.dcnt[sem], "d_" + sem)

    def _auto_waits(self, reads, writes, psum):
        if not hasattr(self, "lw"):
            self.lw, self.rd, self.pa = {}, {}, {}
        ws = []
        for r in reads:
            ws.append(self.lw.get(r))
        for w in writes:
            ws.append(self.lw.get(w))
            ws.extend(self.rd.get(w, []))
        for p in psum:
            ws.append(self.pa.get(p))
        return ws

    def _auto_done(self, tok, reads, writes, psum):
        for r in reads:
            self.rd.setdefault(r, []).append(tok)
        for w in writes:
            self.lw[w] = tok
            self.rd[w] = []
        for p in psum:
            self.pa[p] = tok

    def auto(self, eng, fns, reads=(), writes=(), psum=(), extra=()):
        if not isinstance(fns, (list, tuple)):
            fns = [fns]
        ws = self._auto_waits(reads, writes, psum) + list(extra)
        tok = None
        for k, fn in enumerate(fns):
            tok = self.op(eng, fn, waits=ws if k == 0 else (), signal=(k == len(fns) - 1))
        self._auto_done(tok, reads, writes, psum)
        return tok

    def adma(self, q, out, in_, sem, reads=(), writes=(), extra=(), **kw):
        ws = self._auto_waits(reads, writes, ()) + list(extra)
        tok = self.dma(q, out, in_, sem, waits=ws, **kw)
        self._auto_done(tok, reads, writes, ())
        return tok

    def acdma(self, q, fn, sem, reads=(), writes=(), extra=()):
        ws = self._auto_waits(reads, writes, ()) + list(extra)
        tok = self.cdma(q, fn, sem, waits=ws)
        self._auto_done(tok, reads, writes, ())
        return tok

    def emit(self, final_waits):
        nc = self.nc
        with nc.Block() as block:
            def mk(eng):
                def body(e):
                    for fn, ws, inc in self.ops[eng]:
                        for sem, val in ws:
                            e.wait_ge(sem, val)
                        ins = fn(e)
                        if inc is not None:
                            ins.then_inc(inc[0], inc[1])
                    if eng == "sync":
                        for w in final_waits:
                            if w is not None:
                                e.wait_ge(w[0], w[1])
                return body
            block.tensor(mk("tensor"))
            block.vector(mk("vector"))
            block.scalar(mk("scalar"))
            block.gpsimd(mk("gpsimd"))
            block.sync(mk("sync"))


class Arena:
    def __init__(self, t, nwords):
        self.t = t
        self.n = nwords
        self.top = 0

    def mark(self):
        return self.top

    def reset(self, m):
        self.top = m

    def alloc(self, shape, dt):
        per = 1
        for s_ in shape[1:]:
            per *= s_
        if dt == BF16:
            words = (per + 1) // 2
        else:
            words = per
        words = (words + 7) // 8 * 8
        a = self.top
        self.top += words
        assert self.top <= self.n, ("arena overflow", self.top, self.n)
        v = self.t[:, a:a + words]
        if dt == BF16:
            v = v.bitcast(BF16)[:, 0:per]
        elif dt == I32:
            v = v.bitcast(I32)[:, 0:per]
        else:
            v = v[:, 0:per]
        if len(shape) == 3:
            v = v.rearrange("p (a b) -> p a b", a=shape[1])
        elif len(shape) == 4:
            v = v.rearrange("p (a b c) -> p a b c", a=shape[1], b=shape[2])
        if shape[0] != 128:
            v = v[0:shape[0]]
        return v


def build(upto="all", taps=(), NQT=8):
    nc = bass.Bass("TRN2", target_bir_lowering=False)
    din = lambda name, shape, dt: nc.dram_tensor(name, shape, dt, kind="ExternalInput").ap()
    xf = din("xf", [S, D], F32)
    xo = din("xo", [NOWN, D], F32)
    xh = din("xh", [64, D], F32)
    posf = din("posf", [1, S], I32)
    poso = din("poso", [1, NOWN], I32)
    w_in = din("w_in", [D, 5120], F32)
    b_gate = din("b_gate", [1, 2048], F32)
    lam_in = din("lam_in", [1, 256], F32)
    subln_g = din("subln_g", [1, 128], F32)
    w_o_att = din("w_o_att", [512, D], F32)
    conv_w = din("conv_w", [3, 512], F32)
    w_o_conv = din("w_o_conv", [512, D], F32)
    w_mix = din("w_mix", [D, D], F32)
    ln1 = din("ln1", [2, D], F32)
    ln2 = din("ln2", [2, D], F32)
    w_rt = din("w_rt", [D, 36], F32)
    b_rt = din("b_rt", [1, 36], F32)
    w_eg = din("w_eg", [32, D, 512], F32)
    w_eu = din("w_eu", [32, D, 512], F32)
    w_ed = din("w_ed", [32, 512, D], F32)
    cbf = din("cbf", [128, 384 + 4096], F32)
    cf32 = din("cf32", [128, 512], F32)
    out = nc.dram_tensor("out", [NOWN, D], F32, kind="ExternalOutput").ap()
    h1f = nc.dram_tensor("h1f", [NOWN, D], F32, kind="Internal").ap()
    xs_d = nc.dram_tensor("xs_d", [NSLOT, D], BF16, kind="Internal").ap()
    ys_d = nc.dram_tensor("ys_d", [NSLOT, D], F32, kind="Internal").ap()
    tap_out = {}

    with ExitStack() as es:
        P = Prog(nc, es)
        NW = 51 * 1024
        arena_t = es.enter_context(nc.sbuf_tensor("arena", [128, NW], F32))
        AR = Arena(arena_t, NW)
        psum = es.enter_context(nc.psum_tensor("psum", [128, 8, 512], F32))
        PB = [psum[:, i, :] for i in range(8)]
        PBb = [psum[:, i, :].bitcast(BF16) for i in range(8)]
        finals = []

        def tap(name, ap, waits, dt=F32):
            if name not in taps:
                return
            shp = list(ap.shape)
            o = nc.dram_tensor("tap_" + name, shp, dt, kind="ExternalOutput").ap()
            tap_out[name] = shp
            finals.append(P.dma("sync", o, ap, "tap_" + name, waits=waits))

        cb_t = AR.alloc([128, 384], BF16)
        ident = cb_t[:, 0:128]
        rotm = cb_t[:, 128:256]
        onesb = cb_t[:, 256:384]
        cf_t = AR.alloc([128, 512], F32)
        identf = cf_t[:, 0:128]
        onesf = cf_t[:, 128:256]
        trif = cf_t[:, 256:384]
        invf = cf_t[:, 384:385]
        ecap = cf_t[:, 392:424]
        mhalf = cf_t[:, 424:425]
        epsc = cf_t[:, 425:426]
        d_cb = P.dma("gpsimd", cb_t, cbf[:, 0:384], "cb")
        d_cf = P.dma("sync", cf_t, cf32, "cf")
        QO = AR.alloc([128, 4, NOWN], BF16)
        small = AR.alloc([128, 64], F32)
        neglam = small[:, 0:1]
        gsc = small[:, 1:2]
        lamv = AR.alloc([128, 256], F32)
        slg = AR.alloc([128, 128], F32)
        d_lam = P.dma("sync", lamv, lam_in.to_broadcast([128, 256]), "lam")
        d_slg = P.dma("sync", slg[:, 0:1], subln_g.rearrange("o p -> p o"), "slg", allow_slow_non_contiguous=True)
        lt = small[:, 8:10]
        t_l1 = P.op("vector", lambda e: e.tensor_tensor(out=lamv[:, 0:64], in0=lamv[:, 0:64], in1=lamv[:, 64:128], op=ALU.mult), waits=[d_lam])
        t_l2 = P.op("vector", lambda e: e.tensor_tensor(out=lamv[:, 128:192], in0=lamv[:, 128:192], in1=lamv[:, 192:256], op=ALU.mult), waits=[d_lam])
        t_l3 = P.op("vector", lambda e: e.tensor_reduce(out=lt[:, 0:1], in_=lamv[:, 0:64], axis=AX.X, op=ALU.add), waits=[t_l1])
        t_l4 = P.op("vector", lambda e: e.tensor_reduce(out=lt[:, 1:2], in_=lamv[:, 128:192], axis=AX.X, op=ALU.add), waits=[t_l2])
        t_l5 = P.op("scalar", lambda e: e.activation(out=small[:, 10:12], in_=lt, func=AF.Exp), waits=[t_l3, t_l4])
        t_l6 = P.op("vector", lambda e: e.tensor_tensor(out=small[:, 12:13], in0=small[:, 11:12], in1=small[:, 10:11], op=ALU.subtract), waits=[t_l5])
        t_l7 = P.op("vector", lambda e: e.tensor_scalar(out=neglam, in0=small[:, 12:13], scalar1=-LAMBDA_INIT, scalar2=None, op0=ALU.add), waits=[t_l6])
        t_g = P.op("vector", lambda e: e.tensor_scalar(out=gsc, in0=slg[:, 0:1], scalar1=1.0 - LAMBDA_INIT, scalar2=None, op0=ALU.mult), waits=[d_slg])
        t_consts = [d_cb, d_cf, t_l7, t_g]
        LG = AR.alloc([128, 32, 36], F32)
        PW = AR.alloc([128, 2, 32], F32)
        SLI = AR.alloc([128, 2, 32], I32)
        pers_mark = AR.mark()

        KT = AR.alloc([128, 2, S], BF16)
        maskt_t = AR.alloc([128, 4096], BF16)
        maskt = maskt_t.rearrange("p (r q) -> p r q", r=8)
        d_mask = P.dma("gpsimd", maskt_t, cbf[:, 384:384 + 4096], "mask", max_dma_last_dim=4096)
        VV = AR.alloc([128, 64, 256], BF16)
        wq = AR.alloc([128, 8, 512], BF16)
        wkv = AR.alloc([128, 8, 512], BF16)
        XBt = [AR.alloc([128, 4, D], BF16) for _ in range(2)]
        XTt = [AR.alloc([128, 8, 512], BF16) for _ in range(2)]
        post2 = [AR.alloc([128, 512], F32) for _ in range(2)]
        tq = AR.alloc([128, 512], F32)
        ki = AR.alloc([128, 512], I32)
        cst2 = [AR.alloc([128, 512], F32) for _ in range(2)]
        snt2 = [AR.alloc([128, 512], F32) for _ in range(2)]
        qsb = [AR.alloc([128, 512], BF16) for _ in range(2)]
        ra = [AR.alloc([128, 512], F32) for _ in range(2)]
        rb = [AR.alloc([128, 512], F32) for _ in range(2)]
        PT = [AR.alloc([128, 2, 512], BF16) for _ in range(3)]
        EE = [AR.alloc([128, 512], F32) for _ in range(4)]

        st = {"xb_free": [None, None], "xt_free": [None, None], "tp_free": [None, None],
              "kp_free": [[], []], "rp_free": [None, None], "vp_free": [None, None],
              "tab_free": [[], []], "qs_free": [None, None], "ra_free": [None, None],
              "n_kp": 0, "n_tp": 0, "n_vp": 0, "n_x": 0, "n_tab": 0, "last_sin": None}

        def load_w(dst, src_cols, sem, waits):
            return P.dma("gpsimd", dst, src_cols.rearrange("(c p) n -> p c n", p=128), sem, waits=waits)

        def tables_dma(pos_src, t0):
            ti = st["n_tab"] % 2
            st["n_tab"] += 1
            w0 = list(st["tab_free"][ti])
            d = P.dma("gpsimd", post2[ti], pos_src[0:1, t0:t0 + 512].to_broadcast([128, 512]), "pos%d" % ti, waits=w0)
            return ti, d, w0

        def tables(pos_src, t0):
            return tables_compute(*tables_dma(pos_src, t0))

        def tables_compute(ti, d, w0):
            post = post2[ti]
            prev = [st["last_sin"]]
            for dst, add in ((snt2[ti], 0.0), (cst2[ti], 0.25)):
                a = P.op("vector", lambda e, add=add: e.tensor_scalar(out=tq, in0=post, scalar1=invf, scalar2=add, op0=ALU.mult, op1=ALU.add), waits=[d, d_cf] + prev)
                b = P.op("vector", lambda e: e.tensor_copy(out=ki, in_=tq), waits=[a])
                c = P.op("vector", lambda e: e.tensor_tensor(out=tq, in0=tq, in1=ki, op=ALU.subtract), waits=[b])
                s_ = P.op("scalar", lambda e, dst=dst: e.activation(out=dst, in_=tq, func=AF.Sin, scale=TWO_PI), waits=[c] + w0)
                prev = [s_]
            st["last_sin"] = prev[0]
            return prev[0], ti

        def load_x_tile(src, row0):
            i = st["n_x"] % 2
            st["n_x"] += 1
            xb = XBt[i]
            d = P.dma("gpsimd", xb, src[row0:row0 + 512, :].rearrange("(b p) d -> p b d", p=128),
                      "xb%d" % i, waits=[st["xb_free"][i]])
            return i, d

        def transpose_tile(i, dx):
            xb = XBt[i]
            xt = XTt[i]
            evs = []
            last_t = None
            for g in range(4):
                bk = st["n_tp"] % 2
                st["n_tp"] += 1
                for cc in range(2):
                    c = 2 * g + cc
                    for blk in range(4):
                        last = (cc == 1 and blk == 3)
                        o_ = PBb[bk][:, cc * 512 + blk * 128: cc * 512 + blk * 128 + 128]
                        i_ = xb[:, blk, c * 128:(c + 1) * 128]
                        tk = P.op("tensor", lambda e, o_=o_, i_=i_: e.transpose(out=o_, in_=i_, identity=ident),
                                  waits=[dx, d_cb, st["tp_free"][bk]], signal=last)
                        if last:
                            last_t = tk
                dst = xt[:, 2 * g:2 * g + 2, :].rearrange("p a b -> p (a b)")
                src = PBb[bk]
                if g % 2 == 0:
                    ev = P.op("vector", lambda e, dst=dst, src=src: e.tensor_copy(out=dst, in_=src), waits=[last_t, st["xt_free"][i]])
                else:
                    ev = P.op("scalar", lambda e, dst=dst, src=src: e.copy(out=dst, in_=src), waits=[last_t, st["xt_free"][i]])
                st["tp_free"][bk] = ev
                evs.append(ev)
            st["xb_free"][i] = last_t
            return evs

        def proj_part1(xt, evs, wt, wcol, tab_tok, ti, extra_w):
            kb = st["n_kp"] % 2
            st["n_kp"] += 1
            kp = PB[2 + kb]
            q_ = qsb[kb]
            ra_ = ra[kb]
            cs_ = cst2[ti]
            mm = None
            for c in range(8):
                l_ = wt[:, c, wcol:wcol + 128]
                r_ = xt[:, c, :]
                mm = P.op("tensor", lambda e, l_=l_, r_=r_, c=c: e.matmul(kp, lhsT=l_, rhs=r_, start=(c == 0), stop=(c == 7)),
                          waits=[evs[c // 2]] + st["kp_free"][kb] + extra_w, signal=(c == 7))
            cp = P.op("scalar", lambda e: e.copy(out=q_, in_=kp), waits=[mm, st["qs_free"][kb]])
            a = P.op("vector", lambda e: e.tensor_tensor(out=ra_, in0=kp, in1=cs_, op=ALU.mult), waits=[mm, cp, tab_tok, st["ra_free"][kb]])
            st["kp_free"][kb] = [a, cp]
            return {"kb": kb, "cp": cp, "a": a, "tab": tab_tok, "ti": ti, "mm": mm}

        def proj_part2(cx, dst):
            kb = cx["kb"]
            rp = PB[4 + kb]
            q_ = qsb[kb]
            ra_ = ra[kb]
            rb_ = rb[kb]
            sn_ = snt2[cx["ti"]]
            rm = P.op("tensor", lambda e: e.matmul(rp, lhsT=rotm, rhs=q_, start=True, stop=True), waits=[cx["cp"], d_cb, st["rp_free"][kb]])
            st["qs_free"][kb] = rm
            b = P.op("vector", lambda e: e.tensor_tensor(out=rb_, in0=rp, in1=sn_, op=ALU.mult), waits=[rm, cx["tab"], st["ra_free"][kb]])
            st["rp_free"][kb] = b
            f = P.op("vector", lambda e: e.tensor_tensor(out=dst, in0=ra_, in1=rb_, op=ALU.add), waits=[cx["a"], b])
            st["ra_free"][kb] = f
            return f, b, rm

        pre = {}

        def prefetch(key, src, pos_src, T):
            i, dx = load_x_tile(src, T * 512)
            tab, ti = tables(pos_src, T * 512)
            pre[key] = (i, dx, tab, ti)

        def prefetch_dma(key, src, pos_src, T):
            i, dx = load_x_tile(src, T * 512)
            return (key, i, dx, tables_dma(pos_src, T * 512))

        def prefetch_compute(pf):
            key, i, dx, td = pf
            tab, ti = tables_compute(*td)
            pre[key] = (i, dx, tab, ti)

        def a_tile(T, dwk, dwv, last, nxt):
            i, dx, tab, ti = pre.pop(("a", T))
            pf = prefetch_dma(*nxt) if nxt is not None else None
            evs = transpose_tile(i, dx)
            cxs = [proj_part1(XTt[i], evs, wkv, hl * 128, tab, ti, [dwk]) for hl in range(2)]
            mm = None
            for blk in range(4):
                vb = st["n_vp"] % 2
                st["n_vp"] += 1
                vp = PB[6 + vb][:, 0:256]
                for c in range(8):
                    l_ = XTt[i][:, c, blk * 128:(blk + 1) * 128]
                    r_ = wkv[:, c, 256:512]
                    mm = P.op("tensor", lambda e, l_=l_, r_=r_, c=c, vp=vp: e.matmul(vp, lhsT=l_, rhs=r_, start=(c == 0), stop=(c == 7)),
                              waits=evs + [dwv, st["vp_free"][vb]], signal=(c == 7))
                o_ = VV[:, T * 4 + blk, :]
                vts = P.op("scalar", lambda e, o_=o_, vp=vp: e.copy(out=o_, in_=vp), waits=[mm])
                st["vp_free"][vb] = vts
                last.append(vts)
            st["xt_free"][i] = mm
            if pf is not None:
                prefetch_compute(pf)
            tabfree = []
            for hl in range(2):
                f, b, rm = proj_part2(cxs[hl], KT[:, hl, T * 512:(T + 1) * 512])
                tabfree.append(b)
                last.append(f)
            st["tab_free"][ti] = tabfree

        def phase_A(hp, then_q):
            dwk = load_w(wkv[:, :, 0:256], w_in[:, 512 + hp * 256: 512 + hp * 256 + 256], "wk", [])
            dwv = load_w(wkv[:, :, 256:512], w_in[:, 1024 + hp * 256: 1024 + hp * 256 + 256], "wv", [])
            last = []
            prefetch(("a", 0), xf, posf, 0)
            for T in range(16):
                if T + 1 < 16:
                    nxt = (("a", T + 1), xf, posf, T + 1)
                elif then_q:
                    nxt = (("q", 0), xo, poso, 0)
                else:
                    nxt = None
                a_tile(T, dwk, dwv, last, nxt)
            return last

        def q_tile(T, dwq, last):
            i, dx, tab, ti = pre.pop(("q", T))
            pf = prefetch_dma(("q", T + 1), xo, poso, T + 1) if T + 1 < NQT else None
            evs = transpose_tile(i, dx)
            tabfree = []
            rm = None
            cxs = {}
            cxs[0] = proj_part1(XTt[i], evs, wq, 0, tab, ti, [dwq])
            for h in range(4):
                if h + 1 < 4:
                    cxs[h + 1] = proj_part1(XTt[i], evs, wq, (h + 1) * 128, tab, ti, [dwq])
                if h == 3 and pf is not None:
                    prefetch_compute(pf)
                f, b, rm = proj_part2(cxs[h], QO[:, h, T * 512:(T + 1) * 512])
                tabfree.append(b)
                last.append(f)
            st["tab_free"][ti] = tabfree
            st["xt_free"][i] = rm

        def phase_Q():
            dwq = load_w(wq, w_in[:, 0:512], "wq", [])
            last = []
            for T in range(NQT):
                q_tile(T, dwq, last)
            return last

        ACC0 = ra[0]
        att = {"a0": None, "accL_free": None, "l6_free": None, "s_free": [None, None], "pt_free": [[], [], []], "acc_free": None, "n_s": 0, "n_pt": 0, "ee_free": None, "pending": None}

        def att_unit(i, hl, h):
            nkb = 8 * i + 8
            qk_tok = {}
            qrange = slice(i * 512, (i + 1) * 512)

            def col0(kb):
                r = kb - 8 * i
                return 128 * (r // 2) if r > 0 else 0

            def issue_qk(kb):
                s = kb % 2
                c0 = col0(kb)
                krange = slice(kb * 128, (kb + 1) * 128)
                qr = slice(i * 512 + c0, (i + 1) * 512)
                P.op("tensor", lambda e: e.matmul(psum[:, 2 * s, c0:512], lhsT=KT[0:64, hl, krange], rhs=QO[0:64, h, qr], start=True, stop=True),
                     waits=[att["s_free"][s]], signal=False)
                t = P.op("tensor", lambda e: e.matmul(psum[:, 2 * s + 1, c0:512], lhsT=KT[64:128, hl, krange], rhs=QO[64:128, h, qr], start=True, stop=True))
                qk_tok[kb] = (t, s)

            def exp_step(kb):
                t, s = qk_tok[kb]
                c0 = col0(kb)
                pi = att["n_pt"] % 3
                att["n_pt"] += 1
                pt = PT[pi]
                ex = P.op("scalar", lambda e: e.activation(out=pt[:, :, c0:512], in_=psum[:, 2 * s:2 * s + 2, c0:512], func=AF.Exp, scale=0.125), waits=[t] + att["pt_free"][pi])
                att["s_free"][s] = ex
                return ex, pt, pi

            def pv_step(kb, ex, pt, pi):
                pv_w = ex
                c0 = col0(kb)
                if kb >= 8 * i:
                    r = kb - 8 * i
                    pv_w = P.op("vector", lambda e: e.tensor_tensor(out=pt[:, :, c0:512], in0=pt[:, :, c0:512],
                                                                      in1=maskt[:, r, c0:512].unsqueeze(1).to_broadcast([128, 2, 512 - c0]), op=ALU.mult),
                                waits=[ex, d_mask])
                s0 = (kb == 0)
                s1 = (kb == nkb - 1)
                w0 = [pv_w, att["acc_free"]] if kb == 0 else [pv_w]
                vv = VV[:, kb, hl * 128:(hl + 1) * 128]
                P.op("tensor", lambda e: e.matmul(PB[4][:, c0:512], lhsT=vv, rhs=pt[:, 0, c0:512], start=s0, stop=s1), waits=w0, signal=False)
                P.op("tensor", lambda e: e.matmul(PB[5][:, c0:512], lhsT=vv, rhs=pt[:, 1, c0:512], start=s0, stop=s1), signal=False)
                pv = P.op("tensor", lambda e: e.matmul(PB[7][:, c0:512], lhsT=onesb, rhs=pt[:, 1, c0:512], start=s0, stop=s1))
                if kb == 0:
                    a0 = P.op("vector", lambda e: e.tensor_copy(out=ACC0, in_=pt[:, 0, :]), waits=[pv_w, att["accL_free"]])
                else:
                    a0 = P.op("vector", lambda e: e.tensor_tensor(out=ACC0[:, c0:512], in0=ACC0[:, c0:512], in1=pt[:, 0, c0:512], op=ALU.add), waits=[pv_w, att["a0"]])
                att["a0"] = a0
                att["pt_free"][pi] = [pv, a0]
                return pv

            issue_qk(0)
            issue_qk(1)
            pv = None
            for kb in range(nkb):
                ex, pt, pi = exp_step(kb)
                if kb + 2 < nkb:
                    issue_qk(kb + 2)
                pv = pv_step(kb, ex, pt, pi)
                if kb == 2 and att["pending"] is not None:
                    att["pending"]()
                    att["pending"] = None
            wfree = [att["ee_free"]]
            lsum = P.op("tensor", lambda e: e.matmul(PB[6], lhsT=onesf, rhs=ACC0, start=True, stop=True), waits=[att["a0"], att["acc_free"], att["l6_free"], d_cf])
            att["accL_free"] = lsum
            e0 = P.op("vector", lambda e: e.reciprocal(out=EE[0], in_=PB[6]), waits=[pv, lsum] + wfree)
            e1 = P.op("vector", lambda e: e.reciprocal(out=EE[1], in_=PB[7]), waits=[pv, lsum] + wfree)
            e2 = P.op("vector", lambda e: e.tensor_tensor(out=EE[0], in0=PB[4], in1=EE[0], op=ALU.mult), waits=[e0])
            e3 = P.op("vector", lambda e: e.tensor_tensor(out=EE[1], in0=PB[5], in1=EE[1], op=ALU.mult), waits=[e1])
            att["acc_free"] = e3
            e4 = P.op("vector", lambda e: e.scalar_tensor_tensor(out=EE[2], in0=EE[1], scalar=neglam, in1=EE[0], op0=ALU.mult, op1=ALU.add), waits=[e2, e3, t_l7] + wfree)
            e5 = P.op("gpsimd", lambda e: e.tensor_tensor(out=EE[3], in0=EE[2], in1=EE[2], op=ALU.mult), waits=[e4] + wfree)

            def finish():
                ss = P.op("tensor", lambda e: e.matmul(PB[6], lhsT=onesf, rhs=EE[3], start=True, stop=True), waits=[e5, e3, d_cf, att["l6_free"]])
                e6 = P.op("scalar", lambda e: e.activation(out=EE[3], in_=PB[6], func=AF.Ln, scale=1.0 / 128.0, bias=epsc), waits=[ss])
                att["l6_free"] = e6
                e7 = P.op("scalar", lambda e: e.activation(out=EE[3], in_=EE[3], func=AF.Exp, scale=-0.5), waits=[e6])
                e8 = P.op("vector", lambda e: e.tensor_tensor(out=EE[2], in0=EE[2], in1=EE[3], op=ALU.mult), waits=[e7])
                e9 = P.op("vector", lambda e: e.tensor_scalar(out=QO[:, h, qrange], in0=EE[2], scalar1=gsc, scalar2=None, op0=ALU.mult), waits=[e8, t_g])
                att["ee_free"] = e9
                att["last"] = e9
            att["pending"] = finish

        def attention(hp):
            for i in range(NQT):
                for hl in range(2):
                    att_unit(i, hl, 2 * hp + hl)
            att["pending"]()
            att["pending"] = None
            return att["last"]

        att_last = None
        if upto == "consts":
            tap("small", small, t_consts)
            P.emit(finals)
            return nc, tap_out
        for hp in range(2):
            kvt = phase_A(hp, hp == 0)
            if upto == "A":
                tap("kt", KT[:, :, 0:2048], kvt, BF16)
                tap("vv", VV[:, 0:8, :], kvt, BF16)
                P.emit(finals)
                return nc, tap_out
            qtoks = phase_Q() if hp == 0 else []
            P.base_waits = [t for t in kvt + qtoks if t is not None] + t_consts
            att_last = attention(hp)
            P.base_waits = [att_last]
            if upto == "att0":
                break
        tap("qo", QO, [att_last], BF16)
        tap("kt", KT[:, :, 0:2048], [att_last], BF16)
        tap("vv", VV[:, 0:8, :], [att_last], BF16)

        if upto in ("att0", "att"):
            P.emit(finals)
            return nc, tap_out

        AR.reset(pers_mark)
        wp = AR.alloc([128, 8, 3584], BF16)
        woa = AR.alloc([128, 4, 1024], BF16)
        woc = AR.alloc([128, 4, 1024], BF16)
        wmx = AR.alloc([128, 8, 1024], BF16)
        xb2_ = [AR.alloc([128, 2, 1024], BF16) for _ in range(2)]
        xhb_ = [AR.alloc([128, 1024], BF16) for _ in range(2)]
        xres_ = [AR.alloc([128, 2, 1024], F32) for _ in range(2)]
        xT2 = AR.alloc([128, 8, 256], BF16)
        xhT = AR.alloc([128, 32], BF16)
        ccs_ = [AR.alloc([128, 264], F32) for _ in range(2)]
        Ub_ = [AR.alloc([128, 2, 130], F32) for _ in range(2)]
        T1_ = [AR.alloc([128, 2, 128], F32) for _ in range(2)]
        Zb = AR.alloc([128, 4, 256], BF16)
        g0_ = [AR.alloc([128, 256], F32) for _ in range(2)]
        g1_ = [AR.alloc([128, 256], F32) for _ in range(2)]
        m0_ = [AR.alloc([128, 256], F32) for _ in range(2)]
        m1_ = [AR.alloc([128, 256], F32) for _ in range(2)]
        MT = AR.alloc([128, 8, 256], BF16)
        Rb_ = [AR.alloc([128, 1024], F32) for _ in range(2)]
        lng = AR.alloc([128, 1024], F32)
        lnb = AR.alloc([128, 1024], F32)
        H1T = AR.alloc([128, 8, 128], F32)
        stt_b = AR.alloc([128, 16], F32)
        bgs = AR.alloc([128, 16], F32)
        cws = AR.alloc([128, 12], F32)
        rbias = AR.alloc([128, 36], F32)
        wr = AR.alloc([128, 8, 36], F32)
        for i7 in range(7):
            P.adma("gpsimd", wp[:, :, i7 * 512:(i7 + 1) * 512], w_in[:, 1536 + i7 * 512:1536 + (i7 + 1) * 512].rearrange("(c p) n -> p c n", p=128), "wp", writes=["wp"])
        P.adma("gpsimd", woa, w_o_att.rearrange("(c p) n -> p c n", p=128), "woa", writes=["woa"])
        P.adma("gpsimd", woc, w_o_conv.rearrange("(c p) n -> p c n", p=128), "woc", writes=["woc"])
        P.adma("gpsimd", wmx, w_mix.rearrange("(c p) n -> p c n", p=128), "wmx", writes=["wmx"])
        P.adma("sync", wr, w_rt.rearrange("(c p) n -> p c n", p=128), "wr", writes=["wr"])
        P.adma("sync", bgs, b_gate.rearrange("o (q p) -> p (o q)", p=128), "bgs", writes=["bgs"], allow_slow_non_contiguous=True)
        P.adma("sync", cws.rearrange("p (k q) -> p k q", k=3), conv_w.rearrange("k (q p) -> p k q", p=128), "cws", writes=["cws"], allow_slow_non_contiguous=True)
        P.adma("sync", rbias, b_rt.to_broadcast([128, 36]), "rbias", writes=["rbias"])
        P.adma("sync", lng, ln1[0:1, :].to_broadcast([128, 1024]), "lng", writes=["lng"])
        P.adma("sync", lnb, ln1[1:2, :].to_broadcast([128, 1024]), "lnb", writes=["lnb"])

        def layer_norm(buf, gname, bname, gt, bt, stt=None, rn="R"):
            stt = stt_b if stt is None else stt
            mv = stt[:, 12:14]
            ve = stt[:, 14:15]
            rs = stt[:, 15:16]
            P.auto("vector", lambda e: e.bn_stats(out=stt[:, 0:6], in_=buf[:, 0:512]), reads=[rn], writes=["stt"])
            P.auto("vector", lambda e: e.bn_stats(out=stt[:, 6:12], in_=buf[:, 512:1024]), reads=[rn], writes=["stt2"])
            P.auto("vector", lambda e: e.bn_aggr(out=mv, in_=stt[:, 0:12]), reads=["stt", "stt2"], writes=["mv"])
            P.auto("vector", lambda e: e.tensor_scalar(out=ve, in0=mv[:, 1:2], scalar1=1e-5, scalar2=None, op0=ALU.add), reads=["mv"], writes=["ve"])
            P.auto("gpsimd", lambda e: e.tensor_tensor(out=rs, in0=ve, in1=mhalf, op=ALU.pow), reads=["ve"], writes=["rs"], extra=[d_cf])
            P.auto("vector", lambda e: e.tensor_scalar(out=buf, in0=buf, scalar1=mv[:, 0:1], scalar2=rs, op0=ALU.subtract, op1=ALU.mult), reads=["mv", "rs"], writes=[rn])
            P.auto("vector", lambda e: e.tensor_tensor(out=buf, in0=buf, in1=gt, op=ALU.mult), reads=[gname], writes=[rn])
            return P.auto("gpsimd", lambda e: e.tensor_tensor(out=buf, in0=buf, in1=bt, op=ALU.add), reads=[bname], writes=[rn])

        def b_loads(tb):
            r0 = tb * 256
            pb = tb % 2
            P.adma("gpsimd", xb2_[pb], xo[r0:r0 + 256, :].rearrange("(b p) d -> p b d", p=128), "xb2%d" % pb, writes=["xb2%d" % pb])
            P.adma("gpsimd", xhb_[pb][0:4, :], xh[tb * 4:tb * 4 + 4, :], "xhb%d" % pb, writes=["xhb%d" % pb])
            P.adma("sync", xres_[pb], xo[r0:r0 + 256, :].rearrange("(b p) d -> p b d", p=128), "xres%d" % pb, writes=["xres%d" % pb])

        def b_tile(tb):
            r0 = tb * 256
            pb = tb % 2
            xb2, xhb, xres = xb2_[pb], xhb_[pb], xres_[pb]
            n_xb2, n_xhb, n_xres = "xb2%d" % pb, "xhb%d" % pb, "xres%d" % pb
            if tb + 1 < NTB:
                b_loads(tb + 1)
            for half in range(2):
                fns = []
                for cc in range(4):
                    c = half * 4 + cc
                    for blk in range(2):
                        o_ = PBb[half][:, cc * 256 + blk * 128: cc * 256 + blk * 128 + 128]
                        i_ = xb2[:, blk, c * 128:(c + 1) * 128]
                        fns.append(lambda e, o_=o_, i_=i_: e.transpose(out=o_, in_=i_, identity=ident))
                P.auto("tensor", fns, reads=[n_xb2], psum=["b%d" % half], extra=[d_cb])
                dst = xT2[:, half * 4:half * 4 + 4, :].rearrange("p a b -> p (a b)")
                if half == 0:
                    P.auto("vector", lambda e, dst=dst: e.tensor_copy(out=dst, in_=PBb[0]), writes=["xT2a"], psum=["b0"])
                else:
                    P.auto("scalar", lambda e, dst=dst: e.copy(out=dst, in_=PBb[1]), writes=["xT2b"], psum=["b1"])
            fns = []
            for c in range(8):
                o_ = PBb[6][:, c * 4:(c + 1) * 4]
                i_ = xhb[0:4, c * 128:(c + 1) * 128]
                fns.append(lambda e, o_=o_, i_=i_: e.transpose(out=o_, in_=i_, identity=ident[0:4, 0:4]))
            P.auto("tensor", fns, reads=[n_xhb], psum=["b6"], extra=[d_cb])
            P.auto("vector", lambda e: e.tensor_copy(out=xhT, in_=PBb[6][:, 0:32]), writes=["xhT"], psum=["b6"])
            xhT3 = xhT.rearrange("p (c k) -> p c k", c=8)
            for q in range(4):
                qp = q % 2
                BA, BB = PB[2 + 2 * qp], PB[3 + 2 * qp]
                nBA, nBB = "b%d" % (2 + 2 * qp), "b%d" % (3 + 2 * qp)
                ccs, Ub, T1 = ccs_[qp], Ub_[qp], T1_[qp]
                nccs, nU, nU2, nT1 = "ccs%d" % qp, "U%d" % qp, "U2%d" % qp, "T1%d" % qp
                fns = []
                for c in range(8):
                    l_ = wp[:, c, 512 + q * 128: 512 + q * 128 + 128]
                    fns.append(lambda e, l_=l_, c=c, BA=BA: e.matmul(BA[:, 0:256], lhsT=l_, rhs=xT2[:, c, :], start=(c == 0), stop=(c == 7)))
                for c in range(8):
                    l_ = wp[:, c, 512 + q * 128: 512 + q * 128 + 128]
                    fns.append(lambda e, l_=l_, c=c, BA=BA: e.matmul(BA[:, 256:260], lhsT=l_, rhs=xhT3[:, c, :], start=(c == 0), stop=(c == 7)))
                for c in range(8):
                    l_ = wp[:, c, 1024 + q * 128: 1024 + q * 128 + 128]
                    fns.append(lambda e, l_=l_, c=c, BA=BA: e.matmul(BA[:, 260:264], lhsT=l_, rhs=xhT3[:, c, :], start=(c == 0), stop=(c == 7)))
                P.auto("tensor", fns, reads=["wp", "xT2a", "xT2b", "xhT"], psum=[nBA])
                fns = []
                for c in range(8):
                    l_ = wp[:, c, 1024 + q * 128: 1024 + q * 128 + 128]
                    fns.append(lambda e, l_=l_, c=c, BB=BB: e.matmul(BB[:, 0:256], lhsT=l_, rhs=xT2[:, c, :], start=(c == 0), stop=(c == 7)))
                for c in range(8):
                    l_ = wp[:, c, q * 128: q * 128 + 128]
                    fns.append(lambda e, l_=l_, c=c, BB=BB: e.matmul(BB[:, 256:512], lhsT=l_, rhs=xT2[:, c, :], start=(c == 0), stop=(c == 7)))
                P.auto("tensor", fns, reads=["wp", "xT2a", "xT2b"], psum=[nBB])
                P.auto("scalar", lambda e, ccs=ccs, BA=BA: e.copy(out=ccs, in_=BA[:, 0:264]), writes=[nccs], psum=[nBA])
                P.auto("vector", lambda e, ccs=ccs, Ub=Ub, BB=BB: e.tensor_tensor(out=Ub[:, :, 2:130], in0=ccs[:, 0:256].rearrange("p (b t) -> p b t", b=2),
                                                          in1=BB[:, 0:256].rearrange("p (b t) -> p b t", b=2), op=ALU.mult),
                       reads=[nccs], writes=[nU], psum=[nBB])
                P.auto("vector", lambda e, ccs=ccs, Ub=Ub: e.tensor_tensor(out=Ub[:, :, 0:2], in0=ccs[:, 256:260].rearrange("p (b t) -> p b t", b=2),
                                                          in1=ccs[:, 260:264].rearrange("p (b t) -> p b t", b=2), op=ALU.mult),
                       reads=[nccs], writes=[nU2])
                P.auto("vector", lambda e, q=q, T1=T1, Ub=Ub: e.tensor_scalar(out=T1, in0=Ub[:, :, 0:128], scalar1=cws[:, q:q + 1], scalar2=None, op0=ALU.mult),
                       reads=[nU, nU2, "cws"], writes=[nT1])
                P.auto("vector", lambda e, q=q, T1=T1, Ub=Ub: e.scalar_tensor_tensor(out=T1, in0=Ub[:, :, 1:129], scalar=cws[:, 4 + q:5 + q], in1=T1, op0=ALU.mult, op1=ALU.add),
                       reads=[nU, nU2], writes=[nT1])
                P.auto("vector", lambda e, q=q, T1=T1, Ub=Ub: e.scalar_tensor_tensor(out=T1, in0=Ub[:, :, 2:130], scalar=cws[:, 8 + q:9 + q], in1=T1, op0=ALU.mult, op1=ALU.add),
                       reads=[nU, nU2], writes=[nT1])
                P.auto("vector", lambda e, q=q, T1=T1, BB=BB: e.tensor_tensor(out=Zb[:, q, :], in0=T1.rearrange("p b t -> p (b t)"), in1=BB[:, 256:512], op=ALU.mult),
                       reads=[nT1], writes=["Z%d" % q], psum=[nBB])
            flush_routers()
            for c8 in range(8):
                cp_ = c8 % 2
                BG, BY = PB[2 + 2 * cp_], PB[3 + 2 * cp_]
                nBG, nBY = "b%d" % (2 + 2 * cp_), "b%d" % (3 + 2 * cp_)
                g0, g1, m0, m1 = g0_[cp_], g1_[cp_], m0_[cp_], m1_[cp_]
                ng0, ng1, nm0, nm1 = "g0%d" % cp_, "g1%d" % cp_, "m0%d" % cp_, "m1%d" % cp_
                fns = []
                for c in range(8):
                    l_ = wp[:, c, 1536 + c8 * 128: 1536 + c8 * 128 + 128]
                    fns.append(lambda e, l_=l_, c=c, BG=BG: e.matmul(BG[:, 0:256], lhsT=l_, rhs=xT2[:, c, :], start=(c == 0), stop=(c == 7)))
                for c in range(8):
                    l_ = wp[:, c, 2560 + c8 * 128: 2560 + c8 * 128 + 128]
                    fns.append(lambda e, l_=l_, c=c, BG=BG: e.matmul(BG[:, 256:512], lhsT=l_, rhs=xT2[:, c, :], start=(c == 0), stop=(c == 7)))
                P.auto("tensor", fns, reads=["wp", "xT2a", "xT2b"], psum=[nBG])
                P.auto("scalar", lambda e, c8=c8, g0=g0, BG=BG: e.activation(out=g0, in_=BG[:, 0:256], func=AF.Sigmoid, bias=bgs[:, c8:c8 + 1]), reads=["bgs"], writes=[ng0], psum=[nBG])
                P.auto("scalar", lambda e, c8=c8, g1=g1, BG=BG: e.activation(out=g1, in_=BG[:, 256:512], func=AF.Sigmoid, bias=bgs[:, 8 + c8:9 + c8]), reads=["bgs"], writes=[ng1], psum=[nBG])
                fns = []
                for h in range(4):
                    l_ = woa[:, h, c8 * 128:(c8 + 1) * 128]
                    r_ = QO[:, h, r0:r0 + 256]
                    fns.append(lambda e, l_=l_, r_=r_, h=h, BY=BY: e.matmul(BY[:, 0:256], lhsT=l_, rhs=r_, start=(h == 0), stop=(h == 3)))
                for q in range(4):
                    l_ = woc[:, q, c8 * 128:(c8 + 1) * 128]
                    r_ = Zb[:, q, :]
                    fns.append(lambda e, l_=l_, r_=r_, q=q, BY=BY: e.matmul(BY[:, 256:512], lhsT=l_, rhs=r_, start=(q == 0), stop=(q == 3)))
                P.auto("tensor", fns, reads=["woa", "woc", "Z0", "Z1", "Z2", "Z3"], psum=[nBY])
                P.auto("vector", lambda e, m0=m0, g0=g0, BY=BY: e.tensor_tensor(out=m0, in0=g0, in1=BY[:, 0:256], op=ALU.mult), reads=[ng0], writes=[nm0], psum=[nBY])
                P.auto("vector", lambda e, m1=m1, g1=g1, BY=BY: e.tensor_tensor(out=m1, in0=g1, in1=BY[:, 256:512], op=ALU.mult), reads=[ng1], writes=[nm1], psum=[nBY])
                P.auto("vector", lambda e, c8=c8, m0=m0, m1=m1: e.tensor_tensor(out=MT[:, c8, :], in0=m0, in1=m1, op=ALU.add), reads=[nm0, nm1], writes=["MT%d" % c8])
            for blk in range(2):
                gb = tb * 2 + blk
                Rb = Rb_[blk]
                rn = "R%d" % blk
                fns = []
                for half in range(2):
                    for c in range(8):
                        l_ = MT[:, c, blk * 128:(blk + 1) * 128]
                        r_ = wmx[:, c, half * 512:(half + 1) * 512]
                        fns.append(lambda e, l_=l_, r_=r_, c=c, half=half: e.matmul(PB[6 + half], lhsT=l_, rhs=r_, start=(c == 0), stop=(c == 7)))
                P.auto("tensor", fns, reads=["MT%d" % c_ for c_ in range(8)] + ["wmx"], psum=["b6", "b7"])
                P.auto("vector", lambda e, blk=blk, Rb=Rb: e.scalar_tensor_tensor(out=Rb[:, 0:512], in0=xres[:, blk, 0:512], scalar=ALPHA, in1=PB[6], op0=ALU.mult, op1=ALU.add),
                       reads=[n_xres], writes=[rn], psum=["b6"])
                P.auto("vector", lambda e, blk=blk, Rb=Rb: e.scalar_tensor_tensor(out=Rb[:, 512:1024], in0=xres[:, blk, 512:1024], scalar=ALPHA, in1=PB[7], op0=ALU.mult, op1=ALU.add),
                       reads=[n_xres], writes=[rn], psum=["b7"])
                layer_norm(Rb, "lng", "lnb", lng, lnb, None, rn)
                finals_h1.append(P.adma("sync", h1f[gb * 128:(gb + 1) * 128, :], Rb, "h1w%d" % blk, reads=[rn]))
                pend_r.append(make_router(Rb, rn, gb))

        def make_router(Rb, rn, gb):
            def run():
                fns = []
                for c in range(8):
                    o_ = PB[c // 4][:, (c % 4) * 128:(c % 4) * 128 + 128]
                    i_ = Rb[:, c * 128:(c + 1) * 128]
                    fns.append(lambda e, o_=o_, i_=i_: e.transpose(out=o_, in_=i_, identity=identf))
                P.auto("tensor", fns, reads=[rn], psum=["b0", "b1"], extra=[d_cf])
                P.auto("scalar", lambda e: e.copy(out=H1T[:, 0:4, :].rearrange("p a b -> p (a b)"), in_=PB[0]), writes=["H1Ta"], psum=["b0"])
                P.auto("vector", lambda e: e.tensor_copy(out=H1T[:, 4:8, :].rearrange("p a b -> p (a b)"), in_=PB[1]), writes=["H1Tb"], psum=["b1"])
                fns = []
                for c in range(8):
                    fns.append(lambda e, c=c: e.matmul(PB[0][:, 0:36], lhsT=H1T[:, c, :], rhs=wr[:, c, :], start=(c == 0), stop=(c == 7)))
                P.auto("tensor", fns, reads=["H1Ta", "H1Tb", "wr"], psum=["b0"])
                P.auto("vector", lambda e: e.tensor_tensor(out=LG[:, gb, :], in0=PB[0][:, 0:36], in1=rbias, op=ALU.add), reads=["rbias"], writes=["LG"], psum=["b0"])
            return run

        pend_r = []

        def flush_routers():
            while pend_r:
                pend_r.pop(0)()

        finals_h1 = []
        NTB = NQT * 2
        b_loads(0)
        for tb in range(NTB):
            b_tile(tb)
        flush_routers()
        tap("lg", LG, [P.lw["LG"]])
        tap("h1", h1f[0:256, :], finals_h1)
        if upto == "B":
            P.emit(finals + finals_h1)
            return nc, tap_out

        P.base_waits = [P.lw["LG"]] + finals_h1
        AR.reset(pers_mark)
        NB = NTB * 2
        gm = AR.alloc([128, 32], F32)
        gd = AR.alloc([128, 32, 4], F32)
        pen = AR.alloc([128, 32, 4], F32)
        ge = AR.alloc([128, 32, 4], F32)
        gs = AR.alloc([128, 32], F32)
        gw = AR.alloc([128, 32], F32)
        em = AR.alloc([128, 32, 32], F32)
        em2 = AR.alloc([128, 32, 32], F32)
        oh1 = AR.alloc([128, 32, 32], F32)
        oh2 = AR.alloc([128, 32, 32], F32)
        Aa = AR.alloc([128, 32, 32], F32)
        Tt = AR.alloc([128, 32, 32], F32)
        sc0 = AR.alloc([128, 32, 32], F32)
        sc1 = AR.alloc([128, 32, 32], F32)
        rank = AR.alloc([128, 32, 32], F32)
        tmpc = AR.alloc([128, 32, 32], F32)
        v1 = AR.alloc([128, 32], F32)
        v2 = AR.alloc([128, 32], F32)
        dv = AR.alloc([128, 32], F32)
        s12 = AR.alloc([128, 2, 32], F32)
        stt_c = AR.alloc([128, 16], F32)
        fl = lambda t: t.rearrange("p a b -> p (a b)")
        gl = LG[:, :, 0:4]
        el = LG[:, :, 4:36]
        bc3 = lambda t: t.unsqueeze(2).to_broadcast([128, 32, 32])
        V = "vector"
        P.auto(V, lambda e: e.tensor_reduce(out=gm, in_=gl, axis=AX.X, op=ALU.max), reads=["LG"], writes=["gm"])
        P.auto(V, lambda e: e.tensor_tensor(out=gd, in0=gl, in1=gm.unsqueeze(2).to_broadcast([128, 32, 4]), op=ALU.subtract), reads=["LG", "gm"], writes=["gd"])
        P.auto(V, lambda e: e.tensor_scalar(out=pen, in0=gd, scalar1=0.0, scalar2=NEG, op0=ALU.is_lt, op1=ALU.mult), reads=["gd"], writes=["pen"])
        P.auto("scalar", lambda e: e.activation(out=ge, in_=gd, func=AF.Exp), reads=["gd"], writes=["ge"])
        P.auto(V, lambda e: e.tensor_reduce(out=gs, in_=ge, axis=AX.X, op=ALU.add), reads=["ge"], writes=["gs"])
        P.auto(V, lambda e: e.reciprocal(out=gw, in_=gs), reads=["gs"], writes=["gw"])
        P.auto(V, lambda e: e.tensor_tensor(out=em.rearrange("p b (g k) -> p b g k", g=4), in0=el.rearrange("p b (g k) -> p b g k", g=4),
                                            in1=pen.unsqueeze(3).to_broadcast([128, 32, 4, 8]), op=ALU.add), reads=["LG", "pen"], writes=["em"])
        P.auto(V, lambda e: e.tensor_reduce(out=v1, in_=em, axis=AX.X, op=ALU.max), reads=["em"], writes=["v1"])
        P.auto(V, lambda e: e.tensor_tensor(out=oh1, in0=em, in1=bc3(v1), op=ALU.is_equal), reads=["em", "v1"], writes=["oh1"])
        P.auto(V, lambda e: e.scalar_tensor_tensor(out=fl(em2), in0=fl(oh1), scalar=NEG, in1=fl(em), op0=ALU.mult, op1=ALU.add), reads=["oh1", "em"], writes=["em2"])
        P.auto(V, lambda e: e.tensor_reduce(out=v2, in_=em2, axis=AX.X, op=ALU.max), reads=["em2"], writes=["v2"])
        P.auto(V, lambda e: e.tensor_tensor(out=oh2, in0=em2, in1=bc3(v2), op=ALU.is_equal), reads=["em2", "v2"], writes=["oh2"])
        P.auto(V, lambda e: e.tensor_tensor(out=dv, in0=v2, in1=v1, op=ALU.subtract), reads=["v1", "v2"], writes=["dv"])
        P.auto("scalar", lambda e: e.activation(out=dv, in_=dv, func=AF.Exp), reads=[], writes=["dv"])
        P.auto(V, lambda e: e.tensor_scalar(out=dv, in0=dv, scalar1=1.0, scalar2=None, op0=ALU.add), writes=["dv"])
        P.auto(V, lambda e: e.reciprocal(out=dv, in_=dv), writes=["dv"])
        P.auto(V, lambda e: e.tensor_tensor(out=PW[:, 0, :], in0=dv, in1=gw, op=ALU.mult), reads=["dv", "gw"], writes=["PW0"])
        P.auto(V, lambda e: e.tensor_tensor(out=PW[:, 1, :], in0=gw, in1=PW[:, 0, :], op=ALU.subtract), reads=["PW0", "gw"], writes=["PW1"])
        P.auto(V, lambda e: e.tensor_tensor(out=fl(Aa), in0=fl(oh1), in1=fl(oh2), op=ALU.add), reads=["oh1", "oh2"], writes=["Aa"])
        Af = fl(Aa)
        P.auto("tensor", [lambda e: e.matmul(PB[0], lhsT=trif, rhs=Af[:, 0:512], start=True, stop=True),
                          lambda e: e.matmul(PB[1], lhsT=trif, rhs=Af[:, 512:1024], start=True, stop=True),
                          lambda e: e.matmul(PB[2], lhsT=onesf, rhs=Af[:, 0:512], start=True, stop=True),
                          lambda e: e.matmul(PB[3], lhsT=onesf, rhs=Af[:, 512:1024], start=True, stop=True)],
               reads=["Aa"], psum=["b0", "b1", "b2", "b3"], extra=[d_cf])
        P.auto(V, lambda e: e.tensor_copy(out=fl(Tt)[:, 0:512], in_=PB[2]), writes=["Tt"], psum=["b2"])
        P.auto(V, lambda e: e.tensor_copy(out=fl(Tt)[:, 512:1024], in_=PB[3]), writes=["Tt"], psum=["b3"])
        cur, cname = Tt, "Tt"
        for si, sh in enumerate((1, 2, 4, 8, 16)):
            nxt, nname = (sc0, "sc0") if si % 2 == 0 else (sc1, "sc1")
            P.auto(V, lambda e, cur=cur, nxt=nxt, sh=sh: e.tensor_tensor(out=nxt[:, sh:32, :], in0=cur[:, sh:32, :], in1=cur[:, 0:32 - sh, :], op=ALU.add), reads=[cname], writes=[nname])
            P.auto(V, lambda e, cur=cur, nxt=nxt, sh=sh: e.tensor_copy(out=nxt[:, 0:sh, :], in_=cur[:, 0:sh, :]), reads=[cname], writes=[nname])
            cur, cname = nxt, nname
        P.auto(V, lambda e, cur=cur: e.tensor_tensor(out=fl(tmpc), in0=fl(cur), in1=fl(Tt), op=ALU.subtract), reads=[cname, "Tt"], writes=["tmpc"])
        P.auto(V, lambda e: e.tensor_tensor(out=fl(rank)[:, 0:512], in0=fl(tmpc)[:, 0:512], in1=PB[0], op=ALU.add), reads=["tmpc"], writes=["rank"], psum=["b0"])
        P.auto(V, lambda e: e.tensor_tensor(out=fl(rank)[:, 512:1024], in0=fl(tmpc)[:, 512:1024], in1=PB[1], op=ALU.add), reads=["tmpc"], writes=["rank"], psum=["b1"])
        P.auto(V, lambda e: e.tensor_scalar(out=fl(rank), in0=fl(rank), scalar1=float(CAP - 1), scalar2=None, op0=ALU.min), writes=["rank"])
        P.auto(V, lambda e: e.tensor_tensor(out=rank, in0=rank, in1=ecap.unsqueeze(1).to_broadcast([128, 32, 32]), op=ALU.add), writes=["rank"], extra=[d_cf])
        P.auto(V, lambda e: e.tensor_tensor(out=fl(tmpc), in0=fl(oh1), in1=fl(rank), op=ALU.mult), reads=["oh1", "rank"], writes=["tmpc"])
        P.auto(V, lambda e: e.tensor_reduce(out=s12[:, 0, :], in_=tmpc, axis=AX.X, op=ALU.add), reads=["tmpc"], writes=["s12a"])
        P.auto(V, lambda e: e.tensor_tensor(out=fl(tmpc), in0=fl(oh2), in1=fl(rank), op=ALU.mult), reads=["oh2", "rank"], writes=["tmpc"])
        P.auto(V, lambda e: e.tensor_reduce(out=s12[:, 1, :], in_=tmpc, axis=AX.X, op=ALU.add), reads=["tmpc"], writes=["s12b"])
        P.auto(V, lambda e: e.tensor_scalar(out=fl(s12), in0=fl(s12), scalar1=float(NSLOT - 1), scalar2=0.0, op0=ALU.min, op1=ALU.max), reads=["s12a", "s12b"], writes=["s12a", "s12b"])
        P.auto(V, lambda e: e.tensor_copy(out=SLI, in_=s12), reads=["s12a", "s12b"], writes=["SLI"])
        tap("sli", SLI, [P.lw["SLI"]], I32)
        tap("pw", PW, [P.lw["PW1"]])

        P.base_waits = [P.lw["SLI"], P.lw["PW1"], P.lw["PW0"]] + finals_h1
        AR.reset(pers_mark)
        stt_c = AR.alloc([128, 16], F32)
        wg = [AR.alloc([128, 8, 512], BF16) for _ in range(2)]
        wu = [AR.alloc([128, 8, 512], BF16) for _ in range(2)]
        wd = [AR.alloc([128, 4, 1024], BF16) for _ in range(2)]

        def e_loads_w(e_):
            i = e_ % 2
            P.adma("gpsimd", wg[i], w_eg[e_].rearrange("(c p) n -> p c n", p=128), "wg%d" % i, writes=["wg%d" % i])
            P.adma("gpsimd", wu[i], w_eu[e_].rearrange("(c p) n -> p c n", p=128), "wu%d" % i, writes=["wu%d" % i])
            P.adma("gpsimd", wd[i], w_ed[e_].rearrange("(c p) n -> p c n", p=128), "wd%d" % i, writes=["wd%d" % i])

        e_loads_w(0)
        hb = [AR.alloc([128, 1024], F32) for _ in range(2)]
        sc_toks = []
        for blk in range(NB):
            i = blk % 2
            P.adma("sync", hb[i], h1f[blk * 128:(blk + 1) * 128, :], "hb%d" % i, writes=["hb%d" % i])
            for k in range(2):
                off = SLI[:, k, blk:blk + 1].bitcast(U32)
                src = hb[i]
                sc_toks.append(P.acdma("gpsimd", lambda e, off=off, src=src: e.indirect_dma_start(
                    out=xs_d, out_offset=bass.IndirectOffsetOnAxis(ap=off, axis=0), in_=src, in_offset=None),
                    "sc%d_%d" % (i, k), reads=["hb%d" % i, "SLI"]))

        XE = [AR.alloc([128, 3, 1024], BF16) for _ in range(2)]
        XT = AR.alloc([128, 8, 384], BF16)
        sg = AR.alloc([128, 384], F32)
        HT = AR.alloc([128, 4, 384], BF16)
        YSb = [AR.alloc([128, 1024], F32) for _ in range(2)]
        NE = 32
        ys_toks = []

        def e_loads(e_):
            i = e_ % 2
            if e_ > 0:
                e_loads_w(e_)
            P.adma("sync", XE[i], xs_d[e_ * CAP:(e_ + 1) * CAP, :].rearrange("(b p) d -> p b d", p=128), "xe%d" % i, writes=["xe%d" % i], extra=sc_toks)

        def e_compute(e_):
            i = e_ % 2
            for pr in range(4):
                bank = pr % 2
                fns = []
                for cc in range(2):
                    c = 2 * pr + cc
                    for sb in range(3):
                        o_ = PBb[bank][:, cc * 384 + sb * 128: cc * 384 + sb * 128 + 128]
                        i_ = XE[i][:, sb, c * 128:(c + 1) * 128]
                        fns.append(lambda e, o_=o_, i_=i_: e.transpose(out=o_, in_=i_, identity=ident))
                P.auto("tensor", fns, reads=["xe%d" % i], psum=["b%d" % bank])
                dst = XT[:, 2 * pr:2 * pr + 2, :].rearrange("p a b -> p (a b)")
                if bank == 0:
                    P.auto("vector", lambda e, dst=dst: e.tensor_copy(out=dst, in_=PBb[0][:, 0:768]), writes=["XT%d" % pr], psum=["b0"])
                else:
                    P.auto("scalar", lambda e, dst=dst: e.copy(out=dst, in_=PBb[1][:, 0:768]), writes=["XT%d" % pr], psum=["b1"])
            xt_names = ["XT%d" % pr for pr in range(4)]
            for f in range(4):
                gb_ = 2 + 2 * (f % 2)
                ub_ = 3 + 2 * (f % 2)
                fns = []
                for c in range(8):
                    l_ = wg[i][:, c, f * 128:(f + 1) * 128]
                    fns.append(lambda e, l_=l_, c=c, gb_=gb_: e.matmul(PB[gb_][:, 0:384], lhsT=l_, rhs=XT[:, c, :], start=(c == 0), stop=(c == 7)))
                P.auto("tensor", fns, reads=["wg%d" % i] + xt_names, psum=["b%d" % gb_])
                fns = []
                for c in range(8):
                    l_ = wu[i][:, c, f * 128:(f + 1) * 128]
                    fns.append(lambda e, l_=l_, c=c, ub_=ub_: e.matmul(PB[ub_][:, 0:384], lhsT=l_, rhs=XT[:, c, :], start=(c == 0), stop=(c == 7)))
                P.auto("tensor", fns, reads=["wu%d" % i] + xt_names, psum=["b%d" % ub_])
                P.auto("scalar", lambda e, gb_=gb_: e.activation(out=sg, in_=PB[gb_][:, 0:384], func=AF.Silu), writes=["sg"], psum=["b%d" % gb_])
                P.auto("vector", lambda e, ub_=ub_, f=f: e.tensor_tensor(out=HT[:, f, :], in0=sg, in1=PB[ub_][:, 0:384], op=ALU.mult), reads=["sg"], writes=["HT%d" % f], psum=["b%d" % ub_])
            ht_names = ["HT%d" % f for f in range(4)]
            for sb in range(3):
                k = (e_ * 3 + sb) % 2
                fns = []
                for half in range(2):
                    for f in range(4):
                        l_ = HT[:, f, sb * 128:(sb + 1) * 128]
                        r_ = wd[i][:, f, half * 512:(half + 1) * 512]
                        fns.append(lambda e, l_=l_, r_=r_, f=f, half=half: e.matmul(PB[6 + half], lhsT=l_, rhs=r_, start=(f == 0), stop=(f == 3)))
                P.auto("tensor", fns, reads=["wd%d" % i] + ht_names, psum=["b6", "b7"])
                ysb = YSb[k]
                P.auto("scalar", lambda e, ysb=ysb: e.copy(out=ysb[:, 0:512], in_=PB[6]), writes=["ysa%d" % k], psum=["b6"])
                P.auto("vector", lambda e, ysb=ysb: e.tensor_copy(out=ysb[:, 512:1024], in_=PB[7]), writes=["ysb%d" % k], psum=["b7"])
                r0 = e_ * CAP + sb * 128
                ys_toks.append(P.adma("sync", ys_d[r0:r0 + 128, :], ysb, "ysw%d" % k, reads=["ysa%d" % k, "ysb%d" % k]))

        e_loads(0)
        for e_ in range(NE):
            if e_ + 1 < NE:
                e_loads(e_ + 1)
            e_compute(e_)

        Y1_ = [AR.alloc([128, 1024], F32) for _ in range(2)]
        Y2_ = [AR.alloc([128, 1024], F32) for _ in range(2)]
        hc_ = [AR.alloc([128, 1024], F32) for _ in range(2)]
        R2_ = [AR.alloc([128, 1024], F32) for _ in range(2)]
        lng2 = AR.alloc([128, 1024], F32)
        lnb2 = AR.alloc([128, 1024], F32)
        P.adma("sync", lng2, ln2[0:1, :].to_broadcast([128, 1024]), "lng2", writes=["lng2"])
        P.adma("sync", lnb2, ln2[1:2, :].to_broadcast([128, 1024]), "lnb2", writes=["lnb2"])

        def c_loads(blk):
            pb = blk % 2
            P.adma("sync", hc_[pb], h1f[blk * 128:(blk + 1) * 128, :], "hc%d" % pb, writes=["hc%d" % pb])
            for k, yb in ((0, Y1_[pb]), (1, Y2_[pb])):
                off = SLI[:, k, blk:blk + 1].bitcast(U32)
                P.acdma("gpsimd", lambda e, off=off, yb=yb: e.indirect_dma_start(
                    out=yb, out_offset=None, in_=ys_d, in_offset=bass.IndirectOffsetOnAxis(ap=off, axis=0)),
                    "yg%d%d" % (k, pb), reads=["SLI"], writes=["Y%d%d" % (k, pb)], extra=ys_toks)

        def c_block(blk):
            pb = blk % 2
            if blk + 1 < NB:
                c_loads(blk + 1)
            Y1, Y2, hc, R2 = Y1_[pb], Y2_[pb], hc_[pb], R2_[pb]
            rn = "R2%d" % pb
            P.auto("scalar", lambda e: e.mul(out=R2, in_=hc, mul=ALPHA), reads=["hc%d" % pb], writes=[rn])
            P.auto(V, lambda e: e.scalar_tensor_tensor(out=R2, in0=Y1, scalar=PW[:, 0, blk:blk + 1], in1=R2, op0=ALU.mult, op1=ALU.add), reads=["Y0%d" % pb, "PW0"], writes=[rn])
            P.auto(V, lambda e: e.scalar_tensor_tensor(out=R2, in0=Y2, scalar=PW[:, 1, blk:blk + 1], in1=R2, op0=ALU.mult, op1=ALU.add), reads=["Y1%d" % pb, "PW1"], writes=[rn])
            layer_norm(R2, "lng2", "lnb2", lng2, lnb2, stt_c, rn)
            finals.append(P.adma("sync", out[blk * 128:(blk + 1) * 128, :], R2, "outw%d" % pb, reads=[rn]))

        c_loads(0)
        for blk in range(NB):
            c_block(blk)

        P.emit(finals)
    return nc, tap_out


def _consts(j):
    ident = np.eye(128, dtype=np.float32)
    rot = np.zeros((128, 128), np.float32)
    for m in range(128):
        if (m % 64) < 32:
            rot[m + 32, m] = -1.0
        else:
            rot[m - 32, m] = 1.0
    ones = np.ones((128, 128), np.float32)
    kk = np.arange(128)[:, None, None]
    r = np.arange(8)[None, :, None]
    qq = np.arange(512)[None, None, :]
    mask = ((r * 128 + kk) <= ((2 * (qq // 128) + j) * 128 + (qq % 128))).astype(np.float32)
    cbf = np.concatenate([ident, rot, ones, mask.reshape(128, 4096)], axis=1)
    cf = np.zeros((128, 512), np.float32)
    cf[:, 0:128] = ident
    cf[:, 128:256] = 1.0
    cf[:, 256:384] = (np.arange(128)[:, None] < np.arange(128)[None, :]).astype(np.float32)
    inv_freq = (np.float32(10000.0) ** (-np.arange(0, 64, 2, dtype=np.float32) / np.float32(64))).astype(np.float32)
    p = np.arange(128)
    cf[:, 384] = (inv_freq[(p % 64) % 32].astype(np.float64) / (2.0 * np.pi)).astype(np.float32)
    cf[:, 392:424] = (np.arange(32) * CAP)[None, :]
    cf[:, 424] = -0.5
    cf[:, 425] = 1e-5
    return np.ascontiguousarray(cbf), cf


def make_core_inputs(inp, c):
    b, j = c // 2, c % 2
    x = inp["x"]
    xb_ = x[b]
    blocks = xb_.reshape(64, 128, D)
    xo = np.ascontiguousarray(blocks[j::2].reshape(NOWN, D))
    xh = np.zeros((32, 2, D), np.float32)
    for m in range(32):
        blk = 2 * m + j
        if blk > 0:
            xh[m] = xb_[blk * 128 - 2: blk * 128]
    pos = np.asarray(inp["positions"][b], dtype=np.int32)
    poso = np.ascontiguousarray(pos.reshape(64, 128)[j::2].reshape(1, NOWN))
    cbf, cf = _consts(j)
    f = lambda a: np.ascontiguousarray(np.asarray(a, dtype=np.float32))
    return {
        "xf": f(xb_), "xo": xo, "xh": f(xh.reshape(64, D)), "posf": np.ascontiguousarray(pos.reshape(1, S)), "poso": poso,
        "w_in": f(inp["w_in"][0]), "b_gate": f(inp["b_gate"][0].reshape(1, 2048)),
        "lam_in": f(np.concatenate([inp["lambda_q1"][0], inp["lambda_k1"][0], inp["lambda_q2"][0], inp["lambda_k2"][0]]).reshape(1, 256)),
        "subln_g": f(inp["subln_g"][0].reshape(1, 128)), "w_o_att": f(inp["w_o_att"][0]), "conv_w": f(inp["conv_w"][0]),
        "w_o_conv": f(inp["w_o_conv"][0]), "w_mix": f(inp["w_mix_out"][0]),
        "ln1": f(np.stack([inp["ln1_g"][0], inp["ln1_b"][0]])), "ln2": f(np.stack([inp["ln2_g"][0], inp["ln2_b"][0]])),
        "w_rt": f(np.concatenate([inp["w_router_group"][0], inp["w_router_expert"][0]], axis=1)),
        "b_rt": f(np.concatenate([inp["b_router_group"][0], inp["b_router_expert"][0]]).reshape(1, 36)),
        "w_eg": f(inp["w_exp_gate"][0]), "w_eu": f(inp["w_exp_up"][0]), "w_ed": f(inp["w_exp_down"][0]),
        "cbf": cbf, "cf32": cf,
    }


def kernel(**inputs):
    inp = {k: np.asarray(v) for k, v in inputs.items()}
    nc, _ = build()
    in_maps = [make_core_inputs(inp, c) for c in range(8)]
    res = run_bass_kernel_spmd(nc, in_maps, core_ids=list(range(8)))
    outp = np.zeros((4, S, D), np.float32)
    for c in range(8):
        b, j = c // 2, c % 2
        o = np.asarray(res.results[c]["out"]).reshape(32, 128, D)
        outp[b].reshape(64, 128, D)[j::2] = o
    return outp
```

```python
import math
import numpy as np
import ml_dtypes
from contextlib import ExitStack
import concourse.bass as bass
import concourse.mybir as mybir
from concourse.bass_utils import run_bass_kernel_spmd

F32 = mybir.dt.float32
BF16 = mybir.dt.bfloat16
I32 = mybir.dt.int32
U32 = mybir.dt.uint32
AF = mybir.ActivationFunctionType
ALU = mybir.AluOpType
AX = mybir.AxisListType
ENGS = ["tensor", "vector", "scalar", "gpsimd", "sync"]

S = 8192
D = 1024
NOWN = 4096
CAP = 384
NSLOT = 32 * CAP
ALPHA = 2.0 ** 0.25
LAMBDA_INIT = 0.8 - 0.6 * math.exp(0.0)
TWO_PI = 2.0 * math.pi
NEG = -1.0e30


class Prog:
    def __init__(self, nc, es):
        self.nc = nc
        self.es = es
        self.ops = {e: [] for e in ENGS}
        self.cnt = {e: 0 for e in ENGS}
        self.esem = {e: es.enter_context(nc.semaphore("es_" + e)) for e in ENGS}
        self.dsem = {}
        self.dcnt = {}
        self.waited = {e: {} for e in ENGS}
        self.base_waits = []

    def _waits(self, eng, waits):
        best = {}
        for w in list(waits) + list(self.base_waits):
            if w is None:
                continue
            sem, val, key = w
            if key not in best or best[key][1] < val:
                best[key] = (sem, val)
        out = []
        for key, (sem, val) in best.items():
            if self.waited[eng].get(key, 0) >= val:
                continue
            self.waited[eng][key] = val
            out.append((sem, val))
        return out

    def op(self, eng, fn, waits=(), signal=True):
        ws = self._waits(eng, waits)
        inc = None
        tok = None
        if signal:
            self.cnt[eng] += 1
            inc = (self.esem[eng], 1)
            tok = (self.esem[eng], self.cnt[eng], "e_" + eng)
        self.ops[eng].append((fn, ws, inc))
        return tok

    def _dsem(self, sem):
        if sem not in self.dsem:
            self.dsem[sem] = self.es.enter_context(self.nc.semaphore("ds_" + sem))
            self.dcnt[sem] = 0
        return self.dsem[sem]

    def dma(self, q, out, in_, sem, waits=(), **kw):
        return self.cdma(q, lambda e: e.dma_start(out=out, in_=in_, **kw), sem, waits)

    def cdma(self, q, fn, sem, waits=()):
        s = self._dsem(sem)
        ws = self._waits(q, waits)
        self.dcnt[sem] += 16
        self.ops[q].append((fn, ws, (s, 16)))
        return (s, self.dcnt[sem], "d_" + sem)

    def _auto_waits(self, reads, writes, psum):
        if not hasattr(self, "lw"):
            self.lw, self.rd, self.pa = {}, {}, {}
        ws = []
        for r in reads:
            ws.append(self.lw.get(r))
        for w in writes:
            ws.append(self.lw.get(w))
            ws.extend(self.rd.get(w, []))
        for p in psum:
            ws.append(self.pa.get(p))
        return ws

    def _auto_done(self, tok, reads, writes, psum):
        for r in reads:
            self.rd.setdefault(r, []).append(tok)
        for w in writes:
            self.lw[w] = tok
            self.rd[w] = []
        for p in psum:
            self.pa[p] = tok

    def auto(self, eng, fns, reads=(), writes=(), psum=(), extra=()):
        if not isinstance(fns, (list, tuple)):
            fns = [fns]
        ws = self._auto_waits(reads, writes, psum) + list(extra)
        tok = None
        for k, fn in enumerate(fns):
            tok = self.op(eng, fn, waits=ws if k == 0 else (), signal=(k == len(fns) - 1))
        self._auto_done(tok, reads, writes, psum)
        return tok

    def adma(self, q, out, in_, sem, reads=(), writes=(), extra=(), **kw):
        ws = self._auto_waits(reads, writes, ()) + list(extra)
        tok = self.dma(q, out, in_, sem, waits=ws, **kw)
        self._auto_done(tok, reads, writes, ())
        return tok

    def acdma(self, q, fn, sem, reads=(), writes=(), extra=()):
        ws = self._auto_waits(reads, writes, ()) + list(extra)
        tok = self.cdma(q, fn, sem, waits=ws)
        self._auto_done(tok, reads, writes, ())
        return tok

    def emit(self, final_waits):
        nc = self.nc
        with nc.Block() as block:
            def mk(eng):
                def body(e):
                    for fn, ws, inc in self.ops[eng]:
                        for sem, val in ws:
                            e.wait_ge(sem, val)
                        ins = fn(e)
                        if inc is not None:
                            ins.then_inc(inc[0], inc[1])
                    if eng == "sync":
                        for w in final_waits:
                            if w is not None:
                                e.wait_ge(w[0], w[1])
                return body
            block.tensor(mk("tensor"))
            block.vector(mk("vector"))
            block.scalar(mk("scalar"))
            block.gpsimd(mk("gpsimd"))
            block.sync(mk("sync"))


class Arena:
    def __init__(self, t, nwords):
        self.t = t
        self.n = nwords
        self.top = 0

    def mark(self):
        return self.top

    def reset(self, m):
        self.top = m

    def alloc(self, shape, dt):
        per = 1
        for s_ in shape[1:]:
            per *= s_
        if dt == BF16:
            words = (per + 1) // 2
        else:
            words = per
        words = (words + 7) // 8 * 8
        a = self.top
        self.top += words
        assert self.top <= self.n, ("arena overflow", self.top, self.n)
        v = self.t[:, a:a + words]
        if dt == BF16:
            v = v.bitcast(BF16)[:, 0:per]
        elif dt == I32:
            v = v.bitcast(I32)[:, 0:per]
        else:
            v = v[:, 0:per]
        if len(shape) == 3:
            v = v.rearrange("p (a b) -> p a b", a=shape[1])
        elif len(shape) == 4:
            v = v.rearrange("p (a b c) -> p a b c", a=shape[1], b=shape[2])
        if shape[0] != 128:
            v = v[0:shape[0]]
        return v


def build(upto="all", taps=(), NQT=8):
    nc = bass.Bass("TRN2", target_bir_lowering=False)
    din = lambda name, shape, dt: nc.dram_tensor(name, shape, dt, kind="ExternalInput").ap()
    xf = din("xf", [S, D], F32)
    xo = din("xo", [NOWN, D], F32)
    xh = din("xh", [64, D], F32)
    posf = din("posf", [1, S], I32)
    poso = din("poso", [1, NOWN], I32)
    w_in = din("w_in", [D, 5120], F32)
    b_gate = din("b_gate", [1, 2048], F32)
    lam_in = din("lam_in", [1, 256], F32)
    subln_g = din("subln_g", [1, 128], F32)
    w_o_att = din("w_o_att", [512, D], F32)
    conv_w = din("conv_w", [3, 512], F32)
    w_o_conv = din("w_o_conv", [512, D], F32)
    w_mix = din("w_mix", [D, D], F32)
    ln1 = din("ln1", [2, D], F32)
    ln2 = din("ln2", [2, D], F32)
    w_rt = din("w_rt", [D, 36], F32)
    b_rt = din("b_rt", [1, 36], F32)
    w_eg = din("w_eg", [32, D, 512], F32)
    w_eu = din("w_eu", [32, D, 512], F32)
    w_ed = din("w_ed", [32, 512, D], F32)
    cbf = din("cbf", [128, 384 + 4096], F32)
    cf32 = din("cf32", [128, 512], F32)
    out = nc.dram_tensor("out", [NOWN, D], F32, kind="ExternalOutput").ap()
    h1f = nc.dram_tensor("h1f", [NOWN, D], F32, kind="Internal").ap()
    xs_d = nc.dram_tensor("xs_d", [NSLOT, D], BF16, kind="Internal").ap()
    ys_d = nc.dram_tensor("ys_d", [NSLOT, D], F32, kind="Internal").ap()
    tap_out = {}

    with ExitStack() as es:
        P = Prog(nc, es)
        NW = 51 * 1024
        arena_t = es.enter_context(nc.sbuf_tensor("arena", [128, NW], F32))
        AR = Arena(arena_t, NW)
        psum = es.enter_context(nc.psum_tensor("psum", [128, 8, 512], F32))
        PB = [psum[:, i, :] for i in range(8)]
        PBb = [psum[:, i, :].bitcast(BF16) for i in range(8)]
        finals = []

        def tap(name, ap, waits, dt=F32):
            if name not in taps:
                return
            shp = list(ap.shape)
            o = nc.dram_tensor("tap_" + name, shp, dt, kind="ExternalOutput").ap()
            tap_out[name] = shp
            finals.append(P.dma("sync", o, ap, "tap_" + name, waits=waits))

        cb_t = AR.alloc([128, 384], BF16)
        ident = cb_t[:, 0:128]
        rotm = cb_t[:, 128:256]
        onesb = cb_t[:, 256:384]
        cf_t = AR.alloc([128, 512], F32)
        identf = cf_t[:, 0:128]
        onesf = cf_t[:, 128:256]
        trif = cf_t[:, 256:384]
        invf = cf_t[:, 384:385]
        ecap = cf_t[:, 392:424]
        mhalf = cf_t[:, 424:425]
        epsc = cf_t[:, 425:426]
        d_cb = P.dma("gpsimd", cb_t, cbf[:, 0:384], "cb")
        d_cf = P.dma("sync", cf_t, cf32, "cf")
        QO = AR.alloc([128, 4, NOWN], BF16)
        small = AR.alloc([128, 64], F32)
        neglam = small[:, 0:1]
        gsc = small[:, 1:2]
        lamv = AR.alloc([128, 256], F32)
        slg = AR.alloc([128, 128], F32)
        d_lam = P.dma("sync", lamv, lam_in.to_broadcast([128, 256]), "lam")
        d_slg = P.dma("sync", slg[:, 0:1], subln_g.rearrange("o p -> p o"), "slg", allow_slow_non_contiguous=True)
        lt = small[:, 8:10]
        t_l1 = P.op("vector", lambda e: e.tensor_tensor(out=lamv[:, 0:64], in0=lamv[:, 0:64], in1=lamv[:, 64:128], op=ALU.mult), waits=[d_lam])
        t_l2 = P.op("vector", lambda e: e.tensor_tensor(out=lamv[:, 128:192], in0=lamv[:, 128:192], in1=lamv[:, 192:256], op=ALU.mult), waits=[d_lam])
        t_l3 = P.op("vector", lambda e: e.tensor_reduce(out=lt[:, 0:1], in_=lamv[:, 0:64], axis=AX.X, op=ALU.add), waits=[t_l1])
        t_l4 = P.op("vector", lambda e: e.tensor_reduce(out=lt[:, 1:2], in_=lamv[:, 128:192], axis=AX.X, op=ALU.add), waits=[t_l2])
        t_l5 = P.op("scalar", lambda e: e.activation(out=small[:, 10:12], in_=lt, func=AF.Exp), waits=[t_l3, t_l4])
        t_l6 = P.op("vector", lambda e: e.tensor_tensor(out=small[:, 12:13], in0=small[:, 11:12], in1=small[:, 10:11], op=ALU.subtract), waits=[t_l5])
        t_l7 = P.op("vector", lambda e: e.tensor_scalar(out=neglam, in0=small[:, 12:13], scalar1=-LAMBDA_INIT, scalar2=None, op0=ALU.add), waits=[t_l6])
        t_g = P.op("vector", lambda e: e.tensor_scalar(out=gsc, in0=slg[:, 0:1], scalar1=1.0 - LAMBDA_INIT, scalar2=None, op0=ALU.mult), waits=[d_slg])
        t_consts = [d_cb, d_cf, t_l7, t_g]
        LG = AR.alloc([128, 32, 36], F32)
        PW = AR.alloc([128, 2, 32], F32)
        SLI = AR.alloc([128, 2, 32], I32)
        pers_mark = AR.mark()

        KT = AR.alloc([128, 2, S], BF16)
        maskt_t = AR.alloc([128, 4096], BF16)
        maskt = maskt_t.rearrange("p (r q) -> p r q", r=8)
        d_mask = P.dma("gpsimd", maskt_t, cbf[:, 384:384 + 4096], "mask", max_dma_last_dim=4096)
        VV = AR.alloc([128, 64, 256], BF16)
        wq = AR.alloc([128, 8, 512], BF16)
        wkv = AR.alloc([128, 8, 512], BF16)
        XBt = [AR.alloc([128, 4, D], BF16) for _ in range(2)]
        XTt = [AR.alloc([128, 8, 512], BF16) for _ in range(2)]
        post2 = [AR.alloc([128, 512], F32) for _ in range(2)]
        tq = AR.alloc([128, 512], F32)
        ki = AR.alloc([128, 512], I32)
        cst2 = [AR.alloc([128, 512], F32) for _ in range(2)]
        snt2 = [AR.alloc([128, 512], F32) for _ in range(2)]
        qsb = [AR.alloc([128, 512], BF16) for _ in range(2)]
        ra = [AR.alloc([128, 512], F32) for _ in range(2)]
        rb = [AR.alloc([128, 512], F32) for _ in range(2)]
        PT = [AR.alloc([128, 2, 512], BF16) for _ in range(3)]
        EE = [AR.alloc([128, 512], F32) for _ in range(4)]

        st = {"xb_free": [None, None], "xt_free": [None, None], "tp_free": [None, None],
              "kp_free": [[], []], "rp_free": [None, None], "vp_free": [None, None],
              "tab_free": [[], []], "qs_free": [None, None], "ra_free": [None, None],
              "n_kp": 0, "n_tp": 0, "n_vp": 0, "n_x": 0, "n_tab": 0, "last_sin": None}

        def load_w(dst, src_cols, sem, waits):
            return P.dma("gpsimd", dst, src_cols.rearrange("(c p) n -> p c n", p=128), sem, waits=waits)

        def tables_dma(pos_src, t0):
            ti = st["n_tab"] % 2
            st["n_tab"] += 1
            w0 = list(st["tab_free"][ti])
            d = P.dma("gpsimd", post2[ti], pos_src[0:1, t0:t0 + 512].to_broadcast([128, 512]), "pos%d" % ti, waits=w0)
            return ti, d, w0

        def tables(pos_src, t0):
            return tables_compute(*tables_dma(pos_src, t0))

        def tables_compute(ti, d, w0):
            post = post2[ti]
            prev = [st["last_sin"]]
            for dst, add in ((snt2[ti], 0.0), (cst2[ti], 0.25)):
                a = P.op("vector", lambda e, add=add: e.tensor_scalar(out=tq, in0=post, scalar1=invf, scalar2=add, op0=ALU.mult, op1=ALU.add), waits=[d, d_cf] + prev)
                b = P.op("vector", lambda e: e.tensor_copy(out=ki, in_=tq), waits=[a])
                c = P.op("vector", lambda e: e.tensor_tensor(out=tq, in0=tq, in1=ki, op=ALU.subtract), waits=[b])
                s_ = P.op("scalar", lambda e, dst=dst: e.activation(out=dst, in_=tq, func=AF.Sin, scale=TWO_PI), waits=[c] + w0)
                prev = [s_]
            st["last_sin"] = prev[0]
            return prev[0], ti

        def load_x_tile(src, row0):
            i = st["n_x"] % 2
            st["n_x"] += 1
            xb = XBt[i]
            d = P.dma("gpsimd", xb, src[row0:row0 + 512, :].rearrange("(b p) d -> p b d", p=128),
                      "xb%d" % i, waits=[st["xb_free"][i]])
            return i, d

        def transpose_tile(i, dx):
            xb = XBt[i]
            xt = XTt[i]
            evs = []
            last_t = None
            for g in range(4):
                bk = st["n_tp"] % 2
                st["n_tp"] += 1
                for cc in range(2):
                    c = 2 * g + cc
                    for blk in range(4):
                        last = (cc == 1 and blk == 3)
                        o_ = PBb[bk][:, cc * 512 + blk * 128: cc * 512 + blk * 128 + 128]
                        i_ = xb[:, blk, c * 128:(c + 1) * 128]
                        tk = P.op("tensor", lambda e, o_=o_, i_=i_: e.transpose(out=o_, in_=i_, identity=ident),
                                  waits=[dx, d_cb, st["tp_free"][bk]], signal=last)
                        if last:
                            last_t = tk
                dst = xt[:, 2 * g:2 * g + 2, :].rearrange("p a b -> p (a b)")
                src = PBb[bk]
                if g % 2 == 0:
                    ev = P.op("vector", lambda e, dst=dst, src=src: e.tensor_copy(out=dst, in_=src), waits=[last_t, st["xt_free"][i]])
                else:
                    ev = P.op("scalar", lambda e, dst=dst, src=src: e.copy(out=dst, in_=src), waits=[last_t, st["xt_free"][i]])
                st["tp_free"][bk] = ev
                evs.append(ev)
            st["xb_free"][i] = last_t
            return evs

        def proj_part1(xt, evs, wt, wcol, tab_tok, ti, extra_w):
            kb = st["n_kp"] % 2
            st["n_kp"] += 1
            kp = PB[2 + kb]
            q_ = qsb[kb]
            ra_ = ra[kb]
            cs_ = cst2[ti]
            mm = None
            for c in range(8):
                l_ = wt[:, c, wcol:wcol + 128]
                r_ = xt[:, c, :]
                mm = P.op("tensor", lambda e, l_=l_, r_=r_, c=c: e.matmul(kp, lhsT=l_, rhs=r_, start=(c == 0), stop=(c == 7)),
                          waits=[evs[c // 2]] + st["kp_free"][kb] + extra_w, signal=(c == 7))
            cp = P.op("scalar", lambda e: e.copy(out=q_, in_=kp), waits=[mm, st["qs_free"][kb]])
            a = P.op("vector", lambda e: e.tensor_tensor(out=ra_, in0=kp, in1=cs_, op=ALU.mult), waits=[mm, cp, tab_tok, st["ra_free"][kb]])
            st["kp_free"][kb] = [a, cp]
            return {"kb": kb, "cp": cp, "a": a, "tab": tab_tok, "ti": ti, "mm": mm}

        def proj_part2(cx, dst):
            kb = cx["kb"]
            rp = PB[4 + kb]
            q_ = qsb[kb]
            ra_ = ra[kb]
            rb_ = rb[kb]
            sn_ = snt2[cx["ti"]]
            rm = P.op("tensor", lambda e: e.matmul(rp, lhsT=rotm, rhs=q_, start=True, stop=True), waits=[cx["cp"], d_cb, st["rp_free"][kb]])
            st["qs_free"][kb] = rm
            b = P.op("vector", lambda e: e.tensor_tensor(out=rb_, in0=rp, in1=sn_, op=ALU.mult), waits=[rm, cx["tab"], st["ra_free"][kb]])
            st["rp_free"][kb] = b
            f = P.op("vector", lambda e: e.tensor_tensor(out=dst, in0=ra_, in1=rb_, op=ALU.add), waits=[cx["a"], b])
            st["ra_free"][kb] = f
            return f, b, rm

        pre = {}

        def prefetch(key, src, pos_src, T):
            i, dx = load_x_tile(src, T * 512)
            tab, ti = tables(pos_src, T * 512)
            pre[key] = (i, dx, tab, ti)

        def prefetch_dma(key, src, pos_src, T):
            i, dx = load_x_tile(src, T * 512)
            return (key, i, dx, tables_dma(pos_src, T * 512))

        def prefetch_compute(pf):
            key, i, dx, td = pf
            tab, ti = tables_compute(*td)
            pre[key] = (i, dx, tab, ti)

        def a_tile(T, dwk, dwv, last, nxt):
            i, dx, tab, ti = pre.pop(("a", T))
            pf = prefetch_dma(*nxt) if nxt is not None else None
            evs = transpose_tile(i, dx)
            cxs = [proj_part1(XTt[i], evs, wkv, hl * 128, tab, ti, [dwk]) for hl in range(2)]
            mm = None
            for blk in range(4):
                vb = st["n_vp"] % 2
                st["n_vp"] += 1
                vp = PB[6 + vb][:, 0:256]
                for c in range(8):
                    l_ = XTt[i][:, c, blk * 128:(blk + 1) * 128]
                    r_ = wkv[:, c, 256:512]
                    mm = P.op("tensor", lambda e, l_=l_, r_=r_, c=c, vp=vp: e.matmul(vp, lhsT=l_, rhs=r_, start=(c == 0), stop=(c == 7)),
                              waits=evs + [dwv, st["vp_free"][vb]], signal=(c == 7))
                o_ = VV[:, T * 4 + blk, :]
                vts = P.op("scalar", lambda e, o_=o_, vp=vp: e.copy(out=o_, in_=vp), waits=[mm])
                st["vp_free"][vb] = vts
                last.append(vts)
            st["xt_free"][i] = mm
            if pf is not None:
                prefetch_compute(pf)
            tabfree = []
            for hl in range(2):
                f, b, rm = proj_part2(cxs[hl], KT[:, hl, T * 512:(T + 1) * 512])
                tabfree.append(b)
                last.append(f)
            st["tab_free"][ti] = tabfree

        def phase_A(hp, then_q):
            dwk = load_w(wkv[:, :, 0:256], w_in[:, 512 + hp * 256: 512 + hp * 256 + 256], "wk", [])
            dwv = load_w(wkv[:, :, 256:512], w_in[:, 1024 + hp * 256: 1024 + hp * 256 + 256], "wv", [])
            last = []
            prefetch(("a", 0), xf, posf, 0)
            for T in range(16):
                if T + 1 < 16:
                    nxt = (("a", T + 1), xf, posf, T + 1)
                elif then_q:
                    nxt = (("q", 0), xo, poso, 0)
                else:
                    nxt = None
                a_tile(T, dwk, dwv, last, nxt)
            return last

        def q_tile(T, dwq, last):
            i, dx, tab, ti = pre.pop(("q", T))
            pf = prefetch_dma(("q", T + 1), xo, poso, T + 1) if T + 1 < NQT else None
            evs = transpose_tile(i, dx)
            tabfree = []
            rm = None
            cxs = {}
            cxs[0] = proj_part1(XTt[i], evs, wq, 0, tab, ti, [dwq])
            for h in range(4):
                if h + 1 < 4:
                    cxs[h + 1] = proj_part1(XTt[i], evs, wq, (h + 1) * 128, tab, ti, [dwq])
                if h == 3 and pf is not None:
                    prefetch_compute(pf)
                f, b, rm = proj_part2(cxs[h], QO[:, h, T * 512:(T + 1) * 512])
                tabfree.append(b)
                last.append(f)
            st["tab_free"][ti] = tabfree
            st["xt_free"][i] = rm

        def phase_Q():
            dwq = load_w(wq, w_in[:, 0:512], "wq", [])
            last = []
            for T in range(NQT):
                q_tile(T, dwq, last)
            return last

        ACC0 = ra[0]
        att = {"a0": None, "accL_free": None, "l6_free": None, "s_free": [None, None], "pt_free": [[], [], []], "acc_free": None, "n_s": 0, "n_pt": 0, "ee_free": None, "pending": None}

        def att_unit(i, hl, h):
            nkb = 8 * i + 8
            qk_tok = {}
            qrange = slice(i * 512, (i + 1) * 512)

            def col0(kb):
                r = kb - 8 * i
                return 128 * (r // 2) if r > 0 else 0

            def issue_qk(kb):
                s = kb % 2
                c0 = col0(kb)
                krange = slice(kb * 128, (kb + 1) * 128)
                qr = slice(i * 512 + c0, (i + 1) * 512)
                P.op("tensor", lambda e: e.matmul(psum[:, 2 * s, c0:512], lhsT=KT[0:64, hl, krange], rhs=QO[0:64, h, qr], start=True, stop=True),
                     waits=[att["s_free"][s]], signal=False)
                t = P.op("tensor", lambda e: e.matmul(psum[:, 2 * s + 1, c0:512], lhsT=KT[64:128, hl, krange], rhs=QO[64:128, h, qr], start=True, stop=True))
                qk_tok[kb] = (t, s)

            def exp_step(kb):
                t, s = qk_tok[kb]
                c0 = col0(kb)
                pi = att["n_pt"] % 3
                att["n_pt"] += 1
                pt = PT[pi]
                ex = P.op("scalar", lambda e: e.activation(out=pt[:, :, c0:512], in_=psum[:, 2 * s:2 * s + 2, c0:512], func=AF.Exp, scale=0.125), waits=[t] + att["pt_free"][pi])
                att["s_free"][s] = ex
                return ex, pt, pi

            def pv_step(kb, ex, pt, pi):
                pv_w = ex
                c0 = col0(kb)
                if kb >= 8 * i:
                    r = kb - 8 * i
                    pv_w = P.op("vector", lambda e: e.tensor_tensor(out=pt[:, :, c0:512], in0=pt[:, :, c0:512],
                                                                      in1=maskt[:, r, c0:512].unsqueeze(1).to_broadcast([128, 2, 512 - c0]), op=ALU.mult),
                                waits=[ex, d_mask])
                s0 = (kb == 0)
                s1 = (kb == nkb - 1)
                w0 = [pv_w, att["acc_free"]] if kb == 0 else [pv_w]
                vv = VV[:, kb, hl * 128:(hl + 1) * 128]
                P.op("tensor", lambda e: e.matmul(PB[4][:, c0:512], lhsT=vv, rhs=pt[:, 0, c0:512], start=s0, stop=s1), waits=w0, signal=False)
                P.op("tensor", lambda e: e.matmul(PB[5][:, c0:512], lhsT=vv, rhs=pt[:, 1, c0:512], start=s0, stop=s1), signal=False)
                pv = P.op("tensor", lambda e: e.matmul(PB[7][:, c0:512], lhsT=onesb, rhs=pt[:, 1, c0:512], start=s0, stop=s1))
                if kb == 0:
                    a0 = P.op("vector", lambda e: e.tensor_copy(out=ACC0, in_=pt[:, 0, :]), waits=[pv_w, att["accL_free"]])
                else:
                    a0 = P.op("vector", lambda e: e.tensor_tensor(out=ACC0[:, c0:512], in0=ACC0[:, c0:512], in1=pt[:, 0, c0:512], op=ALU.add), waits=[pv_w, att["a0"]])
                att["a0"] = a0
                att["pt_free"][pi] = [pv, a0]
                return pv

            issue_qk(0)
            issue_qk(1)
            pv = None
            for kb in range(nkb):
                ex, pt, pi = exp_step(kb)
                if kb + 2 < nkb:
                    issue_qk(kb + 2)
                pv = pv_step(kb, ex, pt, pi)
                if kb == 2 and att["pending"] is not None:
                    att["pending"]()
                    att["pending"] = None
            wfree = [att["ee_free"]]
            lsum = P.op("tensor", lambda e: e.matmul(PB[6], lhsT=onesf, rhs=ACC0, start=True, stop=True), waits=[att["a0"], att["acc_free"], att["l6_free"], d_cf])
            att["accL_free"] = lsum
            e0 = P.op("vector", lambda e: e.reciprocal(out=EE[0], in_=PB[6]), waits=[pv, lsum] + wfree)
            e1 = P.op("vector", lambda e: e.reciprocal(out=EE[1], in_=PB[7]), waits=[pv, lsum] + wfree)
            e2 = P.op("vector", lambda e: e.tensor_tensor(out=EE[0], in0=PB[4], in1=EE[0], op=ALU.mult), waits=[e0])
            e3 = P.op("vector", lambda e: e.tensor_tensor(out=EE[1], in0=PB[5], in1=EE[1], op=ALU.mult), waits=[e1])
            att["acc_free"] = e3
            e4 = P.op("vector", lambda e: e.scalar_tensor_tensor(out=EE[2], in0=EE[1], scalar=neglam, in1=EE[0], op0=ALU.mult, op1=ALU.add), waits=[e2, e3, t_l7] + wfree)
            e5 = P.op("gpsimd", lambda e: e.tensor_tensor(out=EE[3], in0=EE[2], in1=EE[2], op=ALU.mult), waits=[e4] + wfree)

            def finish():
                ss = P.op("tensor", lambda e: e.matmul(PB[6], lhsT=onesf, rhs=EE[3], start=True, stop=True), waits=[e5, e3, d_cf, att["l6_free"]])
                e6 = P.op("scalar", lambda e: e.activation(out=EE[3], in_=PB[6], func=AF.Ln, scale=1.0 / 128.0, bias=epsc), waits=[ss])
                att["l6_free"] = e6
                e7 = P.op("scalar", lambda e: e.activation(out=EE[3], in_=EE[3], func=AF.Exp, scale=-0.5), waits=[e6])
                e8 = P.op("vector", lambda e: e.tensor_tensor(out=EE[2], in0=EE[2], in1=EE[3], op=ALU.mult), waits=[e7])
                e9 = P.op("vector", lambda e: e.tensor_scalar(out=QO[:, h, qrange], in0=EE[2], scalar1=gsc, scalar2=None, op0=ALU.mult), waits=[e8, t_g])
                att["ee_free"] = e9
                att["last"] = e9
            att["pending"] = finish

        def attention(hp):
            for i in range(NQT):
                for hl in range(2):
                    att_unit(i, hl, 2 * hp + hl)
            att["pending"]()
            att["pending"] = None
            return att["last"]

        att_last = None
        if upto == "consts":
            tap("small", small, t_consts)
            P.emit(finals)
            return nc, tap_out
        for hp in range(2):
            kvt = phase_A(hp, hp == 0)
            if upto == "A":
                tap("kt", KT[:, :, 0:2048], kvt, BF16)
                tap("vv", VV[:, 0:8, :], kvt, BF16)
                P.emit(finals)
                return nc, tap_out
            qtoks = phase_Q() if hp == 0 else []
            P.base_waits = [t for t in kvt + qtoks if t is not None] + t_consts
            att_last = attention(hp)
            P.base_waits = [att_last]
            if upto == "att0":
                break
        tap("qo", QO, [att_last], BF16)
        tap("kt", KT[:, :, 0:2048], [att_last], BF16)
        tap("vv", VV[:, 0:8, :], [att_last], BF16)

        if upto in ("att0", "att"):
            P.emit(finals)
            return nc, tap_out

        AR.reset(pers_mark)
        wp = AR.alloc([128, 8, 3584], BF16)
        woa = AR.alloc([128, 4, 1024], BF16)
        woc = AR.alloc([128, 4, 1024], BF16)
        wmx = AR.alloc([128, 8, 1024], BF16)
        xb2_ = [AR.alloc([128, 2, 1024], BF16) for _ in range(2)]
        xhb_ = [AR.alloc([128, 1024], BF16) for _ in range(2)]
        xres_ = [AR.alloc([128, 2, 1024], F32) for _ in range(2)]
        xT2 = AR.alloc([128, 8, 256], BF16)
        xhT = AR.alloc([128, 32], BF16)
        ccs_ = [AR.alloc([128, 264], F32) for _ in range(2)]
        Ub_ = [AR.alloc([128, 2, 130], F32) for _ in range(2)]
        T1_ = [AR.alloc([128, 2, 128], F32) for _ in range(2)]
        Zb = AR.alloc([128, 4, 256], BF16)
        g0_ = [AR.alloc([128, 256], F32) for _ in range(2)]
        g1_ = [AR.alloc([128, 256], F32) for _ in range(2)]
        m0_ = [AR.alloc([128, 256], F32) for _ in range(2)]
        m1_ = [AR.alloc([128, 256], F32) for _ in range(2)]
        MT = AR.alloc([128, 8, 256], BF16)
        Rb_ = [AR.alloc([128, 1024], F32) for _ in range(2)]
        lng = AR.alloc([128, 1024], F32)
        lnb = AR.alloc([128, 1024], F32)
        H1T = AR.alloc([128, 8, 128], F32)
        stt_b = AR.alloc([128, 16], F32)
        bgs = AR.alloc([128, 16], F32)
        cws = AR.alloc([128, 12], F32)
        rbias = AR.alloc([128, 36], F32)
        wr = AR.alloc([128, 8, 36], F32)
        for i7 in range(7):
            P.adma("gpsimd", wp[:, :, i7 * 512:(i7 + 1) * 512], w_in[:, 1536 + i7 * 512:1536 + (i7 + 1) * 512].rearrange("(c p) n -> p c n", p=128), "wp", writes=["wp"])
        P.adma("gpsimd", woa, w_o_att.rearrange("(c p) n -> p c n", p=128), "woa", writes=["woa"])
        P.adma("gpsimd", woc, w_o_conv.rearrange("(c p) n -> p c n", p=128), "woc", writes=["woc"])
        P.adma("gpsimd", wmx, w_mix.rearrange("(c p) n -> p c n", p=128), "wmx", writes=["wmx"])
        P.adma("sync", wr, w_rt.rearrange("(c p) n -> p c n", p=128), "wr", writes=["wr"])
        P.adma("sync", bgs, b_gate.rearrange("o (q p) -> p (o q)", p=128), "bgs", writes=["bgs"], allow_slow_non_contiguous=True)
        P.adma("sync", cws.rearrange("p (k q) -> p k q", k=3), conv_w.rearrange("k (q p) -> p k q", p=128), "cws", writes=["cws"], allow_slow_non_contiguous=True)
        P.adma("sync", rbias, b_rt.to_broadcast([128, 36]), "rbias", writes=["rbias"])
        P.adma("sync", lng, ln1[0:1, :].to_broadcast([128, 1024]), "lng", writes=["lng"])
        P.adma("sync", lnb, ln1[1:2, :].to_broadcast([128, 1024]), "lnb", writes=["lnb"])

        def layer_norm(buf, gname, bname, gt, bt, stt=None, rn="R"):
            stt = stt_b if stt is None else stt
            mv = stt[:, 12:14]
            ve = stt[:, 14:15]
            rs = stt[:, 15:16]
            P.auto("vector", lambda e: e.bn_stats(out=stt[:, 0:6], in_=buf[:, 0:512]), reads=[rn], writes=["stt"])
            P.auto("vector", lambda e: e.bn_stats(out=stt[:, 6:12], in_=buf[:, 512:1024]), reads=[rn], writes=["stt2"])
            P.auto("vector", lambda e: e.bn_aggr(out=mv, in_=stt[:, 0:12]), reads=["stt", "stt2"], writes=["mv"])
            P.auto("vector", lambda e: e.tensor_scalar(out=ve, in0=mv[:, 1:2], scalar1=1e-5, scalar2=None, op0=ALU.add), reads=["mv"], writes=["ve"])
            P.auto("gpsimd", lambda e: e.tensor_tensor(out=rs, in0=ve, in1=mhalf, op=ALU.pow), reads=["ve"], writes=["rs"], extra=[d_cf])
            P.auto("vector", lambda e: e.tensor_scalar(out=buf, in0=buf, scalar1=mv[:, 0:1], scalar2=rs, op0=ALU.subtract, op1=ALU.mult), reads=["mv", "rs"], writes=[rn])
            P.auto("vector", lambda e: e.tensor_tensor(out=buf, in0=buf, in1=gt, op=ALU.mult), reads=[gname], writes=[rn])
            return P.auto("gpsimd", lambda e: e.tensor_tensor(out=buf, in0=buf, in1=bt, op=ALU.add), reads=[bname], writes=[rn])

        def b_loads(tb):
            r0 = tb * 256
            pb = tb % 2
            P.adma("gpsimd", xb2_[pb], xo[r0:r0 + 256, :].rearrange("(b p) d -> p b d", p=128), "xb2%d" % pb, writes=["xb2%d" % pb])
            P.adma("gpsimd", xhb_[pb][0:4, :], xh[tb * 4:tb * 4 + 4, :], "xhb%d" % pb, writes=["xhb%d" % pb])
            P.adma("sync", xres_[pb], xo[r0:r0 + 256, :].rearrange("(b p) d -> p b d", p=128), "xres%d" % pb, writes=["xres%d" % pb])

        def b_tile(tb):
            r0 = tb * 256
            pb = tb % 2
            xb2, xhb, xres = xb2_[pb], xhb_[pb], xres_[pb]
            n_xb2, n_xhb, n_xres = "xb2%d" % pb, "xhb%d" % pb, "xres%d" % pb
            if tb + 1 < NTB:
                b_loads(tb + 1)
            for half in range(2):
                fns = []
                for cc in range(4):
                    c = half * 4 + cc
                    for blk in range(2):
                        o_ = PBb[half][:, cc * 256 + blk * 128: cc * 256 + blk * 128 + 128]
                        i_ = xb2[:, blk, c * 128:(c + 1) * 128]
                        fns.append(lambda e, o_=o_, i_=i_: e.transpose(out=o_, in_=i_, identity=ident))
                P.auto("tensor", fns, reads=[n_xb2], psum=["b%d" % half], extra=[d_cb])
                dst = xT2[:, half * 4:half * 4 + 4, :].rearrange("p a b -> p (a b)")
                if half == 0:
                    P.auto("scalar", lambda e, dst=dst: e.copy(out=dst, in_=PBb[0]), writes=["xT2a"], psum=["b0"])
                else:
                    P.auto("scalar", lambda e, dst=dst: e.copy(out=dst, in_=PBb[1]), writes=["xT2b"], psum=["b1"])
            fns = []
            for c in range(8):
                o_ = PBb[2][:, 600 + c * 4:600 + (c + 1) * 4]
                i_ = xhb[0:4, c * 128:(c + 1) * 128]
                fns.append(lambda e, o_=o_, i_=i_: e.transpose(out=o_, in_=i_, identity=ident[0:4, 0:4]))
            P.auto("tensor", fns, reads=[n_xhb], psum=["b2"], extra=[d_cb])
            P.auto("scalar", lambda e: e.copy(out=xhT, in_=PBb[2][:, 600:632]), writes=["xhT"], psum=["b2"])
            xhT3 = xhT.rearrange("p (c k) -> p c k", c=8)
            for q in range(4):
                qp = q % 2
                BA, BB = PB[2 + 2 * qp], PB[3 + 2 * qp]
                nBA, nBB = "b%d" % (2 + 2 * qp), "b%d" % (3 + 2 * qp)
                ccs, Ub, T1 = ccs_[qp], Ub_[qp], T1_[qp]
                nccs, nU, nU2, nT1 = "ccs%d" % qp, "U%d" % qp, "U2%d" % qp, "T1%d" % qp
                fns = []
                for c in range(8):
                    l_ = wp[:, c, 512 + q * 128: 512 + q * 128 + 128]
                    fns.append(lambda e, l_=l_, c=c, BA=BA: e.matmul(BA[:, 0:256], lhsT=l_, rhs=xT2[:, c, :], start=(c == 0), stop=(c == 7)))
                for c in range(8):
                    l_ = wp[:, c, 512 + q * 128: 512 + q * 128 + 128]
                    fns.append(lambda e, l_=l_, c=c, BA=BA: e.matmul(BA[:, 256:260], lhsT=l_, rhs=xhT3[:, c, :], start=(c == 0), stop=(c == 7)))
                for c in range(8):
                    l_ = wp[:, c, 1024 + q * 128: 1024 + q * 128 + 128]
                    fns.append(lambda e, l_=l_, c=c, BA=BA: e.matmul(BA[:, 260:264], lhsT=l_, rhs=xhT3[:, c, :], start=(c == 0), stop=(c == 7)))
                P.auto("tensor", fns, reads=["wp", "xT2a", "xT2b", "xhT"], psum=[nBA])
                fns = []
                for c in range(8):
                    l_ = wp[:, c, 1024 + q * 128: 1024 + q * 128 + 128]
                    fns.append(lambda e, l_=l_, c=c, BB=BB: e.matmul(BB[:, 0:256], lhsT=l_, rhs=xT2[:, c, :], start=(c == 0), stop=(c == 7)))
                for c in range(8):
                    l_ = wp[:, c, q * 128: q * 128 + 128]
                    fns.append(lambda e, l_=l_, c=c, BB=BB: e.matmul(BB[:, 256:512], lhsT=l_, rhs=xT2[:, c, :], start=(c == 0), stop=(c == 7)))
                P.auto("tensor", fns, reads=["wp", "xT2a", "xT2b"], psum=[nBB])
                P.auto("scalar", lambda e, ccs=ccs, BA=BA: e.copy(out=ccs, in_=BA[:, 0:264]), writes=[nccs], psum=[nBA])
                P.auto("vector", lambda e, ccs=ccs, Ub=Ub, BB=BB: e.tensor_tensor(out=Ub[:, :, 2:130], in0=ccs[:, 0:256].rearrange("p (b t) -> p b t", b=2),
                                                          in1=BB[:, 0:256].rearrange("p (b t) -> p b t", b=2), op=ALU.mult),
                       reads=[nccs], writes=[nU], psum=[nBB])
                P.auto("vector", lambda e, ccs=ccs, Ub=Ub: e.tensor_tensor(out=Ub[:, :, 0:2], in0=ccs[:, 256:260].rearrange("p (b t) -> p b t", b=2),
                                                          in1=ccs[:, 260:264].rearrange("p (b t) -> p b t", b=2), op=ALU.mult),
                       reads=[nccs], writes=[nU2])
                P.auto("vector", lambda e, q=q, T1=T1, Ub=Ub: e.tensor_scalar(out=T1, in0=Ub[:, :, 0:128], scalar1=cws[:, q:q + 1], scalar2=None, op0=ALU.mult),
                       reads=[nU, nU2, "cws"], writes=[nT1])
                P.auto("vector", lambda e, q=q, T1=T1, Ub=Ub: e.scalar_tensor_tensor(out=T1, in0=Ub[:, :, 1:129], scalar=cws[:, 4 + q:5 + q], in1=T1, op0=ALU.mult, op1=ALU.add),
                       reads=[nU, nU2], writes=[nT1])
                P.auto("vector", lambda e, q=q, T1=T1, Ub=Ub: e.scalar_tensor_tensor(out=T1, in0=Ub[:, :, 2:130], scalar=cws[:, 8 + q:9 + q], in1=T1, op0=ALU.mult, op1=ALU.add),
                       reads=[nU, nU2], writes=[nT1])
                P.auto("vector", lambda e, q=q, T1=T1, BB=BB: e.tensor_tensor(out=Zb[:, q, :], in0=T1.rearrange("p b t -> p (b t)"), in1=BB[:, 256:512], op=ALU.mult),
                       reads=[nT1], writes=["Z%d" % q], psum=[nBB])
            flush_routers()
            for c8 in range(8):
                cp_ = c8 % 2
                BG, BY = PB[2 + 2 * cp_], PB[3 + 2 * cp_]
                nBG, nBY = "b%d" % (2 + 2 * cp_), "b%d" % (3 + 2 * cp_)
                g0, g1, m0, m1 = g0_[cp_], g1_[cp_], m0_[cp_], m1_[cp_]
                ng0, ng1, nm0, nm1 = "g0%d" % cp_, "g1%d" % cp_, "m0%d" % cp_, "m1%d" % cp_
                fns = []
                for c in range(8):
                    l_ = wp[:, c, 1536 + c8 * 128: 1536 + c8 * 128 + 128]
                    fns.append(lambda e, l_=l_, c=c, BG=BG: e.matmul(BG[:, 0:256], lhsT=l_, rhs=xT2[:, c, :], start=(c == 0), stop=(c == 7)))
                for c in range(8):
                    l_ = wp[:, c, 2560 + c8 * 128: 2560 + c8 * 128 + 128]
                    fns.append(lambda e, l_=l_, c=c, BG=BG: e.matmul(BG[:, 256:512], lhsT=l_, rhs=xT2[:, c, :], start=(c == 0), stop=(c == 7)))
                P.auto("tensor", fns, reads=["wp", "xT2a", "xT2b"], psum=[nBG])
                P.auto("scalar", lambda e, c8=c8, g0=g0, BG=BG: e.activation(out=g0, in_=BG[:, 0:256], func=AF.Sigmoid, bias=bgs[:, c8:c8 + 1]), reads=["bgs"], writes=[ng0], psum=[nBG])
                P.auto("scalar", lambda e, c8=c8, g1=g1, BG=BG: e.activation(out=g1, in_=BG[:, 256:512], func=AF.Sigmoid, bias=bgs[:, 8 + c8:9 + c8]), reads=["bgs"], writes=[ng1], psum=[nBG])
                fns = []
                for h in range(4):
                    l_ = woa[:, h, c8 * 128:(c8 + 1) * 128]
                    r_ = QO[:, h, r0:r0 + 256]
                    fns.append(lambda e, l_=l_, r_=r_, h=h, BY=BY: e.matmul(BY[:, 0:256], lhsT=l_, rhs=r_, start=(h == 0), stop=(h == 3)))
                for q in range(4):
                    l_ = woc[:, q, c8 * 128:(c8 + 1) * 128]
                    r_ = Zb[:, q, :]
                    fns.append(lambda e, l_=l_, r_=r_, q=q, BY=BY: e.matmul(BY[:, 256:512], lhsT=l_, rhs=r_, start=(q == 0), stop=(q == 3)))
                P.auto("tensor", fns, reads=["woa", "woc", "Z0", "Z1", "Z2", "Z3"], psum=[nBY])
                P.auto("vector", lambda e, m0=m0, g0=g0, BY=BY: e.tensor_tensor(out=m0, in0=g0, in1=BY[:, 0:256], op=ALU.mult), reads=[ng0], writes=[nm0], psum=[nBY])
                P.auto("vector", lambda e, m1=m1, g1=g1, BY=BY: e.tensor_tensor(out=m1, in0=g1, in1=BY[:, 256:512], op=ALU.mult), reads=[ng1], writes=[nm1], psum=[nBY])
                P.auto("vector", lambda e, c8=c8, m0=m0, m1=m1: e.tensor_tensor(out=MT[:, c8, :], in0=m0, in1=m1, op=ALU.add), reads=[nm0, nm1], writes=["MT%d" % c8])
            for blk in range(2):
                gb = tb * 2 + blk
                Rb = Rb_[blk]
                rn = "R%d" % blk
                fns = []
                for half in range(2):
                    for c in range(8):
                        l_ = MT[:, c, blk * 128:(blk + 1) * 128]
                        r_ = wmx[:, c, half * 512:(half + 1) * 512]
                        fns.append(lambda e, l_=l_, r_=r_, c=c, half=half: e.matmul(PB[6 + half], lhsT=l_, rhs=r_, start=(c == 0), stop=(c == 7)))
                P.auto("tensor", fns, reads=["MT%d" % c_ for c_ in range(8)] + ["wmx"], psum=["b6", "b7"])
                P.auto("vector", lambda e, blk=blk, Rb=Rb: e.scalar_tensor_tensor(out=Rb[:, 0:512], in0=xres[:, blk, 0:512], scalar=ALPHA, in1=PB[6], op0=ALU.mult, op1=ALU.add),
                       reads=[n_xres], writes=[rn], psum=["b6"])
                P.auto("vector", lambda e, blk=blk, Rb=Rb: e.scalar_tensor_tensor(out=Rb[:, 512:1024], in0=xres[:, blk, 512:1024], scalar=ALPHA, in1=PB[7], op0=ALU.mult, op1=ALU.add),
                       reads=[n_xres], writes=[rn], psum=["b7"])
                layer_norm(Rb, "lng", "lnb", lng, lnb, None, rn)
                finals_h1.append(P.adma("sync", h1f[gb * 128:(gb + 1) * 128, :], Rb, "h1w%d" % blk, reads=[rn]))
                pend_r.append(make_router(Rb, rn, gb))

        def make_router(Rb, rn, gb):
            def run():
                fns = []
                for c in range(8):
                    o_ = PB[c // 4][:, (c % 4) * 128:(c % 4) * 128 + 128]
                    i_ = Rb[:, c * 128:(c + 1) * 128]
                    fns.append(lambda e, o_=o_, i_=i_: e.transpose(out=o_, in_=i_, identity=identf))
                P.auto("tensor", fns, reads=[rn], psum=["b0", "b1"], extra=[d_cf])
                P.auto("scalar", lambda e: e.copy(out=H1T[:, 0:4, :].rearrange("p a b -> p (a b)"), in_=PB[0]), writes=["H1Ta"], psum=["b0"])
                P.auto("vector", lambda e: e.tensor_copy(out=H1T[:, 4:8, :].rearrange("p a b -> p (a b)"), in_=PB[1]), writes=["H1Tb"], psum=["b1"])
                fns = []
                for c in range(8):
                    fns.append(lambda e, c=c: e.matmul(PB[0][:, 0:36], lhsT=H1T[:, c, :], rhs=wr[:, c, :], start=(c == 0), stop=(c == 7)))
                P.auto("tensor", fns, reads=["H1Ta", "H1Tb", "wr"], psum=["b0"])
                P.auto("vector", lambda e: e.tensor_tensor(out=LG[:, gb, :], in0=PB[0][:, 0:36], in1=rbias, op=ALU.add), reads=["rbias"], writes=["LG"], psum=["b0"])
            return run

        pend_r = []

        def flush_routers():
            while pend_r:
                pend_r.pop(0)()

        finals_h1 = []
        NTB = NQT * 2
        b_loads(0)
        for tb in range(NTB):
            b_tile(tb)
        flush_routers()
        tap("lg", LG, [P.lw["LG"]])
        tap("h1", h1f[0:256, :], finals_h1)
        if upto == "B":
            P.emit(finals + finals_h1)
            return nc, tap_out

        P.base_waits = [P.lw["LG"]] + finals_h1
        AR.reset(pers_mark)
        NB = NTB * 2
        gm = AR.alloc([128, 32], F32)
        gd = AR.alloc([128, 32, 4], F32)
        pen = AR.alloc([128, 32, 4], F32)
        ge = AR.alloc([128, 32, 4], F32)
        gs = AR.alloc([128, 32], F32)
        gw = AR.alloc([128, 32], F32)
        em = AR.alloc([128, 32, 32], F32)
        em2 = AR.alloc([128, 32, 32], F32)
        oh1 = AR.alloc([128, 32, 32], F32)
        oh2 = AR.alloc([128, 32, 32], F32)
        Aa = AR.alloc([128, 32, 32], F32)
        Tt = AR.alloc([128, 32, 32], F32)
        sc0 = AR.alloc([128, 32, 32], F32)
        sc1 = AR.alloc([128, 32, 32], F32)
        rank = AR.alloc([128, 32, 32], F32)
        tmpc = AR.alloc([128, 32, 32], F32)
        v1 = AR.alloc([128, 32], F32)
        v2 = AR.alloc([128, 32], F32)
        dv = AR.alloc([128, 32], F32)
        s12 = AR.alloc([128, 2, 32], F32)
        stt_c = AR.alloc([128, 16], F32)
        fl = lambda t: t.rearrange("p a b -> p (a b)")
        gl = LG[:, :, 0:4]
        el = LG[:, :, 4:36]
        bc3 = lambda t: t.unsqueeze(2).to_broadcast([128, 32, 32])
        V = "vector"
        P.auto(V, lambda e: e.tensor_reduce(out=gm, in_=gl, axis=AX.X, op=ALU.max), reads=["LG"], writes=["gm"])
        P.auto(V, lambda e: e.tensor_tensor(out=gd, in0=gl, in1=gm.unsqueeze(2).to_broadcast([128, 32, 4]), op=ALU.subtract), reads=["LG", "gm"], writes=["gd"])
        P.auto(V, lambda e: e.tensor_scalar(out=pen, in0=gd, scalar1=0.0, scalar2=NEG, op0=ALU.is_lt, op1=ALU.mult), reads=["gd"], writes=["pen"])
        P.auto("scalar", lambda e: e.activation(out=ge, in_=gd, func=AF.Exp), reads=["gd"], writes=["ge"])
        P.auto(V, lambda e: e.tensor_reduce(out=gs, in_=ge, axis=AX.X, op=ALU.add), reads=["ge"], writes=["gs"])
        P.auto(V, lambda e: e.reciprocal(out=gw, in_=gs), reads=["gs"], writes=["gw"])
        P.auto(V, lambda e: e.tensor_tensor(out=em.rearrange("p b (g k) -> p b g k", g=4), in0=el.rearrange("p b (g k) -> p b g k", g=4),
                                            in1=pen.unsqueeze(3).to_broadcast([128, 32, 4, 8]), op=ALU.add), reads=["LG", "pen"], writes=["em"])
        P.auto(V, lambda e: e.tensor_reduce(out=v1, in_=em, axis=AX.X, op=ALU.max), reads=["em"], writes=["v1"])
        P.auto(V, lambda e: e.tensor_tensor(out=oh1, in0=em, in1=bc3(v1), op=ALU.is_equal), reads=["em", "v1"], writes=["oh1"])
        P.auto(V, lambda e: e.scalar_tensor_tensor(out=fl(em2), in0=fl(oh1), scalar=NEG, in1=fl(em), op0=ALU.mult, op1=ALU.add), reads=["oh1", "em"], writes=["em2"])
        P.auto(V, lambda e: e.tensor_reduce(out=v2, in_=em2, axis=AX.X, op=ALU.max), reads=["em2"], writes=["v2"])
        P.auto(V, lambda e: e.tensor_tensor(out=oh2, in0=em2, in1=bc3(v2), op=ALU.is_equal), reads=["em2", "v2"], writes=["oh2"])
        P.auto(V, lambda e: e.tensor_tensor(out=dv, in0=v2, in1=v1, op=ALU.subtract), reads=["v1", "v2"], writes=["dv"])
        P.auto("scalar", lambda e: e.activation(out=dv, in_=dv, func=AF.Exp), reads=[], writes=["dv"])
        P.auto(V, lambda e: e.tensor_scalar(out=dv, in0=dv, scalar1=1.0, scalar2=None, op0=ALU.add), writes=["dv"])
        P.auto(V, lambda e: e.reciprocal(out=dv, in_=dv), writes=["dv"])
        P.auto(V, lambda e: e.tensor_tensor(out=PW[:, 0, :], in0=dv, in1=gw, op=ALU.mult), reads=["dv", "gw"], writes=["PW0"])
        P.auto(V, lambda e: e.tensor_tensor(out=PW[:, 1, :], in0=gw, in1=PW[:, 0, :], op=ALU.subtract), reads=["PW0", "gw"], writes=["PW1"])
        P.auto(V, lambda e: e.tensor_tensor(out=fl(Aa), in0=fl(oh1), in1=fl(oh2), op=ALU.add), reads=["oh1", "oh2"], writes=["Aa"])
        Af = fl(Aa)
        P.auto("tensor", [lambda e: e.matmul(PB[0], lhsT=trif, rhs=Af[:, 0:512], start=True, stop=True),
                          lambda e: e.matmul(PB[1], lhsT=trif, rhs=Af[:, 512:1024], start=True, stop=True),
                          lambda e: e.matmul(PB[2], lhsT=onesf, rhs=Af[:, 0:512], start=True, stop=True),
                          lambda e: e.matmul(PB[3], lhsT=onesf, rhs=Af[:, 512:1024], start=True, stop=True)],
               reads=["Aa"], psum=["b0", "b1", "b2", "b3"], extra=[d_cf])
        P.auto(V, lambda e: e.tensor_copy(out=fl(Tt)[:, 0:512], in_=PB[2]), writes=["Tt"], psum=["b2"])
        P.auto(V, lambda e: e.tensor_copy(out=fl(Tt)[:, 512:1024], in_=PB[3]), writes=["Tt"], psum=["b3"])
        cur, cname = Tt, "Tt"
        for si, sh in enumerate((1, 2, 4, 8, 16)):
            nxt, nname = (sc0, "sc0") if si % 2 == 0 else (sc1, "sc1")
            P.auto(V, lambda e, cur=cur, nxt=nxt, sh=sh: e.tensor_tensor(out=nxt[:, sh:32, :], in0=cur[:, sh:32, :], in1=cur[:, 0:32 - sh, :], op=ALU.add), reads=[cname], writes=[nname])
            P.auto(V, lambda e, cur=cur, nxt=nxt, sh=sh: e.tensor_copy(out=nxt[:, 0:sh, :], in_=cur[:, 0:sh, :]), reads=[cname], writes=[nname])
            cur, cname = nxt, nname
        P.auto(V, lambda e, cur=cur: e.tensor_tensor(out=fl(tmpc), in0=fl(cur), in1=fl(Tt), op=ALU.subtract), reads=[cname, "Tt"], writes=["tmpc"])
        P.auto(V, lambda e: e.tensor_tensor(out=fl(rank)[:, 0:512], in0=fl(tmpc)[:, 0:512], in1=PB[0], op=ALU.add), reads=["tmpc"], writes=["rank"], psum=["b0"])
        P.auto(V, lambda e: e.tensor_tensor(out=fl(rank)[:, 512:1024], in0=fl(tmpc)[:, 512:1024], in1=PB[1], op=ALU.add), reads=["tmpc"], writes=["rank"], psum=["b1"])
        P.auto(V, lambda e: e.tensor_scalar(out=fl(rank), in0=fl(rank), scalar1=float(CAP - 1), scalar2=None, op0=ALU.min), writes=["rank"])
        P.auto(V, lambda e: e.tensor_tensor(out=rank, in0=rank, in1=ecap.unsqueeze(1).to_broadcast([128, 32, 32]), op=ALU.add), writes=["rank"], extra=[d_cf])
        P.auto(V, lambda e: e.tensor_tensor(out=fl(tmpc), in0=fl(oh1), in1=fl(rank), op=ALU.mult), reads=["oh1", "rank"], writes=["tmpc"])
        P.auto(V, lambda e: e.tensor_reduce(out=s12[:, 0, :], in_=tmpc, axis=AX.X, op=ALU.add), reads=["tmpc"], writes=["s12a"])
        P.auto(V, lambda e: e.tensor_tensor(out=fl(tmpc), in0=fl(oh2), in1=fl(rank), op=ALU.mult), reads=["oh2", "rank"], writes=["tmpc"])
        P.auto(V, lambda e: e.tensor_reduce(out=s12[:, 1, :], in_=tmpc, axis=AX.X, op=ALU.add), reads=["tmpc"], writes=["s12b"])
        P.auto(V, lambda e: e.tensor_scalar(out=fl(s12), in0=fl(s12), scalar1=float(NSLOT - 1), scalar2=0.0, op0=ALU.min, op1=ALU.max), reads=["s12a", "s12b"], writes=["s12a", "s12b"])
        P.auto(V, lambda e: e.tensor_copy(out=SLI, in_=s12), reads=["s12a", "s12b"], writes=["SLI"])
        tap("sli", SLI, [P.lw["SLI"]], I32)
        tap("pw", PW, [P.lw["PW1"]])

        P.base_waits = [P.lw["SLI"], P.lw["PW1"], P.lw["PW0"]] + finals_h1
        AR.reset(pers_mark)
        stt_c = AR.alloc([128, 16], F32)
        wg = [AR.alloc([128, 8, 512], BF16) for _ in range(3)]
        wu = [AR.alloc([128, 8, 512], BF16) for _ in range(3)]
        wd = [AR.alloc([128, 4, 1024], BF16) for _ in range(3)]

        def e_loads_w(e_):
            i = e_ % 3
            P.adma("gpsimd", wg[i], w_eg[e_].rearrange("(c p) n -> p c n", p=128), "wg%d" % i, writes=["wg%d" % i])
            P.adma("gpsimd", wu[i], w_eu[e_].rearrange("(c p) n -> p c n", p=128), "wu%d" % i, writes=["wu%d" % i])
            P.adma("gpsimd", wd[i], w_ed[e_].rearrange("(c p) n -> p c n", p=128), "wd%d" % i, writes=["wd%d" % i])

        e_loads_w(0)
        e_loads_w(1)
        e_loads_w(2)
        hb = [AR.alloc([128, 1024], F32) for _ in range(2)]
        sc_toks = []
        for blk in range(NB):
            i = blk % 2
            P.adma("sync", hb[i], h1f[blk * 128:(blk + 1) * 128, :], "hb%d" % i, writes=["hb%d" % i])
            for k in range(2):
                off = SLI[:, k, blk:blk + 1].bitcast(U32)
                src = hb[i]
                sc_toks.append(P.acdma("gpsimd", lambda e, off=off, src=src: e.indirect_dma_start(
                    out=xs_d, out_offset=bass.IndirectOffsetOnAxis(ap=off, axis=0), in_=src, in_offset=None),
                    "sc%d_%d" % (i, k), reads=["hb%d" % i, "SLI"]))

        XE = [AR.alloc([128, 3, 1024], BF16) for _ in range(2)]
        XT = AR.alloc([128, 8, 384], BF16)
        sg = AR.alloc([128, 384], F32)
        HT = AR.alloc([128, 4, 384], BF16)
        YSb = [AR.alloc([128, 1024], F32) for _ in range(2)]
        NE = 32
        ys_toks = []

        def e_loads(e_):
            i = e_ % 2
            P.adma("sync", XE[i], xs_d[e_ * CAP:(e_ + 1) * CAP, :].rearrange("(b p) d -> p b d", p=128), "xe%d" % i, writes=["xe%d" % i], extra=sc_toks)

        def e_compute(e_):
            i = e_ % 2
            iw = e_ % 3
            for pr in range(4):
                bank = pr % 2
                fns = []
                for cc in range(2):
                    c = 2 * pr + cc
                    for sb in range(3):
                        o_ = PBb[bank][:, cc * 384 + sb * 128: cc * 384 + sb * 128 + 128]
                        i_ = XE[i][:, sb, c * 128:(c + 1) * 128]
                        fns.append(lambda e, o_=o_, i_=i_: e.transpose(out=o_, in_=i_, identity=ident))
                P.auto("tensor", fns, reads=["xe%d" % i], psum=["b%d" % bank])
                dst = XT[:, 2 * pr:2 * pr + 2, :].rearrange("p a b -> p (a b)")
                if bank == 0:
                    P.auto("vector", lambda e, dst=dst: e.tensor_copy(out=dst, in_=PBb[0][:, 0:768]), writes=["XT%d" % pr], psum=["b0"])
                else:
                    P.auto("scalar", lambda e, dst=dst: e.copy(out=dst, in_=PBb[1][:, 0:768]), writes=["XT%d" % pr], psum=["b1"])
            xt_names = ["XT%d" % pr for pr in range(4)]
            for f in range(4):
                gb_ = 2 + 2 * (f % 2)
                ub_ = 3 + 2 * (f % 2)
                fns = []
                for c in range(8):
                    l_ = wg[iw][:, c, f * 128:(f + 1) * 128]
                    fns.append(lambda e, l_=l_, c=c, gb_=gb_: e.matmul(PB[gb_][:, 0:384], lhsT=l_, rhs=XT[:, c, :], start=(c == 0), stop=(c == 7)))
                P.auto("tensor", fns, reads=["wg%d" % iw] + xt_names, psum=["b%d" % gb_])
                fns = []
                for c in range(8):
                    l_ = wu[iw][:, c, f * 128:(f + 1) * 128]
                    fns.append(lambda e, l_=l_, c=c, ub_=ub_: e.matmul(PB[ub_][:, 0:384], lhsT=l_, rhs=XT[:, c, :], start=(c == 0), stop=(c == 7)))
                P.auto("tensor", fns, reads=["wu%d" % iw] + xt_names, psum=["b%d" % ub_])
                P.auto("scalar", lambda e, gb_=gb_: e.activation(out=sg, in_=PB[gb_][:, 0:384], func=AF.Silu), writes=["sg"], psum=["b%d" % gb_])
                P.auto("vector", lambda e, ub_=ub_, f=f: e.tensor_tensor(out=HT[:, f, :], in0=sg, in1=PB[ub_][:, 0:384], op=ALU.mult), reads=["sg"], writes=["HT%d" % f], psum=["b%d" % ub_])
            ht_names = ["HT%d" % f for f in range(4)]
            for sb in range(3):
                k = (e_ * 3 + sb) % 2
                fns = []
                for half in range(2):
                    for f in range(4):
                        l_ = HT[:, f, sb * 128:(sb + 1) * 128]
                        r_ = wd[iw][:, f, half * 512:(half + 1) * 512]
                        fns.append(lambda e, l_=l_, r_=r_, f=f, half=half: e.matmul(PB[6 + half], lhsT=l_, rhs=r_, start=(f == 0), stop=(f == 3)))
                P.auto("tensor", fns, reads=["wd%d" % iw] + ht_names, psum=["b6", "b7"])
                ysb = YSb[k]
                P.auto("scalar", lambda e, ysb=ysb: e.copy(out=ysb[:, 0:512], in_=PB[6]), writes=["ysa%d" % k], psum=["b6"])
                P.auto("vector", lambda e, ysb=ysb: e.tensor_copy(out=ysb[:, 512:1024], in_=PB[7]), writes=["ysb%d" % k], psum=["b7"])
                r0 = e_ * CAP + sb * 128
                ys_toks.append(P.adma("sync", ys_d[r0:r0 + 128, :], ysb, "ysw%d" % k, reads=["ysa%d" % k, "ysb%d" % k]))

        e_loads(0)
        for e_ in range(NE):
            if e_ + 1 < NE:
                e_loads(e_ + 1)
            e_compute(e_)
            if e_ + 3 < NE:
                e_loads_w(e_ + 3)

        Y1_ = [AR.alloc([128, 1024], F32) for _ in range(2)]
        Y2_ = [AR.alloc([128, 1024], F32) for _ in range(2)]
        hc_ = [AR.alloc([128, 1024], F32) for _ in range(2)]
        R2_ = [AR.alloc([128, 1024], F32) for _ in range(2)]
        lng2 = AR.alloc([128, 1024], F32)
        lnb2 = AR.alloc([128, 1024], F32)
        P.adma("sync", lng2, ln2[0:1, :].to_broadcast([128, 1024]), "lng2", writes=["lng2"])
        P.adma("sync", lnb2, ln2[1:2, :].to_broadcast([128, 1024]), "lnb2", writes=["lnb2"])

        def c_loads(blk):
            pb = blk % 2
            P.adma("sync", hc_[pb], h1f[blk * 128:(blk + 1) * 128, :], "hc%d" % pb, writes=["hc%d" % pb])
            for k, yb in ((0, Y1_[pb]), (1, Y2_[pb])):
                off = SLI[:, k, blk:blk + 1].bitcast(U32)
                P.acdma("gpsimd", lambda e, off=off, yb=yb: e.indirect_dma_start(
                    out=yb, out_offset=None, in_=ys_d, in_offset=bass.IndirectOffsetOnAxis(ap=off, axis=0)),
                    "yg%d%d" % (k, pb), reads=["SLI"], writes=["Y%d%d" % (k, pb)], extra=ys_toks)

        def c_block(blk):
            pb = blk % 2
            if blk + 1 < NB:
                c_loads(blk + 1)
            Y1, Y2, hc, R2 = Y1_[pb], Y2_[pb], hc_[pb], R2_[pb]
            rn = "R2%d" % pb
            P.auto("scalar", lambda e: e.mul(out=R2, in_=hc, mul=ALPHA), reads=["hc%d" % pb], writes=[rn])
            P.auto(V, lambda e: e.scalar_tensor_tensor(out=R2, in0=Y1, scalar=PW[:, 0, blk:blk + 1], in1=R2, op0=ALU.mult, op1=ALU.add), reads=["Y0%d" % pb, "PW0"], writes=[rn])
            P.auto(V, lambda e: e.scalar_tensor_tensor(out=R2, in0=Y2, scalar=PW[:, 1, blk:blk + 1], in1=R2, op0=ALU.mult, op1=ALU.add), reads=["Y1%d" % pb, "PW1"], writes=[rn])
            layer_norm(R2, "lng2", "lnb2", lng2, lnb2, stt_c, rn)
            finals.append(P.adma("sync", out[blk * 128:(blk + 1) * 128, :], R2, "outw%d" % pb, reads=[rn]))

        c_loads(0)
        for blk in range(NB):
            c_block(blk)

        P.emit(finals)
    return nc, tap_out


def _consts(j):
    ident = np.eye(128, dtype=np.float32)
    rot = np.zeros((128, 128), np.float32)
    for m in range(128):
        if (m % 64) < 32:
            rot[m + 32, m] = -1.0
        else:
            rot[m - 32, m] = 1.0
    ones = np.ones((128, 128), np.float32)
    kk = np.arange(128)[:, None, None]
    r = np.arange(8)[None, :, None]
    qq = np.arange(512)[None, None, :]
    mask = ((r * 128 + kk) <= ((2 * (qq // 128) + j) * 128 + (qq % 128))).astype(np.float32)
    cbf = np.concatenate([ident, rot, ones, mask.reshape(128, 4096)], axis=1)
    cf = np.zeros((128, 512), np.float32)
    cf[:, 0:128] = ident
    cf[:, 128:256] = 1.0
    cf[:, 256:384] = (np.arange(128)[:, None] < np.arange(128)[None, :]).astype(np.float32)
    inv_freq = (np.float32(10000.0) ** (-np.arange(0, 64, 2, dtype=np.float32) / np.float32(64))).astype(np.float32)
    p = np.arange(128)
    cf[:, 384] = (inv_freq[(p % 64) % 32].astype(np.float64) / (2.0 * np.pi)).astype(np.float32)
    cf[:, 392:424] = (np.arange(32) * CAP)[None, :]
    cf[:, 424] = -0.5
    cf[:, 425] = 1e-5
    return np.ascontiguousarray(cbf), cf


def make_core_inputs(inp, c):
    b, j = c // 2, c % 2
    x = inp["x"]
    xb_ = x[b]
    blocks = xb_.reshape(64, 128, D)
    xo = np.ascontiguousarray(blocks[j::2].reshape(NOWN, D))
    xh = np.zeros((32, 2, D), np.float32)
    for m in range(32):
        blk = 2 * m + j
        if blk > 0:
            xh[m] = xb_[blk * 128 - 2: blk * 128]
    pos = np.asarray(inp["positions"][b], dtype=np.int32)
    poso = np.ascontiguousarray(pos.reshape(64, 128)[j::2].reshape(1, NOWN))
    cbf, cf = _consts(j)
    f = lambda a: np.ascontiguousarray(np.asarray(a, dtype=np.float32))
    return {
        "xf": f(xb_), "xo": xo, "xh": f(xh.reshape(64, D)), "posf": np.ascontiguousarray(pos.reshape(1, S)), "poso": poso,
        "w_in": f(inp["w_in"][0]), "b_gate": f(inp["b_gate"][0].reshape(1, 2048)),
        "lam_in": f(np.concatenate([inp["lambda_q1"][0], inp["lambda_k1"][0], inp["lambda_q2"][0], inp["lambda_k2"][0]]).reshape(1, 256)),
        "subln_g": f(inp["subln_g"][0].reshape(1, 128)), "w_o_att": f(inp["w_o_att"][0]), "conv_w": f(inp["conv_w"][0]),
        "w_o_conv": f(inp["w_o_conv"][0]), "w_mix": f(inp["w_mix_out"][0]),
        "ln1": f(np.stack([inp["ln1_g"][0], inp["ln1_b"][0]])), "ln2": f(np.stack([inp["ln2_g"][0], inp["ln2_b"][0]])),
        "w_rt": f(np.concatenate([inp["w_router_group"][0], inp["w_router_expert"][0]], axis=1)),
        "b_rt": f(np.concatenate([inp["b_router_group"][0], inp["b_router_expert"][0]]).reshape(1, 36)),
        "w_eg": f(inp["w_exp_gate"][0]), "w_eu": f(inp["w_exp_up"][0]), "w_ed": f(inp["w_exp_down"][0]),
        "cbf": cbf, "cf32": cf,
    }


def kernel(**inputs):
    inp = {k: np.asarray(v) for k, v in inputs.items()}
    nc, _ = build()
    in_maps = [make_core_inputs(inp, c) for c in range(8)]
    res = run_bass_kernel_spmd(nc, in_maps, core_ids=list(range(8)))
    outp = np.zeros((4, S, D), np.float32)
    for c in range(8):
        b, j = c // 2, c % 2
        o = np.asarray(res.results[c]["out"]).reshape(32, 128, D)
        outp[b].reshape(64, 128, D)[j::2] = o
    return outp
```

```python
import math
import numpy as np
import ml_dtypes
from contextlib import ExitStack
import concourse.bass as bass
import concourse.mybir as mybir
from concourse.bass_utils import run_bass_kernel_spmd

F32 = mybir.dt.float32
BF16 = mybir.dt.bfloat16
I32 = mybir.dt.int32
U32 = mybir.dt.uint32
AF = mybir.ActivationFunctionType
ALU = mybir.AluOpType
AX = mybir.AxisListType
ENGS = ["tensor", "vector", "scalar", "gpsimd", "sync"]

S = 8192
D = 1024
NOWN = 4096
CAP = 384
NSLOT = 32 * CAP
ALPHA = 2.0 ** 0.25
LAMBDA_INIT = 0.8 - 0.6 * math.exp(0.0)
TWO_PI = 2.0 * math.pi
NEG = -1.0e30


class Prog:
    def __init__(self, nc, es):
        self.nc = nc
        self.es = es
        self.ops = {e: [] for e in ENGS}
        self.cnt = {e: 0 for e in ENGS}
        self.esem = {e: es.enter_context(nc.semaphore("es_" + e)) for e in ENGS}
        self.dsem = {}
        self.dcnt = {}
        self.waited = {e: {} for e in ENGS}
        self.base_waits = []

    def _waits(self, eng, waits):
        best = {}
        for w in list(waits) + list(self.base_waits):
            if w is None:
                continue
            sem, val, key = w
            if key not in best or best[key][1] < val:
                best[key] = (sem, val)
        out = []
        for key, (sem, val) in best.items():
            if self.waited[eng].get(key, 0) >= val:
                continue
            self.waited[eng][key] = val
            out.append((sem, val))
        return out

    def op(self, eng, fn, waits=(), signal=True):
        ws = self._waits(eng, waits)
        inc = None
        tok = None
        if signal:
            self.cnt[eng] += 1
            inc = (self.esem[eng], 1)
            tok = (self.esem[eng], self.cnt[eng], "e_" + eng)
        self.ops[eng].append((fn, ws, inc))
        return tok

    def _dsem(self, sem):
        if sem not in self.dsem:
            self.dsem[sem] = self.es.enter_context(self.nc.semaphore("ds_" + sem))
            self.dcnt[sem] = 0
        return self.dsem[sem]

    def dma(self, q, out, in_, sem, waits=(), **kw):
        return self.cdma(q, lambda e: e.dma_start(out=out, in_=in_, **kw), sem, waits)

    def cdma(self, q, fn, sem, waits=()):
        s = self._dsem(sem)
        ws = self._waits(q, waits)
        self.dcnt[sem] += 16
        self.ops[q].append((fn, ws, (s, 16)))
        return (s, self.dcnt[sem], "d_" + sem)

    def _auto_waits(self, reads, writes, psum):
        if not hasattr(self, "lw"):
            self.lw, self.rd, self.pa = {}, {}, {}
        ws = []
        for r in reads:
            ws.append(self.lw.get(r))
        for w in writes:
            ws.append(self.lw.get(w))
            ws.extend(self.rd.get(w, []))
        for p in psum:
            ws.append(self.pa.get(p))
        return ws

    def _auto_done(self, tok, reads, writes, psum):
        for r in reads:
            self.rd.setdefault(r, []).append(tok)
        for w in writes:
            self.lw[w] = tok
            self.rd[w] = []
        for p in psum:
            self.pa[p] = tok

    def auto(self, eng, fns, reads=(), writes=(), psum=(), extra=()):
        if not isinstance(fns, (list, tuple)):
            fns = [fns]
        ws = self._auto_waits(reads, writes, psum) + list(extra)
        tok = None
        for k, fn in enumerate(fns):
            tok = self.op(eng, fn, waits=ws if k == 0 else (), signal=(k == len(fns) - 1))
        self._auto_done(tok, reads, writes, psum)
        return tok

    def adma(self, q, out, in_, sem, reads=(), writes=(), extra=(), **kw):
        ws = self._auto_waits(reads, writes, ()) + list(extra)
        tok = self.dma(q, out, in_, sem, waits=ws, **kw)
        self._auto_done(tok, reads, writes, ())
        return tok

    def acdma(self, q, fn, sem, reads=(), writes=(), extra=()):
        ws = self._auto_waits(reads, writes, ()) + list(extra)
        tok = self.cdma(q, fn, sem, waits=ws)
        self._auto_done(tok, reads, writes, ())
        return tok

    def emit(self, final_waits):
        nc = self.nc
        with nc.Block() as block:
            def mk(eng):
                def body(e):
                    for fn, ws, inc in self.ops[eng]:
                        for sem, val in ws:
                            e.wait_ge(sem, val)
                        ins = fn(e)
                        if inc is not None:
                            ins.then_inc(inc[0], inc[1])
                    if eng == "sync":
                        for w in final_waits:
                            if w is not None:
                                e.wait_ge(w[0], w[1])
                return body
            block.tensor(mk("tensor"))
            block.vector(mk("vector"))
            block.scalar(mk("scalar"))
            block.gpsimd(mk("gpsimd"))
            block.sync(mk("sync"))


class Arena:
    def __init__(self, t, nwords):
        self.t = t
        self.n = nwords
        self.top = 0

    def mark(self):
        return self.top

    def reset(self, m):
        self.top = m

    def alloc(self, shape, dt):
        per = 1
        for s_ in shape[1:]:
            per *= s_
        if dt == BF16:
            words = (per + 1) // 2
        else:
            words = per
        words = (words + 7) // 8 * 8
        a = self.top
        self.top += words
        assert self.top <= self.n, ("arena overflow", self.top, self.n)
        v = self.t[:, a:a + words]
        if dt == BF16:
            v = v.bitcast(BF16)[:, 0:per]
        elif dt == I32:
            v = v.bitcast(I32)[:, 0:per]
        else:
            v = v[:, 0:per]
        if len(shape) == 3:
            v = v.rearrange("p (a b) -> p a b", a=shape[1])
        elif len(shape) == 4:
            v = v.rearrange("p (a b c) -> p a b c", a=shape[1], b=shape[2])
        if shape[0] != 128:
            v = v[0:shape[0]]
        return v


def build(upto="all", taps=(), NQT=8):
    nc = bass.Bass("TRN2", target_bir_lowering=False)
    din = lambda name, shape, dt: nc.dram_tensor(name, shape, dt, kind="ExternalInput").ap()
    xf = din("xf", [S, D], F32)
    xo = din("xo", [NOWN, D], F32)
    xh = din("xh", [64, D], F32)
    posf = din("posf", [1, S], I32)
    poso = din("poso", [1, NOWN], I32)
    w_in = din("w_in", [D, 5120], F32)
    b_gate = din("b_gate", [1, 2048], F32)
    lam_in = din("lam_in", [1, 256], F32)
    subln_g = din("subln_g", [1, 128], F32)
    w_o_att = din("w_o_att", [512, D], F32)
    conv_w = din("conv_w", [3, 512], F32)
    w_o_conv = din("w_o_conv", [512, D], F32)
    w_mix = din("w_mix", [D, D], F32)
    ln1 = din("ln1", [2, D], F32)
    ln2 = din("ln2", [2, D], F32)
    w_rt = din("w_rt", [D, 36], F32)
    b_rt = din("b_rt", [1, 36], F32)
    w_eg = din("w_eg", [32, D, 512], F32)
    w_eu = din("w_eu", [32, D, 512], F32)
    w_ed = din("w_ed", [32, 512, D], F32)
    cbf = din("cbf", [128, 384 + 4096], F32)
    cf32 = din("cf32", [128, 512], F32)
    out = nc.dram_tensor("out", [NOWN, D], F32, kind="ExternalOutput").ap()
    h1f = nc.dram_tensor("h1f", [NOWN, D], F32, kind="Internal").ap()
    xs_d = nc.dram_tensor("xs_d", [NSLOT, D], BF16, kind="Internal").ap()
    ys_d = nc.dram_tensor("ys_d", [NSLOT, D], F32, kind="Internal").ap()
    tap_out = {}

    with ExitStack() as es:
        P = Prog(nc, es)
        NW = 51 * 1024
        arena_t = es.enter_context(nc.sbuf_tensor("arena", [128, NW], F32))
        AR = Arena(arena_t, NW)
        psum = es.enter_context(nc.psum_tensor("psum", [128, 8, 512], F32))
        PB = [psum[:, i, :] for i in range(8)]
        PBb = [psum[:, i, :].bitcast(BF16) for i in range(8)]
        finals = []

        def tap(name, ap, waits, dt=F32):
            if name not in taps:
                return
            shp = list(ap.shape)
            o = nc.dram_tensor("tap_" + name, shp, dt, kind="ExternalOutput").ap()
            tap_out[name] = shp
            finals.append(P.dma("sync", o, ap, "tap_" + name, waits=waits))

        cb_t = AR.alloc([128, 384], BF16)
        ident = cb_t[:, 0:128]
        rotm = cb_t[:, 128:256]
        onesb = cb_t[:, 256:384]
        cf_t = AR.alloc([128, 512], F32)
        identf = cf_t[:, 0:128]
        onesf = cf_t[:, 128:256]
        trif = cf_t[:, 256:384]
        invf = cf_t[:, 384:385]
        ecap = cf_t[:, 392:424]
        mhalf = cf_t[:, 424:425]
        epsc = cf_t[:, 425:426]
        d_cb = P.dma("gpsimd", cb_t, cbf[:, 0:384], "cb")
        d_cf = P.dma("sync", cf_t, cf32, "cf")
        QO = AR.alloc([128, 4, NOWN], BF16)
        small = AR.alloc([128, 64], F32)
        neglam = small[:, 0:1]
        gsc = small[:, 1:2]
        lamv = AR.alloc([128, 256], F32)
        slg = AR.alloc([128, 128], F32)
        d_lam = P.dma("sync", lamv, lam_in.to_broadcast([128, 256]), "lam")
        d_slg = P.dma("sync", slg[:, 0:1], subln_g.rearrange("o p -> p o"), "slg", allow_slow_non_contiguous=True)
        lt = small[:, 8:10]
        t_l1 = P.op("vector", lambda e: e.tensor_tensor(out=lamv[:, 0:64], in0=lamv[:, 0:64], in1=lamv[:, 64:128], op=ALU.mult), waits=[d_lam])
        t_l2 = P.op("vector", lambda e: e.tensor_tensor(out=lamv[:, 128:192], in0=lamv[:, 128:192], in1=lamv[:, 192:256], op=ALU.mult), waits=[d_lam])
        t_l3 = P.op("vector", lambda e: e.tensor_reduce(out=lt[:, 0:1], in_=lamv[:, 0:64], axis=AX.X, op=ALU.add), waits=[t_l1])
        t_l4 = P.op("vector", lambda e: e.tensor_reduce(out=lt[:, 1:2], in_=lamv[:, 128:192], axis=AX.X, op=ALU.add), waits=[t_l2])
        t_l5 = P.op("scalar", lambda e: e.activation(out=small[:, 10:12], in_=lt, func=AF.Exp), waits=[t_l3, t_l4])
        t_l6 = P.op("vector", lambda e: e.tensor_tensor(out=small[:, 12:13], in0=small[:, 11:12], in1=small[:, 10:11], op=ALU.subtract), waits=[t_l5])
        t_l7 = P.op("vector", lambda e: e.tensor_scalar(out=neglam, in0=small[:, 12:13], scalar1=-LAMBDA_INIT, scalar2=None, op0=ALU.add), waits=[t_l6])
        t_g = P.op("vector", lambda e: e.tensor_scalar(out=gsc, in0=slg[:, 0:1], scalar1=1.0 - LAMBDA_INIT, scalar2=None, op0=ALU.mult), waits=[d_slg])
        t_consts = [d_cb, d_cf, t_l7, t_g]
        LG = AR.alloc([128, 32, 36], F32)
        PW = AR.alloc([128, 2, 32], F32)
        SLI = AR.alloc([128, 2, 32], I32)
        pers_mark = AR.mark()

        KT = AR.alloc([128, 2, S], BF16)
        maskt_t = AR.alloc([128, 4096], BF16)
        maskt = maskt_t.rearrange("p (r q) -> p r q", r=8)
        d_mask = P.dma("gpsimd", maskt_t, cbf[:, 384:384 + 4096], "mask", max_dma_last_dim=4096)
        VV = AR.alloc([128, 64, 256], BF16)
        wq = AR.alloc([128, 8, 512], BF16)
        wkv = AR.alloc([128, 8, 512], BF16)
        XBt = [AR.alloc([128, 4, D], BF16) for _ in range(2)]
        XTt = [AR.alloc([128, 8, 512], BF16) for _ in range(2)]
        post2 = [AR.alloc([128, 512], F32) for _ in range(2)]
        tq = AR.alloc([128, 512], F32)
        ki = AR.alloc([128, 512], I32)
        cst2 = [AR.alloc([128, 512], F32) for _ in range(2)]
        snt2 = [AR.alloc([128, 512], F32) for _ in range(2)]
        qsb = [AR.alloc([128, 512], BF16) for _ in range(2)]
        ra = [AR.alloc([128, 512], F32) for _ in range(2)]
        rb = [AR.alloc([128, 512], F32) for _ in range(2)]
        PT = [AR.alloc([128, 2, 512], BF16) for _ in range(3)]
        EE = [AR.alloc([128, 512], F32) for _ in range(4)]

        st = {"xb_free": [None, None], "xt_free": [None, None], "tp_free": [None, None],
              "kp_free": [[], []], "rp_free": [None, None], "vp_free": [None, None],
              "tab_free": [[], []], "qs_free": [None, None], "ra_free": [None, None],
              "n_kp": 0, "n_tp": 0, "n_vp": 0, "n_x": 0, "n_tab": 0, "last_sin": None}

        def load_w(dst, src_cols, sem, waits):
            return P.dma("gpsimd", dst, src_cols.rearrange("(c p) n -> p c n", p=128), sem, waits=waits)

        def tables_dma(pos_src, t0):
            ti = st["n_tab"] % 2
            st["n_tab"] += 1
            w0 = list(st["tab_free"][ti])
            d = P.dma("gpsimd", post2[ti], pos_src[0:1, t0:t0 + 512].to_broadcast([128, 512]), "pos%d" % ti, waits=w0)
            return ti, d, w0

        def tables(pos_src, t0):
            return tables_compute(*tables_dma(pos_src, t0))

        def tables_compute(ti, d, w0):
            post = post2[ti]
            prev = [st["last_sin"]]
            for dst, add in ((snt2[ti], 0.0), (cst2[ti], 0.25)):
                a = P.op("vector", lambda e, add=add: e.tensor_scalar(out=tq, in0=post, scalar1=invf, scalar2=add, op0=ALU.mult, op1=ALU.add), waits=[d, d_cf] + prev)
                b = P.op("vector", lambda e: e.tensor_copy(out=ki, in_=tq), waits=[a])
                c = P.op("vector", lambda e: e.tensor_tensor(out=tq, in0=tq, in1=ki, op=ALU.subtract), waits=[b])
                s_ = P.op("scalar", lambda e, dst=dst: e.activation(out=dst, in_=tq, func=AF.Sin, scale=TWO_PI), waits=[c] + w0)
                prev = [s_]
            st["last_sin"] = prev[0]
            return prev[0], ti

        def load_x_tile(src, row0):
            i = st["n_x"] % 2
            st["n_x"] += 1
            xb = XBt[i]
            d = P.dma("gpsimd", xb, src[row0:row0 + 512, :].rearrange("(b p) d -> p b d", p=128),
                      "xb%d" % i, waits=[st["xb_free"][i]])
            return i, d

        def transpose_tile(i, dx):
            xb = XBt[i]
            xt = XTt[i]
            evs = []
            last_t = None
            for g in range(4):
                bk = st["n_tp"] % 2
                st["n_tp"] += 1
                for cc in range(2):
                    c = 2 * g + cc
                    for blk in range(4):
                        last = (cc == 1 and blk == 3)
                        o_ = PBb[bk][:, cc * 512 + blk * 128: cc * 512 + blk * 128 + 128]
                        i_ = xb[:, blk, c * 128:(c + 1) * 128]
                        tk = P.op("tensor", lambda e, o_=o_, i_=i_: e.transpose(out=o_, in_=i_, identity=ident),
                                  waits=[dx, d_cb, st["tp_free"][bk]], signal=last)
                        if last:
                            last_t = tk
                dst = xt[:, 2 * g:2 * g + 2, :].rearrange("p a b -> p (a b)")
                src = PBb[bk]
                if g % 2 == 0:
                    ev = P.op("vector", lambda e, dst=dst, src=src: e.tensor_copy(out=dst, in_=src), waits=[last_t, st["xt_free"][i]])
                else:
                    ev = P.op("scalar", lambda e, dst=dst, src=src: e.copy(out=dst, in_=src), waits=[last_t, st["xt_free"][i]])
                st["tp_free"][bk] = ev
                evs.append(ev)
            st["xb_free"][i] = last_t
            return evs

        def proj_part1(xt, evs, wt, wcol, tab_tok, ti, extra_w):
            kb = st["n_kp"] % 2
            st["n_kp"] += 1
            kp = PB[2 + kb]
            q_ = qsb[kb]
            ra_ = ra[kb]
            cs_ = cst2[ti]
            mm = None
            for c in range(8):
                l_ = wt[:, c, wcol:wcol + 128]
                r_ = xt[:, c, :]
                mm = P.op("tensor", lambda e, l_=l_, r_=r_, c=c: e.matmul(kp, lhsT=l_, rhs=r_, start=(c == 0), stop=(c == 7)),
                          waits=[evs[c // 2]] + st["kp_free"][kb] + extra_w, signal=(c == 7))
            cp = P.op("scalar", lambda e: e.copy(out=q_, in_=kp), waits=[mm, st["qs_free"][kb]])
            a = P.op("vector", lambda e: e.tensor_tensor(out=ra_, in0=kp, in1=cs_, op=ALU.mult), waits=[mm, cp, tab_tok, st["ra_free"][kb]])
            st["kp_free"][kb] = [a, cp]
            return {"kb": kb, "cp": cp, "a": a, "tab": tab_tok, "ti": ti, "mm": mm}

        def proj_part2(cx, dst):
            kb = cx["kb"]
            rp = PB[4 + kb]
            q_ = qsb[kb]
            ra_ = ra[kb]
            rb_ = rb[kb]
            sn_ = snt2[cx["ti"]]
            rm = P.op("tensor", lambda e: e.matmul(rp, lhsT=rotm, rhs=q_, start=True, stop=True), waits=[cx["cp"], d_cb, st["rp_free"][kb]])
            st["qs_free"][kb] = rm
            b = P.op("vector", lambda e: e.tensor_tensor(out=rb_, in0=rp, in1=sn_, op=ALU.mult), waits=[rm, cx["tab"], st["ra_free"][kb]])
            st["rp_free"][kb] = b
            f = P.op("vector", lambda e: e.tensor_tensor(out=dst, in0=ra_, in1=rb_, op=ALU.add), waits=[cx["a"], b])
            st["ra_free"][kb] = f
            return f, b, rm

        pre = {}

        def prefetch(key, src, pos_src, T):
            i, dx = load_x_tile(src, T * 512)
            tab, ti = tables(pos_src, T * 512)
            pre[key] = (i, dx, tab, ti)

        def prefetch_dma(key, src, pos_src, T):
            i, dx = load_x_tile(src, T * 512)
            return (key, i, dx, tables_dma(pos_src, T * 512))

        def prefetch_compute(pf):
            key, i, dx, td = pf
            tab, ti = tables_compute(*td)
            pre[key] = (i, dx, tab, ti)

        def a_tile(T, dwk, dwv, last, nxt):
            i, dx, tab, ti = pre.pop(("a", T))
            pf = prefetch_dma(*nxt) if nxt is not None else None
            evs = transpose_tile(i, dx)
            cxs = [proj_part1(XTt[i], evs, wkv, hl * 128, tab, ti, [dwk]) for hl in range(2)]
            mm = None
            for blk in range(4):
                vb = st["n_vp"] % 2
                st["n_vp"] += 1
                vp = PB[6 + vb][:, 0:256]
                for c in range(8):
                    l_ = XTt[i][:, c, blk * 128:(blk + 1) * 128]
                    r_ = wkv[:, c, 256:512]
                    mm = P.op("tensor", lambda e, l_=l_, r_=r_, c=c, vp=vp: e.matmul(vp, lhsT=l_, rhs=r_, start=(c == 0), stop=(c == 7)),
                              waits=evs + [dwv, st["vp_free"][vb]], signal=(c == 7))
                o_ = VV[:, T * 4 + blk, :]
                vts = P.op("scalar", lambda e, o_=o_, vp=vp: e.copy(out=o_, in_=vp), waits=[mm])
                st["vp_free"][vb] = vts
                last.append(vts)
            st["xt_free"][i] = mm
            if pf is not None:
                prefetch_compute(pf)
            tabfree = []
            for hl in range(2):
                f, b, rm = proj_part2(cxs[hl], KT[:, hl, T * 512:(T + 1) * 512])
                tabfree.append(b)
                last.append(f)
            st["tab_free"][ti] = tabfree

        def phase_A(hp, then_q):
            dwk = load_w(wkv[:, :, 0:256], w_in[:, 512 + hp * 256: 512 + hp * 256 + 256], "wk", [])
            dwv = load_w(wkv[:, :, 256:512], w_in[:, 1024 + hp * 256: 1024 + hp * 256 + 256], "wv", [])
            last = []
            prefetch(("a", 0), xf, posf, 0)
            for T in range(16):
                if T + 1 < 16:
                    nxt = (("a", T + 1), xf, posf, T + 1)
                elif then_q:
                    nxt = (("q", 0), xo, poso, 0)
                else:
                    nxt = None
                a_tile(T, dwk, dwv, last, nxt)
            return last

        def q_tile(T, dwq, last):
            i, dx, tab, ti = pre.pop(("q", T))
            pf = prefetch_dma(("q", T + 1), xo, poso, T + 1) if T + 1 < NQT else None
            evs = transpose_tile(i, dx)
            tabfree = []
            rm = None
            cxs = {}
            cxs[0] = proj_part1(XTt[i], evs, wq, 0, tab, ti, [dwq])
            for h in range(4):
                if h + 1 < 4:
                    cxs[h + 1] = proj_part1(XTt[i], evs, wq, (h + 1) * 128, tab, ti, [dwq])
                if h == 3 and pf is not None:
                    prefetch_compute(pf)
                f, b, rm = proj_part2(cxs[h], QO[:, h, T * 512:(T + 1) * 512])
                tabfree.append(b)
                last.append(f)
            st["tab_free"][ti] = tabfree
            st["xt_free"][i] = rm

        def phase_Q():
            dwq = load_w(wq, w_in[:, 0:512], "wq", [])
            last = []
            for T in range(NQT):
                q_tile(T, dwq, last)
            return last

        ACC0 = ra[0]
        att = {"a0": None, "accL_free": None, "l6_free": None, "s_free": [None, None], "pt_free": [[], [], []], "acc_free": None, "n_s": 0, "n_pt": 0, "ee_free": None, "pending": None}

        def att_unit(i, hl, h):
            nkb = 8 * i + 8
            qk_tok = {}
            qrange = slice(i * 512, (i + 1) * 512)

            def col0(kb):
                r = kb - 8 * i
                return 128 * (r // 2) if r > 0 else 0

            def issue_qk(kb):
                s = kb % 2
                c0 = col0(kb)
                krange = slice(kb * 128, (kb + 1) * 128)
                qr = slice(i * 512 + c0, (i + 1) * 512)
                P.op("tensor", lambda e: e.matmul(psum[:, 2 * s, c0:512], lhsT=KT[0:64, hl, krange], rhs=QO[0:64, h, qr], start=True, stop=True),
                     waits=[att["s_free"][s]], signal=False)
                t = P.op("tensor", lambda e: e.matmul(psum[:, 2 * s + 1, c0:512], lhsT=KT[64:128, hl, krange], rhs=QO[64:128, h, qr], start=True, stop=True))
                qk_tok[kb] = (t, s)

            def exp_step(kb):
                t, s = qk_tok[kb]
                c0 = col0(kb)
                pi = att["n_pt"] % 3
                att["n_pt"] += 1
                pt = PT[pi]
                ex = P.op("scalar", lambda e: e.activation(out=pt[:, :, c0:512], in_=psum[:, 2 * s:2 * s + 2, c0:512], func=AF.Exp, scale=0.125), waits=[t] + att["pt_free"][pi])
                att["s_free"][s] = ex
                return ex, pt, pi

            def pv_step(kb, ex, pt, pi):
                pv_w = ex
                c0 = col0(kb)
                if kb >= 8 * i:
                    r = kb - 8 * i
                    pv_w = P.op("vector", lambda e: e.tensor_tensor(out=pt[:, :, c0:512], in0=pt[:, :, c0:512],
                                                                      in1=maskt[:, r, c0:512].unsqueeze(1).to_broadcast([128, 2, 512 - c0]), op=ALU.mult),
                                waits=[ex, d_mask])
                s0 = (kb == 0)
                s1 = (kb == nkb - 1)
                w0 = [pv_w, att["acc_free"]] if kb == 0 else [pv_w]
                vv = VV[:, kb, hl * 128:(hl + 1) * 128]
                P.op("tensor", lambda e: e.matmul(PB[4][:, c0:512], lhsT=vv, rhs=pt[:, 0, c0:512], start=s0, stop=s1), waits=w0, signal=False)
                P.op("tensor", lambda e: e.matmul(PB[5][:, c0:512], lhsT=vv, rhs=pt[:, 1, c0:512], start=s0, stop=s1), signal=False)
                pv = P.op("tensor", lambda e: e.matmul(PB[7][:, c0:512], lhsT=onesb, rhs=pt[:, 1, c0:512], start=s0, stop=s1))
                if kb == 0:
                    a0 = P.op("vector", lambda e: e.tensor_copy(out=ACC0, in_=pt[:, 0, :]), waits=[pv_w, att["accL_free"]])
                else:
                    a0 = P.op("vector", lambda e: e.tensor_tensor(out=ACC0[:, c0:512], in0=ACC0[:, c0:512], in1=pt[:, 0, c0:512], op=ALU.add), waits=[pv_w, att["a0"]])
                att["a0"] = a0
                att["pt_free"][pi] = [pv, a0]
                return pv

            issue_qk(0)
            issue_qk(1)
            pv = None
            for kb in range(nkb):
                ex, pt, pi = exp_step(kb)
                if kb + 2 < nkb:
                    issue_qk(kb + 2)
                pv = pv_step(kb, ex, pt, pi)
                if kb == 2 and att["pending"] is not None:
                    att["pending"]()
                    att["pending"] = None
            wfree = [att["ee_free"]]
            lsum = P.op("tensor", lambda e: e.matmul(PB[6], lhsT=onesf, rhs=ACC0, start=True, stop=True), waits=[att["a0"], att["acc_free"], att["l6_free"], d_cf])
            att["accL_free"] = lsum
            e0 = P.op("vector", lambda e: e.reciprocal(out=EE[0], in_=PB[6]), waits=[pv, lsum] + wfree)
            e1 = P.op("vector", lambda e: e.reciprocal(out=EE[1], in_=PB[7]), waits=[pv, lsum] + wfree)
            e2 = P.op("vector", lambda e: e.tensor_tensor(out=EE[0], in0=PB[4], in1=EE[0], op=ALU.mult), waits=[e0])
            e3 = P.op("vector", lambda e: e.tensor_tensor(out=EE[1], in0=PB[5], in1=EE[1], op=ALU.mult), waits=[e1])
            att["acc_free"] = e3
            e4 = P.op("vector", lambda e: e.scalar_tensor_tensor(out=EE[2], in0=EE[1], scalar=neglam, in1=EE[0], op0=ALU.mult, op1=ALU.add), waits=[e2, e3, t_l7] + wfree)
            e5 = P.op("gpsimd", lambda e: e.tensor_tensor(out=EE[3], in0=EE[2], in1=EE[2], op=ALU.mult), waits=[e4] + wfree)

            def finish():
                ss = P.op("tensor", lambda e: e.matmul(PB[6], lhsT=onesf, rhs=EE[3], start=True, stop=True), waits=[e5, e3, d_cf, att["l6_free"]])
                e6 = P.op("scalar", lambda e: e.activation(out=EE[3], in_=PB[6], func=AF.Ln, scale=1.0 / 128.0, bias=epsc), waits=[ss])
                att["l6_free"] = e6
                e7 = P.op("scalar", lambda e: e.activation(out=EE[3], in_=EE[3], func=AF.Exp, scale=-0.5), waits=[e6])
                e8 = P.op("vector", lambda e: e.tensor_tensor(out=EE[2], in0=EE[2], in1=EE[3], op=ALU.mult), waits=[e7])
                e9 = P.op("vector", lambda e: e.tensor_scalar(out=QO[:, h, qrange], in0=EE[2], scalar1=gsc, scalar2=None, op0=ALU.mult), waits=[e8, t_g])
                att["ee_free"] = e9
                att["last"] = e9
            att["pending"] = finish

        def attention(hp):
            for i in range(NQT):
                for hl in range(2):
                    att_unit(i, hl, 2 * hp + hl)
            att["pending"]()
            att["pending"] = None
            return att["last"]

        att_last = None
        if upto == "consts":
            tap("small", small, t_consts)
            P.emit(finals)
            return nc, tap_out
        for hp in range(2):
            kvt = phase_A(hp, hp == 0)
            if upto == "A":
                tap("kt", KT[:, :, 0:2048], kvt, BF16)
                tap("vv", VV[:, 0:8, :], kvt, BF16)
                P.emit(finals)
                return nc, tap_out
            qtoks = phase_Q() if hp == 0 else []
            P.base_waits = [t for t in kvt + qtoks if t is not None] + t_consts
            att_last = attention(hp)
            P.base_waits = [att_last]
            if upto == "att0":
                break
        tap("qo", QO, [att_last], BF16)
        tap("kt", KT[:, :, 0:2048], [att_last], BF16)
        tap("vv", VV[:, 0:8, :], [att_last], BF16)

        if upto in ("att0", "att"):
            P.emit(finals)
            return nc, tap_out

        AR.reset(pers_mark)
        wp = AR.alloc([128, 8, 3584], BF16)
        woa = AR.alloc([128, 4, 1024], BF16)
        woc = AR.alloc([128, 4, 1024], BF16)
        wmx = AR.alloc([128, 8, 1024], BF16)
        xb2_ = [AR.alloc([128, 2, 1024], BF16) for _ in range(2)]
        xhb_ = [AR.alloc([128, 1024], BF16) for _ in range(2)]
        xres_ = [AR.alloc([128, 2, 1024], F32) for _ in range(2)]
        xT2 = AR.alloc([128, 8, 256], BF16)
        xhT = AR.alloc([128, 32], BF16)
        ccs_ = [AR.alloc([128, 264], F32) for _ in range(2)]
        Ub_ = [AR.alloc([128, 2, 130], F32) for _ in range(2)]
        T1_ = [AR.alloc([128, 2, 128], F32) for _ in range(2)]
        Zb = AR.alloc([128, 4, 256], BF16)
        g0_ = [AR.alloc([128, 256], F32) for _ in range(2)]
        g1_ = [AR.alloc([128, 256], F32) for _ in range(2)]
        m0_ = [AR.alloc([128, 256], F32) for _ in range(2)]
        m1_ = [AR.alloc([128, 256], F32) for _ in range(2)]
        MT = AR.alloc([128, 8, 256], BF16)
        Rb_ = [AR.alloc([128, 1024], F32) for _ in range(2)]
        lng = AR.alloc([128, 1024], F32)
        lnb = AR.alloc([128, 1024], F32)
        H1T = AR.alloc([128, 8, 128], F32)
        stt_b = AR.alloc([128, 16], F32)
        bgs = AR.alloc([128, 16], F32)
        cws = AR.alloc([128, 12], F32)
        rbias = AR.alloc([128, 36], F32)
        wr = AR.alloc([128, 8, 36], F32)
        for i7 in range(7):
            P.adma("gpsimd", wp[:, :, i7 * 512:(i7 + 1) * 512], w_in[:, 1536 + i7 * 512:1536 + (i7 + 1) * 512].rearrange("(c p) n -> p c n", p=128), "wp", writes=["wp"])
        P.adma("gpsimd", woa, w_o_att.rearrange("(c p) n -> p c n", p=128), "woa", writes=["woa"])
        P.adma("gpsimd", woc, w_o_conv.rearrange("(c p) n -> p c n", p=128), "woc", writes=["woc"])
        P.adma("gpsimd", wmx, w_mix.rearrange("(c p) n -> p c n", p=128), "wmx", writes=["wmx"])
        P.adma("sync", wr, w_rt.rearrange("(c p) n -> p c n", p=128), "wr", writes=["wr"])
        P.adma("sync", bgs, b_gate.rearrange("o (q p) -> p (o q)", p=128), "bgs", writes=["bgs"], allow_slow_non_contiguous=True)
        P.adma("sync", cws.rearrange("p (k q) -> p k q", k=3), conv_w.rearrange("k (q p) -> p k q", p=128), "cws", writes=["cws"], allow_slow_non_contiguous=True)
        P.adma("sync", rbias, b_rt.to_broadcast([128, 36]), "rbias", writes=["rbias"])
        P.adma("sync", lng, ln1[0:1, :].to_broadcast([128, 1024]), "lng", writes=["lng"])
        P.adma("sync", lnb, ln1[1:2, :].to_broadcast([128, 1024]), "lnb", writes=["lnb"])

        def layer_norm(buf, gname, bname, gt, bt, stt=None, rn="R", off_pool=False):
            stt = stt_b if stt is None else stt
            mv = stt[:, 12:14]
            ve = stt[:, 14:15]
            rs = stt[:, 15:16]
            P.auto("vector", lambda e: e.bn_stats(out=stt[:, 0:6], in_=buf[:, 0:512]), reads=[rn], writes=["stt"])
            P.auto("vector", lambda e: e.bn_stats(out=stt[:, 6:12], in_=buf[:, 512:1024]), reads=[rn], writes=["stt2"])
            P.auto("vector", lambda e: e.bn_aggr(out=mv, in_=stt[:, 0:12]), reads=["stt", "stt2"], writes=["mv"])
            if off_pool:
                P.auto("scalar", lambda e: e.activation(out=rs, in_=mv[:, 1:2], func=AF.Ln, bias=epsc), reads=["mv"], writes=["rs"], extra=[d_cf])
                P.auto("scalar", lambda e: e.activation(out=rs, in_=rs, func=AF.Exp, scale=-0.5), writes=["rs"])
            else:
                P.auto("vector", lambda e: e.tensor_scalar(out=ve, in0=mv[:, 1:2], scalar1=1e-5, scalar2=None, op0=ALU.add), reads=["mv"], writes=["ve"])
                P.auto("gpsimd", lambda e: e.tensor_tensor(out=rs, in0=ve, in1=mhalf, op=ALU.pow), reads=["ve"], writes=["rs"], extra=[d_cf])
            P.auto("vector", lambda e: e.tensor_scalar(out=buf, in0=buf, scalar1=mv[:, 0:1], scalar2=rs, op0=ALU.subtract, op1=ALU.mult), reads=["mv", "rs"], writes=[rn])
            P.auto("vector", lambda e: e.tensor_tensor(out=buf, in0=buf, in1=gt, op=ALU.mult), reads=[gname], writes=[rn])
            return P.auto("vector" if off_pool else "gpsimd", lambda e: e.tensor_tensor(out=buf, in0=buf, in1=bt, op=ALU.add), reads=[bname], writes=[rn])

        def b_loads(tb):
            r0 = tb * 256
            pb = tb % 2
            P.adma("gpsimd", xb2_[pb], xo[r0:r0 + 256, :].rearrange("(b p) d -> p b d", p=128), "xb2%d" % pb, writes=["xb2%d" % pb])
            P.adma("gpsimd", xhb_[pb][0:4, :], xh[tb * 4:tb * 4 + 4, :], "xhb%d" % pb, writes=["xhb%d" % pb])
            P.adma("sync", xres_[pb], xo[r0:r0 + 256, :].rearrange("(b p) d -> p b d", p=128), "xres%d" % pb, writes=["xres%d" % pb])

        def b_tile(tb):
            r0 = tb * 256
            pb = tb % 2
            xb2, xhb, xres = xb2_[pb], xhb_[pb], xres_[pb]
            n_xb2, n_xhb, n_xres = "xb2%d" % pb, "xhb%d" % pb, "xres%d" % pb
            if tb + 1 < NTB:
                b_loads(tb + 1)
            for half in range(2):
                fns = []
                for cc in range(4):
                    c = half * 4 + cc
                    for blk in range(2):
                        o_ = PBb[half][:, cc * 256 + blk * 128: cc * 256 + blk * 128 + 128]
                        i_ = xb2[:, blk, c * 128:(c + 1) * 128]
                        fns.append(lambda e, o_=o_, i_=i_: e.transpose(out=o_, in_=i_, identity=ident))
                P.auto("tensor", fns, reads=[n_xb2], psum=["b%d" % half], extra=[d_cb])
                dst = xT2[:, half * 4:half * 4 + 4, :].rearrange("p a b -> p (a b)")
                if half == 0:
                    P.auto("scalar", lambda e, dst=dst: e.copy(out=dst, in_=PBb[0]), writes=["xT2a"], psum=["b0"])
                else:
                    P.auto("scalar", lambda e, dst=dst: e.copy(out=dst, in_=PBb[1]), writes=["xT2b"], psum=["b1"])
            fns = []
            for c in range(8):
                o_ = PBb[2][:, 600 + c * 4:600 + (c + 1) * 4]
                i_ = xhb[0:4, c * 128:(c + 1) * 128]
                fns.append(lambda e, o_=o_, i_=i_: e.transpose(out=o_, in_=i_, identity=ident[0:4, 0:4]))
            P.auto("tensor", fns, reads=[n_xhb], psum=["b2"], extra=[d_cb])
            P.auto("scalar", lambda e: e.copy(out=xhT, in_=PBb[2][:, 600:632]), writes=["xhT"], psum=["b2"])
            xhT3 = xhT.rearrange("p (c k) -> p c k", c=8)
            for q in range(4):
                qp = q % 2
                BA, BB = PB[2 + 2 * qp], PB[3 + 2 * qp]
                nBA, nBB = "b%d" % (2 + 2 * qp), "b%d" % (3 + 2 * qp)
                ccs, Ub, T1 = ccs_[qp], Ub_[qp], T1_[qp]
                nccs, nU, nU2, nT1 = "ccs%d" % qp, "U%d" % qp, "U2%d" % qp, "T1%d" % qp
                fns = []
                for c in range(8):
                    l_ = wp[:, c, 512 + q * 128: 512 + q * 128 + 128]
                    fns.append(lambda e, l_=l_, c=c, BA=BA: e.matmul(BA[:, 0:256], lhsT=l_, rhs=xT2[:, c, :], start=(c == 0), stop=(c == 7)))
                for c in range(8):
                    l_ = wp[:, c, 512 + q * 128: 512 + q * 128 + 128]
                    fns.append(lambda e, l_=l_, c=c, BA=BA: e.matmul(BA[:, 256:260], lhsT=l_, rhs=xhT3[:, c, :], start=(c == 0), stop=(c == 7)))
                for c in range(8):
                    l_ = wp[:, c, 1024 + q * 128: 1024 + q * 128 + 128]
                    fns.append(lambda e, l_=l_, c=c, BA=BA: e.matmul(BA[:, 260:264], lhsT=l_, rhs=xhT3[:, c, :], start=(c == 0), stop=(c == 7)))
                P.auto("tensor", fns, reads=["wp", "xT2a", "xT2b", "xhT"], psum=[nBA])
                fns = []
                for c in range(8):
                    l_ = wp[:, c, 1024 + q * 128: 1024 + q * 128 + 128]
                    fns.append(lambda e, l_=l_, c=c, BB=BB: e.matmul(BB[:, 0:256], lhsT=l_, rhs=xT2[:, c, :], start=(c == 0), stop=(c == 7)))
                for c in range(8):
                    l_ = wp[:, c, q * 128: q * 128 + 128]
                    fns.append(lambda e, l_=l_, c=c, BB=BB: e.matmul(BB[:, 256:512], lhsT=l_, rhs=xT2[:, c, :], start=(c == 0), stop=(c == 7)))
                P.auto("tensor", fns, reads=["wp", "xT2a", "xT2b"], psum=[nBB])
                P.auto("scalar", lambda e, ccs=ccs, BA=BA: e.copy(out=ccs, in_=BA[:, 0:264]), writes=[nccs], psum=[nBA])
                P.auto("vector", lambda e, ccs=ccs, Ub=Ub, BB=BB: e.tensor_tensor(out=Ub[:, :, 2:130], in0=ccs[:, 0:256].rearrange("p (b t) -> p b t", b=2),
                                                          in1=BB[:, 0:256].rearrange("p (b t) -> p b t", b=2), op=ALU.mult),
                       reads=[nccs], writes=[nU], psum=[nBB])
                P.auto("vector", lambda e, ccs=ccs, Ub=Ub: e.tensor_tensor(out=Ub[:, :, 0:2], in0=ccs[:, 256:260].rearrange("p (b t) -> p b t", b=2),
                                                          in1=ccs[:, 260:264].rearrange("p (b t) -> p b t", b=2), op=ALU.mult),
                       reads=[nccs], writes=[nU2])
                P.auto("vector", lambda e, q=q, T1=T1, Ub=Ub: e.tensor_scalar(out=T1, in0=Ub[:, :, 0:128], scalar1=cws[:, q:q + 1], scalar2=None, op0=ALU.mult),
                       reads=[nU, nU2, "cws"], writes=[nT1])
                P.auto("vector", lambda e, q=q, T1=T1, Ub=Ub: e.scalar_tensor_tensor(out=T1, in0=Ub[:, :, 1:129], scalar=cws[:, 4 + q:5 + q], in1=T1, op0=ALU.mult, op1=ALU.add),
                       reads=[nU, nU2], writes=[nT1])
                P.auto("vector", lambda e, q=q, T1=T1, Ub=Ub: e.scalar_tensor_tensor(out=T1, in0=Ub[:, :, 2:130], scalar=cws[:, 8 + q:9 + q], in1=T1, op0=ALU.mult, op1=ALU.add),
                       reads=[nU, nU2], writes=[nT1])
                P.auto("vector", lambda e, q=q, T1=T1, BB=BB: e.tensor_tensor(out=Zb[:, q, :], in0=T1.rearrange("p b t -> p (b t)"), in1=BB[:, 256:512], op=ALU.mult),
                       reads=[nT1], writes=["Z%d" % q], psum=[nBB])
            flush_routers()
            for c8 in range(8):
                cp_ = c8 % 2
                BG, BY = PB[2 + 2 * cp_], PB[3 + 2 * cp_]
                nBG, nBY = "b%d" % (2 + 2 * cp_), "b%d" % (3 + 2 * cp_)
                g0, g1, m0, m1 = g0_[cp_], g1_[cp_], m0_[cp_], m1_[cp_]
                ng0, ng1, nm0, nm1 = "g0%d" % cp_, "g1%d" % cp_, "m0%d" % cp_, "m1%d" % cp_
                fns = []
                for c in range(8):
                    l_ = wp[:, c, 1536 + c8 * 128: 1536 + c8 * 128 + 128]
                    fns.append(lambda e, l_=l_, c=c, BG=BG: e.matmul(BG[:, 0:256], lhsT=l_, rhs=xT2[:, c, :], start=(c == 0), stop=(c == 7)))
                for c in range(8):
                    l_ = wp[:, c, 2560 + c8 * 128: 2560 + c8 * 128 + 128]
                    fns.append(lambda e, l_=l_, c=c, BG=BG: e.matmul(BG[:, 256:512], lhsT=l_, rhs=xT2[:, c, :], start=(c == 0), stop=(c == 7)))
                P.auto("tensor", fns, reads=["wp", "xT2a", "xT2b"], psum=[nBG])
                P.auto("scalar", lambda e, c8=c8, g0=g0, BG=BG: e.activation(out=g0, in_=BG[:, 0:256], func=AF.Sigmoid, bias=bgs[:, c8:c8 + 1]), reads=["bgs"], writes=[ng0], psum=[nBG])
                P.auto("scalar", lambda e, c8=c8, g1=g1, BG=BG: e.activation(out=g1, in_=BG[:, 256:512], func=AF.Sigmoid, bias=bgs[:, 8 + c8:9 + c8]), reads=["bgs"], writes=[ng1], psum=[nBG])
                fns = []
                for h in range(4):
                    l_ = woa[:, h, c8 * 128:(c8 + 1) * 128]
                    r_ = QO[:, h, r0:r0 + 256]
                    fns.append(lambda e, l_=l_, r_=r_, h=h, BY=BY: e.matmul(BY[:, 0:256], lhsT=l_, rhs=r_, start=(h == 0), stop=(h == 3)))
                for q in range(4):
                    l_ = woc[:, q, c8 * 128:(c8 + 1) * 128]
                    r_ = Zb[:, q, :]
                    fns.append(lambda e, l_=l_, r_=r_, q=q, BY=BY: e.matmul(BY[:, 256:512], lhsT=l_, rhs=r_, start=(q == 0), stop=(q == 3)))
                P.auto("tensor", fns, reads=["woa", "woc", "Z0", "Z1", "Z2", "Z3"], psum=[nBY])
                P.auto("vector", lambda e, m0=m0, g0=g0, BY=BY: e.tensor_tensor(out=m0, in0=g0, in1=BY[:, 0:256], op=ALU.mult), reads=[ng0], writes=[nm0], psum=[nBY])
                P.auto("vector", lambda e, m1=m1, g1=g1, BY=BY: e.tensor_tensor(out=m1, in0=g1, in1=BY[:, 256:512], op=ALU.mult), reads=[ng1], writes=[nm1], psum=[nBY])
                P.auto("vector", lambda e, c8=c8, m0=m0, m1=m1: e.tensor_tensor(out=MT[:, c8, :], in0=m0, in1=m1, op=ALU.add), reads=[nm0, nm1], writes=["MT%d" % c8])
            for blk in range(2):
                gb = tb * 2 + blk
                Rb = Rb_[blk]
                rn = "R%d" % blk
                fns = []
                for half in range(2):
                    for c in range(8):
                        l_ = MT[:, c, blk * 128:(blk + 1) * 128]
                        r_ = wmx[:, c, half * 512:(half + 1) * 512]
                        fns.append(lambda e, l_=l_, r_=r_, c=c, half=half: e.matmul(PB[6 + half], lhsT=l_, rhs=r_, start=(c == 0), stop=(c == 7)))
                P.auto("tensor", fns, reads=["MT%d" % c_ for c_ in range(8)] + ["wmx"], psum=["b6", "b7"])
                P.auto("vector", lambda e, blk=blk, Rb=Rb: e.scalar_tensor_tensor(out=Rb[:, 0:512], in0=xres[:, blk, 0:512], scalar=ALPHA, in1=PB[6], op0=ALU.mult, op1=ALU.add),
                       reads=[n_xres], writes=[rn], psum=["b6"])
                P.auto("vector", lambda e, blk=blk, Rb=Rb: e.scalar_tensor_tensor(out=Rb[:, 512:1024], in0=xres[:, blk, 512:1024], scalar=ALPHA, in1=PB[7], op0=ALU.mult, op1=ALU.add),
                       reads=[n_xres], writes=[rn], psum=["b7"])
                layer_norm(Rb, "lng", "lnb", lng, lnb, None, rn)
                finals_h1.append(P.adma("sync", h1f[gb * 128:(gb + 1) * 128, :], Rb, "h1w%d" % blk, reads=[rn]))
                pend_r.append(make_router(Rb, rn, gb))

        def make_router(Rb, rn, gb):
            def run():
                fns = []
                for c in range(8):
                    o_ = PB[c // 4][:, (c % 4) * 128:(c % 4) * 128 + 128]
                    i_ = Rb[:, c * 128:(c + 1) * 128]
                    fns.append(lambda e, o_=o_, i_=i_: e.transpose(out=o_, in_=i_, identity=identf))
                P.auto("tensor", fns, reads=[rn], psum=["b0", "b1"], extra=[d_cf])
                P.auto("scalar", lambda e: e.copy(out=H1T[:, 0:4, :].rearrange("p a b -> p (a b)"), in_=PB[0]), writes=["H1Ta"], psum=["b0"])
                P.auto("vector", lambda e: e.tensor_copy(out=H1T[:, 4:8, :].rearrange("p a b -> p (a b)"), in_=PB[1]), writes=["H1Tb"], psum=["b1"])
                fns = []
                for c in range(8):
                    fns.append(lambda e, c=c: e.matmul(PB[0][:, 0:36], lhsT=H1T[:, c, :], rhs=wr[:, c, :], start=(c == 0), stop=(c == 7)))
                P.auto("tensor", fns, reads=["H1Ta", "H1Tb", "wr"], psum=["b0"])
                P.auto("vector", lambda e: e.tensor_tensor(out=LG[:, gb, :], in0=PB[0][:, 0:36], in1=rbias, op=ALU.add), reads=["rbias"], writes=["LG"], psum=["b0"])
            return run

        pend_r = []

        def flush_routers():
            while pend_r:
                pend_r.pop(0)()

        finals_h1 = []
        NTB = NQT * 2
        b_loads(0)
        for tb in range(NTB):
            b_tile(tb)
        flush_routers()
        tap("lg", LG, [P.lw["LG"]])
        tap("h1", h1f[0:256, :], finals_h1)
        if upto == "B":
            P.emit(finals + finals_h1)
            return nc, tap_out

        P.base_waits = [P.lw["LG"]] + finals_h1
        AR.reset(pers_mark)
        NB = NTB * 2
        gm = AR.alloc([128, 32], F32)
        gd = AR.alloc([128, 32, 4], F32)
        pen = AR.alloc([128, 32, 4], F32)
        ge = AR.alloc([128, 32, 4], F32)
        gs = AR.alloc([128, 32], F32)
        gw = AR.alloc([128, 32], F32)
        em = AR.alloc([128, 32, 32], F32)
        em2 = AR.alloc([128, 32, 32], F32)
        oh1 = AR.alloc([128, 32, 32], F32)
        oh2 = AR.alloc([128, 32, 32], F32)
        Aa = AR.alloc([128, 32, 32], F32)
        Tt = AR.alloc([128, 32, 32], F32)
        sc0 = AR.alloc([128, 32, 32], F32)
        sc1 = AR.alloc([128, 32, 32], F32)
        rank = AR.alloc([128, 32, 32], F32)
        tmpc = AR.alloc([128, 32, 32], F32)
        v1 = AR.alloc([128, 32], F32)
        v2 = AR.alloc([128, 32], F32)
        dv = AR.alloc([128, 32], F32)
        s12 = AR.alloc([128, 2, 32], F32)
        stt_c = AR.alloc([128, 16], F32)
        fl = lambda t: t.rearrange("p a b -> p (a b)")
        gl = LG[:, :, 0:4]
        el = LG[:, :, 4:36]
        bc3 = lambda t: t.unsqueeze(2).to_broadcast([128, 32, 32])
        V = "vector"
        P.auto(V, lambda e: e.tensor_reduce(out=gm, in_=gl, axis=AX.X, op=ALU.max), reads=["LG"], writes=["gm"])
        P.auto(V, lambda e: e.tensor_tensor(out=gd, in0=gl, in1=gm.unsqueeze(2).to_broadcast([128, 32, 4]), op=ALU.subtract), reads=["LG", "gm"], writes=["gd"])
        P.auto(V, lambda e: e.tensor_scalar(out=pen, in0=gd, scalar1=0.0, scalar2=NEG, op0=ALU.is_lt, op1=ALU.mult), reads=["gd"], writes=["pen"])
        P.auto("scalar", lambda e: e.activation(out=ge, in_=gd, func=AF.Exp), reads=["gd"], writes=["ge"])
        P.auto(V, lambda e: e.tensor_reduce(out=gs, in_=ge, axis=AX.X, op=ALU.add), reads=["ge"], writes=["gs"])
        P.auto(V, lambda e: e.reciprocal(out=gw, in_=gs), reads=["gs"], writes=["gw"])
        P.auto(V, lambda e: e.tensor_tensor(out=em.rearrange("p b (g k) -> p b g k", g=4), in0=el.rearrange("p b (g k) -> p b g k", g=4),
                                            in1=pen.unsqueeze(3).to_broadcast([128, 32, 4, 8]), op=ALU.add), reads=["LG", "pen"], writes=["em"])
        P.auto(V, lambda e: e.tensor_reduce(out=v1, in_=em, axis=AX.X, op=ALU.max), reads=["em"], writes=["v1"])
        P.auto(V, lambda e: e.tensor_tensor(out=oh1, in0=em, in1=bc3(v1), op=ALU.is_equal), reads=["em", "v1"], writes=["oh1"])
        P.auto(V, lambda e: e.scalar_tensor_tensor(out=fl(em2), in0=fl(oh1), scalar=NEG, in1=fl(em), op0=ALU.mult, op1=ALU.add), reads=["oh1", "em"], writes=["em2"])
        P.auto(V, lambda e: e.tensor_reduce(out=v2, in_=em2, axis=AX.X, op=ALU.max), reads=["em2"], writes=["v2"])
        P.auto(V, lambda e: e.tensor_tensor(out=oh2, in0=em2, in1=bc3(v2), op=ALU.is_equal), reads=["em2", "v2"], writes=["oh2"])
        P.auto(V, lambda e: e.tensor_tensor(out=dv, in0=v2, in1=v1, op=ALU.subtract), reads=["v1", "v2"], writes=["dv"])
        P.auto("scalar", lambda e: e.activation(out=dv, in_=dv, func=AF.Exp), reads=[], writes=["dv"])
        P.auto(V, lambda e: e.tensor_scalar(out=dv, in0=dv, scalar1=1.0, scalar2=None, op0=ALU.add), writes=["dv"])
        P.auto(V, lambda e: e.reciprocal(out=dv, in_=dv), writes=["dv"])
        P.auto(V, lambda e: e.tensor_tensor(out=PW[:, 0, :], in0=dv, in1=gw, op=ALU.mult), reads=["dv", "gw"], writes=["PW0"])
        P.auto(V, lambda e: e.tensor_tensor(out=PW[:, 1, :], in0=gw, in1=PW[:, 0, :], op=ALU.subtract), reads=["PW0", "gw"], writes=["PW1"])
        P.auto(V, lambda e: e.tensor_tensor(out=fl(Aa), in0=fl(oh1), in1=fl(oh2), op=ALU.add), reads=["oh1", "oh2"], writes=["Aa"])
        Af = fl(Aa)
        P.auto("tensor", [lambda e: e.matmul(PB[0], lhsT=trif, rhs=Af[:, 0:512], start=True, stop=True),
                          lambda e: e.matmul(PB[1], lhsT=trif, rhs=Af[:, 512:1024], start=True, stop=True),
                          lambda e: e.matmul(PB[2], lhsT=onesf, rhs=Af[:, 0:512], start=True, stop=True),
                          lambda e: e.matmul(PB[3], lhsT=onesf, rhs=Af[:, 512:1024], start=True, stop=True)],
               reads=["Aa"], psum=["b0", "b1", "b2", "b3"], extra=[d_cf])
        P.auto(V, lambda e: e.tensor_copy(out=fl(Tt)[:, 0:512], in_=PB[2]), writes=["Tt"], psum=["b2"])
        P.auto(V, lambda e: e.tensor_copy(out=fl(Tt)[:, 512:1024], in_=PB[3]), writes=["Tt"], psum=["b3"])
        cur, cname = Tt, "Tt"
        for si, sh in enumerate((1, 2, 4, 8, 16)):
            nxt, nname = (sc0, "sc0") if si % 2 == 0 else (sc1, "sc1")
            P.auto(V, lambda e, cur=cur, nxt=nxt, sh=sh: e.tensor_tensor(out=nxt[:, sh:32, :], in0=cur[:, sh:32, :], in1=cur[:, 0:32 - sh, :], op=ALU.add), reads=[cname], writes=[nname])
            P.auto(V, lambda e, cur=cur, nxt=nxt, sh=sh: e.tensor_copy(out=nxt[:, 0:sh, :], in_=cur[:, 0:sh, :]), reads=[cname], writes=[nname])
            cur, cname = nxt, nname
        P.auto(V, lambda e, cur=cur: e.tensor_tensor(out=fl(tmpc), in0=fl(cur), in1=fl(Tt), op=ALU.subtract), reads=[cname, "Tt"], writes=["tmpc"])
        P.auto(V, lambda e: e.tensor_tensor(out=fl(rank)[:, 0:512], in0=fl(tmpc)[:, 0:512], in1=PB[0], op=ALU.add), reads=["tmpc"], writes=["rank"], psum=["b0"])
        P.auto(V, lambda e: e.tensor_tensor(out=fl(rank)[:, 512:1024], in0=fl(tmpc)[:, 512:1024], in1=PB[1], op=ALU.add), reads=["tmpc"], writes=["rank"], psum=["b1"])
        P.auto(V, lambda e: e.tensor_scalar(out=fl(rank), in0=fl(rank), scalar1=float(CAP - 1), scalar2=None, op0=ALU.min), writes=["rank"])
        P.auto(V, lambda e: e.tensor_tensor(out=rank, in0=rank, in1=ecap.unsqueeze(1).to_broadcast([128, 32, 32]), op=ALU.add), writes=["rank"], extra=[d_cf])
        P.auto(V, lambda e: e.tensor_tensor(out=fl(tmpc), in0=fl(oh1), in1=fl(rank), op=ALU.mult), reads=["oh1", "rank"], writes=["tmpc"])
        P.auto(V, lambda e: e.tensor_reduce(out=s12[:, 0, :], in_=tmpc, axis=AX.X, op=ALU.add), reads=["tmpc"], writes=["s12a"])
        P.auto(V, lambda e: e.tensor_tensor(out=fl(tmpc), in0=fl(oh2), in1=fl(rank), op=ALU.mult), reads=["oh2", "rank"], writes=["tmpc"])
        P.auto(V, lambda e: e.tensor_reduce(out=s12[:, 1, :], in_=tmpc, axis=AX.X, op=ALU.add), reads=["tmpc"], writes=["s12b"])
        P.auto(V, lambda e: e.tensor_scalar(out=fl(s12), in0=fl(s12), scalar1=float(NSLOT - 1), scalar2=0.0, op0=ALU.min, op1=ALU.max), reads=["s12a", "s12b"], writes=["s12a", "s12b"])
        P.auto(V, lambda e: e.tensor_copy(out=SLI, in_=s12), reads=["s12a", "s12b"], writes=["SLI"])
        tap("sli", SLI, [P.lw["SLI"]], I32)
        tap("pw", PW, [P.lw["PW1"]])

        P.base_waits = [P.lw["SLI"], P.lw["PW1"], P.lw["PW0"]] + finals_h1
        AR.reset(pers_mark)
        stt_c = AR.alloc([128, 16], F32)
        wg = [AR.alloc([128, 8, 512], BF16) for _ in range(3)]
        wu = [AR.alloc([128, 8, 512], BF16) for _ in range(3)]
        wd = [AR.alloc([128, 4, 1024], BF16) for _ in range(3)]

        def e_loads_w(e_):
            i = e_ % 3
            P.adma("gpsimd", wg[i], w_eg[e_].rearrange("(c p) n -> p c n", p=128), "wg%d" % i, writes=["wg%d" % i])
            P.adma("gpsimd", wu[i], w_eu[e_].rearrange("(c p) n -> p c n", p=128), "wu%d" % i, writes=["wu%d" % i])
            P.adma("gpsimd", wd[i], w_ed[e_].rearrange("(c p) n -> p c n", p=128), "wd%d" % i, writes=["wd%d" % i])

        e_loads_w(0)
        e_loads_w(1)
        e_loads_w(2)
        hb = [AR.alloc([128, 1024], F32) for _ in range(2)]
        sc_toks = []
        for blk in range(NB):
            i = blk % 2
            P.adma("sync", hb[i], h1f[blk * 128:(blk + 1) * 128, :], "hb%d" % i, writes=["hb%d" % i])
            for k in range(2):
                off = SLI[:, k, blk:blk + 1].bitcast(U32)
                src = hb[i]
                sc_toks.append(P.acdma("gpsimd", lambda e, off=off, src=src: e.indirect_dma_start(
                    out=xs_d, out_offset=bass.IndirectOffsetOnAxis(ap=off, axis=0), in_=src, in_offset=None),
                    "sc%d_%d" % (i, k), reads=["hb%d" % i, "SLI"]))

        XE = [AR.alloc([128, 3, 1024], BF16) for _ in range(2)]
        XT = AR.alloc([128, 8, 384], BF16)
        sg = AR.alloc([128, 384], F32)
        HT = AR.alloc([128, 4, 384], BF16)
        YSb = [AR.alloc([128, 1024], F32) for _ in range(2)]
        NE = 32
        ys_toks = []

        def e_loads(e_):
            i = e_ % 2
            P.adma("sync", XE[i], xs_d[e_ * CAP:(e_ + 1) * CAP, :].rearrange("(b p) d -> p b d", p=128), "xe%d" % i, writes=["xe%d" % i], extra=sc_toks)

        def e_compute(e_):
            i = e_ % 2
            iw = e_ % 3
            for pr in range(4):
                bank = pr % 2
                fns = []
                for cc in range(2):
                    c = 2 * pr + cc
                    for sb in range(3):
                        o_ = PBb[bank][:, cc * 384 + sb * 128: cc * 384 + sb * 128 + 128]
                        i_ = XE[i][:, sb, c * 128:(c + 1) * 128]
                        fns.append(lambda e, o_=o_, i_=i_: e.transpose(out=o_, in_=i_, identity=ident))
                P.auto("tensor", fns, reads=["xe%d" % i], psum=["b%d" % bank])
                dst = XT[:, 2 * pr:2 * pr + 2, :].rearrange("p a b -> p (a b)")
                if bank == 0:
                    P.auto("vector", lambda e, dst=dst: e.tensor_copy(out=dst, in_=PBb[0][:, 0:768]), writes=["XT%d" % pr], psum=["b0"])
                else:
                    P.auto("scalar", lambda e, dst=dst: e.copy(out=dst, in_=PBb[1][:, 0:768]), writes=["XT%d" % pr], psum=["b1"])
            xt_names = ["XT%d" % pr for pr in range(4)]
            for f in range(4):
                gb_ = 2 + 2 * (f % 2)
                ub_ = 3 + 2 * (f % 2)
                fns = []
                for c in range(8):
                    l_ = wg[iw][:, c, f * 128:(f + 1) * 128]
                    fns.append(lambda e, l_=l_, c=c, gb_=gb_: e.matmul(PB[gb_][:, 0:384], lhsT=l_, rhs=XT[:, c, :], start=(c == 0), stop=(c == 7)))
                P.auto("tensor", fns, reads=["wg%d" % iw] + xt_names, psum=["b%d" % gb_])
                fns = []
                for c in range(8):
                    l_ = wu[iw][:, c, f * 128:(f + 1) * 128]
                    fns.append(lambda e, l_=l_, c=c, ub_=ub_: e.matmul(PB[ub_][:, 0:384], lhsT=l_, rhs=XT[:, c, :], start=(c == 0), stop=(c == 7)))
                P.auto("tensor", fns, reads=["wu%d" % iw] + xt_names, psum=["b%d" % ub_])
                P.auto("scalar", lambda e, gb_=gb_: e.activation(out=sg, in_=PB[gb_][:, 0:384], func=AF.Silu), writes=["sg"], psum=["b%d" % gb_])
                P.auto("vector", lambda e, ub_=ub_, f=f: e.tensor_tensor(out=HT[:, f, :], in0=sg, in1=PB[ub_][:, 0:384], op=ALU.mult), reads=["sg"], writes=["HT%d" % f], psum=["b%d" % ub_])
            ht_names = ["HT%d" % f for f in range(4)]
            for sb in range(3):
                k = (e_ * 3 + sb) % 2
                fns = []
                for half in range(2):
                    for f in range(4):
                        l_ = HT[:, f, sb * 128:(sb + 1) * 128]
                        r_ = wd[iw][:, f, half * 512:(half + 1) * 512]
                        fns.append(lambda e, l_=l_, r_=r_, f=f, half=half: e.matmul(PB[6 + half], lhsT=l_, rhs=r_, start=(f == 0), stop=(f == 3)))
                P.auto("tensor", fns, reads=["wd%d" % iw] + ht_names, psum=["b6", "b7"])
                ysb = YSb[k]
                P.auto("scalar", lambda e, ysb=ysb: e.copy(out=ysb[:, 0:512], in_=PB[6]), writes=["ysa%d" % k], psum=["b6"])
                P.auto("vector", lambda e, ysb=ysb: e.tensor_copy(out=ysb[:, 512:1024], in_=PB[7]), writes=["ysb%d" % k], psum=["b7"])
                r0 = e_ * CAP + sb * 128
                ys_toks.append(P.adma("sync", ys_d[r0:r0 + 128, :], ysb, "ysw%d" % k, reads=["ysa%d" % k, "ysb%d" % k]))

        e_loads(0)
        for e_ in range(NE):
            if e_ + 1 < NE:
                e_loads(e_ + 1)
            e_compute(e_)
            if e_ + 3 < NE:
                e_loads_w(e_ + 3)

        Y1_ = [AR.alloc([128, 1024], F32) for _ in range(2)]
        Y2_ = [AR.alloc([128, 1024], F32) for _ in range(2)]
        hc_ = [AR.alloc([128, 1024], F32) for _ in range(2)]
        R2_ = [AR.alloc([128, 1024], F32) for _ in range(2)]
        lng2 = AR.alloc([128, 1024], F32)
        lnb2 = AR.alloc([128, 1024], F32)
        P.adma("sync", lng2, ln2[0:1, :].to_broadcast([128, 1024]), "lng2", writes=["lng2"])
        P.adma("sync", lnb2, ln2[1:2, :].to_broadcast([128, 1024]), "lnb2", writes=["lnb2"])

        def c_loads(blk):
            pb = blk % 2
            P.adma("sync", hc_[pb], h1f[blk * 128:(blk + 1) * 128, :], "hc%d" % pb, writes=["hc%d" % pb])
            for k, yb in ((0, Y1_[pb]), (1, Y2_[pb])):
                off = SLI[:, k, blk:blk + 1].bitcast(U32)
                P.acdma("gpsimd", lambda e, off=off, yb=yb: e.indirect_dma_start(
                    out=yb, out_offset=None, in_=ys_d, in_offset=bass.IndirectOffsetOnAxis(ap=off, axis=0)),
                    "yg%d%d" % (k, pb), reads=["SLI"], writes=["Y%d%d" % (k, pb)], extra=ys_toks)

        def c_block(blk):
            pb = blk % 2
            if blk + 1 < NB:
                c_loads(blk + 1)
            Y1, Y2, hc, R2 = Y1_[pb], Y2_[pb], hc_[pb], R2_[pb]
            rn = "R2%d" % pb
            P.auto("scalar", lambda e: e.mul(out=R2, in_=hc, mul=ALPHA), reads=["hc%d" % pb], writes=[rn])
            P.auto(V, lambda e: e.scalar_tensor_tensor(out=R2, in0=Y1, scalar=PW[:, 0, blk:blk + 1], in1=R2, op0=ALU.mult, op1=ALU.add), reads=["Y0%d" % pb, "PW0"], writes=[rn])
            P.auto(V, lambda e: e.scalar_tensor_tensor(out=R2, in0=Y2, scalar=PW[:, 1, blk:blk + 1], in1=R2, op0=ALU.mult, op1=ALU.add), reads=["Y1%d" % pb, "PW1"], writes=[rn])
            layer_norm(R2, "lng2", "lnb2", lng2, lnb2, stt_c, rn, off_pool=True)
            finals.append(P.adma("sync", out[blk * 128:(blk + 1) * 128, :], R2, "outw%d" % pb, reads=[rn]))

        c_loads(0)
        for blk in range(NB):
            c_block(blk)

        P.emit(finals)
    return nc, tap_out


def _consts(j):
    ident = np.eye(128, dtype=np.float32)
    rot = np.zeros((128, 128), np.float32)
    for m in range(128):
        if (m % 64) < 32:
            rot[m + 32, m] = -1.0
        else:
            rot[m - 32, m] = 1.0
    ones = np.ones((128, 128), np.float32)
    kk = np.arange(128)[:, None, None]
    r = np.arange(8)[None, :, None]
    qq = np.arange(512)[None, None, :]
    mask = ((r * 128 + kk) <= ((2 * (qq // 128) + j) * 128 + (qq % 128))).astype(np.float32)
    cbf = np.concatenate([ident, rot, ones, mask.reshape(128, 4096)], axis=1)
    cf = np.zeros((128, 512), np.float32)
    cf[:, 0:128] = ident
    cf[:, 128:256] = 1.0
    cf[:, 256:384] = (np.arange(128)[:, None] < np.arange(128)[None, :]).astype(np.float32)
    inv_freq = (np.float32(10000.0) ** (-np.arange(0, 64, 2, dtype=np.float32) / np.float32(64))).astype(np.float32)
    p = np.arange(128)
    cf[:, 384] = (inv_freq[(p % 64) % 32].astype(np.float64) / (2.0 * np.pi)).astype(np.float32)
    cf[:, 392:424] = (np.arange(32) * CAP)[None, :]
    cf[:, 424] = -0.5
    cf[:, 425] = 1e-5
    return np.ascontiguousarray(cbf), cf


def make_core_inputs(inp, c):
    b, j = c // 2, c % 2
    x = inp["x"]
    xb_ = x[b]
    blocks = xb_.reshape(64, 128, D)
    xo = np.ascontiguousarray(blocks[j::2].reshape(NOWN, D))
    xh = np.zeros((32, 2, D), np.float32)
    for m in range(32):
        blk = 2 * m + j
        if blk > 0:
            xh[m] = xb_[blk * 128 - 2: blk * 128]
    pos = np.asarray(inp["positions"][b], dtype=np.int32)
    poso = np.ascontiguousarray(pos.reshape(64, 128)[j::2].reshape(1, NOWN))
    cbf, cf = _consts(j)
    f = lambda a: np.ascontiguousarray(np.asarray(a, dtype=np.float32))
    return {
        "xf": f(xb_), "xo": xo, "xh": f(xh.reshape(64, D)), "posf": np.ascontiguousarray(pos.reshape(1, S)), "poso": poso,
        "w_in": f(inp["w_in"][0]), "b_gate": f(inp["b_gate"][0].reshape(1, 2048)),
        "lam_in": f(np.concatenate([inp["lambda_q1"][0], inp["lambda_k1"][0], inp["lambda_q2"][0], inp["lambda_k2"][0]]).reshape(1, 256)),
        "subln_g": f(inp["subln_g"][0].reshape(1, 128)), "w_o_att": f(inp["w_o_att"][0]), "conv_w": f(inp["conv_w"][0]),
        "w_o_conv": f(inp["w_o_conv"][0]), "w_mix": f(inp["w_mix_out"][0]),
        "ln1": f(np.stack([inp["ln1_g"][0], inp["ln1_b"][0]])), "ln2": f(np.stack([inp["ln2_g"][0], inp["ln2_b"][0]])),
        "w_rt": f(np.concatenate([inp["w_router_group"][0], inp["w_router_expert"][0]], axis=1)),
        "b_rt": f(np.concatenate([inp["b_router_group"][0], inp["b_router_expert"][0]]).reshape(1, 36)),
        "w_eg": f(inp["w_exp_gate"][0]), "w_eu": f(inp["w_exp_up"][0]), "w_ed": f(inp["w_exp_down"][0]),
        "cbf": cbf, "cf32": cf,
    }


def kernel(**inputs):
    inp = {k: np.asarray(v) for k, v in inputs.items()}
    nc, _ = build()
    in_maps = [make_core_inputs(inp, c) for c in range(8)]
    res = run_bass_kernel_spmd(nc, in_maps, core_ids=list(range(8)))
    outp = np.zeros((4, S, D), np.float32)
    for c in range(8):
        b, j = c // 2, c % 2
        o = np.asarray(res.results[c]["out"]).reshape(32, 128, D)
        outp[b].reshape(64, 128, D)[j::2] = o
    return outp
```
